# Optimizing a Trainium2 kernel written in Bass

```python
import math
import jax
import jax.numpy as jnp
from jax import lax
import numpy as np


D_MODEL = 1024
BATCH = 8
SEQ = 8192
DEPTH = 4

GRID_W = 64
CTX_LEN = 256
N_MIXERS = 3
NORM_EPS = 1e-6

ATTN_HEADS = 8
ATTN_KV_HEADS = 2
ATTN_HEAD_DIM = D_MODEL // ATTN_HEADS
ATTN_GROUP = ATTN_HEADS // ATTN_KV_HEADS
ROPE_THETA = 10000.0
Q_BLOCK = 128

S5_GROUP = 16
S5_GROUPS = D_MODEL // S5_GROUP
S5_STATE = 64
S5_CHUNK = 128
S5_DT_MIN = 1e-3
S5_DT_MAX = 1e-1

SSD_INNER = 2 * D_MODEL
SSD_HEAD_DIM = 64
SSD_HEADS = SSD_INNER // SSD_HEAD_DIM
SSD_GROUPS = 4
SSD_HEADS_PER_GROUP = SSD_HEADS // SSD_GROUPS
SSD_STATE = 128
SSD_CONV = 5
SSD_CHUNK = 128
SSD_CONV_CH = SSD_INNER + 2 * SSD_GROUPS * SSD_STATE
SSD_PROJ = SSD_INNER + SSD_CONV_CH + 2 * SSD_HEADS
SSD_DT_MIN = 1e-3
SSD_DT_MAX = 1e-1

MOE_GROUPS = 4
MOE_EXPERTS_PER_GROUP = 8
MOE_EXPERTS = MOE_GROUPS * MOE_EXPERTS_PER_GROUP
MOE_TOP_K = 2
MOE_HIDDEN = 256

kernel_name = 'hybrid_gqa_s5_ssd_hmoe_prefix_dit'


def rmsnorm(h, g):
    h32 = h.astype(jnp.float32)
    h32 = h32 * lax.rsqrt(jnp.mean(h32 * h32, axis=-1, keepdims=True) + NORM_EPS)
    return h32.astype(h.dtype) * g


def modulate(h, shift, scale):
    return h * (1.0 + scale) + shift


def axial_rope_tables(rows, dtype):
    row, col = jnp.meshgrid(jnp.arange(rows), jnp.arange(GRID_W), indexing='ij')
    n_tokens = rows * GRID_W
    pos = jnp.stack([row.reshape(-1), col.reshape(-1)], axis=-1).astype(jnp.float32)
    n_freq = ATTN_HEAD_DIM // 4
    inv_freq = ROPE_THETA ** (-jnp.arange(n_freq, dtype=jnp.float32) / n_freq)
    ang = jnp.broadcast_to(pos[:, :, None, None] * inv_freq, (n_tokens, 2, 2, n_freq))
    ang = ang.reshape(n_tokens, ATTN_HEAD_DIM)
    return jnp.cos(ang).astype(dtype), jnp.sin(ang).astype(dtype)


def axial_rope(x, cos, sin):
    xs = x.reshape(x.shape[:-1] + (2, 2, ATTN_HEAD_DIM // 4))
    rot = jnp.stack([-xs[..., 1, :], xs[..., 0, :]], axis=-2).reshape(x.shape)
    return x * cos[:, None, :] + rot * sin[:, None, :]


def gqa_axial_attention(u_lat, u_ctx, cos, sin, w_qkv, q_gain, k_gain, w_o, need_ctx):
    bn, s_len, _ = u_lat.shape
    hd = ATTN_HEAD_DIM

    def project(u):
        n = u.shape[1]
        q, k, v = jnp.split(u @ w_qkv, [ATTN_HEADS * hd, (ATTN_HEADS + ATTN_KV_HEADS) * hd], axis=-1)
        q = rmsnorm(q.reshape(bn, n, ATTN_HEADS, hd), q_gain)
        k = rmsnorm(k.reshape(bn, n, ATTN_KV_HEADS, hd), k_gain)
        v = v.reshape(bn, n, ATTN_KV_HEADS, hd)
        return q, k, v

    def attend(q, k, v):
        sc = jnp.einsum('bqkgd,blkd->bkgql', q, k).astype(jnp.float32) * (hd ** -0.5)
        p = jax.nn.softmax(sc, axis=-1).astype(v.dtype)
        return jnp.einsum('bkgql,blkd->bqkgd', p, v)

    q_l, k_l, v_l = project(u_lat)
    q_l = axial_rope(q_l, cos, sin)
    k_l = axial_rope(k_l, cos, sin)
    q_c, k_c, v_c = project(u_ctx)
    k_all = jnp.concatenate([k_c, k_l], axis=1)
    v_all = jnp.concatenate([v_c, v_l], axis=1)
    nb = s_len // Q_BLOCK
    q_blocks = q_l.reshape(bn, nb, Q_BLOCK, ATTN_KV_HEADS, ATTN_GROUP, hd).swapaxes(0, 1)
    o = lax.map(lambda qb: attend(qb, k_all, v_all), q_blocks)
    o = o.swapaxes(0, 1).reshape(bn, s_len, ATTN_HEADS * hd)
    y_lat = o @ w_o
    y_ctx = None
    if need_ctx:
        n_c = u_ctx.shape[1]
        oc = attend(q_c.reshape(bn, n_c, ATTN_KV_HEADS, ATTN_GROUP, hd), k_c, v_c)
        y_ctx = oc.reshape(bn, n_c, ATTN_HEADS * hd) @ w_o
    return y_lat, y_ctx


def s5_discretise(a_re, a_im, log_dt, b_re, b_im):
    dt = jnp.exp(log_dt)[:, None]
    mag = jnp.exp(a_re * dt)
    ab_re = mag * jnp.cos(a_im * dt)
    ab_im = mag * jnp.sin(a_im * dt)
    den = a_re * a_re + a_im * a_im
    num_re = ab_re - 1.0
    coef_re = (num_re * a_re + ab_im * a_im) / den
    coef_im = (ab_im * a_re - num_re * a_im) / den
    bb_re = coef_re[..., None] * b_re - coef_im[..., None] * b_im
    bb_im = coef_re[..., None] * b_im + coef_im[..., None] * b_re
    return ab_re, ab_im, bb_re, bb_im


def s5_chunk_scan(u, ab_re, ab_im, bb_re, bb_im, c_re, c_im, s_re, s_im):
    bn, n = u.shape[:2]
    uc = u.reshape(bn, n // S5_CHUNK, S5_CHUNK, S5_GROUPS, S5_GROUP).swapaxes(0, 1)

    def combine(e1, e2):
        a1r, a1i, b1r, b1i = e1
        a2r, a2i, b2r, b2i = e2
        return (a2r * a1r - a2i * a1i, a2r * a1i + a2i * a1r,
                a2r * b1r - a2i * b1i + b2r, a2r * b1i + a2i * b1r + b2i)

    def step(carry, ub):
        sr, si = carry
        bu_re = jnp.einsum('bqgc,gpc->bqgp', ub, bb_re)
        bu_im = jnp.einsum('bqgc,gpc->bqgp', ub, bb_im)
        bu_re = bu_re.at[:, 0].add(ab_re * sr - ab_im * si)
        bu_im = bu_im.at[:, 0].add(ab_re * si + ab_im * sr)
        ar = jnp.broadcast_to(ab_re, bu_re.shape)
        ai = jnp.broadcast_to(ab_im, bu_im.shape)
        _, _, xr, xi = lax.associative_scan(combine, (ar, ai, bu_re, bu_im), axis=1)
        y = jnp.einsum('bqgp,gcp->bqgc', xr, c_re) - jnp.einsum('bqgp,gcp->bqgc', xi, c_im)
        return (xr[:, -1], xi[:, -1]), y

    (sr, si), ys = lax.scan(step, (s_re, s_im), uc)
    return ys.swapaxes(0, 1).reshape(u.shape), sr, si


def s5_mixer(u_lat, u_ctx, a_re, a_im, log_dt, b_re, b_im, c_re, c_im, d_skip, w_glu, b_glu, need_ctx):
    f32 = jnp.float32
    bn = u_lat.shape[0]

    def groups(u):
        return u.astype(f32).reshape(u.shape[0], u.shape[1], S5_GROUPS, S5_GROUP)

    def flip(t):
        return jnp.flip(t, axis=1)

    g_lat, g_ctx = groups(u_lat), groups(u_ctx)
    dsk = d_skip.astype(f32).reshape(S5_GROUPS, S5_GROUP)
    y_lat = g_lat * dsk
    y_ctx = g_ctx * dsk
    s0 = jnp.zeros((bn, S5_GROUPS, S5_STATE), f32)
    for dr in range(2):
        disc = s5_discretise(a_re[dr].astype(f32), a_im[dr].astype(f32), log_dt[dr].astype(f32),
                             b_re[dr].astype(f32), b_im[dr].astype(f32))
        cr, ci = c_re[dr].astype(f32), c_im[dr].astype(f32)
        seq_c = g_ctx if dr == 0 else flip(g_ctx)
        seq_l = g_lat if dr == 0 else flip(g_lat)
        yc, s_re, s_im = s5_chunk_scan(seq_c, *disc, cr, ci, s0, s0)
        yl, _, _ = s5_chunk_scan(seq_l, *disc, cr, ci, s_re, s_im)
        if dr == 1:
            yc, yl = flip(yc), flip(yl)
        y_lat = y_lat + yl
        y_ctx = y_ctx + yc

    def glu(y, ref):
        z = jax.nn.gelu(y.reshape(ref.shape).astype(ref.dtype))
        a, b = jnp.split(z @ w_glu + b_glu, 2, axis=-1)
        return a * jax.nn.sigmoid(b)

    return glu(y_lat, u_lat), (glu(y_ctx, u_ctx) if need_ctx else None)


def centred_depthwise_conv(h, w, b):
    out = lax.conv_general_dilated(h, w[:, None, :].astype(h.dtype), window_strides=(1,),
                                   padding=[(SSD_CONV // 2, SSD_CONV // 2)],
                                   dimension_numbers=('NWC', 'WIO', 'NWC'),
                                   feature_group_count=h.shape[-1])
    return out + b


def ssd_chunk_scan(xs, dt, a, bm, cm, s0):
    bn, n = xs.shape[:2]
    nc = n // SSD_CHUNK

    def chunks(t):
        return t.reshape((bn, nc, SSD_CHUNK) + t.shape[2:]).swapaxes(0, 1)

    lower = jnp.tril(jnp.ones((SSD_CHUNK, SSD_CHUNK), dtype=bool))

    def step(s, blk):
        xq, dtq, bq, cq = blk
        cum = jnp.cumsum(dtq * a, axis=1)
        seg = cum[:, :, None] - cum[:, None, :]
        decay = jnp.exp(jnp.where(lower[None, :, :, None, None], seg, -jnp.inf))
        xdt = xq * dtq[..., None]
        cb = jnp.einsum('bign,bjgn->bijg', cq, bq)
        y = jnp.einsum('bijg,bijgh,bjghp->bighp', cb, decay, xdt)
        y = y + jnp.einsum('bign,bghpn,bigh->bighp', cq, s, jnp.exp(cum))
        tail = jnp.exp(cum[:, -1:] - cum)
        s = jnp.exp(cum[:, -1])[..., None, None] * s + jnp.einsum('bjgn,bjgh,bjghp->bghpn', bq, tail, xdt)
        return s, y

    s_fin, ys = lax.scan(step, s0, (chunks(xs), chunks(dt), chunks(bm), chunks(cm)))
    return ys.swapaxes(0, 1).reshape(xs.shape), s_fin


def ssd_mixer(u_lat, u_ctx, w_in, conv_w, conv_b, dt_bias, a_log, d_skip, norm_g, w_out, need_ctx):
    f32 = jnp.float32
    bn = u_lat.shape[0]
    G, Hg = SSD_GROUPS, SSD_HEADS_PER_GROUP

    def prep(u):
        n = u.shape[1]
        z, xbc, dt_raw = jnp.split(u @ w_in, [SSD_INNER, SSD_INNER + SSD_CONV_CH], axis=-1)
        xbc = jax.nn.silu(centred_depthwise_conv(xbc, conv_w, conv_b))
        xs, bm, cm = jnp.split(xbc, [SSD_INNER, SSD_INNER + G * SSD_STATE], axis=-1)
        xs = xs.reshape(bn, n, G, Hg, SSD_HEAD_DIM).astype(f32)
        bm = bm.reshape(bn, n, G, SSD_STATE).astype(f32)
        cm = cm.reshape(bn, n, G, SSD_STATE).astype(f32)
        dt = jax.nn.softplus(dt_raw.astype(f32).reshape(bn, n, 2, G, Hg) + dt_bias.astype(f32).reshape(2, G, Hg))
        return z, xs, bm, cm, dt

    def flip(t):
        return jnp.flip(t, axis=1)

    z_l, x_l, b_l, c_l, dt_l = prep(u_lat)
    z_c, x_c, b_c, c_c, dt_c = prep(u_ctx)
    a = -jnp.exp(a_log.astype(f32)).reshape(2, G, Hg)
    s0 = jnp.zeros((bn, G, Hg, SSD_HEAD_DIM, SSD_STATE), f32)
    yc_f, sc_f = ssd_chunk_scan(x_c, dt_c[:, :, 0], a[0], b_c, c_c, s0)
    yl_f, _ = ssd_chunk_scan(x_l, dt_l[:, :, 0], a[0], b_l, c_l, sc_f)
    yc_b, sc_b = ssd_chunk_scan(flip(x_c), flip(dt_c[:, :, 1]), a[1], flip(b_c), flip(c_c), s0)
    yl_b, _ = ssd_chunk_scan(flip(x_l), flip(dt_l[:, :, 1]), a[1], flip(b_l), flip(c_l), sc_b)
    dsk = d_skip.astype(f32).reshape(G, Hg, 1)

    def finish(y_f, y_b, xs, z):
        y = (y_f + flip(y_b) + xs * dsk).reshape(z.shape).astype(z.dtype)
        return rmsnorm(y * jax.nn.silu(z), norm_g) @ w_out

    y_lat = finish(yl_f, yl_b, x_l, z_l)
    y_ctx = finish(yc_f, yc_b, x_c, z_c) if need_ctx else None
    return y_lat, y_ctx


def hierarchical_moe(h, w_group, b_group, w_router, b_router, w_gate, w_up, w_down):
    shp = h.shape
    t = h.reshape(-1, D_MODEL)
    g_logits = (t @ w_group + b_group).astype(jnp.float32)
    g_idx = jnp.argmax(g_logits, axis=-1)
    g_w = jnp.max(jax.nn.softmax(g_logits, axis=-1), axis=-1, keepdims=True)
    e_logits = (t @ w_router + b_router).astype(jnp.float32).reshape(-1, MOE_GROUPS, MOE_EXPERTS_PER_GROUP)
    e_logits = jnp.einsum('tg,tge->te', jax.nn.one_hot(g_idx, MOE_GROUPS, dtype=jnp.float32), e_logits)
    e_w, e_idx = lax.top_k(jax.nn.softmax(e_logits, axis=-1), MOE_TOP_K)
    e_w = e_w / jnp.sum(e_w, axis=-1, keepdims=True)
    expert = g_idx[:, None] * MOE_EXPERTS_PER_GROUP + e_idx
    combine = jnp.einsum('tk,tke->te', g_w * e_w,
                         jax.nn.one_hot(expert, MOE_EXPERTS, dtype=jnp.float32)).astype(t.dtype)
    y = jnp.zeros_like(t)
    for e in range(MOE_EXPERTS):
        hid = jax.nn.silu(t @ w_gate[e]) * (t @ w_up[e])
        y = y + combine[:, e:e + 1] * (hid @ w_down[e])
    return y.reshape(shp)


def setup_inputs(seed: int = 0) -> dict:
    key = jax.random.key(seed)
    keys = iter(jax.random.split(key, 48))
    f32 = jnp.float32
    n_attn = len(range(0, DEPTH, N_MIXERS))
    n_s5 = len(range(1, DEPTH, N_MIXERS))
    n_ssd = len(range(2, DEPTH, N_MIXERS))

    def normal(shape, scale=1.0):
        return jax.random.normal(next(keys), shape, f32) * scale

    def uniform(shape, lo, hi):
        return jax.random.uniform(next(keys), shape, f32, lo, hi)

    def gain(shape):
        return 1.0 + normal(shape, 0.05)

    qkv_cols = (ATTN_HEADS + 2 * ATTN_KV_HEADS) * ATTN_HEAD_DIM
    ssd_dt = jnp.exp(uniform((n_ssd, 2, SSD_HEADS), math.log(SSD_DT_MIN), math.log(SSD_DT_MAX)))
    return {
        'x': normal((BATCH, SEQ, D_MODEL)),
        'c': normal((BATCH, D_MODEL)),
        'ctx': normal((BATCH, CTX_LEN, D_MODEL)),
        'c_ctx': normal((D_MODEL,)),
        'mod_w': normal((DEPTH, D_MODEL, 6 * D_MODEL), 0.5 * D_MODEL ** -0.5),
        'mod_b': normal((DEPTH, 6 * D_MODEL), 0.02),
        'norm1_g': gain((DEPTH, D_MODEL)),
        'norm2_g': gain((DEPTH, D_MODEL)),
        'attn_w_qkv': normal((n_attn, D_MODEL, qkv_cols), D_MODEL ** -0.5),
        'attn_q_gain': gain((n_attn, ATTN_HEAD_DIM)),
        'attn_k_gain': gain((n_attn, ATTN_HEAD_DIM)),
        'attn_w_o': normal((n_attn, ATTN_HEADS * ATTN_HEAD_DIM, D_MODEL), (ATTN_HEADS * ATTN_HEAD_DIM) ** -0.5),
        's5_a_re': -0.5 + normal((n_s5, 2, S5_GROUPS, S5_STATE), 0.01),
        's5_a_im': math.pi * jnp.arange(S5_STATE, dtype=f32) + normal((n_s5, 2, S5_GROUPS, S5_STATE), 0.01),
        's5_log_dt': uniform((n_s5, 2, S5_GROUPS), math.log(S5_DT_MIN), math.log(S5_DT_MAX)),
        's5_b_re': normal((n_s5, 2, S5_GROUPS, S5_STATE, S5_GROUP), (2 * S5_GROUP) ** -0.5),
        's5_b_im': normal((n_s5, 2, S5_GROUPS, S5_STATE, S5_GROUP), (2 * S5_GROUP) ** -0.5),
        's5_c_re': normal((n_s5, 2, S5_GROUPS, S5_GROUP, S5_STATE), S5_STATE ** -0.5),
        's5_c_im': normal((n_s5, 2, S5_GROUPS, S5_GROUP, S5_STATE), S5_STATE ** -0.5),
        's5_d': normal((n_s5, D_MODEL)),
        's5_w_glu': normal((n_s5, D_MODEL, 2 * D_MODEL), D_MODEL ** -0.5),
        's5_b_glu': normal((n_s5, 2 * D_MODEL), 0.02),
        'ssd_w_in': normal((n_ssd, D_MODEL, SSD_PROJ), D_MODEL ** -0.5),
        'ssd_conv_w': normal((n_ssd, SSD_CONV, SSD_CONV_CH), SSD_CONV ** -0.5),
        'ssd_conv_b': normal((n_ssd, SSD_CONV_CH), 0.02),
        'ssd_dt_bias': ssd_dt + jnp.log(-jnp.expm1(-ssd_dt)),
        'ssd_a_log': jnp.log(uniform((n_ssd, 2, SSD_HEADS), 1.0, 16.0)),
        'ssd_d': gain((n_ssd, SSD_HEADS)),
        'ssd_norm_g': gain((n_ssd, SSD_INNER)),
        'ssd_w_out': normal((n_ssd, SSD_INNER, D_MODEL), SSD_INNER ** -0.5),
        'moe_w_group': normal((DEPTH, D_MODEL, MOE_GROUPS), D_MODEL ** -0.5),
        'moe_b_group': normal((DEPTH, MOE_GROUPS), 0.01),
        'moe_w_router': normal((DEPTH, D_MODEL, MOE_EXPERTS), D_MODEL ** -0.5),
        'moe_b_router': normal((DEPTH, MOE_EXPERTS), 0.01),
        'moe_w_gate': normal((DEPTH, MOE_EXPERTS, D_MODEL, MOE_HIDDEN), D_MODEL ** -0.5),
        'moe_w_up': normal((DEPTH, MOE_EXPERTS, D_MODEL, MOE_HIDDEN), D_MODEL ** -0.5),
        'moe_w_down': normal((DEPTH, MOE_EXPERTS, MOE_HIDDEN, D_MODEL), MOE_HIDDEN ** -0.5),
    }


def reference(x, c, ctx, c_ctx, mod_w, mod_b, norm1_g, norm2_g,
              attn_w_qkv, attn_q_gain, attn_k_gain, attn_w_o,
              s5_a_re, s5_a_im, s5_log_dt, s5_b_re, s5_b_im, s5_c_re, s5_c_im, s5_d, s5_w_glu, s5_b_glu,
              ssd_w_in, ssd_conv_w, ssd_conv_b, ssd_dt_bias, ssd_a_log, ssd_d, ssd_norm_g, ssd_w_out,
              moe_w_group, moe_b_group, moe_w_router, moe_b_router, moe_w_gate, moe_w_up, moe_w_down):
    rows = x.shape[1] // GRID_W
    cos, sin = axial_rope_tables(rows, x.dtype)
    h_lat, h_ctx = x, ctx
    for i in range(DEPTH):
        need_ctx = i < DEPTH - 1
        m_l = jnp.split((jax.nn.silu(c) @ mod_w[i] + mod_b[i])[:, None, :], 6, axis=-1)
        m_c = jnp.split(jax.nn.silu(c_ctx) @ mod_w[i] + mod_b[i], 6, axis=-1)
        u_lat = modulate(rmsnorm(h_lat, norm1_g[i]), m_l[0], m_l[1])
        u_ctx = modulate(rmsnorm(h_ctx, norm1_g[i]), m_c[0], m_c[1])
        kind, j = i % N_MIXERS, i // N_MIXERS
        if kind == 0:
            y_lat, y_ctx = gqa_axial_attention(u_lat, u_ctx, cos, sin, attn_w_qkv[j], attn_q_gain[j],
                                               attn_k_gain[j], attn_w_o[j], need_ctx)
        elif kind == 1:
            y_lat, y_ctx = s5_mixer(u_lat, u_ctx, s5_a_re[j], s5_a_im[j], s5_log_dt[j], s5_b_re[j], s5_b_im[j],
                                    s5_c_re[j], s5_c_im[j], s5_d[j], s5_w_glu[j], s5_b_glu[j], need_ctx)
        else:
            y_lat, y_ctx = ssd_mixer(u_lat, u_ctx, ssd_w_in[j], ssd_conv_w[j], ssd_conv_b[j], ssd_dt_bias[j],
                                     ssd_a_log[j], ssd_d[j], ssd_norm_g[j], ssd_w_out[j], need_ctx)
        moe_args = (moe_w_group[i], moe_b_group[i], moe_w_router[i], moe_b_router[i],
                    moe_w_gate[i], moe_w_up[i], moe_w_down[i])
        h_lat = h_lat + m_l[2] * y_lat
        h_lat = h_lat + m_l[5] * hierarchical_moe(modulate(rmsnorm(h_lat, norm2_g[i]), m_l[3], m_l[4]), *moe_args)
        if need_ctx:
            h_ctx = h_ctx + m_c[2] * y_ctx
            h_ctx = h_ctx + m_c[5] * hierarchical_moe(modulate(rmsnorm(h_ctx, norm2_g[i]), m_c[3], m_c[4]), *moe_args)
    return h_lat
```

```python
import numpy as np
from contextlib import ExitStack
import concourse.bass as bass
import concourse.mybir as mybir
from concourse.bass_utils import run_bass_kernel_spmd

F32, BF16 = mybir.dt.float32, mybir.dt.bfloat16
AF = mybir.ActivationFunctionType
ALU = mybir.AluOpType
AX = mybir.AxisListType

D = 1024
KT = 8
TC = 256
EPS = 1e-6
NE = 32
KEEP = ('wgb', 'wub', 'wdb', 'wqkvb', 'wob', 'wglub', 'winb', 'woutb')
HID = 256


class Sched:
    BLK = 8000
    NDMA = 12

    def __init__(self, nc, es):
        self.nc, self.es = nc, es
        self.eng = {'pe': nc.tensor, 'act': nc.scalar, 'dve': nc.vector,
                    'pool': nc.gpsimd, 'sp': nc.sync}
        self.cnt = {e: 0 for e in self.eng}
        self.sems = {e: [] for e in self.eng}
        self.seen = {e: {} for e in self.eng}
        self.lastw, self.readers = {}, {}
        self.dq = {}
        self.nsem = 0

    def _newsem(self, name):
        self.nsem += 1
        return self.es.enter_context(self.nc.semaphore(name))

    def _deps(self, r, w):
        toks = []
        for k in r:
            t = self.lastw.get(k)
            if t:
                toks.append(t)
        for k in w:
            t = self.lastw.get(k)
            if t:
                toks.append(t)
            toks.extend(self.readers.get(k, {}).values())
        return toks

    def _wait(self, e, toks, skip_pe=False):
        need = {}
        for (te, tb, sem, val) in toks:
            if skip_pe and te == 'pe':
                continue
            cur = need.get(te)
            if cur is None or (tb, val) > (cur[0], cur[1]):
                need[te] = (tb, val, sem)
        for te, (tb, val, sem) in need.items():
            s = self.seen[e].get(te)
            if s is not None and s >= (tb, val):
                continue
            self.eng[e].wait_ge(sem, val)
            self.seen[e][te] = (tb, val)

    def _reg(self, tok, r, w):
        for k in r:
            self.readers.setdefault(k, {})[tok[0]] = tok
        for k in w:
            self.lastw[k] = tok
            self.readers[k] = {}

    def op(self, e, fn, r=(), w=()):
        self._wait(e, self._deps(r, w), skip_pe=(e == 'pe'))
        ins = fn()
        k = self.cnt[e]
        b = k // self.BLK
        while len(self.sems[e]) <= b:
            self.sems[e].append(self._newsem(f"s_{e}_{len(self.sems[e])}"))
        sem, val = self.sems[e][b], k % self.BLK + 1
        ins.then_inc(sem, 1)
        self.cnt[e] += 1
        self._reg((e, b, sem, val), r, w)

    def dma(self, out, in_, r=(), w=(), q='sp', grp='m', **kw):
        key = (q, grp)
        if key not in self.dq:
            self.dq[key] = {'rr': 0, 'sems': [[self._newsem(f"d_{q}_{grp}_{i}"), 0]
                                             for i in range(self.NDMA)]}
        st = self.dq[key]
        i = st['rr']
        st['rr'] = (i + 1) % self.NDMA
        sem, n = st['sems'][i]
        te = ('dma', q, grp, i)
        toks = self._deps(r, w)
        if n > 0:
            toks.append((te, 0, sem, 16 * n))
        self._wait(q, toks)
        ins = self.eng[q].dma_start(out=out, in_=in_, **kw)
        ins.then_inc(sem, 16)
        st['sems'][i][1] = n + 1
        self._reg((te, 0, sem, 16 * (n + 1)), r, w)

    def barrier(self, keep=()):
        toks = []
        for e in ('pe', 'act', 'dve', 'pool'):
            k = self.cnt[e]
            if k > 0:
                b = (k - 1) // self.BLK
                toks.append((e, b, self.sems[e][b], (k - 1) % self.BLK + 1))
        for (q, grp), st in self.dq.items():
            if grp == 'async':
                continue
            for i, (sem, n) in enumerate(st['sems']):
                if n > 0:
                    toks.append((('dma', q, grp, i), 0, sem, 16 * n))
        for e in self.eng:
            self._wait(e, toks)
        lw = {k: v for k, v in self.lastw.items() if k[0] in keep}
        self.lastw, self.readers = lw, {}

    def finish(self):
        toks = []
        for (q, grp), st in self.dq.items():
            for i, (sem, n) in enumerate(st['sems']):
                if n > 0:
                    toks.append((('dma', q, grp, i), 0, sem, 16 * n))
        self._wait('sp', toks)


class Prog:
    def __init__(self, TL, layers, n_layers_w=4, dbg=None):
        self.TL, self.T = TL, TC + TL
        self.layers = layers
        self.dbg = dbg
        self.spans = [(0, TC, True)] + [(TC + i * 512, 512, False) for i in range(TL // 512)]
        self.nc = bass.Bass("TRN2", target_bir_lowering=False)
        self.build()

    def dram_in(self, name, shape, dt=F32):
        self.in_names.append(name)
        return self.nc.dram_tensor(name, list(shape), dt, kind="ExternalInput").ap()

    def dram_scr(self, name, shape, dt=F32):
        return self.nc.dram_tensor(name, list(shape), dt, kind="Internal").ap()

    def sb(self, es, name, shape, dt=F32):
        self._nsb = getattr(self, '_nsb', 0) + 1
        return es.enter_context(self.nc.sbuf_tensor(f"sb{self._nsb}_{name}", list(shape), dt))

    def mm(self, out, lhsT, rhs, start, stop, r, w):
        nc = self.nc
        self.S.op('pe', lambda: nc.tensor.matmul(out, lhsT=lhsT, rhs=rhs, start=start, stop=stop), r=r, w=w)

    def act(self, out, in_, func, r, w, **kw):
        nc = self.nc
        self.S.op('act', lambda: nc.scalar.activation(out=out, in_=in_, func=func, **kw), r=r, w=w)

    def tt(self, out, in0, in1, op, r, w, e='dve'):
        eng = self.S.eng[e]
        self.S.op(e, lambda: eng.tensor_tensor(out=out, in0=in0, in1=in1, op=op), r=r, w=w)

    def ts(self, out, in0, s1, op0, r, w, s2=None, op1=None, e='dve', **kw):
        eng = self.S.eng[e]
        if op1 is None:
            self.S.op(e, lambda: eng.tensor_scalar(out=out, in0=in0, scalar1=s1, scalar2=None, op0=op0, **kw), r=r, w=w)
        else:
            self.S.op(e, lambda: eng.tensor_scalar(out=out, in0=in0, scalar1=s1, scalar2=s2, op0=op0, op1=op1, **kw), r=r, w=w)

    def stt(self, out, in0, scalar, in1, op0, op1, r, w):
        nc = self.nc
        self.S.op('dve', lambda: nc.vector.scalar_tensor_tensor(out=out, in0=in0, scalar=scalar, in1=in1, op0=op0, op1=op1), r=r, w=w)

    def cp(self, out, in_, r, w, e='dve'):
        eng = self.S.eng[e]
        if e == 'act':
            self.S.op(e, lambda: eng.copy(out=out, in_=in_), r=r, w=w)
        else:
            self.S.op(e, lambda: eng.tensor_copy(out=out, in_=in_), r=r, w=w)

    def build(self):
        nc = self.nc
        self.in_names = []
        TL, T = self.TL, self.T
        I = self.I = {}
        I['x'] = self.dram_in('x', [TL, D])
        I['ctx'] = self.dram_in('ctx', [TC, D])
        I['cc'] = self.dram_in('cc', [2, D])
        I['ident'] = self.dram_in('ident', [128, 128])
        I['mod_w'] = self.dram_in('mod_w', [4, D, 6 * D])
        I['mod_b'] = self.dram_in('mod_b', [4, 6 * D])
        I['ng'] = self.dram_in('ng', [128, 2 * 4 * KT])
        I['moe_wr'] = self.dram_in('moe_wr', [4, D, 36])
        I['moe_br'] = self.dram_in('moe_br', [4, 36])
        I['moe_wg'] = self.dram_in('moe_wg', [4, NE, D, HID])
        I['moe_wu'] = self.dram_in('moe_wu', [4, NE, D, HID])
        I['moe_wd'] = self.dram_in('moe_wd', [4, NE, HID, D])
        I['attn_wqkv'] = self.dram_in('attn_wqkv', [2, D, 1536])
        I['attn_wo'] = self.dram_in('attn_wo', [2, D, D])
        I['attn_g'] = self.dram_in('attn_g', [128, 4])
        I['rope_cos'] = self.dram_in('rope_cos', [128, T])
        I['rope_sin'] = self.dram_in('rope_sin', [128, T])
        I['rope_pm'] = self.dram_in('rope_pm', [128, 128])
        self.wqkvb = self.dram_scr('wqkvb', [2, D, 1536], BF16)
        self.wob = self.dram_scr('wob', [2, D, D], BF16)
        self.QT = self.dram_scr('QT', [D, T], BF16)
        self.KT_ = self.dram_scr('KTs', [256, T], BF16)
        self.Vs = self.dram_scr('Vs', [T, 256], BF16)
        self.OT = self.dram_scr('OT', [D, T], BF16)
        I['s5_par'] = self.dram_in('s5_par', [128, 3 * 64])
        I['s5_B'] = self.dram_in('s5_B', [128, 2 * 64 * 32])
        I['s5_C'] = self.dram_in('s5_C', [128, 2 * 64 * 32])
        I['s5_d'] = self.dram_in('s5_d', [32, 32])
        I['s5_wglu'] = self.dram_in('s5_wglu', [1, D, 2 * D])
        I['s5_bglu'] = self.dram_in('s5_bglu', [128, 16])
        self.wglub = self.dram_scr('wglub', [1, D, 2 * D], BF16)
        self.UT = self.dram_scr('UT', [D, T], BF16)
        self.YF = self.dram_scr('YF', [D, T])
        self.YB = self.dram_scr('YB', [D, T])
        I['ssd_win'] = self.dram_in('ssd_win', [1, D, 5184])
        I['ssd_wout'] = self.dram_in('ssd_wout', [1, 2 * D, D])
        I['ssd_cw'] = self.dram_in('ssd_cw', [128, 24 * 5])
        I['ssd_cb'] = self.dram_in('ssd_cb', [128, 24])
        I['ssd_dtb'] = self.dram_in('ssd_dtb', [128, 64])
        I['ssd_alog'] = self.dram_in('ssd_alog', [128, 64])
        I['ssd_dh'] = self.dram_in('ssd_dh', [128, 32])
        I['ssd_ng'] = self.dram_in('ssd_ng', [128, 2 * D])
        I['ssd_msk'] = self.dram_in('ssd_msk', [128, 4 * 128])
        self.winb = self.dram_scr('winb', [1, D, 5184], BF16)
        self.woutb = self.dram_scr('woutb', [1, 2 * D, D], BF16)
        self.ZS = self.dram_scr('ZS', [T, 2 * D], BF16)
        self.XBC = self.dram_scr('XBC', [3072, T], BF16)
        self.DTs = self.dram_scr('DTs', [T, 64])
        self.XTK = self.dram_scr('XTK', [T, 2 * D], BF16)
        self.BTK = self.dram_scr('BTK', [T, 512], BF16)
        self.BCf = self.dram_scr('BCf', [1024, T], BF16)
        self.YD = self.dram_scr('YD', [2, T, 2 * D])
        self.out = nc.dram_tensor('out', [TL, D], F32, kind="ExternalOutput").ap()
        self.hT = self.dram_scr('hT', [D, T])
        self.wgb = self.dram_scr('wgb', [4, NE, D, HID], BF16)
        self.wub = self.dram_scr('wub', [4, NE, D, HID], BF16)
        self.wdb = self.dram_scr('wdb', [4, NE, HID, D], BF16)
        if self.dbg:
            self.dbg_out = nc.dram_tensor('dbg', [D, T], F32, kind="ExternalOutput").ap()

        with ExitStack() as es:
            self.S = S = Sched(nc, es)
            self.ident = self.sb(es, 'ident', [128, 128])
            self.identb = self.sb(es, 'identb', [128, 128], BF16)
            self.ones32 = self.sb(es, 'ones32', [128, 128])
            self.onesb = self.sb(es, 'onesb', [128, 128], BF16)
            self.mv = self.sb(es, 'mv', [128, 4, 96])
            self.ng = self.sb(es, 'ng', [128, 2, 4, KT])
            self.ps = [es.enter_context(nc.psum_tensor(f'ps{i}', [128, 512], F32)) for i in range(8)]
            S.dma(self.ident[:], I['ident'], w=['ident'])
            S.dma(self.ng[:].rearrange("p n l k -> p (n l k)"), I['ng'], w=['ng'])
            self.cp(self.identb[:], self.ident[:], r=['ident'], w=['identb'])
            S.op('dve', lambda: nc.vector.memset(self.ones32[:], 1.0), w=['ones32'])
            S.op('dve', lambda: nc.vector.memset(self.onesb[:], 1.0), w=['onesb'])
            S.barrier()
            self.async_casts(self.layers[0])
            self.phase_mod()
            self.phase_tin()
            for n_, L in enumerate(self.layers):
                if n_ + 1 < len(self.layers):
                    self.async_casts(self.layers[n_ + 1])
                self.layer(*L)
            self.phase_tout()
            S.finish()

    def cast_dram(self, dst, src, key):
        R_, C_ = src.shape
        a = R_ // 128
        sv = src.rearrange("(p a) n -> p a n", p=128)
        dv = dst.rearrange("(p a) n -> p a n", p=128)
        step = max(1, 2048 // C_)
        for a0 in range(0, a, step):
            a1 = min(a, a0 + step)
            self.S.dma(dv[:, a0:a1, :], sv[:, a0:a1, :], w=[key], q='pool', grp='async')

    def async_casts(self, L):
        I = self.I
        li, kind, ki, nctx = L
        if kind == 's':
            self.cast_dram(self.wglub[ki], I['s5_wglu'][ki], ('wglub', ki))
        if kind == 'd':
            self.cast_dram(self.winb[ki], I['ssd_win'][ki], ('winb', ki))
            self.cast_dram(self.woutb[ki], I['ssd_wout'][ki], ('woutb', ki))
        if kind == 'a':
            self.cast_dram(self.wqkvb[ki], I['attn_wqkv'][ki], ('wqkvb', ki))
            self.cast_dram(self.wob[ki], I['attn_wo'][ki], ('wob', ki))
        for e in range(NE):
            self.cast_dram(self.wgb[li, e], I['moe_wg'][li, e], ('wgb', li, e))
            self.cast_dram(self.wub[li, e], I['moe_wu'][li, e], ('wub', li, e))
            self.cast_dram(self.wdb[li, e], I['moe_wd'][li, e], ('wdb', li, e))

    def phase_mod(self):
        nc, S, I = self.nc, self.S, self.I
        with ExitStack() as es:
            ccT = self.sb(es, 'ccT', [128, KT, 2])
            scT = self.sb(es, 'scT', [128, KT, 2])
            wch = [self.sb(es, f'modw{i}', [128, KT, 512]) for i in range(2)]
            brow = self.sb(es, 'modb', [1, 6 * D])
            with nc.allow_non_contiguous_dma(reason="tiny transposed load of c"):
                for r_ in range(2):
                    S.dma(ccT[:, :, r_], I['cc'][r_].rearrange("(k p) -> p k", p=128), w=['ccT'])
            self.act(scT[:], ccT[:], AF.Silu, r=['ccT'], w=['scT'])
            ci = 0
            for (li, kind, ki, nctx) in self.layers:
                S.dma(brow[:], I['mod_b'][li:li + 1, :], w=['modb'])
                pb = self.ps[li % 2]
                for j in range(12):
                    wt = wch[ci % 2]
                    S.dma(wt[:], I['mod_w'][li][:, j * 512:(j + 1) * 512].rearrange("(k p) n -> p k n", p=128),
                          w=[('modw', ci % 2)])
                    for b4 in range(4):
                        blk = j * 4 + b4
                        o = pb[:, blk * 2:blk * 2 + 2]
                        for k in range(KT):
                            self.mm(o, wt[:, k, b4 * 128:(b4 + 1) * 128], scT[:, k, :], k == 0, False,
                                    r=[('modw', ci % 2), 'scT'], w=[('psm', li % 2)])
                        self.mm(o, brow[0:1, blk * 128:(blk + 1) * 128], self.ones32[0:1, 0:2], False, True,
                                r=['modb', 'ones32'], w=[('psm', li % 2)])
                    ci += 1
                self.cp(self.mv[:, li, :], pb[:, 0:96], r=[('psm', li % 2)], w=[('mv', li)])
        S.barrier(keep=KEEP)

    def hT_span(self, t0, w):
        return self.hT.rearrange("(k p) t -> p k t", p=128)[:, :, t0:t0 + w]

    def phase_tin(self):
        nc, S, I = self.nc, self.S, self.I
        with ExitStack() as es:
            xt = [self.sb(es, f'tin_x{i}', [128, D]) for i in range(2)]
            stg = [self.sb(es, f'tin_s{i}', [128, KT, 512]) for i in range(2)]
            ti = 0
            for si, (t0, w, isc) in enumerate(self.spans):
                st = stg[si % 2]
                for tt_ in range(w // 128):
                    tok = t0 + tt_ * 128
                    src = I['ctx'][tok:tok + 128, :] if isc else I['x'][tok - TC:tok - TC + 128, :]
                    xs = xt[ti % 2]
                    S.dma(xs[:], src, w=[('tinx', ti % 2)])
                    for half in range(2):
                        pb = self.ps[(ti * 2 + half) % 4]
                        for q in range(4):
                            k = half * 4 + q
                            S.op('pe', lambda: nc.tensor.transpose(out=pb[:, q * 128:(q + 1) * 128],
                                                                   in_=xs[:, k * 128:(k + 1) * 128], identity=self.ident[:]),
                                 r=[('tinx', ti % 2)], w=[('pst', (ti * 2 + half) % 4)])
                        self.cp(st[:, half * 4:half * 4 + 4, tt_ * 128:(tt_ + 1) * 128],
                                pb[:].rearrange("p (q t) -> p q t", q=4),
                                r=[('pst', (ti * 2 + half) % 4)], w=[('tins', si % 2)],
                                e='dve' if half == 0 else 'act')
                    ti += 1
                S.dma(self.hT_span(t0, w), st[:, :, :w], r=[('tins', si % 2)], w=[('hT', si)])
        S.barrier(keep=KEEP)

    def phase_tout(self):
        nc, S = self.nc, self.S
        if self.dbg:
            with ExitStack() as es:
                t_ = [self.sb(es, f'dbg{i}', [128, KT, 512]) for i in range(2)]
                for si, (t0, w, isc) in enumerate(self.spans):
                    S.dma(t_[si % 2][:, :, :w], self.hT_span(t0, w), w=[('dbgt', si % 2)])
                    S.dma(self.dbg_out.rearrange("(k p) t -> p k t", p=128)[:, :, t0:t0 + w], t_[si % 2][:, :, :w],
                          r=[('dbgt', si % 2)], w=[('dbgo', si)])
            S.barrier()
        with ExitStack() as es:
            hs = [self.sb(es, f'to_h{i}', [128, KT, 512]) for i in range(2)]
            ot = [self.sb(es, f'to_o{i}', [128, D]) for i in range(2)]
            ti = 0
            for si, (t0, w, isc) in enumerate(self.spans):
                if isc:
                    continue
                h = hs[si % 2]
                S.dma(h[:, :, :w], self.hT_span(t0, w), w=[('toh', si % 2)])
                for tt_ in range(w // 128):
                    o = ot[ti % 2]
                    for half in range(2):
                        pb = self.ps[(ti * 2 + half) % 4]
                        for q in range(4):
                            k = half * 4 + q
                            S.op('pe', lambda: nc.tensor.transpose(out=pb[:, q * 128:(q + 1) * 128],
                                                                   in_=h[:, k, tt_ * 128:(tt_ + 1) * 128], identity=self.ident[:]),
                                 r=[('toh', si % 2)], w=[('pst', (ti * 2 + half) % 4)])
                        self.cp(o[:, half * 512:(half + 1) * 512], pb[:],
                                r=[('pst', (ti * 2 + half) % 4)], w=[('too', ti % 2)],
                                e='dve' if half == 0 else 'act')
                    tok = t0 - TC + tt_ * 128
                    S.dma(self.out[tok:tok + 128, :], o[:], r=[('too', ti % 2)], w=[('out', ti)])
                    ti += 1

    def layer_scalars(self, es, li):
        S = self.S
        ab = self.sb(es, f'ab{li}', [128, 2, 6, KT])
        mvv = self.mv[:, li, :].rearrange("p (j k r) -> p r j k", j=6, k=KT, r=2)
        key = ('ab', li)
        for r_ in range(2):
            self.stt(ab[:, r_, 0, :], mvv[:, r_, 1, :], 1.0, self.ng[:, 0, li, :], ALU.add, ALU.mult, r=[('mv', li), 'ng'], w=[key])
            self.cp(ab[:, r_, 1, :], mvv[:, r_, 0, :], r=[('mv', li)], w=[key])
            self.cp(ab[:, r_, 2, :], mvv[:, r_, 2, :], r=[('mv', li)], w=[key])
            self.stt(ab[:, r_, 3, :], mvv[:, r_, 4, :], 1.0, self.ng[:, 1, li, :], ALU.add, ALU.mult, r=[('mv', li), 'ng'], w=[key])
            self.cp(ab[:, r_, 4, :], mvv[:, r_, 3, :], r=[('mv', li)], w=[key])
            self.cp(ab[:, r_, 5, :], mvv[:, r_, 5, :], r=[('mv', li)], w=[key])
        return ab

    def norm_span(self, hs, hkey, w, A, B, abkey, u32, u32key, ub, ubkey, scr):
        nc, S = self.nc, self.S
        sq, rs = scr['sq'], scr['rs']
        pss = self.ps[7]
        self.act(sq[:, :, :w], hs[:, :, :w], AF.Square, r=[hkey], w=['sq'])
        for k in range(KT):
            self.mm(pss[:, :w], self.onesb[:], sq[:, k, :w], k == 0, k == KT - 1, r=['sq', 'onesb'], w=[('ps', 7)])
        self.act(rs[:, :w], pss[:, :w], AF.Sqrt, r=[('ps', 7), 'epsb'], w=['rs'], scale=1.0 / D, bias=self.epsb[:, 0:1])
        S.op('dve', lambda: nc.vector.reciprocal(out=rs[:, :w], in_=rs[:, :w]), r=['rs'], w=['rs'])
        xn = scr['xn']
        self.tt(xn[:, :, :w], hs[:, :, :w], rs[:, :w].unsqueeze(1).to_broadcast([128, KT, w]), ALU.mult, r=[hkey, 'rs'],
                w=[('acc', k) for k in range(KT)])
        for k in range(KT):
            dst = u32[:, k, :w] if u32 is not None else ub[:, k, :w]
            dkey = u32key if u32 is not None else ubkey
            self.act(dst, xn[:, k, :w], AF.Identity, r=[('acc', k), abkey], w=[dkey], scale=A[:, k:k + 1], bias=B[:, k:k + 1])
        if u32 is not None:
            self.cp(ub[:, :, :w], u32[:, :, :w], r=[u32key], w=[ubkey], e='pool')

    def moe_span(self, li, hs, hkey, w, u32, ub, G5, abkey, M):
        nc, S = self.nc, self.S
        ps = self.ps
        ntt = w // 128
        cw, lgs, sm = M['cw'], M['lgs'], M['sm']
        for tt_ in range(ntt):
            pl = ps[6]
            for k in range(KT):
                self.mm(pl[:, 0:36], u32[:, k, tt_ * 128:(tt_ + 1) * 128], M['wr'][:, k, :], k == 0, False,
                        r=['u32', 'wr'], w=[('ps', 6)])
            self.mm(pl[:, 0:36], self.ones32[0:1, :], M['br'][0:1, :], False, True, r=['ones32', 'br'], w=[('ps', 6)])
            L = lgs
            self.cp(L[:, 0:36], pl[:, 0:36], r=[('ps', 6)], w=['lgs'])
            rk, wk = ['lgs', 'sm'], ['sm']
            S.op('dve', lambda: nc.vector.tensor_reduce(out=sm[:, 0:1], in_=L[:, 0:4], op=ALU.max, axis=AX.X), r=rk, w=wk)
            self.ts(sm[:, 1:2], sm[:, 0:1], -1.0, ALU.mult, r=rk, w=wk)
            self.ts(L[:, 36:40], L[:, 0:4], sm[:, 0:1], ALU.is_equal, r=rk, w=['lgs'])
            self.act(L[:, 40:44], L[:, 0:4], AF.Exp, r=rk, w=['lgs', 'sm'], bias=sm[:, 1:2], scale=1.0, accum_out=sm[:, 2:3])
            S.op('dve', lambda: nc.vector.reciprocal(out=sm[:, 3:4], in_=sm[:, 2:3]), r=rk, w=wk)
            self.ts(L[:, 44:52], L[:, 4:12], L[:, 36:37], ALU.mult, r=rk, w=['lgs'])
            for g in range(1, 4):
                self.stt(L[:, 44:52], L[:, 4 + 8 * g:12 + 8 * g], L[:, 36 + g:37 + g], L[:, 44:52], ALU.mult, ALU.add, r=rk, w=['lgs'])
            S.op('dve', lambda: nc.vector.tensor_reduce(out=sm[:, 4:5], in_=L[:, 44:52], op=ALU.max, axis=AX.X), r=rk, w=wk)
            self.ts(L[:, 52:60], L[:, 44:52], sm[:, 4:5], ALU.is_equal, r=rk, w=['lgs'])
            self.stt(L[:, 60:68], L[:, 52:60], -1e30, L[:, 44:52], ALU.mult, ALU.add, r=rk, w=['lgs'])
            S.op('dve', lambda: nc.vector.tensor_reduce(out=sm[:, 5:6], in_=L[:, 60:68], op=ALU.max, axis=AX.X), r=rk, w=wk)
            self.ts(L[:, 68:76], L[:, 60:68], sm[:, 5:6], ALU.is_equal, r=rk, w=['lgs'])
            self.tt(sm[:, 6:7], sm[:, 5:6], sm[:, 4:5], ALU.subtract, r=rk, w=wk)
            self.act(sm[:, 7:8], sm[:, 6:7], AF.Exp, r=rk, w=wk)
            self.ts(sm[:, 8:9], sm[:, 7:8], 1.0, ALU.add, r=rk, w=wk)
            S.op('dve', lambda: nc.vector.reciprocal(out=sm[:, 8:9], in_=sm[:, 8:9]), r=rk, w=wk)
            self.tt(sm[:, 9:10], sm[:, 7:8], sm[:, 8:9], ALU.mult, r=rk, w=wk)
            self.tt(sm[:, 10:11], sm[:, 8:9], sm[:, 3:4], ALU.mult, r=rk, w=wk)
            self.tt(sm[:, 11:12], sm[:, 9:10], sm[:, 3:4], ALU.mult, r=rk, w=wk)
            self.ts(L[:, 76:84], L[:, 52:60], sm[:, 10:11], ALU.mult, r=rk, w=['lgs'])
            self.stt(L[:, 76:84], L[:, 68:76], sm[:, 11:12], L[:, 76:84], ALU.mult, ALU.add, r=rk, w=['lgs'])
            for g in range(4):
                self.ts(cw[:, tt_, g * 8:(g + 1) * 8], L[:, 76:84], L[:, 36 + g:37 + g], ALU.mult, r=rk, w=['cw'])
        wgs, wus, wds = M['wg'], M['wu'], M['wd']
        acc = M['acc']

        def load_w(e):
            sl = e % 2
            S.dma(wgs[sl][:], self.wgb[li, e].rearrange("(k p) n -> p k n", p=128), r=[('wgb', li, e)], w=[('wg', sl)])
            S.dma(wus[sl][:], self.wub[li, e].rearrange("(k p) n -> p k n", p=128), r=[('wub', li, e)], w=[('wu', sl)])
            S.dma(wds[sl][:], self.wdb[li, e].rearrange("(k p) n -> p k n", p=128), r=[('wdb', li, e)], w=[('wd', sl)])
        load_w(0)
        for e in range(NE):
            sl = e % 2
            if e + 1 < NE:
                load_w(e + 1)
            pc = ps[4 + e % 2]
            for tt_ in range(ntt):
                self.mm(pc[:, tt_ * 128:(tt_ + 1) * 128], cw[:, tt_, e:e + 1].to_broadcast([128, 128]), self.ident[:], True, True,
                        r=['cw', 'ident'], w=[('ps', 4 + e % 2)])
            for j in range(2):
                b0 = 2 * ((e * 2 + j) % 2)
                for (wt, wkey, bank) in ((wgs[sl], ('wg', sl), b0), (wus[sl], ('wu', sl), b0 + 1)):
                    for k in range(KT):
                        self.mm(ps[bank][:, :w], wt[:, k, j * 128:(j + 1) * 128], ub[:, k, :w], k == 0, k == KT - 1,
                                r=[wkey, 'ub'], w=[('ps', bank)])
                sg, t2 = M['sg'][j], M['t2'][j]
                hid = M['hid'][sl]
                self.act(sg[:, :w], ps[b0][:, :w], AF.Silu, r=[('ps', b0)], w=[('sg', j)])
                self.tt(t2[:, :w], sg[:, :w], ps[b0 + 1][:, :w], ALU.mult, r=[('sg', j), ('ps', b0 + 1)], w=[('t2', j)])
                self.tt(hid[:, j, :w], t2[:, :w], pc[:, :w], ALU.mult, r=[('t2', j), ('ps', 4 + e % 2)], w=[('hid', sl, j)])
            for ct in range(KT):
                pd = ps[6 + ct % 2]
                for j in range(2):
                    self.mm(pd[:, :w], wds[sl][:, j, ct * 128:(ct + 1) * 128], M['hid'][sl][:, j, :w], j == 0, j == 1,
                            r=[('wd', sl), ('hid', sl, j)], w=[('ps', 6 + ct % 2)])
                if e == 0:
                    self.cp(acc[:, ct, :w], pd[:, :w], r=[('ps', 6 + ct % 2)], w=[('acc', ct)])
                else:
                    self.tt(acc[:, ct, :w], acc[:, ct, :w], pd[:, :w], ALU.add, r=[('ps', 6 + ct % 2), ('acc', ct)], w=[('acc', ct)])
        for ct in range(KT):
            self.stt(hs[:, ct, :w], acc[:, ct, :w], G5[:, ct:ct + 1], hs[:, ct, :w], ALU.mult, ALU.add,
                     r=[('acc', ct), hkey, abkey], w=[hkey])

    def moe_alloc(self, es, li):
        S, I = self.S, self.I
        M = {}
        M['wr'] = self.sb(es, 'moe_wr', [128, KT, 36])
        M['br'] = self.sb(es, 'moe_br', [1, 36])
        M['cw'] = self.sb(es, 'moe_cw', [128, 4, NE])
        M['lgs'] = self.sb(es, 'moe_lgs', [128, 96])
        M['sm'] = self.sb(es, 'moe_sm', [128, 16])
        M['wg'] = [self.sb(es, f'moe_wg{i}', [128, KT, HID], BF16) for i in range(2)]
        M['wu'] = [self.sb(es, f'moe_wu{i}', [128, KT, HID], BF16) for i in range(2)]
        M['wd'] = [self.sb(es, f'moe_wd{i}', [128, 2, D], BF16) for i in range(2)]
        M['sg'] = [self.sb(es, f'moe_sg{i}', [128, 512]) for i in range(2)]
        M['t2'] = [self.sb(es, f'moe_t2{i}', [128, 512]) for i in range(2)]
        M['hid'] = [self.sb(es, f'moe_hid{i}', [128, 2, 512], BF16) for i in range(2)]
        M['acc'] = self.sb(es, 'moe_acc', [128, KT, 512])
        S.dma(M['wr'][:], I['moe_wr'][li].rearrange("(k p) n -> p k n", p=128), w=['wr'])
        S.dma(M['br'][:], I['moe_br'][li:li + 1, :], w=['br'])
        return M

    def norm_alloc(self, es):
        scr = {'sq': self.sb(es, 'n_sq', [128, KT, 512], BF16), 'rs': self.sb(es, 'n_rs', [128, 512])}
        self.epsb = self.sb(es, 'epsb', [128, 1])
        nc = self.nc
        self.S.op('dve', lambda: nc.vector.memset(self.epsb[:], EPS), w=['epsb'])
        return scr

    def attn_a1(self, li, ki, ab, abkey):
        nc, S, I = self.nc, self.S, self.I
        ps = self.ps
        with ExitStack() as es:
            scr = self.norm_alloc(es)
            scr['xn'] = self.sb(es, 'a1_xn', [128, KT, 512])
            wq = self.sb(es, 'a1_wqkv', [128, KT, 1536], BF16)
            pmb = self.sb(es, 'a1_pmb', [128, 128], BF16)
            pm32 = self.sb(es, 'a1_pm32', [128, 128])
            gq = self.sb(es, 'a1_g', [128, 4])
            hsb = [self.sb(es, f'a1_hs{i}', [128, KT, 512]) for i in range(2)]
            ub = self.sb(es, 'a1_ub', [128, KT, 512], BF16)
            cs = [self.sb(es, f'a1_cos{i}', [128, 512]) for i in range(2)]
            sn = [self.sb(es, f'a1_sin{i}', [128, 512]) for i in range(2)]
            sq = [self.sb(es, f'a1_sq{i}', [128, 512], BF16) for i in range(2)]
            rsq = [self.sb(es, f'a1_rs{i}', [128, 512]) for i in range(2)]
            yb = [self.sb(es, f'a1_yb{i}', [128, 512], BF16) for i in range(2)]
            t1 = [self.sb(es, f'a1_t1{i}', [128, 512]) for i in range(2)]
            t2 = [self.sb(es, f'a1_t2{i}', [128, 512]) for i in range(2)]
            qst = [self.sb(es, f'a1_qst{i}', [128, 10, 512], BF16) for i in range(2)]
            vst = [self.sb(es, f'a1_vst{i}', [128, 4, 256], BF16) for i in range(2)]
            S.dma(wq[:], self.wqkvb[ki].rearrange("(k p) n -> p k n", p=128), r=[('wqkvb', ki)], w=['wq'])
            S.dma(pm32[:], I['rope_pm'], w=['pm32'])
            S.dma(gq[:], I['attn_g'], w=['gq'])
            self.cp(pmb[:], pm32[:], r=['pm32'], w=['pmb'])
            self.ts(gq[:, 2 * ki:2 * ki + 1], gq[:, 2 * ki:2 * ki + 1], 128.0 ** -0.5, ALU.mult, r=['gq'], w=['gq'])
            for si, (t0, w, isc) in enumerate(self.spans):
                hs, hkey = hsb[si % 2], ('hs', si % 2)
                r_ = 1 if isc else 0
                S.dma(hs[:, :, :w], self.hT_span(t0, w), r=[('hT', si)], w=[hkey])
                S.dma(cs[si % 2][:, :w], I['rope_cos'][:, t0:t0 + w], w=[('cos', si % 2)])
                S.dma(sn[si % 2][:, :w], I['rope_sin'][:, t0:t0 + w], w=[('sin', si % 2)])
                self.norm_span(hs, hkey, w, ab[:, r_, 0, :], ab[:, r_, 1, :], abkey, None, None, ub, 'ub', scr)
                qs_ = qst[si % 2]
                for c in range(10):
                    b = c % 2
                    pq, pss_, pr = ps[b], ps[2 + b], ps[4 + b]
                    for k in range(KT):
                        self.mm(pq[:, :w], wq[:, k, c * 128:(c + 1) * 128], ub[:, k, :w], k == 0, k == KT - 1,
                                r=['wq', 'ub'], w=[('ps', b)])
                    self.act(sq[b][:, :w], pq[:, :w], AF.Square, r=[('ps', b)], w=[('sq2', b)])
                    self.mm(pss_[:, :w], self.onesb[:], sq[b][:, :w], True, True, r=[('sq2', b), 'onesb'], w=[('ps', 2 + b)])
                    self.act(rsq[b][:, :w], pss_[:, :w], AF.Sqrt, r=[('ps', 2 + b), 'epsb'], w=[('rsq', b)],
                             scale=1.0 / 128, bias=self.epsb[:, 0:1])
                    S.op('dve', lambda: nc.vector.reciprocal(out=rsq[b][:, :w], in_=rsq[b][:, :w]), r=[('rsq', b)], w=[('rsq', b)])
                    gcol = 2 * ki + (0 if c < 8 else 1)
                    self.stt(yb[b][:, :w], pq[:, :w], gq[:, gcol:gcol + 1], rsq[b][:, :w], ALU.mult, ALU.mult,
                             r=[('ps', b), 'gq', ('rsq', b)], w=[('yb', b)])
                    self.mm(pr[:, :w], pmb[:], yb[b][:, :w], True, True, r=['pmb', ('yb', b)], w=[('ps', 4 + b)])
                    self.tt(t1[b][:, :w], yb[b][:, :w], cs[si % 2][:, :w], ALU.mult, r=[('yb', b), ('cos', si % 2)], w=[('t1', b)])
                    self.tt(t2[b][:, :w], pr[:, :w], sn[si % 2][:, :w], ALU.mult, r=[('ps', 4 + b), ('sin', si % 2)], w=[('t2', b)])
                    self.tt(qs_[:, c, :w], t1[b][:, :w], t2[b][:, :w], ALU.add, r=[('t1', b), ('t2', b)], w=[('qst', si % 2)])
                S.dma(self.QT.rearrange("(h p) t -> p h t", p=128)[:, :, t0:t0 + w], qs_[:, 0:8, :w], r=[('qst', si % 2)], w=[('QT', si)])
                S.dma(self.KT_.rearrange("(h p) t -> p h t", p=128)[:, :, t0:t0 + w], qs_[:, 8:10, :w], r=[('qst', si % 2)], w=[('KT', si)])
                vs_ = vst[si % 2]
                for tt_ in range(w // 128):
                    for k in range(KT):
                        self.mm(ps[6][:, 0:256], ub[:, k, tt_ * 128:(tt_ + 1) * 128], wq[:, k, 1280:1536], k == 0, k == KT - 1,
                                r=['wq', 'ub'], w=[('ps', 6)])
                    self.cp(vs_[:, tt_, :], ps[6][:, 0:256], r=[('ps', 6)], w=[('vst', si % 2)], e='act')
                S.dma(self.Vs[t0:t0 + w, :].rearrange("(a p) n -> p a n", p=128), vs_[:, :w // 128, :], r=[('vst', si % 2)], w=[('Vs', si)])
        S.barrier(keep=KEEP)

    def attn_a2(self, need_ctx):
        nc, S = self.nc, self.S
        ps = self.ps
        T = self.T
        NB = T // 128
        with ExitStack() as es:
            kT = self.sb(es, 'a2_kT', [128, 2, T], BF16)
            vt = self.sb(es, 'a2_v', [128, NB, 256], BF16)
            qsb = [self.sb(es, f'a2_q{i}', [128, 512], BF16) for i in range(2)]
            pt = [self.sb(es, f'a2_p{i}', [128, 512], BF16) for i in range(4)]
            rl = [self.sb(es, f'a2_rl{i}', [128, 512]) for i in range(2)]
            ost = [self.sb(es, f'a2_o{i}', [128, 512], BF16) for i in range(2)]
            S.dma(kT[:], self.KT_.rearrange("(h p) t -> p h t", p=128), w=['kT'])
            for a0 in range(0, NB, 8):
                a1 = min(NB, a0 + 8)
                S.dma(vt[:, a0:a1, :], self.Vs[a0 * 128:a1 * 128, :].rearrange("(a p) n -> p a n", p=128), w=['vt'])
            it = 0
            ie = 0
            for g in range(2):
                for hq in range(4):
                    h = g * 4 + hq
                    for si, (t0, w, isc) in enumerate(self.spans):
                        if isc and not need_ctx:
                            continue
                        nkb = 2 if isc else NB
                        q_, qk = qsb[it % 2], ('q', it % 2)
                        S.dma(q_[:, :w], self.QT[h * 128:(h + 1) * 128, t0:t0 + w], w=[qk])
                        po, pl = ps[4 + it % 2], ps[6 + it % 2]
                        for kb in range(nkb):
                            sb_ = ie % 4
                            self.mm(ps[sb_][:, :w], kT[:, g, kb * 128:(kb + 1) * 128], q_[:, :w], True, True,
                                    r=['kT', qk], w=[('ps', sb_)])
                            self.act(pt[sb_][:, :w], ps[sb_][:, :w], AF.Exp, r=[('ps', sb_)], w=[('pt', sb_)])
                            self.mm(po[:, :w], vt[:, kb, g * 128:(g + 1) * 128], pt[sb_][:, :w], kb == 0, kb == nkb - 1,
                                    r=['vt', ('pt', sb_)], w=[('ps', 4 + it % 2)])
                            self.mm(pl[:, :w], self.onesb[:], pt[sb_][:, :w], kb == 0, kb == nkb - 1,
                                    r=['onesb', ('pt', sb_)], w=[('ps', 6 + it % 2)])
                            ie += 1
                        S.op('dve', lambda: nc.vector.reciprocal(out=rl[it % 2][:, :w], in_=pl[:, :w]),
                             r=[('ps', 6 + it % 2)], w=[('rl', it % 2)])
                        self.tt(ost[it % 2][:, :w], po[:, :w], rl[it % 2][:, :w], ALU.mult,
                                r=[('ps', 4 + it % 2), ('rl', it % 2)], w=[('ost', it % 2)])
                        S.dma(self.OT[h * 128:(h + 1) * 128, t0:t0 + w], ost[it % 2][:, :w], r=[('ost', it % 2)], w=[('OT', h, si)])
                        it += 1
        S.barrier(keep=KEEP)

    def s5_s1(self, li, ab, abkey):
        S = self.S
        with ExitStack() as es:
            scr = self.norm_alloc(es)
            scr['xn'] = self.sb(es, 's1_xn', [128, KT, 512])
            hsb = [self.sb(es, f's1_hs{i}', [128, KT, 512]) for i in range(2)]
            ubb = [self.sb(es, f's1_ub{i}', [128, KT, 512], BF16) for i in range(2)]
            for si, (t0, w, isc) in enumerate(self.spans):
                hs, hkey = hsb[si % 2], ('hs', si % 2)
                r_ = 1 if isc else 0
                S.dma(hs[:, :, :w], self.hT_span(t0, w), r=[('hT', si)], w=[hkey])
                self.norm_span(hs, hkey, w, ab[:, r_, 0, :], ab[:, r_, 1, :], abkey, None, None, ubb[si % 2], ('ub', si % 2), scr)
                S.dma(self.UT.rearrange("(k p) t -> p k t", p=128)[:, :, t0:t0 + w], ubb[si % 2][:, :, :w],
                      r=[('ub', si % 2)], w=[('UT', si)])
        S.barrier(keep=KEEP)

    def s5_s2(self, ki):
        nc, S, I = self.nc, self.S, self.I
        ps = self.ps
        T = self.T
        PI = float(np.pi)
        with ExitStack() as es:
            es0 = ExitStack()
            par = self.sb(es, 's5par', [128, 3, 64])
            d32 = self.sb(es, 's5d', [32, 32])
            dsk = self.sb(es, 's5dsk', [32, 32, 32], BF16)
            BT = self.sb(es, 's5BT', [32, 64, 2, 128], BF16)
            Cw = self.sb(es, 's5Cw', [128, 3, 64, 32], BF16)
            v = {n: self.sb(es, 's5_' + n, [128, 64]) for n in
                 ('dt', 'mag', 'th', 't', 's', 'y', 'm', 'cos', 'sin', 'abr', 'abi', 'den', 'nr', 'cre', 'cim', 'u1', 'u2')}
            vi = self.sb(es, 's5_ti', [128, 64], mybir.dt.int32)
            PL = self.sb(es, 's5PL', [128, 10, 2, 64])
            nGs = self.sb(es, 's5nGs', [128, 2, 64])
            Bt = self.sb(es0, 's5B', [128, 2, 64, 32])
            Ct = self.sb(es0, 's5C', [128, 2, 64, 32])
            bb = self.sb(es0, 's5bb', [128, 2, 64, 32])
            tmpb = self.sb(es0, 's5tmpb', [128, 64, 32])
            with nc.allow_non_contiguous_dma(reason="small parameter tables"):
                S.dma(par[:].rearrange("p a b -> p (a b)"), I['s5_par'], w=['par'])
                S.dma(Bt[:].rearrange("p a b c -> p (a b c)"), I['s5_B'], w=['Bt'])
                S.dma(Ct[:].rearrange("p a b c -> p (a b c)"), I['s5_C'], w=['Ct'])
                S.dma(d32[:], I['s5_d'], w=['d32'])
            K_ = ['dk']

            def TS(o, i, s1, op0, s2=None, op1=None):
                self.ts(o, i, s1, op0, r=K_ + ['par'], w=K_, s2=s2, op1=op1)

            def TT(o, a, b, op):
                self.tt(o, a, b, op, r=K_ + ['par'], w=K_)
            are, aim, ldt = par[:, 0, :], par[:, 1, :], par[:, 2, :]
            self.act(v['dt'][:], ldt, AF.Exp, r=['par'], w=K_)
            TT(v['t'][:], are, v['dt'][:], ALU.mult)
            self.act(v['mag'][:], v['t'][:], AF.Exp, r=K_, w=K_)
            TT(v['th'][:], aim, v['dt'][:], ALU.mult)

            def sin_of(dst, shift):
                TS(v['t'][:], v['th'][:], 1.0 / (2 * PI), ALU.mult, s2=shift / (2 * PI), op1=ALU.add)
                self.cp(vi[:], v['t'][:], r=K_, w=K_)
                self.cp(v['t'][:], vi[:], r=K_, w=K_)
                TS(v['s'][:], v['th'][:], shift, ALU.add)
                self.stt(v['y'][:], v['t'][:], -2 * PI, v['s'][:], ALU.mult, ALU.add, r=K_, w=K_)
                TS(v['m'][:], v['y'][:], PI, ALU.is_gt)
                self.stt(v['y'][:], v['m'][:], -2 * PI, v['y'][:], ALU.mult, ALU.add, r=K_, w=K_)
                TS(v['m'][:], v['y'][:], -PI, ALU.is_lt)
                self.stt(v['y'][:], v['m'][:], 2 * PI, v['y'][:], ALU.mult, ALU.add, r=K_, w=K_)
                self.act(dst, v['y'][:], AF.Sin, r=K_, w=K_)
            sin_of(v['sin'][:], 0.0)
            sin_of(v['cos'][:], PI / 2)
            TT(v['abr'][:], v['mag'][:], v['cos'][:], ALU.mult)
            TT(v['abi'][:], v['mag'][:], v['sin'][:], ALU.mult)
            TT(v['den'][:], are, are, ALU.mult)
            TT(v['u1'][:], aim, aim, ALU.mult)
            TT(v['den'][:], v['den'][:], v['u1'][:], ALU.add)
            S.op('dve', lambda: nc.vector.reciprocal(out=v['den'][:], in_=v['den'][:]), r=K_, w=K_)
            TS(v['nr'][:], v['abr'][:], -1.0, ALU.add)
            TT(v['u1'][:], v['nr'][:], are, ALU.mult)
            TT(v['u2'][:], v['abi'][:], aim, ALU.mult)
            TT(v['u1'][:], v['u1'][:], v['u2'][:], ALU.add)
            TT(v['cre'][:], v['u1'][:], v['den'][:], ALU.mult)
            TT(v['u1'][:], v['abi'][:], are, ALU.mult)
            TT(v['u2'][:], v['nr'][:], aim, ALU.mult)
            TT(v['u1'][:], v['u1'][:], v['u2'][:], ALU.subtract)
            TT(v['cim'][:], v['u1'][:], v['den'][:], ALU.mult)
            creb = v['cre'][:].unsqueeze(2).to_broadcast([128, 64, 32])
            cimb = v['cim'][:].unsqueeze(2).to_broadcast([128, 64, 32])
            self.tt(bb[:, 0], Bt[:, 0], creb, ALU.mult, r=K_ + ['Bt'], w=['bb'])
            self.tt(tmpb[:], Bt[:, 1], cimb, ALU.mult, r=K_ + ['Bt'], w=['tmpb'])
            self.tt(bb[:, 0], bb[:, 0], tmpb[:], ALU.subtract, r=['bb', 'tmpb'], w=['bb'])
            self.tt(bb[:, 1], Bt[:, 1], creb, ALU.mult, r=K_ + ['Bt'], w=['bb'])
            self.tt(tmpb[:], Bt[:, 0], cimb, ALU.mult, r=K_ + ['Bt', 'bb'], w=['tmpb'])
            self.tt(bb[:, 1], bb[:, 1], tmpb[:], ALU.add, r=['bb', 'tmpb'], w=['bb'])
            n_ = 0
            for dj in range(64):
                for ri in range(2):
                    bank = 6 + (n_ // 4) % 2
                    S.op('pe', lambda: nc.tensor.transpose(out=ps[bank][0:32, (n_ % 4) * 128:(n_ % 4 + 1) * 128],
                                                           in_=bb[:, ri, dj, :], identity=self.ident[:]),
                         r=['bb', 'ident'], w=[('ps', bank)])
                    if n_ % 4 == 3:
                        dj0 = dj - 1
                        self.cp(BT[:, dj0:dj0 + 2, :, :].rearrange("p a b c -> p (a b c)"), ps[bank][0:32, :],
                                r=[('ps', bank)], w=['BT'], e='act')
                    n_ += 1
            self.cp(Cw[:, 0], Ct[:, 0], r=['Ct'], w=['Cw'])
            self.ts(Cw[:, 1], Ct[:, 0], -1.0, ALU.mult, r=['Ct'], w=['Cw'])
            self.ts(Cw[:, 2], Ct[:, 1], -1.0, ALU.mult, r=['Ct'], w=['Cw'])
            self.tt(dsk[:], self.ident[0:32, 0:32].unsqueeze(1).to_broadcast([32, 32, 32]),
                    d32[:].unsqueeze(2).to_broadcast([32, 32, 32]), ALU.mult, r=['ident', 'd32'], w=['dsk'])
            self.cp(PL[:, 0, 0, :], v['cos'][:], r=K_, w=['PL'])
            self.cp(PL[:, 0, 1, :], v['sin'][:], r=K_, w=['PL'])
            for l in range(9):
                c_, s_ = PL[:, l, 0, :], PL[:, l, 1, :]
                self.tt(v['u1'][:], c_, c_, ALU.mult, r=['PL'] + K_, w=K_)
                self.tt(v['u2'][:], s_, s_, ALU.mult, r=['PL'] + K_, w=K_)
                self.tt(PL[:, l + 1, 0, :], v['u1'][:], v['u2'][:], ALU.subtract, r=K_ + ['PL'], w=['PL'])
                self.tt(v['u1'][:], c_, s_, ALU.mult, r=['PL'] + K_, w=K_)
                self.ts(PL[:, l + 1, 1, :], v['u1'][:], 2.0, ALU.mult, r=K_ + ['PL'], w=['PL'])
            self.ts(nGs[:, 0, :], PL[:, 8, 1, :], -1.0, ALU.mult, r=['PL'], w=['nGs'])
            self.ts(nGs[:, 1, :], PL[:, 9, 1, :], -1.0, ALU.mult, r=['PL'], w=['nGs'])

            S.barrier(keep=KEEP)
            es0.close()
            cosT = self.sb(es, 's5cosT', [128, 4, 512])
            sinT = self.sb(es, 's5sinT', [128, 4, 512])
            Rt = self.sb(es, 's5Rt', [128, 4, 512])
            ga = self.sb(es, 's5ga', [128, 4, 256])
            gb_ = self.sb(es, 's5gb', [128, 4, 256])
            ujb = [self.sb(es, f's5uj{i}', [32, T], BF16) for i in range(2)]
            yst = self.sb(es, 's5yst', [32, T])
            W_ = {n: self.sb(es, 's5w_' + n, [128, 512]) for n in ('t1', 't2', 'wir', 'wii', 'wr', 'wi')}
            Pp = [self.sb(es, f's5P{i}', [128, 512], BF16) for i in range(4)]
            car = self.sb(es, 's5car', [128, 4])
            lat = [s_ for s_ in enumerate(self.spans) if not s_[1][2]]
            ctxs = [s_ for s_ in enumerate(self.spans) if s_[1][2]]
            orders = [ctxs + lat, ctxs + lat[::-1]]
            it = 0
            for dr in range(2):
                Y = self.YF if dr == 0 else self.YB
                for jb in range(8):
                    cols = slice(dr * 32 + 4 * jb, dr * 32 + 4 * jb + 4)
                    tk = ['tab']
                    S.op('pool', lambda: nc.gpsimd.memset(cosT[:, :, 0:1], 1.0), r=[], w=tk)
                    S.op('pool', lambda: nc.gpsimd.memset(sinT[:, :, 0:1], 0.0), r=[], w=tk)
                    for l in range(9):
                        n = 1 << l
                        pc = PL[:, l, 0, cols].unsqueeze(2).to_broadcast([128, 4, n])
                        ps_ = PL[:, l, 1, cols].unsqueeze(2).to_broadcast([128, 4, n])
                        c0, s0 = cosT[:, :, 0:n], sinT[:, :, 0:n]
                        self.tt(ga[:, :, 0:n], c0, pc, ALU.mult, r=tk + ['PL'], w=['ga'], e='pool')
                        self.tt(gb_[:, :, 0:n], s0, ps_, ALU.mult, r=tk + ['PL'], w=['gb'], e='pool')
                        self.tt(cosT[:, :, n:2 * n], ga[:, :, 0:n], gb_[:, :, 0:n], ALU.subtract, r=['ga', 'gb'], w=tk, e='pool')
                        self.tt(ga[:, :, 0:n], s0, pc, ALU.mult, r=tk + ['PL'], w=['ga'], e='pool')
                        self.tt(gb_[:, :, 0:n], c0, ps_, ALU.mult, r=tk + ['PL'], w=['gb'], e='pool')
                        self.tt(sinT[:, :, n:2 * n], ga[:, :, 0:n], gb_[:, :, 0:n], ALU.add, r=['ga', 'gb'], w=tk, e='pool')
                    self.cp(Rt[:], v['mag'][:, cols].unsqueeze(2).to_broadcast([128, 4, 512]), r=K_, w=tk, e='pool')
                    for jj in range(4):
                        j = 4 * jb + jj
                        dj = dr * 32 + j
                        uj, ukey = ujb[it % 2], ('uj', it % 2)
                        S.dma(uj[:], self.UT[32 * j:32 * j + 32, :], w=[ukey])
                        S.op('dve', lambda: nc.vector.memset(car[:], 0.0), r=[], w=['car'])
                        cT, sT, rT = cosT[:, jj, :], sinT[:, jj, :], Rt[:, jj, :]
                        for n2, (si, (t0, w, isc)) in enumerate(orders[dr]):
                            b2 = n2 % 2
                            pbr, pbi, py = ps[2 * b2], ps[2 * b2 + 1], ps[4 + b2]
                            usl = uj[:, t0:t0 + w]
                            rhs = usl if dr == 0 else usl[:, ::-1]
                            self.mm(pbr[:, :w], BT[:, dj, 0, :], rhs, True, True, r=['BT', ukey], w=[('ps', 2 * b2)])
                            self.mm(pbi[:, :w], BT[:, dj, 1, :], rhs, True, True, r=['BT', ukey], w=[('ps', 2 * b2 + 1)])
                            self.tt(W_['t1'][:, :w], pbr[:, :w], cT[:, :w], ALU.mult, r=[('ps', 2 * b2)] + tk, w=['t1'])
                            self.tt(W_['t2'][:, :w], pbi[:, :w], sT[:, :w], ALU.mult, r=[('ps', 2 * b2 + 1)] + tk, w=['t2'])
                            self.tt(W_['wir'][:, :w], W_['t1'][:, :w], W_['t2'][:, :w], ALU.add, r=['t1', 't2'], w=['wir'])
                            self.tt(W_['t1'][:, :w], pbi[:, :w], cT[:, :w], ALU.mult, r=[('ps', 2 * b2 + 1)] + tk, w=['t1'])
                            self.tt(W_['t2'][:, :w], pbr[:, :w], sT[:, :w], ALU.mult, r=[('ps', 2 * b2)] + tk, w=['t2'])
                            self.tt(W_['wii'][:, :w], W_['t1'][:, :w], W_['t2'][:, :w], ALU.subtract, r=['t1', 't2'], w=['wii'])
                            S.op('dve', lambda: nc.vector.tensor_tensor_scan(out=W_['wr'][:, :w], data0=rT[:, :w], data1=W_['wir'][:, :w],
                                                                           initial=car[:, 0:1], op0=ALU.mult, op1=ALU.add),
                                 r=['wir', 'car'] + tk, w=['wr'])
                            S.op('dve', lambda: nc.vector.tensor_tensor_scan(out=W_['wi'][:, :w], data0=rT[:, :w], data1=W_['wii'][:, :w],
                                                                           initial=car[:, 1:2], op0=ALU.mult, op1=ALU.add),
                                 r=['wii', 'car'] + tk, w=['wi'])
                            lv = 0 if w == 256 else 1
                            Gc, Gs, nG = PL[:, 8 + lv, 0, dj:dj + 1], PL[:, 8 + lv, 1, dj:dj + 1], nGs[:, lv, dj:dj + 1]
                            wrl, wil = W_['wr'][:, w - 1:w], W_['wi'][:, w - 1:w]
                            self.ts(car[:, 2:3], wrl, Gc, ALU.mult, r=['wr', 'PL', 'car'], w=['car'])
                            self.stt(car[:, 0:1], wil, nG, car[:, 2:3], ALU.mult, ALU.add, r=['wi', 'nGs', 'car'], w=['car'])
                            self.ts(car[:, 3:4], wil, Gc, ALU.mult, r=['wi', 'PL', 'car'], w=['car'])
                            self.stt(car[:, 1:2], wrl, Gs, car[:, 3:4], ALU.mult, ALU.add, r=['wr', 'PL', 'car'], w=['car'])
                            self.tt(Pp[0][:, :w], cT[:, :w], W_['wr'][:, :w], ALU.mult, r=tk + ['wr'], w=[('P', 0)], e='pool')
                            self.tt(Pp[1][:, :w], sT[:, :w], W_['wi'][:, :w], ALU.mult, r=tk + ['wi'], w=[('P', 1)], e='pool')
                            self.tt(Pp[2][:, :w], sT[:, :w], W_['wr'][:, :w], ALU.mult, r=tk + ['wr'], w=[('P', 2)], e='pool')
                            self.tt(Pp[3][:, :w], cT[:, :w], W_['wi'][:, :w], ALU.mult, r=tk + ['wi'], w=[('P', 3)], e='pool')
                            yk = ('ps', 4 + b2)
                            self.mm(py[0:32, :w], Cw[:, 0, dj, :], Pp[0][:, :w], True, False, r=['Cw', ('P', 0)], w=[yk])
                            self.mm(py[0:32, :w], Cw[:, 1, dj, :], Pp[1][:, :w], False, False, r=['Cw', ('P', 1)], w=[yk])
                            self.mm(py[0:32, :w], Cw[:, 2, dj, :], Pp[2][:, :w], False, False, r=['Cw', ('P', 2)], w=[yk])
                            self.mm(py[0:32, :w], Cw[:, 2, dj, :], Pp[3][:, :w], False, dr == 1, r=['Cw', ('P', 3)], w=[yk])
                            if dr == 0:
                                self.mm(py[0:32, :w], dsk[:, j, :], usl, False, True, r=['dsk', ukey], w=[yk])
                            src = py[0:32, :w] if dr == 0 else py[0:32, :w][:, ::-1]
                            self.cp(yst[:, t0:t0 + w], src, r=[yk], w=['yst'], e='act')
                        S.dma(Y[32 * j:32 * j + 32, :], yst[:], r=['yst'], w=[('Y', dr, j)])
                        it += 1
        S.barrier(keep=KEEP)

    def ssd_d1(self, li, ki, ab, abkey):
        nc, S, I = self.nc, self.S, self.I
        ps = self.ps
        with ExitStack() as es:
            scr = self.norm_alloc(es)
            scr['xn'] = self.sb(es, 'd1_xn', [128, KT, 512])
            win = self.sb(es, 'd1_win', [128, KT, 5184], BF16)
            dtb = self.sb(es, 'd1_dtb', [128, 64])
            hsb = [self.sb(es, f'd1_hs{i}', [128, KT, 512]) for i in range(2)]
            ub = self.sb(es, 'd1_ub', [128, KT, 512], BF16)
            zst = [self.sb(es, f'd1_zst{i}', [128, 2 * D], BF16) for i in range(2)]
            xst = [self.sb(es, f'd1_xst{i}', [128, 8, 512], BF16) for i in range(2)]
            dst = [self.sb(es, f'd1_dst{i}', [128, 64]) for i in range(2)]
            for k in range(KT):
                S.dma(win[:, k, :], self.winb[ki][k * 128:(k + 1) * 128, :], r=[('winb', ki)], w=['win'])
            S.dma(dtb[:], I['ssd_dtb'], w=['dtb'])
            nz = nx = nd = 0
            for si, (t0, w, isc) in enumerate(self.spans):
                hs, hkey = hsb[si % 2], ('hs', si % 2)
                r_ = 1 if isc else 0
                S.dma(hs[:, :, :w], self.hT_span(t0, w), r=[('hT', si)], w=[hkey])
                self.norm_span(hs, hkey, w, ab[:, r_, 0, :], ab[:, r_, 1, :], abkey, None, None, ub, 'ub', scr)
                for tt_ in range(w // 128):
                    zt, zk = zst[nz % 2], ('zst', nz % 2)
                    for cg in range(4):
                        b = cg % 2
                        for k in range(KT):
                            self.mm(ps[b][:, :], ub[:, k, tt_ * 128:(tt_ + 1) * 128], win[:, k, cg * 512:(cg + 1) * 512],
                                    k == 0, k == KT - 1, r=['win', 'ub'], w=[('ps', b)])
                        self.act(zt[:, cg * 512:(cg + 1) * 512], ps[b][:, :], AF.Silu, r=[('ps', b)], w=[zk])
                    S.dma(self.ZS[t0 + tt_ * 128:t0 + (tt_ + 1) * 128, :], zt[:], r=[zk], w=[('ZS', nz)])
                    nz += 1
                    dt_, dk = dst[nd % 2], ('dst', nd % 2)
                    for k in range(KT):
                        self.mm(ps[6][:, 0:64], ub[:, k, tt_ * 128:(tt_ + 1) * 128], win[:, k, 5120:5184],
                                k == 0, k == KT - 1, r=['win', 'ub'], w=[('ps', 6)])
                    self.tt(dt_[:], ps[6][:, 0:64], dtb[:], ALU.add, r=[('ps', 6), 'dtb'], w=[dk])
                    self.act(dt_[:], dt_[:], AF.Exp, r=[dk], w=[dk])
                    self.act(dt_[:], dt_[:], AF.Ln, r=[dk], w=[dk], bias=1.0, scale=1.0)
                    S.dma(self.DTs[t0 + tt_ * 128:t0 + (tt_ + 1) * 128, :], dt_[:], r=[dk], w=[('DTs', nd)])
                    nd += 1
                for c8 in range(3):
                    xt, xk = xst[nx % 2], ('xst', nx % 2)
                    for c in range(8):
                        ct = c8 * 8 + c
                        b = 2 + ct % 2
                        for k in range(KT):
                            self.mm(ps[b][:, :w], win[:, k, 2048 + ct * 128:2048 + (ct + 1) * 128], ub[:, k, :w],
                                    k == 0, k == KT - 1, r=['win', 'ub'], w=[('ps', b)])
                        self.cp(xt[:, c, :w], ps[b][:, :w], r=[('ps', b)], w=[xk], e='dve' if c % 2 == 0 else 'act')
                    S.dma(self.XBC.rearrange("(k p) t -> p k t", p=128)[:, c8 * 8:(c8 + 1) * 8, t0:t0 + w], xt[:, :, :w],
                          r=[xk], w=[('XBC', nx)])
                    nx += 1
        S.barrier(keep=KEEP)

    def ssd_d2(self):
        nc, S, I = self.nc, self.S, self.I
        ps = self.ps
        T = self.T
        with ExitStack() as es:
            cw = self.sb(es, 'd2_cw', [128, 24, 5])
            cb = self.sb(es, 'd2_cb', [128, 24])
            xin = [self.sb(es, f'd2_xin{i}', [128, 24, 516], BF16) for i in range(2)]
            xc = [self.sb(es, f'd2_xc{i}', [128, 24, 512], BF16) for i in range(2)]
            cv = [self.sb(es, f'd2_cv{i}', [128, 512]) for i in range(2)]
            xts = [self.sb(es, f'd2_xts{i}', [128, 2 * D], BF16) for i in range(2)]
            bts = [self.sb(es, f'd2_bts{i}', [128, 512], BF16) for i in range(2)]
            S.dma(cw[:].rearrange("p a b -> p (a b)"), I['ssd_cw'], w=['cw'])
            S.dma(cb[:], I['ssd_cb'], w=['cb'])
            nt = 0
            for si, (t0, w, isc) in enumerate(self.spans):
                s0, s1 = (0, TC) if isc else (TC, T)
                lo, hi = max(t0 - 2, s0), min(t0 + w + 2, s1)
                xi, xik = xin[si % 2], ('xin', si % 2)
                if lo > t0 - 2:
                    S.op('dve', lambda: nc.vector.memset(xi[:, :, 0:2], 0.0), w=[xik])
                if hi < t0 + w + 2:
                    S.op('dve', lambda: nc.vector.memset(xi[:, :, w + 2:w + 4], 0.0), w=[xik])
                for c8 in range(3):
                    S.dma(xi[:, c8 * 8:(c8 + 1) * 8, lo - (t0 - 2):hi - (t0 - 2)],
                          self.XBC.rearrange("(k p) t -> p k t", p=128)[:, c8 * 8:(c8 + 1) * 8, lo:hi], w=[xik])
                xo, xok = xc[si % 2], ('xc', si % 2)
                for ct in range(24):
                    c_, ck = cv[ct % 2], ('cv', ct % 2)
                    self.ts(c_[:, :w], xi[:, ct, 0:w], cw[:, ct, 0:1], ALU.mult, r=[xik, 'cw', 'cb'], w=[ck], s2=cb[:, ct:ct + 1], op1=ALU.add)
                    for k in range(1, 5):
                        self.stt(c_[:, :w], xi[:, ct, k:k + w], cw[:, ct, k:k + 1], c_[:, :w], ALU.mult, ALU.add, r=[xik, 'cw', ck], w=[ck])
                    self.act(xo[:, ct, :w], c_[:, :w], AF.Silu, r=[ck], w=[xok])
                S.dma(self.BCf.rearrange("(k p) t -> p k t", p=128)[:, :, t0:t0 + w], xo[:, 16:24, :w], r=[xok], w=[('BCf', si)])
                for tt_ in range(w // 128):
                    tok = t0 + tt_ * 128
                    xt, xtk_ = xts[nt % 2], ('xts', nt % 2)
                    for half in range(2):
                        bank = half
                        pbv = ps[bank][:].bitcast(BF16)
                        for q in range(8):
                            ct = half * 8 + q
                            S.op('pe', lambda: nc.tensor.transpose(out=pbv[:, q * 128:(q + 1) * 128],
                                                                   in_=xo[:, ct, tt_ * 128:(tt_ + 1) * 128], identity=self.identb[:]),
                                 r=[xok, 'identb'], w=[('ps', bank)])
                        self.cp(xt[:, half * 1024:(half + 1) * 1024], pbv[:, 0:1024], r=[('ps', bank)], w=[xtk_],
                                e='dve' if half == 0 else 'act')
                    S.dma(self.XTK[tok:tok + 128, :], xt[:], r=[xtk_], w=[('XTK', nt)])
                    bt, btk_ = bts[nt % 2], ('bts', nt % 2)
                    pbv = ps[2][:].bitcast(BF16)
                    for q in range(4):
                        S.op('pe', lambda: nc.tensor.transpose(out=pbv[:, q * 128:(q + 1) * 128],
                                                               in_=xo[:, 16 + q, tt_ * 128:(tt_ + 1) * 128], identity=self.identb[:]),
                             r=[xok, 'identb'], w=[('ps', 2)])
                    self.cp(bt[:], pbv[:, 0:512], r=[('ps', 2)], w=[btk_])
                    S.dma(self.BTK[tok:tok + 128, :], bt[:], r=[btk_], w=[('BTK', nt)])
                    nt += 1
        S.barrier(keep=KEEP)

    def ssd_d3(self):
        nc, S, I = self.nc, self.S, self.I
        ps = self.ps
        T = self.T
        NB = T // 128
        with ExitStack() as es:
            msk = self.sb(es, 'd3_msk', [128, 4, 128])
            aneg = self.sb(es, 'd3_aneg', [128, 64])
            Sst = self.sb(es, 'd3_S', [128, 4, 512])
            Sb = self.sb(es, 'd3_Sb', [128, 4, 512], BF16)
            xtk = [self.sb(es, f'd3_xtk{i}', [128, 32, 64], BF16) for i in range(2)]
            btk = [self.sb(es, f'd3_btk{i}', [128, 512], BF16) for i in range(2)]
            bcf = [self.sb(es, f'd3_bcf{i}', [128, 8, 128], BF16) for i in range(2)]
            dtt_ = [self.sb(es, f'd3_dt{i}', [128, 32]) for i in range(2)]
            sm = self.sb(es, 'd3_sm', [128, 8, 32])
            cs = self.sb(es, 'd3_cs', [128, 64])
            xdt = self.sb(es, 'd3_xdt', [128, 32, 64], BF16)
            txdt = self.sb(es, 'd3_txdt', [128, 32, 64], BF16)
            dec = [self.sb(es, f'd3_dec{i}', [128, 128]) for i in range(4)]
            MT = [self.sb(es, f'd3_MT{i}', [128, 128], BF16) for i in range(4)]
            ytmp = [self.sb(es, f'd3_ytmp{i}', [128, 512]) for i in range(2)]
            ych = [self.sb(es, f'd3_ych{i}', [128, 2 * D]) for i in range(2)]
            S.dma(msk[:].rearrange("p a b -> p (a b)"), I['ssd_msk'], w=['msk'])
            S.dma(aneg[:], I['ssd_alog'], w=['aneg'])
            self.act(aneg[:], aneg[:], AF.Exp, r=['aneg'], w=['aneg'])
            self.ts(aneg[:], aneg[:], -1.0, ALU.mult, r=['aneg'], w=['aneg'])
            ctx_ch = list(range(TC // 128))
            lat_ch = list(range(TC // 128, NB))
            orders = [ctx_ch + lat_ch, ctx_ch[::-1] + lat_ch[::-1]]
            it = 0
            for dr in range(2):
                S.op('dve', lambda: nc.vector.memset(Sst[:], 0.0), w=['S'])
                S.op('dve', lambda: nc.vector.memset(Sb[:], 0.0), w=['Sb'])
                tri, mneg = msk[:, dr, :], msk[:, 2 + dr, :]
                for c in orders[dr]:
                    tok = c * 128
                    p2 = it % 2
                    x_, xk = xtk[p2], ('xtk', p2)
                    b_, bk = btk[p2], ('btk', p2)
                    f_, fk = bcf[p2], ('bcf', p2)
                    d_, dk = dtt_[p2], ('dt', p2)
                    S.dma(x_[:].rearrange("p h q -> p (h q)"), self.XTK[tok:tok + 128, :], w=[xk])
                    S.dma(b_[:], self.BTK[tok:tok + 128, :], w=[bk])
                    S.dma(f_[:], self.BCf.rearrange("(k p) t -> p k t", p=128)[:, :, tok:tok + 128], w=[fk])
                    with nc.allow_non_contiguous_dma(reason="dt half rows"):
                        S.dma(d_[:], self.DTs[tok:tok + 128, dr * 32:(dr + 1) * 32], w=[dk])
                    smk = ['sm']
                    self.tt(sm[:, 0, :], d_[:], aneg[:, dr * 32:(dr + 1) * 32], ALU.mult, r=[dk, 'aneg'], w=smk)
                    self.mm(ps[0][:, 0:32], tri, sm[:, 0, :], True, True, r=['msk'] + smk, w=[('ps', 0)])
                    self.mm(ps[0][:, 32:64], self.ones32[:], sm[:, 0, :], True, True, r=['ones32'] + smk, w=[('ps', 0)])
                    self.cp(cs[:], ps[0][:, 0:64], r=[('ps', 0)], w=['cs'])
                    self.ts(sm[:, 1, :], cs[:, 0:32], -1.0, ALU.mult, r=['cs'], w=smk)
                    self.act(sm[:, 2, :], cs[:, 0:32], AF.Exp, r=['cs'], w=smk)
                    self.act(sm[:, 3, :], cs[:, 32:64], AF.Exp, r=['cs'], w=smk)
                    self.tt(sm[:, 4, :], cs[:, 32:64], cs[:, 0:32], ALU.subtract, r=['cs'], w=smk)
                    self.act(sm[:, 5, :], sm[:, 4, :], AF.Exp, r=smk, w=smk)
                    self.tt(sm[:, 6, :], d_[:], sm[:, 5, :], ALU.mult, r=[dk] + smk, w=smk)
                    self.tt(xdt[:], x_[:], d_[:].unsqueeze(2).to_broadcast([128, 32, 64]), ALU.mult, r=[xk, dk], w=['xdt'])
                    self.tt(txdt[:], x_[:], sm[:, 6, :].unsqueeze(2).to_broadcast([128, 32, 64]), ALU.mult, r=[xk] + smk, w=['txdt'], e='pool')
                    for g in range(4):
                        self.mm(ps[1][:, g * 128:(g + 1) * 128], f_[:, g, :], f_[:, 4 + g, :], True, True, r=[fk], w=[('ps', 1)])
                    y_, yk = ych[p2], ('ych', p2)
                    for g in range(4):
                        pa, pbk = ps[4 + g % 2], ps[6 + g % 2]
                        for hh in range(8):
                            h = g * 8 + hh
                            bkn = 2 + (h // 4) % 2
                            col = (h % 4) * 128
                            self.mm(ps[bkn][:, col:col + 128], cs[:, h:h + 1].to_broadcast([128, 128]), self.ident[:], True, False,
                                    r=['cs', 'ident'], w=[('ps', bkn)])
                            self.mm(ps[bkn][:, col:col + 128], self.ident[:], mneg, False, True, r=['ident', 'msk'], w=[('ps', bkn)])
                            self.act(dec[h % 4][:], ps[bkn][:, col:col + 128], AF.Exp, r=[('ps', bkn)] + smk, w=[('dec', h % 4)],
                                     bias=sm[:, 1, h:h + 1], scale=1.0)
                            self.tt(MT[h % 4][:], dec[h % 4][:], ps[1][:, g * 128:(g + 1) * 128], ALU.mult,
                                    r=[('dec', h % 4), ('ps', 1)], w=[('MT', h % 4)])
                            self.mm(pa[:, hh * 64:(hh + 1) * 64], MT[h % 4][:], xdt[:, h, :], True, True,
                                    r=[('MT', h % 4), 'xdt'], w=[('ps', 4 + g % 2)])
                        self.mm(pbk[:, :], f_[:, 4 + g, :], Sb[:, g, :], True, True, r=[fk, 'Sb'], w=[('ps', 6 + g % 2)])
                        self.tt(ytmp[g % 2][:].rearrange("p (h q) -> p h q", h=8), pbk[:, :].rearrange("p (h q) -> p h q", h=8),
                                sm[:, 2, g * 8:(g + 1) * 8].unsqueeze(2).to_broadcast([128, 8, 64]), ALU.mult,
                                r=[('ps', 6 + g % 2)] + smk, w=[('ytmp', g % 2)])
                        self.tt(y_[:, g * 512:(g + 1) * 512], pa[:, :], ytmp[g % 2][:], ALU.add,
                                r=[('ps', 4 + g % 2), ('ytmp', g % 2)], w=[yk])
                    S.dma(self.YD[dr, tok:tok + 128, :], y_[:], r=[yk], w=[('YD', dr, c)])
                    for g in range(4):
                        pst = ps[2 + g % 2]
                        self.mm(pst[:, :], b_[:, g * 128:(g + 1) * 128], txdt[:, g * 8:(g + 1) * 8, :].rearrange("p h q -> p (h q)"),
                                True, True, r=[bk, 'txdt'], w=[('ps', 2 + g % 2)])
                        sv = Sst[:, g, :].rearrange("p (h q) -> p h q", h=8)
                        self.tt(sv, sv, sm[:, 3, g * 8:(g + 1) * 8].unsqueeze(2).to_broadcast([128, 8, 64]), ALU.mult, r=['S'] + smk, w=['S'])
                        self.tt(Sst[:, g, :], Sst[:, g, :], pst[:, :], ALU.add, r=['S', ('ps', 2 + g % 2)], w=['S'])
                        self.cp(Sb[:, g, :], Sst[:, g, :], r=['S'], w=['Sb'], e='pool')
                    it += 1
        S.barrier(keep=KEEP)

    def ssd_d4(self, li, ki, ab, abkey, need_ctx):
        nc, S, I = self.nc, self.S, self.I
        ps = self.ps
        with ExitStack() as es:
            wout = self.sb(es, 'd4_wout', [128, 16, D], BF16)
            ngr = self.sb(es, 'd4_ng', [128, 2 * D])
            dh = self.sb(es, 'd4_dh', [128, 32])
            eps_ = self.sb(es, 'd4_eps', [128, 1])
            hsb = [self.sb(es, f'd4_hs{i}', [128, KT, 512]) for i in range(2)]
            yf = [self.sb(es, f'd4_yf{i}', [128, 2 * D]) for i in range(2)]
            yb = [self.sb(es, f'd4_yb{i}', [128, 2 * D]) for i in range(2)]
            xk_ = [self.sb(es, f'd4_x{i}', [128, 2 * D], BF16) for i in range(2)]
            zs = [self.sb(es, f'd4_z{i}', [128, 2 * D], BF16) for i in range(2)]
            tmp = self.sb(es, 'd4_tmp', [128, 2 * D])
            ynb = self.sb(es, 'd4_ynb', [128, 2 * D], BF16)
            ynT = self.sb(es, 'd4_ynT', [128, 16, 512], BF16)
            st = self.sb(es, 'd4_st', [128, 4])
            S.dma(wout[:], self.woutb[ki].rearrange("(k p) n -> p k n", p=128), r=[('woutb', ki)], w=['wout'])
            S.dma(ngr[:], I['ssd_ng'], w=['ngr'])
            S.dma(dh[:], I['ssd_dh'], w=['dh'])
            S.op('dve', lambda: nc.vector.memset(eps_[:], EPS), w=['eps'])
            spans = [s_ for s_ in enumerate(self.spans) if need_ctx or not s_[1][2]]
            nt = 0
            for n_, (si, (t0, w, isc)) in enumerate(spans):
                hs, hkey = hsb[n_ % 2], ('hs', n_ % 2)
                r_ = 1 if isc else 0
                S.dma(hs[:, :, :w], self.hT_span(t0, w), r=[('hT', si)], w=[hkey])
                for tt_ in range(w // 128):
                    tok = t0 + tt_ * 128
                    p2 = nt % 2
                    S.dma(yf[p2][:], self.YD[0, tok:tok + 128, :], w=[('yf', p2)])
                    S.dma(yb[p2][:], self.YD[1, tok:tok + 128, :], w=[('yb', p2)])
                    S.dma(xk_[p2][:], self.XTK[tok:tok + 128, :], w=[('x', p2)])
                    S.dma(zs[p2][:], self.ZS[tok:tok + 128, :], w=[('z', p2)])
                    y = yf[p2]
                    yk = ('yf', p2)
                    self.tt(y[:], y[:], yb[p2][:], ALU.add, r=[yk, ('yb', p2)], w=[yk], e='pool')
                    self.tt(tmp[:].rearrange("p (h q) -> p h q", h=32), xk_[p2][:].rearrange("p (h q) -> p h q", h=32),
                            dh[:].unsqueeze(2).to_broadcast([128, 32, 64]), ALU.mult, r=[('x', p2), 'dh'], w=['tmp'], e='pool')
                    self.tt(y[:], y[:], tmp[:], ALU.add, r=[yk, 'tmp'], w=[yk])
                    self.tt(y[:], y[:], zs[p2][:], ALU.mult, r=[yk, ('z', p2)], w=[yk])
                    self.act(tmp[:], y[:], AF.Square, r=[yk], w=['tmp', 'st'], accum_out=st[:, 0:1])
                    self.act(st[:, 1:2], st[:, 0:1], AF.Sqrt, r=['st', 'eps'], w=['st'], scale=1.0 / (2 * D), bias=eps_[:, 0:1])
                    S.op('dve', lambda: nc.vector.reciprocal(out=st[:, 2:3], in_=st[:, 1:2]), r=['st'], w=['st'])
                    self.stt(ynb[:], y[:], st[:, 2:3], ngr[:], ALU.mult, ALU.mult, r=[yk, 'st', 'ngr'], w=['ynb'])
                    for half in range(2):
                        bank = half
                        pbv = ps[bank][:].bitcast(BF16)
                        for q in range(8):
                            k = half * 8 + q
                            S.op('pe', lambda: nc.tensor.transpose(out=pbv[:, q * 128:(q + 1) * 128],
                                                                   in_=ynb[:, k * 128:(k + 1) * 128], identity=self.identb[:]),
                                 r=['ynb', 'identb'], w=[('ps', bank)])
                        self.cp(ynT[:, half * 8:(half + 1) * 8, tt_ * 128:(tt_ + 1) * 128],
                                pbv[:, 0:1024].rearrange("p (k t) -> p k t", k=8), r=[('ps', bank)], w=['ynT'],
                                e='dve' if half == 0 else 'act')
                    nt += 1
                for ct in range(KT):
                    pb = ps[2 + ct % 2]
                    for k in range(16):
                        self.mm(pb[:, :w], wout[:, k, ct * 128:(ct + 1) * 128], ynT[:, k, :w], k == 0, k == 15,
                                r=['wout', 'ynT'], w=[('ps', 2 + ct % 2)])
                    self.stt(hs[:, ct, :w], pb[:, :w], ab[:, r_, 2, ct:ct + 1], hs[:, ct, :w], ALU.mult, ALU.add,
                             r=[('ps', 2 + ct % 2), abkey, hkey], w=[hkey])
                S.dma(self.hT_span(t0, w), hs[:, :, :w], r=[hkey], w=[('hT', si)])
        S.barrier(keep=KEEP)

    def layer(self, li, kind, ki, need_ctx):
        S = self.S
        ps = self.ps
        with ExitStack() as es:
            ab = self.layer_scalars(es, li)
            abkey = ('ab', li)
            if kind == 'a':
                self.attn_a1(li, ki, ab, abkey)
                self.attn_a2(need_ctx)
            if kind == 's':
                self.s5_s1(li, ab, abkey)
                self.s5_s2(ki)
            if kind == 'd':
                self.ssd_d1(li, ki, ab, abkey)
                self.ssd_d2()
                self.ssd_d3()
                self.ssd_d4(li, ki, ab, abkey, need_ctx)
            with ExitStack() as es2:
                scr = self.norm_alloc(es2)
                M = self.moe_alloc(es2, li)
                scr['xn'] = M['acc']
                hsb = [self.sb(es2, f'hs{i}', [128, KT, 512]) for i in range(2)]
                u32 = self.sb(es2, 'u32', [128, KT, 512])
                ub = self.sb(es2, 'ub', [128, KT, 512], BF16)
                if kind == 'a':
                    wo = self.sb(es2, 'a3_wo', [128, KT, D], BF16)
                    otb = [self.sb(es2, f'a3_ot{i}', [128, KT, 512], BF16) for i in range(2)]
                    S.dma(wo[:], self.wob[ki].rearrange("(k p) n -> p k n", p=128), r=[('wob', ki)], w=['wo'])
                if kind == 's':
                    wgl = self.sb(es2, 's3_wglu', [128, KT, 2 * D], BF16)
                    bgl = self.sb(es2, 's3_bglu', [128, 16])
                    sgl = [self.sb(es2, f's3_sig{i}', [128, 512]) for i in range(2)]
                    ymx = [self.sb(es2, f's3_ymx{i}', [128, 512]) for i in range(2)]
                    S.dma(wgl[:], self.wglub[ki].rearrange("(k p) n -> p k n", p=128), r=[('wglub', ki)], w=['wgl'])
                    S.dma(bgl[:], self.I['s5_bglu'], w=['bgl'])
                spans = [s for s in enumerate(self.spans) if need_ctx or not s[1][2]]
                for n_, (si, (t0, w, isc)) in enumerate(spans):
                    hs, hkey = hsb[n_ % 2], ('hs', n_ % 2)
                    r_ = 1 if isc else 0
                    S.dma(hs[:, :, :w], self.hT_span(t0, w), r=[('hT', si)], w=[hkey])
                    if kind == 's':
                        acc = M['acc']
                        akeys = [('acc', k) for k in range(KT)]
                        S.dma(u32[:, :, :w], self.YF.rearrange("(k p) t -> p k t", p=128)[:, :, t0:t0 + w], w=['u32'])
                        S.dma(acc[:, :, :w], self.YB.rearrange("(k p) t -> p k t", p=128)[:, :, t0:t0 + w], w=akeys)
                        self.tt(u32[:, :, :w], u32[:, :, :w], acc[:, :, :w], ALU.add, r=['u32'] + akeys, w=['u32'])
                        self.tt(acc[:, :, :w], u32[:, :, :w], u32[:, :, :w], ALU.mult, r=['u32'], w=akeys, e='pool')
                        self.ts(acc[:, :, :w], acc[:, :, :w], 0.044715, ALU.mult, r=akeys, w=akeys, s2=1.0, op1=ALU.add)
                        self.tt(acc[:, :, :w], acc[:, :, :w], u32[:, :, :w], ALU.mult, r=['u32'] + akeys, w=akeys, e='pool')
                        self.act(acc[:, :, :w], acc[:, :, :w], AF.Sigmoid, r=akeys, w=akeys, scale=2.0 * float(np.sqrt(2.0 / np.pi)))
                        self.tt(ub[:, :, :w], acc[:, :, :w], u32[:, :, :w], ALU.mult, r=['u32'] + akeys, w=['ub'])
                        for ct in range(KT):
                            b = ct % 2
                            pa, pb2 = ps[b], ps[2 + b]
                            for k in range(KT):
                                self.mm(pa[:, :w], wgl[:, k, ct * 128:(ct + 1) * 128], ub[:, k, :w], k == 0, k == KT - 1,
                                        r=['wgl', 'ub'], w=[('ps', b)])
                            for k in range(KT):
                                self.mm(pb2[:, :w], wgl[:, k, D + ct * 128:D + (ct + 1) * 128], ub[:, k, :w], k == 0, k == KT - 1,
                                        r=['wgl', 'ub'], w=[('ps', 2 + b)])
                            self.act(sgl[b][:, :w], pb2[:, :w], AF.Sigmoid, r=[('ps', 2 + b), 'bgl'], w=[('sgl', b)],
                                     bias=bgl[:, 8 + ct:9 + ct], scale=1.0)
                            self.stt(ymx[b][:, :w], pa[:, :w], bgl[:, ct:ct + 1], sgl[b][:, :w], ALU.add, ALU.mult,
                                     r=[('ps', b), 'bgl', ('sgl', b)], w=[('ymx', b)])
                            self.stt(hs[:, ct, :w], ymx[b][:, :w], ab[:, r_, 2, ct:ct + 1], hs[:, ct, :w], ALU.mult, ALU.add,
                                     r=[('ymx', b), abkey, hkey], w=[hkey])
                    if kind == 'a':
                        ot, okey = otb[n_ % 2], ('ot', n_ % 2)
                        S.dma(ot[:, :, :w], self.OT.rearrange("(k p) t -> p k t", p=128)[:, :, t0:t0 + w], w=[okey])
                        for ct in range(KT):
                            pb = ps[ct % 2]
                            for k in range(KT):
                                self.mm(pb[:, :w], wo[:, k, ct * 128:(ct + 1) * 128], ot[:, k, :w], k == 0, k == KT - 1,
                                        r=['wo', okey], w=[('ps', ct % 2)])
                            self.stt(hs[:, ct, :w], pb[:, :w], ab[:, r_, 2, ct:ct + 1], hs[:, ct, :w], ALU.mult, ALU.add,
                                     r=[('ps', ct % 2), abkey, hkey], w=[hkey])
                    self.norm_span(hs, hkey, w, ab[:, r_, 3, :], ab[:, r_, 4, :], abkey, u32, 'u32', ub, 'ub', scr)
                    self.moe_span(li, hs, hkey, w, u32, ub, ab[:, r_, 5, :], abkey, M)
                    S.dma(self.hT_span(t0, w), hs[:, :, :w], r=[hkey], w=[('hT', si)])
            S.barrier(keep=KEEP)


_ROPE = {}


def rope_consts(TL):
    if TL in _ROPE:
        return _ROPE[TL]
    f = np.float32
    t = np.arange(TL)
    pos = np.stack([t // 64, t % 64], axis=-1).astype(f)
    inv = (f(10000.0) ** (-np.arange(32, dtype=f) / f(32))).astype(f)
    ang = np.broadcast_to(pos[:, :, None, None] * inv, (TL, 2, 2, 32)).reshape(TL, 128).astype(f)
    cos = np.concatenate([np.ones((TC, 128), f), np.cos(ang).astype(f)], axis=0).T
    sin = np.concatenate([np.zeros((TC, 128), f), np.sin(ang).astype(f)], axis=0).T
    pm = np.zeros((128, 128), f)
    for a in range(2):
        for j in range(32):
            pm[a * 64 + 32 + j, a * 64 + j] = -1.0
            pm[a * 64 + j, a * 64 + 32 + j] = 1.0
    _ROPE[TL] = (np.ascontiguousarray(cos), np.ascontiguousarray(sin), pm)
    return _ROPE[TL]


def ssd_masks():
    f = np.float32
    k = np.arange(128)[:, None]
    i = np.arange(128)[None, :]
    trif = (k <= i).astype(f)
    trib = (k >= i).astype(f)
    mf = np.where(k <= i, 0.0, -30000.0).astype(f)
    mb = np.where(k >= i, 0.0, -30000.0).astype(f)
    return np.ascontiguousarray(np.stack([trif, trib, mf, mb], axis=1).reshape(128, 512))


def host_inputs(inp, b, TL, layers):
    f = np.float32
    d = {}
    d['x'] = np.ascontiguousarray(inp['x'][b, :TL])
    d['ctx'] = np.ascontiguousarray(inp['ctx'][b])
    d['cc'] = np.ascontiguousarray(np.stack([inp['c'][b], inp['c_ctx']]))
    d['ident'] = np.eye(128, dtype=f)
    d['mod_w'] = inp['mod_w']
    d['mod_b'] = inp['mod_b']
    ng = np.stack([inp['norm1_g'], inp['norm2_g']])
    d['ng'] = np.ascontiguousarray(ng.reshape(2, 4, KT, 128).transpose(3, 0, 1, 2).reshape(128, -1))
    d['attn_wqkv'] = inp['attn_w_qkv']
    d['attn_wo'] = inp['attn_w_o']
    d['attn_g'] = np.ascontiguousarray(np.stack([inp['attn_q_gain'], inp['attn_k_gain']], axis=1).reshape(4, 128).T)
    f = np.float32
    ki = 0
    def gl_p(a):
        sh = a.shape
        a = a.reshape((2, 32, 2, 64) + sh[3:])
        return np.moveaxis(a, (2, 3), (0, 1)).reshape((128, 2, 32) + sh[3:])
    ldt_b = np.broadcast_to(inp['s5_log_dt'][ki][:, :, None], (2, 64, 64))
    par = np.stack([gl_p(inp['s5_a_re'][ki]), gl_p(inp['s5_a_im'][ki]), gl_p(np.ascontiguousarray(ldt_b))], axis=1)
    d['s5_par'] = np.ascontiguousarray(par.reshape(128, 3 * 64)).astype(f)
    def blockdiag(a):
        o = np.zeros((128, 2, 32, 2, 16), f)
        o[:64, :, :, 0, :] = a[:64]
        o[64:, :, :, 1, :] = a[64:]
        return o.reshape(128, 2, 32, 32)
    Bre, Bim = blockdiag(gl_p(inp['s5_b_re'][ki])), blockdiag(gl_p(inp['s5_b_im'][ki]))
    d['s5_B'] = np.ascontiguousarray(np.stack([Bre, Bim], axis=1).reshape(128, -1))
    cre = np.swapaxes(inp['s5_c_re'][ki], 2, 3)
    cim = np.swapaxes(inp['s5_c_im'][ki], 2, 3)
    Cre, Cim = blockdiag(gl_p(np.ascontiguousarray(cre))), blockdiag(gl_p(np.ascontiguousarray(cim)))
    d['s5_C'] = np.ascontiguousarray(np.stack([Cre, Cim], axis=1).reshape(128, -1))
    d['s5_d'] = np.ascontiguousarray(inp['s5_d'][ki].reshape(32, 32).T)
    d['s5_wglu'] = inp['s5_w_glu']
    d['s5_bglu'] = np.ascontiguousarray(inp['s5_b_glu'][ki].reshape(16, 128).T)
    d['ssd_win'] = inp['ssd_w_in']
    d['ssd_wout'] = inp['ssd_w_out']
    d['ssd_cw'] = np.ascontiguousarray(inp['ssd_conv_w'][ki].reshape(5, 24, 128).transpose(2, 1, 0).reshape(128, 120))
    d['ssd_cb'] = np.ascontiguousarray(inp['ssd_conv_b'][ki].reshape(24, 128).T)
    d['ssd_dtb'] = np.ascontiguousarray(np.broadcast_to(inp['ssd_dt_bias'][ki].reshape(1, 64), (128, 64)))
    d['ssd_alog'] = np.ascontiguousarray(np.broadcast_to(inp['ssd_a_log'][ki].reshape(1, 64), (128, 64)))
    d['ssd_dh'] = np.ascontiguousarray(np.broadcast_to(inp['ssd_d'][ki].reshape(1, 32), (128, 32)))
    d['ssd_ng'] = np.ascontiguousarray(np.broadcast_to(inp['ssd_norm_g'][ki].reshape(1, 2048), (128, 2048)))
    d['ssd_msk'] = ssd_masks()
    cos, sin, pm = rope_consts(TL)
    d['rope_cos'], d['rope_sin'], d['rope_pm'] = cos, sin, pm
    d['moe_wr'] = np.ascontiguousarray(np.concatenate([inp['moe_w_group'], inp['moe_w_router']], axis=-1))
    d['moe_br'] = np.ascontiguousarray(np.concatenate([inp['moe_b_group'], inp['moe_b_router']], axis=-1))
    d['moe_wg'] = inp['moe_w_gate']
    d['moe_wu'] = inp['moe_w_up']
    d['moe_wd'] = inp['moe_w_down']
    return d


FULL_LAYERS = [(0, 'a', 0, True), (1, 's', 0, True), (2, 'd', 0, True), (3, 'a', 1, False)]


def kernel(**inputs):
    inp = {k: np.asarray(v) for k, v in inputs.items()}
    B, TL = inp['x'].shape[0], inp['x'].shape[1]
    prog = Prog(TL, FULL_LAYERS)
    in_maps = []
    for b in range(B):
        d = host_inputs(inp, b, TL, FULL_LAYERS)
        in_maps.append({k: d[k] for k in prog.in_names})
    res = run_bass_kernel_spmd(prog.nc, in_maps, core_ids=list(range(B)))
    return np.stack([r['out'] for r in res.results], axis=0)
```

```python
import numpy as np
from contextlib import ExitStack
import concourse.bass as bass
import concourse.mybir as mybir
from concourse.bass_utils import run_bass_kernel_spmd

F32, BF16 = mybir.dt.float32, mybir.dt.bfloat16
AF = mybir.ActivationFunctionType
ALU = mybir.AluOpType
AX = mybir.AxisListType

D = 1024
KT = 8
TC = 256
EPS = 1e-6
NE = 32
KEEP = ('wgb', 'wub', 'wdb', 'wqkvb', 'wob', 'wglub', 'winb', 'woutb')
HID = 256


class Sched:
    BLK = 8000
    NDMA = 12

    def __init__(self, nc, es):
        self.nc, self.es = nc, es
        self.eng = {'pe': nc.tensor, 'act': nc.scalar, 'dve': nc.vector,
                    'pool': nc.gpsimd, 'sp': nc.sync}
        self.cnt = {e: 0 for e in self.eng}
        self.sems = {e: [] for e in self.eng}
        self.seen = {e: {} for e in self.eng}
        self.lastw, self.readers = {}, {}
        self.dq = {}
        self.nsem = 0

    def _newsem(self, name):
        self.nsem += 1
        return self.es.enter_context(self.nc.semaphore(name))

    def _deps(self, r, w):
        toks = []
        for k in r:
            t = self.lastw.get(k)
            if t:
                toks.append(t)
        for k in w:
            t = self.lastw.get(k)
            if t:
                toks.append(t)
            toks.extend(self.readers.get(k, {}).values())
        return toks

    def _wait(self, e, toks, skip_pe=False):
        need = {}
        for (te, tb, sem, val) in toks:
            if skip_pe and te == 'pe':
                continue
            cur = need.get(te)
            if cur is None or (tb, val) > (cur[0], cur[1]):
                need[te] = (tb, val, sem)
        for te, (tb, val, sem) in need.items():
            s = self.seen[e].get(te)
            if s is not None and s >= (tb, val):
                continue
            self.eng[e].wait_ge(sem, val)
            self.seen[e][te] = (tb, val)

    def _reg(self, tok, r, w):
        for k in r:
            self.readers.setdefault(k, {})[tok[0]] = tok
        for k in w:
            self.lastw[k] = tok
            self.readers[k] = {}

    def op(self, e, fn, r=(), w=()):
        self._wait(e, self._deps(r, w), skip_pe=(e == 'pe'))
        ins = fn()
        k = self.cnt[e]
        b = k // self.BLK
        while len(self.sems[e]) <= b:
            self.sems[e].append(self._newsem(f"s_{e}_{len(self.sems[e])}"))
        sem, val = self.sems[e][b], k % self.BLK + 1
        ins.then_inc(sem, 1)
        self.cnt[e] += 1
        self._reg((e, b, sem, val), r, w)

    def dma(self, out, in_, r=(), w=(), q='sp', grp='m', **kw):
        key = (q, grp)
        if key not in self.dq:
            self.dq[key] = {'rr': 0, 'sems': [[self._newsem(f"d_{q}_{grp}_{i}"), 0]
                                             for i in range(self.NDMA)]}
        st = self.dq[key]
        i = st['rr']
        st['rr'] = (i + 1) % self.NDMA
        sem, n = st['sems'][i]
        te = ('dma', q, grp, i)
        toks = self._deps(r, w)
        if n > 0:
            toks.append((te, 0, sem, 16 * n))
        self._wait(q, toks)
        ins = self.eng[q].dma_start(out=out, in_=in_, **kw)
        ins.then_inc(sem, 16)
        st['sems'][i][1] = n + 1
        self._reg((te, 0, sem, 16 * (n + 1)), r, w)

    def barrier(self, keep=()):
        toks = []
        for e in ('pe', 'act', 'dve', 'pool'):
            k = self.cnt[e]
            if k > 0:
                b = (k - 1) // self.BLK
                toks.append((e, b, self.sems[e][b], (k - 1) % self.BLK + 1))
        for (q, grp), st in self.dq.items():
            if grp == 'async':
                continue
            for i, (sem, n) in enumerate(st['sems']):
                if n > 0:
                    toks.append((('dma', q, grp, i), 0, sem, 16 * n))
        for e in self.eng:
            self._wait(e, toks)
        lw = {k: v for k, v in self.lastw.items() if k[0] in keep}
        self.lastw, self.readers = lw, {}

    def finish(self):
        toks = []
        for (q, grp), st in self.dq.items():
            for i, (sem, n) in enumerate(st['sems']):
                if n > 0:
                    toks.append((('dma', q, grp, i), 0, sem, 16 * n))
        self._wait('sp', toks)


class Prog:
    def __init__(self, TL, layers, n_layers_w=4, dbg=None):
        self.TL, self.T = TL, TC + TL
        self.layers = layers
        self.dbg = dbg
        self.spans = [(0, TC, True)] + [(TC + i * 512, 512, False) for i in range(TL // 512)]
        self.nc = bass.Bass("TRN2", target_bir_lowering=False)
        self.build()

    def dram_in(self, name, shape, dt=F32):
        self.in_names.append(name)
        return self.nc.dram_tensor(name, list(shape), dt, kind="ExternalInput").ap()

    def dram_scr(self, name, shape, dt=F32):
        return self.nc.dram_tensor(name, list(shape), dt, kind="Internal").ap()

    def sb(self, es, name, shape, dt=F32):
        self._nsb = getattr(self, '_nsb', 0) + 1
        return es.enter_context(self.nc.sbuf_tensor(f"sb{self._nsb}_{name}", list(shape), dt))

    def mm(self, out, lhsT, rhs, start, stop, r, w):
        nc = self.nc
        self.S.op('pe', lambda: nc.tensor.matmul(out, lhsT=lhsT, rhs=rhs, start=start, stop=stop), r=r, w=w)

    def act(self, out, in_, func, r, w, **kw):
        nc = self.nc
        self.S.op('act', lambda: nc.scalar.activation(out=out, in_=in_, func=func, **kw), r=r, w=w)

    def tt(self, out, in0, in1, op, r, w, e='dve'):
        eng = self.S.eng[e]
        self.S.op(e, lambda: eng.tensor_tensor(out=out, in0=in0, in1=in1, op=op), r=r, w=w)

    def ts(self, out, in0, s1, op0, r, w, s2=None, op1=None, e='dve', **kw):
        eng = self.S.eng[e]
        if op1 is None:
            self.S.op(e, lambda: eng.tensor_scalar(out=out, in0=in0, scalar1=s1, scalar2=None, op0=op0, **kw), r=r, w=w)
        else:
            self.S.op(e, lambda: eng.tensor_scalar(out=out, in0=in0, scalar1=s1, scalar2=s2, op0=op0, op1=op1, **kw), r=r, w=w)

    def stt(self, out, in0, scalar, in1, op0, op1, r, w):
        nc = self.nc
        self.S.op('dve', lambda: nc.vector.scalar_tensor_tensor(out=out, in0=in0, scalar=scalar, in1=in1, op0=op0, op1=op1), r=r, w=w)

    def cp(self, out, in_, r, w, e='dve'):
        eng = self.S.eng[e]
        if e == 'act':
            self.S.op(e, lambda: eng.copy(out=out, in_=in_), r=r, w=w)
        else:
            self.S.op(e, lambda: eng.tensor_copy(out=out, in_=in_), r=r, w=w)

    def build(self):
        nc = self.nc
        self.in_names = []
        TL, T = self.TL, self.T
        I = self.I = {}
        I['x'] = self.dram_in('x', [TL, D])
        I['ctx'] = self.dram_in('ctx', [TC, D])
        I['cc'] = self.dram_in('cc', [2, D])
        I['ident'] = self.dram_in('ident', [128, 128])
        I['mod_w'] = self.dram_in('mod_w', [4, D, 6 * D])
        I['mod_b'] = self.dram_in('mod_b', [4, 6 * D])
        I['ng'] = self.dram_in('ng', [128, 2 * 4 * KT])
        I['moe_wr'] = self.dram_in('moe_wr', [4, D, 36])
        I['moe_br'] = self.dram_in('moe_br', [4, 36])
        I['moe_wg'] = self.dram_in('moe_wg', [4, NE, D, HID])
        I['moe_wu'] = self.dram_in('moe_wu', [4, NE, D, HID])
        I['moe_wd'] = self.dram_in('moe_wd', [4, NE, HID, D])
        I['attn_wqkv'] = self.dram_in('attn_wqkv', [2, D, 1536])
        I['attn_wo'] = self.dram_in('attn_wo', [2, D, D])
        I['attn_g'] = self.dram_in('attn_g', [128, 4])
        I['rope_cos'] = self.dram_in('rope_cos', [128, T])
        I['rope_sin'] = self.dram_in('rope_sin', [128, T])
        I['rope_pm'] = self.dram_in('rope_pm', [128, 128])
        self.wqkvb = self.dram_scr('wqkvb', [2, D, 1536], BF16)
        self.wob = self.dram_scr('wob', [2, D, D], BF16)
        self.QT = self.dram_scr('QT', [D, T], BF16)
        self.KT_ = self.dram_scr('KTs', [256, T], BF16)
        self.Vs = self.dram_scr('Vs', [T, 256], BF16)
        self.OT = self.dram_scr('OT', [D, T], BF16)
        I['s5_par'] = self.dram_in('s5_par', [128, 3 * 64])
        I['s5_B'] = self.dram_in('s5_B', [128, 2 * 64 * 32])
        I['s5_C'] = self.dram_in('s5_C', [128, 2 * 64 * 32])
        I['s5_d'] = self.dram_in('s5_d', [32, 32])
        I['s5_wglu'] = self.dram_in('s5_wglu', [1, D, 2 * D])
        I['s5_bglu'] = self.dram_in('s5_bglu', [128, 16])
        self.wglub = self.dram_scr('wglub', [1, D, 2 * D], BF16)
        self.UT = self.dram_scr('UT', [D, T], BF16)
        self.YF = self.dram_scr('YF', [D, T])
        self.YB = self.dram_scr('YB', [D, T])
        I['ssd_win'] = self.dram_in('ssd_win', [1, D, 5184])
        I['ssd_wout'] = self.dram_in('ssd_wout', [1, 2 * D, D])
        I['ssd_cw'] = self.dram_in('ssd_cw', [128, 24 * 5])
        I['ssd_cb'] = self.dram_in('ssd_cb', [128, 24])
        I['ssd_dtb'] = self.dram_in('ssd_dtb', [128, 64])
        I['ssd_alog'] = self.dram_in('ssd_alog', [128, 64])
        I['ssd_dh'] = self.dram_in('ssd_dh', [128, 32])
        I['ssd_ng'] = self.dram_in('ssd_ng', [128, 2 * D])
        I['ssd_msk'] = self.dram_in('ssd_msk', [128, 4 * 128])
        self.winb = self.dram_scr('winb', [1, D, 5184], BF16)
        self.woutb = self.dram_scr('woutb', [1, 2 * D, D], BF16)
        self.ZS = self.dram_scr('ZS', [T, 2 * D], BF16)
        self.XBC = self.dram_scr('XBC', [3072, T], BF16)
        self.DTs = self.dram_scr('DTs', [T, 64])
        self.XTK = self.dram_scr('XTK', [T, 2 * D], BF16)
        self.BTK = self.dram_scr('BTK', [T, 512], BF16)
        self.BCf = self.dram_scr('BCf', [1024, T], BF16)
        self.YD = self.dram_scr('YD', [2, T, 2 * D])
        self.out = nc.dram_tensor('out', [TL, D], F32, kind="ExternalOutput").ap()
        self.hT = self.dram_scr('hT', [D, T])
        self.wgb = self.dram_scr('wgb', [4, NE, D, HID], BF16)
        self.wub = self.dram_scr('wub', [4, NE, D, HID], BF16)
        self.wdb = self.dram_scr('wdb', [4, NE, HID, D], BF16)
        if self.dbg:
            self.dbg_out = nc.dram_tensor('dbg', [D, T], F32, kind="ExternalOutput").ap()

        with ExitStack() as es:
            self.S = S = Sched(nc, es)
            self.ident = self.sb(es, 'ident', [128, 128])
            self.identb = self.sb(es, 'identb', [128, 128], BF16)
            self.ones32 = self.sb(es, 'ones32', [128, 128])
            self.onesb = self.sb(es, 'onesb', [128, 128], BF16)
            self.mv = self.sb(es, 'mv', [128, 4, 96])
            self.ng = self.sb(es, 'ng', [128, 2, 4, KT])
            self.ps = [es.enter_context(nc.psum_tensor(f'ps{i}', [128, 512], F32)) for i in range(8)]
            S.dma(self.ident[:], I['ident'], w=['ident'])
            S.dma(self.ng[:].rearrange("p n l k -> p (n l k)"), I['ng'], w=['ng'])
            self.cp(self.identb[:], self.ident[:], r=['ident'], w=['identb'])
            S.op('dve', lambda: nc.vector.memset(self.ones32[:], 1.0), w=['ones32'])
            S.op('dve', lambda: nc.vector.memset(self.onesb[:], 1.0), w=['onesb'])
            S.barrier()
            self.async_casts(self.layers[0])
            self.phase_mod()
            self.phase_tin()
            for n_, L in enumerate(self.layers):
                if n_ + 1 < len(self.layers):
                    self.async_casts(self.layers[n_ + 1])
                self.layer(*L)
            self.phase_tout()
            S.finish()

    def cast_dram(self, dst, src, key):
        R_, C_ = src.shape
        a = R_ // 128
        sv = src.rearrange("(p a) n -> p a n", p=128)
        dv = dst.rearrange("(p a) n -> p a n", p=128)
        step = max(1, 2048 // C_)
        for a0 in range(0, a, step):
            a1 = min(a, a0 + step)
            self.S.dma(dv[:, a0:a1, :], sv[:, a0:a1, :], w=[key], q='pool', grp='async')

    def async_casts(self, L):
        I = self.I
        li, kind, ki, nctx = L
        if kind == 's':
            self.cast_dram(self.wglub[ki], I['s5_wglu'][ki], ('wglub', ki))
        if kind == 'd':
            self.cast_dram(self.winb[ki], I['ssd_win'][ki], ('winb', ki))
            self.cast_dram(self.woutb[ki], I['ssd_wout'][ki], ('woutb', ki))
        if kind == 'a':
            self.cast_dram(self.wqkvb[ki], I['attn_wqkv'][ki], ('wqkvb', ki))
            self.cast_dram(self.wob[ki], I['attn_wo'][ki], ('wob', ki))
        for e in range(NE):
            self.cast_dram(self.wgb[li, e], I['moe_wg'][li, e], ('wgb', li, e))
            self.cast_dram(self.wub[li, e], I['moe_wu'][li, e], ('wub', li, e))
            self.cast_dram(self.wdb[li, e], I['moe_wd'][li, e], ('wdb', li, e))

    def phase_mod(self):
        nc, S, I = self.nc, self.S, self.I
        with ExitStack() as es:
            ccT = self.sb(es, 'ccT', [128, KT, 2])
            scT = self.sb(es, 'scT', [128, KT, 2])
            wch = [self.sb(es, f'modw{i}', [128, KT, 512]) for i in range(2)]
            brow = self.sb(es, 'modb', [1, 6 * D])
            with nc.allow_non_contiguous_dma(reason="tiny transposed load of c"):
                for r_ in range(2):
                    S.dma(ccT[:, :, r_], I['cc'][r_].rearrange("(k p) -> p k", p=128), w=['ccT'])
            self.act(scT[:], ccT[:], AF.Silu, r=['ccT'], w=['scT'])
            ci = 0
            for (li, kind, ki, nctx) in self.layers:
                S.dma(brow[:], I['mod_b'][li:li + 1, :], w=['modb'])
                pb = self.ps[li % 2]
                for j in range(12):
                    wt = wch[ci % 2]
                    S.dma(wt[:], I['mod_w'][li][:, j * 512:(j + 1) * 512].rearrange("(k p) n -> p k n", p=128),
                          w=[('modw', ci % 2)])
                    for b4 in range(4):
                        blk = j * 4 + b4
                        o = pb[:, blk * 2:blk * 2 + 2]
                        for k in range(KT):
                            self.mm(o, wt[:, k, b4 * 128:(b4 + 1) * 128], scT[:, k, :], k == 0, False,
                                    r=[('modw', ci % 2), 'scT'], w=[('psm', li % 2)])
                        self.mm(o, brow[0:1, blk * 128:(blk + 1) * 128], self.ones32[0:1, 0:2], False, True,
                                r=['modb', 'ones32'], w=[('psm', li % 2)])
                    ci += 1
                self.cp(self.mv[:, li, :], pb[:, 0:96], r=[('psm', li % 2)], w=[('mv', li)])
        S.barrier(keep=KEEP)

    def hT_span(self, t0, w):
        return self.hT.rearrange("(k p) t -> p k t", p=128)[:, :, t0:t0 + w]

    def phase_tin(self):
        nc, S, I = self.nc, self.S, self.I
        with ExitStack() as es:
            xt = [self.sb(es, f'tin_x{i}', [128, D]) for i in range(2)]
            stg = [self.sb(es, f'tin_s{i}', [128, KT, 512]) for i in range(2)]
            ti = 0
            for si, (t0, w, isc) in enumerate(self.spans):
                st = stg[si % 2]
                for tt_ in range(w // 128):
                    tok = t0 + tt_ * 128
                    src = I['ctx'][tok:tok + 128, :] if isc else I['x'][tok - TC:tok - TC + 128, :]
                    xs = xt[ti % 2]
                    S.dma(xs[:], src, w=[('tinx', ti % 2)])
                    for half in range(2):
                        pb = self.ps[(ti * 2 + half) % 4]
                        for q in range(4):
                            k = half * 4 + q
                            S.op('pe', lambda: nc.tensor.transpose(out=pb[:, q * 128:(q + 1) * 128],
                                                                   in_=xs[:, k * 128:(k + 1) * 128], identity=self.ident[:]),
                                 r=[('tinx', ti % 2)], w=[('pst', (ti * 2 + half) % 4)])
                        self.cp(st[:, half * 4:half * 4 + 4, tt_ * 128:(tt_ + 1) * 128],
                                pb[:].rearrange("p (q t) -> p q t", q=4),
                                r=[('pst', (ti * 2 + half) % 4)], w=[('tins', si % 2)],
                                e='dve' if half == 0 else 'act')
                    ti += 1
                S.dma(self.hT_span(t0, w), st[:, :, :w], r=[('tins', si % 2)], w=[('hT', si)])
        S.barrier(keep=KEEP)

    def phase_tout(self):
        nc, S = self.nc, self.S
        if self.dbg:
            with ExitStack() as es:
                t_ = [self.sb(es, f'dbg{i}', [128, KT, 512]) for i in range(2)]
                for si, (t0, w, isc) in enumerate(self.spans):
                    S.dma(t_[si % 2][:, :, :w], self.hT_span(t0, w), w=[('dbgt', si % 2)])
                    S.dma(self.dbg_out.rearrange("(k p) t -> p k t", p=128)[:, :, t0:t0 + w], t_[si % 2][:, :, :w],
                          r=[('dbgt', si % 2)], w=[('dbgo', si)])
            S.barrier()
        with ExitStack() as es:
            hs = [self.sb(es, f'to_h{i}', [128, KT, 512]) for i in range(2)]
            ot = [self.sb(es, f'to_o{i}', [128, D]) for i in range(2)]
            ti = 0
            for si, (t0, w, isc) in enumerate(self.spans):
                if isc:
                    continue
                h = hs[si % 2]
                S.dma(h[:, :, :w], self.hT_span(t0, w), w=[('toh', si % 2)])
                for tt_ in range(w // 128):
                    o = ot[ti % 2]
                    for half in range(2):
                        pb = self.ps[(ti * 2 + half) % 4]
                        for q in range(4):
                            k = half * 4 + q
                            S.op('pe', lambda: nc.tensor.transpose(out=pb[:, q * 128:(q + 1) * 128],
                                                                   in_=h[:, k, tt_ * 128:(tt_ + 1) * 128], identity=self.ident[:]),
                                 r=[('toh', si % 2)], w=[('pst', (ti * 2 + half) % 4)])
                        self.cp(o[:, half * 512:(half + 1) * 512], pb[:],
                                r=[('pst', (ti * 2 + half) % 4)], w=[('too', ti % 2)],
                                e='dve' if half == 0 else 'act')
                    tok = t0 - TC + tt_ * 128
                    S.dma(self.out[tok:tok + 128, :], o[:], r=[('too', ti % 2)], w=[('out', ti)])
                    ti += 1

    def layer_scalars(self, es, li):
        S = self.S
        ab = self.sb(es, f'ab{li}', [128, 2, 6, KT])
        mvv = self.mv[:, li, :].rearrange("p (j k r) -> p r j k", j=6, k=KT, r=2)
        key = ('ab', li)
        for r_ in range(2):
            self.stt(ab[:, r_, 0, :], mvv[:, r_, 1, :], 1.0, self.ng[:, 0, li, :], ALU.add, ALU.mult, r=[('mv', li), 'ng'], w=[key])
            self.cp(ab[:, r_, 1, :], mvv[:, r_, 0, :], r=[('mv', li)], w=[key])
            self.cp(ab[:, r_, 2, :], mvv[:, r_, 2, :], r=[('mv', li)], w=[key])
            self.stt(ab[:, r_, 3, :], mvv[:, r_, 4, :], 1.0, self.ng[:, 1, li, :], ALU.add, ALU.mult, r=[('mv', li), 'ng'], w=[key])
            self.cp(ab[:, r_, 4, :], mvv[:, r_, 3, :], r=[('mv', li)], w=[key])
            self.cp(ab[:, r_, 5, :], mvv[:, r_, 5, :], r=[('mv', li)], w=[key])
        return ab

    def norm_span(self, hs, hkey, w, A, B, abkey, u32, u32key, ub, ubkey, scr):
        nc, S = self.nc, self.S
        sq, rs = scr['sq'], scr['rs']
        pss = self.ps[7]
        self.act(sq[:, :, :w], hs[:, :, :w], AF.Square, r=[hkey], w=['sq'])
        for k in range(KT):
            self.mm(pss[:, :w], self.onesb[:], sq[:, k, :w], k == 0, k == KT - 1, r=['sq', 'onesb'], w=[('ps', 7)])
        self.act(rs[:, :w], pss[:, :w], AF.Sqrt, r=[('ps', 7), 'epsb'], w=['rs'], scale=1.0 / D, bias=self.epsb[:, 0:1])
        S.op('dve', lambda: nc.vector.reciprocal(out=rs[:, :w], in_=rs[:, :w]), r=['rs'], w=['rs'])
        xn = scr['xn']
        self.tt(xn[:, :, :w], hs[:, :, :w], rs[:, :w].unsqueeze(1).to_broadcast([128, KT, w]), ALU.mult, r=[hkey, 'rs'],
                w=[('acc', k) for k in range(KT)])
        for k in range(KT):
            dst = u32[:, k, :w] if u32 is not None else ub[:, k, :w]
            dkey = u32key if u32 is not None else ubkey
            self.act(dst, xn[:, k, :w], AF.Identity, r=[('acc', k), abkey], w=[dkey], scale=A[:, k:k + 1], bias=B[:, k:k + 1])
        if u32 is not None:
            self.cp(ub[:, :, :w], u32[:, :, :w], r=[u32key], w=[ubkey], e='pool')

    def moe_span(self, li, hs, hkey, w, u32, ub, G5, abkey, M):
        nc, S = self.nc, self.S
        ps = self.ps
        ntt = w // 128
        cw, lgs, sm = M['cw'], M['lgs'], M['sm']
        for tt_ in range(ntt):
            pl = ps[6]
            for k in range(KT):
                self.mm(pl[:, 0:36], u32[:, k, tt_ * 128:(tt_ + 1) * 128], M['wr'][:, k, :], k == 0, False,
                        r=['u32', 'wr'], w=[('ps', 6)])
            self.mm(pl[:, 0:36], self.ones32[0:1, :], M['br'][0:1, :], False, True, r=['ones32', 'br'], w=[('ps', 6)])
            L = lgs
            self.cp(L[:, 0:36], pl[:, 0:36], r=[('ps', 6)], w=['lgs'])
            rk, wk = ['lgs', 'sm'], ['sm']
            S.op('dve', lambda: nc.vector.tensor_reduce(out=sm[:, 0:1], in_=L[:, 0:4], op=ALU.max, axis=AX.X), r=rk, w=wk)
            self.ts(sm[:, 1:2], sm[:, 0:1], -1.0, ALU.mult, r=rk, w=wk)
            self.ts(L[:, 36:40], L[:, 0:4], sm[:, 0:1], ALU.is_equal, r=rk, w=['lgs'])
            self.act(L[:, 40:44], L[:, 0:4], AF.Exp, r=rk, w=['lgs', 'sm'], bias=sm[:, 1:2], scale=1.0, accum_out=sm[:, 2:3])
            S.op('dve', lambda: nc.vector.reciprocal(out=sm[:, 3:4], in_=sm[:, 2:3]), r=rk, w=wk)
            self.ts(L[:, 44:52], L[:, 4:12], L[:, 36:37], ALU.mult, r=rk, w=['lgs'])
            for g in range(1, 4):
                self.stt(L[:, 44:52], L[:, 4 + 8 * g:12 + 8 * g], L[:, 36 + g:37 + g], L[:, 44:52], ALU.mult, ALU.add, r=rk, w=['lgs'])
            S.op('dve', lambda: nc.vector.tensor_reduce(out=sm[:, 4:5], in_=L[:, 44:52], op=ALU.max, axis=AX.X), r=rk, w=wk)
            self.ts(L[:, 52:60], L[:, 44:52], sm[:, 4:5], ALU.is_equal, r=rk, w=['lgs'])
            self.stt(L[:, 60:68], L[:, 52:60], -1e30, L[:, 44:52], ALU.mult, ALU.add, r=rk, w=['lgs'])
            S.op('dve', lambda: nc.vector.tensor_reduce(out=sm[:, 5:6], in_=L[:, 60:68], op=ALU.max, axis=AX.X), r=rk, w=wk)
            self.ts(L[:, 68:76], L[:, 60:68], sm[:, 5:6], ALU.is_equal, r=rk, w=['lgs'])
            self.tt(sm[:, 6:7], sm[:, 5:6], sm[:, 4:5], ALU.subtract, r=rk, w=wk)
            self.act(sm[:, 7:8], sm[:, 6:7], AF.Exp, r=rk, w=wk)
            self.ts(sm[:, 8:9], sm[:, 7:8], 1.0, ALU.add, r=rk, w=wk)
            S.op('dve', lambda: nc.vector.reciprocal(out=sm[:, 8:9], in_=sm[:, 8:9]), r=rk, w=wk)
            self.tt(sm[:, 9:10], sm[:, 7:8], sm[:, 8:9], ALU.mult, r=rk, w=wk)
            self.tt(sm[:, 10:11], sm[:, 8:9], sm[:, 3:4], ALU.mult, r=rk, w=wk)
            self.tt(sm[:, 11:12], sm[:, 9:10], sm[:, 3:4], ALU.mult, r=rk, w=wk)
            self.ts(L[:, 76:84], L[:, 52:60], sm[:, 10:11], ALU.mult, r=rk, w=['lgs'])
            self.stt(L[:, 76:84], L[:, 68:76], sm[:, 11:12], L[:, 76:84], ALU.mult, ALU.add, r=rk, w=['lgs'])
            for g in range(4):
                self.ts(cw[:, tt_, g * 8:(g + 1) * 8], L[:, 76:84], L[:, 36 + g:37 + g], ALU.mult, r=rk, w=['cw'])
        wgs, wus, wds = M['wg'], M['wu'], M['wd']
        acc = M['acc']

        def load_w(e):
            sl = e % 2
            S.dma(wgs[sl][:], self.wgb[li, e].rearrange("(k p) n -> p k n", p=128), r=[('wgb', li, e)], w=[('wg', sl)])
            S.dma(wus[sl][:], self.wub[li, e].rearrange("(k p) n -> p k n", p=128), r=[('wub', li, e)], w=[('wu', sl)])
            S.dma(wds[sl][:], self.wdb[li, e].rearrange("(k p) n -> p k n", p=128), r=[('wdb', li, e)], w=[('wd', sl)])
        load_w(0)
        for e in range(NE):
            sl = e % 2
            if e + 1 < NE:
                load_w(e + 1)
            pc = ps[4 + e % 2]
            for tt_ in range(ntt):
                self.mm(pc[:, tt_ * 128:(tt_ + 1) * 128], cw[:, tt_, e:e + 1].to_broadcast([128, 128]), self.ident[:], True, True,
                        r=['cw', 'ident'], w=[('ps', 4 + e % 2)])
            for j in range(2):
                b0 = 2 * ((e * 2 + j) % 2)
                for (wt, wkey, bank) in ((wgs[sl], ('wg', sl), b0), (wus[sl], ('wu', sl), b0 + 1)):
                    for k in range(KT):
                        self.mm(ps[bank][:, :w], wt[:, k, j * 128:(j + 1) * 128], ub[:, k, :w], k == 0, k == KT - 1,
                                r=[wkey, 'ub'], w=[('ps', bank)])
                sg, t2 = M['sg'][j], M['t2'][j]
                hid = M['hid'][sl]
                self.act(sg[:, :w], ps[b0][:, :w], AF.Silu, r=[('ps', b0)], w=[('sg', j)])
                self.tt(t2[:, :w], sg[:, :w], ps[b0 + 1][:, :w], ALU.mult, r=[('sg', j), ('ps', b0 + 1)], w=[('t2', j)])
                self.tt(hid[:, j, :w], t2[:, :w], pc[:, :w], ALU.mult, r=[('t2', j), ('ps', 4 + e % 2)], w=[('hid', sl, j)])
            for ct in range(KT):
                pd = ps[6 + ct % 2]
                for j in range(2):
                    self.mm(pd[:, :w], wds[sl][:, j, ct * 128:(ct + 1) * 128], M['hid'][sl][:, j, :w], j == 0, j == 1,
                            r=[('wd', sl), ('hid', sl, j)], w=[('ps', 6 + ct % 2)])
                if e == 0:
                    self.cp(acc[:, ct, :w], pd[:, :w], r=[('ps', 6 + ct % 2)], w=[('acc', ct)])
                else:
                    self.tt(acc[:, ct, :w], acc[:, ct, :w], pd[:, :w], ALU.add, r=[('ps', 6 + ct % 2), ('acc', ct)], w=[('acc', ct)])
        for ct in range(KT):
            self.stt(hs[:, ct, :w], acc[:, ct, :w], G5[:, ct:ct + 1], hs[:, ct, :w], ALU.mult, ALU.add,
                     r=[('acc', ct), hkey, abkey], w=[hkey])

    def moe_alloc(self, es, li):
        S, I = self.S, self.I
        M = {}
        M['wr'] = self.sb(es, 'moe_wr', [128, KT, 36])
        M['br'] = self.sb(es, 'moe_br', [1, 36])
        M['cw'] = self.sb(es, 'moe_cw', [128, 4, NE])
        M['lgs'] = self.sb(es, 'moe_lgs', [128, 96])
        M['sm'] = self.sb(es, 'moe_sm', [128, 16])
        M['wg'] = [self.sb(es, f'moe_wg{i}', [128, KT, HID], BF16) for i in range(2)]
        M['wu'] = [self.sb(es, f'moe_wu{i}', [128, KT, HID], BF16) for i in range(2)]
        M['wd'] = [self.sb(es, f'moe_wd{i}', [128, 2, D], BF16) for i in range(2)]
        M['sg'] = [self.sb(es, f'moe_sg{i}', [128, 512]) for i in range(2)]
        M['t2'] = [self.sb(es, f'moe_t2{i}', [128, 512]) for i in range(2)]
        M['hid'] = [self.sb(es, f'moe_hid{i}', [128, 2, 512], BF16) for i in range(2)]
        M['acc'] = self.sb(es, 'moe_acc', [128, KT, 512])
        S.dma(M['wr'][:], I['moe_wr'][li].rearrange("(k p) n -> p k n", p=128), w=['wr'])
        S.dma(M['br'][:], I['moe_br'][li:li + 1, :], w=['br'])
        return M

    def norm_alloc(self, es):
        scr = {'sq': self.sb(es, 'n_sq', [128, KT, 512], BF16), 'rs': self.sb(es, 'n_rs', [128, 512])}
        self.epsb = self.sb(es, 'epsb', [128, 1])
        nc = self.nc
        self.S.op('dve', lambda: nc.vector.memset(self.epsb[:], EPS), w=['epsb'])
        return scr

    def attn_a1(self, li, ki, ab, abkey):
        nc, S, I = self.nc, self.S, self.I
        ps = self.ps
        with ExitStack() as es:
            scr = self.norm_alloc(es)
            scr['xn'] = self.sb(es, 'a1_xn', [128, KT, 512])
            wq = self.sb(es, 'a1_wqkv', [128, KT, 1536], BF16)
            pmb = self.sb(es, 'a1_pmb', [128, 128], BF16)
            pm32 = self.sb(es, 'a1_pm32', [128, 128])
            gq = self.sb(es, 'a1_g', [128, 4])
            hsb = [self.sb(es, f'a1_hs{i}', [128, KT, 512]) for i in range(2)]
            ub = self.sb(es, 'a1_ub', [128, KT, 512], BF16)
            cs = [self.sb(es, f'a1_cos{i}', [128, 512]) for i in range(2)]
            sn = [self.sb(es, f'a1_sin{i}', [128, 512]) for i in range(2)]
            sq = [self.sb(es, f'a1_sq{i}', [128, 512], BF16) for i in range(2)]
            rsq = [self.sb(es, f'a1_rs{i}', [128, 512]) for i in range(2)]
            yb = [self.sb(es, f'a1_yb{i}', [128, 512], BF16) for i in range(2)]
            t1 = [self.sb(es, f'a1_t1{i}', [128, 512]) for i in range(2)]
            t2 = [self.sb(es, f'a1_t2{i}', [128, 512]) for i in range(2)]
            qst = [self.sb(es, f'a1_qst{i}', [128, 10, 512], BF16) for i in range(2)]
            vst = [self.sb(es, f'a1_vst{i}', [128, 4, 256], BF16) for i in range(2)]
            S.dma(wq[:], self.wqkvb[ki].rearrange("(k p) n -> p k n", p=128), r=[('wqkvb', ki)], w=['wq'])
            S.dma(pm32[:], I['rope_pm'], w=['pm32'])
            S.dma(gq[:], I['attn_g'], w=['gq'])
            self.cp(pmb[:], pm32[:], r=['pm32'], w=['pmb'])
            self.ts(gq[:, 2 * ki:2 * ki + 1], gq[:, 2 * ki:2 * ki + 1], 128.0 ** -0.5, ALU.mult, r=['gq'], w=['gq'])
            for si, (t0, w, isc) in enumerate(self.spans):
                hs, hkey = hsb[si % 2], ('hs', si % 2)
                r_ = 1 if isc else 0
                S.dma(hs[:, :, :w], self.hT_span(t0, w), r=[('hT', si)], w=[hkey])
                S.dma(cs[si % 2][:, :w], I['rope_cos'][:, t0:t0 + w], w=[('cos', si % 2)])
                S.dma(sn[si % 2][:, :w], I['rope_sin'][:, t0:t0 + w], w=[('sin', si % 2)])
                self.norm_span(hs, hkey, w, ab[:, r_, 0, :], ab[:, r_, 1, :], abkey, None, None, ub, 'ub', scr)
                qs_ = qst[si % 2]
                for c in range(10):
                    b = c % 2
                    pq, pss_, pr = ps[b], ps[2 + b], ps[4 + b]
                    for k in range(KT):
                        self.mm(pq[:, :w], wq[:, k, c * 128:(c + 1) * 128], ub[:, k, :w], k == 0, k == KT - 1,
                                r=['wq', 'ub'], w=[('ps', b)])
                    self.act(sq[b][:, :w], pq[:, :w], AF.Square, r=[('ps', b)], w=[('sq2', b)])
                    self.mm(pss_[:, :w], self.onesb[:], sq[b][:, :w], True, True, r=[('sq2', b), 'onesb'], w=[('ps', 2 + b)])
                    self.act(rsq[b][:, :w], pss_[:, :w], AF.Sqrt, r=[('ps', 2 + b), 'epsb'], w=[('rsq', b)],
                             scale=1.0 / 128, bias=self.epsb[:, 0:1])
                    S.op('dve', lambda: nc.vector.reciprocal(out=rsq[b][:, :w], in_=rsq[b][:, :w]), r=[('rsq', b)], w=[('rsq', b)])
                    gcol = 2 * ki + (0 if c < 8 else 1)
                    self.stt(yb[b][:, :w], pq[:, :w], gq[:, gcol:gcol + 1], rsq[b][:, :w], ALU.mult, ALU.mult,
                             r=[('ps', b), 'gq', ('rsq', b)], w=[('yb', b)])
                    self.mm(pr[:, :w], pmb[:], yb[b][:, :w], True, True, r=['pmb', ('yb', b)], w=[('ps', 4 + b)])
                    self.tt(t1[b][:, :w], yb[b][:, :w], cs[si % 2][:, :w], ALU.mult, r=[('yb', b), ('cos', si % 2)], w=[('t1', b)])
                    self.tt(t2[b][:, :w], pr[:, :w], sn[si % 2][:, :w], ALU.mult, r=[('ps', 4 + b), ('sin', si % 2)], w=[('t2', b)])
                    self.tt(qs_[:, c, :w], t1[b][:, :w], t2[b][:, :w], ALU.add, r=[('t1', b), ('t2', b)], w=[('qst', si % 2)])
                S.dma(self.QT.rearrange("(h p) t -> p h t", p=128)[:, :, t0:t0 + w], qs_[:, 0:8, :w], r=[('qst', si % 2)], w=[('QT', si)])
                S.dma(self.KT_.rearrange("(h p) t -> p h t", p=128)[:, :, t0:t0 + w], qs_[:, 8:10, :w], r=[('qst', si % 2)], w=[('KT', si)])
                vs_ = vst[si % 2]
                for tt_ in range(w // 128):
                    for k in range(KT):
                        self.mm(ps[6][:, 0:256], ub[:, k, tt_ * 128:(tt_ + 1) * 128], wq[:, k, 1280:1536], k == 0, k == KT - 1,
                                r=['wq', 'ub'], w=[('ps', 6)])
                    self.cp(vs_[:, tt_, :], ps[6][:, 0:256], r=[('ps', 6)], w=[('vst', si % 2)], e='act')
                S.dma(self.Vs[t0:t0 + w, :].rearrange("(a p) n -> p a n", p=128), vs_[:, :w // 128, :], r=[('vst', si % 2)], w=[('Vs', si)])
        S.barrier(keep=KEEP)

    def attn_a2(self, need_ctx):
        nc, S = self.nc, self.S
        ps = self.ps
        T = self.T
        NB = T // 128
        with ExitStack() as es:
            kT = self.sb(es, 'a2_kT', [128, 2, T], BF16)
            vt = self.sb(es, 'a2_v', [128, NB, 256], BF16)
            qsb = [self.sb(es, f'a2_q{i}', [128, 512], BF16) for i in range(2)]
            pt = [self.sb(es, f'a2_p{i}', [128, 512], BF16) for i in range(4)]
            pt2 = [self.sb(es, f'a2_pp{i}', [128, 512], BF16) for i in range(2)]
            rl = [self.sb(es, f'a2_rl{i}', [128, 512]) for i in range(2)]
            ost = [self.sb(es, f'a2_o{i}', [128, 512], BF16) for i in range(2)]
            S.dma(kT[:], self.KT_.rearrange("(h p) t -> p h t", p=128), w=['kT'])
            for a0 in range(0, NB, 8):
                a1 = min(NB, a0 + 8)
                S.dma(vt[:, a0:a1, :], self.Vs[a0 * 128:a1 * 128, :].rearrange("(a p) n -> p a n", p=128), w=['vt'])
            it = 0
            ie = 0
            for g in range(2):
                for hq in range(4):
                    h = g * 4 + hq
                    for si, (t0, w, isc) in enumerate(self.spans):
                        if isc and not need_ctx:
                            continue
                        nkb = 2 if isc else NB
                        q_, qk = qsb[it % 2], ('q', it % 2)
                        S.dma(q_[:, :w], self.QT[h * 128:(h + 1) * 128, t0:t0 + w], w=[qk])
                        po, pl = ps[4 + it % 2], ps[6 + it % 2]
                        base = ie

                        def stA(kb):
                            sb_ = (base + kb) % 4
                            self.mm(ps[sb_][:, :w], kT[:, g, kb * 128:(kb + 1) * 128], q_[:, :w], True, True,
                                    r=['kT', qk], w=[('ps', sb_)])

                        def stB(kb):
                            sb_ = (base + kb) % 4
                            self.act(pt[sb_][:, :w], ps[sb_][:, :w], AF.Exp, r=[('ps', sb_)], w=[('pt', sb_)])

                        def stC(kb):
                            sb_ = (base + kb) % 4
                            self.mm(po[:, :w], vt[:, kb, g * 128:(g + 1) * 128], pt[sb_][:, :w], kb == 0, kb == nkb - 1,
                                    r=['vt', ('pt', sb_)], w=[('ps', 4 + it % 2)])
                            if kb % 2 == 1:
                                sp_ = (base + kb - 1) % 4
                                p2 = pt2[(kb // 2) % 2]
                                self.tt(p2[:, :w], pt[sp_][:, :w], pt[sb_][:, :w], ALU.add,
                                        r=[('pt', sp_), ('pt', sb_)], w=[('pt2', (kb // 2) % 2)])
                                self.mm(pl[:, :w], self.onesb[:], p2[:, :w], kb == 1, kb == nkb - 1,
                                        r=['onesb', ('pt2', (kb // 2) % 2)], w=[('ps', 6 + it % 2)])
                        PF = 2
                        for kb in range(min(PF, nkb)):
                            stA(kb)
                            stB(kb)
                        for kb in range(nkb):
                            if kb + PF < nkb:
                                stA(kb + PF)
                                stB(kb + PF)
                            stC(kb)
                        ie += nkb
                        S.op('dve', lambda: nc.vector.reciprocal(out=rl[it % 2][:, :w], in_=pl[:, :w]),
                             r=[('ps', 6 + it % 2)], w=[('rl', it % 2)])
                        self.tt(ost[it % 2][:, :w], po[:, :w], rl[it % 2][:, :w], ALU.mult,
                                r=[('ps', 4 + it % 2), ('rl', it % 2)], w=[('ost', it % 2)])
                        S.dma(self.OT[h * 128:(h + 1) * 128, t0:t0 + w], ost[it % 2][:, :w], r=[('ost', it % 2)], w=[('OT', h, si)])
                        it += 1
        S.barrier(keep=KEEP)

    def s5_s1(self, li, ab, abkey):
        S = self.S
        with ExitStack() as es:
            scr = self.norm_alloc(es)
            scr['xn'] = self.sb(es, 's1_xn', [128, KT, 512])
            hsb = [self.sb(es, f's1_hs{i}', [128, KT, 512]) for i in range(2)]
            ubb = [self.sb(es, f's1_ub{i}', [128, KT, 512], BF16) for i in range(2)]
            for si, (t0, w, isc) in enumerate(self.spans):
                hs, hkey = hsb[si % 2], ('hs', si % 2)
                r_ = 1 if isc else 0
                S.dma(hs[:, :, :w], self.hT_span(t0, w), r=[('hT', si)], w=[hkey])
                self.norm_span(hs, hkey, w, ab[:, r_, 0, :], ab[:, r_, 1, :], abkey, None, None, ubb[si % 2], ('ub', si % 2), scr)
                S.dma(self.UT.rearrange("(k p) t -> p k t", p=128)[:, :, t0:t0 + w], ubb[si % 2][:, :, :w],
                      r=[('ub', si % 2)], w=[('UT', si)])
        S.barrier(keep=KEEP)

    def s5_s2(self, ki):
        nc, S, I = self.nc, self.S, self.I
        ps = self.ps
        T = self.T
        PI = float(np.pi)
        with ExitStack() as es:
            es0 = ExitStack()
            par = self.sb(es, 's5par', [128, 3, 64])
            d32 = self.sb(es, 's5d', [32, 32])
            dsk = self.sb(es, 's5dsk', [32, 32, 32], BF16)
            BT = self.sb(es, 's5BT', [32, 64, 2, 128], BF16)
            Cw = self.sb(es, 's5Cw', [128, 3, 64, 32], BF16)
            v = {n: self.sb(es, 's5_' + n, [128, 64]) for n in
                 ('dt', 'mag', 'th', 't', 's', 'y', 'm', 'cos', 'sin', 'abr', 'abi', 'den', 'nr', 'cre', 'cim', 'u1', 'u2')}
            vi = self.sb(es, 's5_ti', [128, 64], mybir.dt.int32)
            PL = self.sb(es, 's5PL', [128, 10, 2, 64])
            nGs = self.sb(es, 's5nGs', [128, 2, 64])
            Bt = self.sb(es0, 's5B', [128, 2, 64, 32])
            Ct = self.sb(es0, 's5C', [128, 2, 64, 32])
            bb = self.sb(es0, 's5bb', [128, 2, 64, 32])
            tmpb = self.sb(es0, 's5tmpb', [128, 64, 32])
            with nc.allow_non_contiguous_dma(reason="small parameter tables"):
                S.dma(par[:].rearrange("p a b -> p (a b)"), I['s5_par'], w=['par'])
                S.dma(Bt[:].rearrange("p a b c -> p (a b c)"), I['s5_B'], w=['Bt'])
                S.dma(Ct[:].rearrange("p a b c -> p (a b c)"), I['s5_C'], w=['Ct'])
                S.dma(d32[:], I['s5_d'], w=['d32'])
            K_ = ['dk']

            def TS(o, i, s1, op0, s2=None, op1=None):
                self.ts(o, i, s1, op0, r=K_ + ['par'], w=K_, s2=s2, op1=op1)

            def TT(o, a, b, op):
                self.tt(o, a, b, op, r=K_ + ['par'], w=K_)
            are, aim, ldt = par[:, 0, :], par[:, 1, :], par[:, 2, :]
            self.act(v['dt'][:], ldt, AF.Exp, r=['par'], w=K_)
            TT(v['t'][:], are, v['dt'][:], ALU.mult)
            self.act(v['mag'][:], v['t'][:], AF.Exp, r=K_, w=K_)
            TT(v['th'][:], aim, v['dt'][:], ALU.mult)

            def sin_of(dst, shift):
                TS(v['t'][:], v['th'][:], 1.0 / (2 * PI), ALU.mult, s2=shift / (2 * PI), op1=ALU.add)
                self.cp(vi[:], v['t'][:], r=K_, w=K_)
                self.cp(v['t'][:], vi[:], r=K_, w=K_)
                TS(v['s'][:], v['th'][:], shift, ALU.add)
                self.stt(v['y'][:], v['t'][:], -2 * PI, v['s'][:], ALU.mult, ALU.add, r=K_, w=K_)
                TS(v['m'][:], v['y'][:], PI, ALU.is_gt)
                self.stt(v['y'][:], v['m'][:], -2 * PI, v['y'][:], ALU.mult, ALU.add, r=K_, w=K_)
                TS(v['m'][:], v['y'][:], -PI, ALU.is_lt)
                self.stt(v['y'][:], v['m'][:], 2 * PI, v['y'][:], ALU.mult, ALU.add, r=K_, w=K_)
                self.act(dst, v['y'][:], AF.Sin, r=K_, w=K_)
            sin_of(v['sin'][:], 0.0)
            sin_of(v['cos'][:], PI / 2)
            TT(v['abr'][:], v['mag'][:], v['cos'][:], ALU.mult)
            TT(v['abi'][:], v['mag'][:], v['sin'][:], ALU.mult)
            TT(v['den'][:], are, are, ALU.mult)
            TT(v['u1'][:], aim, aim, ALU.mult)
            TT(v['den'][:], v['den'][:], v['u1'][:], ALU.add)
            S.op('dve', lambda: nc.vector.reciprocal(out=v['den'][:], in_=v['den'][:]), r=K_, w=K_)
            TS(v['nr'][:], v['abr'][:], -1.0, ALU.add)
            TT(v['u1'][:], v['nr'][:], are, ALU.mult)
            TT(v['u2'][:], v['abi'][:], aim, ALU.mult)
            TT(v['u1'][:], v['u1'][:], v['u2'][:], ALU.add)
            TT(v['cre'][:], v['u1'][:], v['den'][:], ALU.mult)
            TT(v['u1'][:], v['abi'][:], are, ALU.mult)
            TT(v['u2'][:], v['nr'][:], aim, ALU.mult)
            TT(v['u1'][:], v['u1'][:], v['u2'][:], ALU.subtract)
            TT(v['cim'][:], v['u1'][:], v['den'][:], ALU.mult)
            creb = v['cre'][:].unsqueeze(2).to_broadcast([128, 64, 32])
            cimb = v['cim'][:].unsqueeze(2).to_broadcast([128, 64, 32])
            self.tt(bb[:, 0], Bt[:, 0], creb, ALU.mult, r=K_ + ['Bt'], w=['bb'])
            self.tt(tmpb[:], Bt[:, 1], cimb, ALU.mult, r=K_ + ['Bt'], w=['tmpb'])
            self.tt(bb[:, 0], bb[:, 0], tmpb[:], ALU.subtract, r=['bb', 'tmpb'], w=['bb'])
            self.tt(bb[:, 1], Bt[:, 1], creb, ALU.mult, r=K_ + ['Bt'], w=['bb'])
            self.tt(tmpb[:], Bt[:, 0], cimb, ALU.mult, r=K_ + ['Bt', 'bb'], w=['tmpb'])
            self.tt(bb[:, 1], bb[:, 1], tmpb[:], ALU.add, r=['bb', 'tmpb'], w=['bb'])
            n_ = 0
            for dj in range(64):
                for ri in range(2):
                    bank = 6 + (n_ // 4) % 2
                    S.op('pe', lambda: nc.tensor.transpose(out=ps[bank][0:32, (n_ % 4) * 128:(n_ % 4 + 1) * 128],
                                                           in_=bb[:, ri, dj, :], identity=self.ident[:]),
                         r=['bb', 'ident'], w=[('ps', bank)])
                    if n_ % 4 == 3:
                        dj0 = dj - 1
                        self.cp(BT[:, dj0:dj0 + 2, :, :].rearrange("p a b c -> p (a b c)"), ps[bank][0:32, :],
                                r=[('ps', bank)], w=['BT'], e='act')
                    n_ += 1
            self.cp(Cw[:, 0], Ct[:, 0], r=['Ct'], w=['Cw'])
            self.ts(Cw[:, 1], Ct[:, 0], -1.0, ALU.mult, r=['Ct'], w=['Cw'])
            self.ts(Cw[:, 2], Ct[:, 1], -1.0, ALU.mult, r=['Ct'], w=['Cw'])
            self.tt(dsk[:], self.ident[0:32, 0:32].unsqueeze(1).to_broadcast([32, 32, 32]),
                    d32[:].unsqueeze(2).to_broadcast([32, 32, 32]), ALU.mult, r=['ident', 'd32'], w=['dsk'])
            self.cp(PL[:, 0, 0, :], v['cos'][:], r=K_, w=['PL'])
            self.cp(PL[:, 0, 1, :], v['sin'][:], r=K_, w=['PL'])
            for l in range(9):
                c_, s_ = PL[:, l, 0, :], PL[:, l, 1, :]
                self.tt(v['u1'][:], c_, c_, ALU.mult, r=['PL'] + K_, w=K_)
                self.tt(v['u2'][:], s_, s_, ALU.mult, r=['PL'] + K_, w=K_)
                self.tt(PL[:, l + 1, 0, :], v['u1'][:], v['u2'][:], ALU.subtract, r=K_ + ['PL'], w=['PL'])
                self.tt(v['u1'][:], c_, s_, ALU.mult, r=['PL'] + K_, w=K_)
                self.ts(PL[:, l + 1, 1, :], v['u1'][:], 2.0, ALU.mult, r=K_ + ['PL'], w=['PL'])
            self.ts(nGs[:, 0, :], PL[:, 8, 1, :], -1.0, ALU.mult, r=['PL'], w=['nGs'])
            self.ts(nGs[:, 1, :], PL[:, 9, 1, :], -1.0, ALU.mult, r=['PL'], w=['nGs'])

            S.barrier(keep=KEEP)
            es0.close()
            cosT = self.sb(es, 's5cosT', [128, 4, 512])
            sinT = self.sb(es, 's5sinT', [128, 4, 512])
            Rt = self.sb(es, 's5Rt', [128, 4, 512])
            ga = self.sb(es, 's5ga', [128, 4, 256])
            gb_ = self.sb(es, 's5gb', [128, 4, 256])
            ujb = [self.sb(es, f's5uj{i}', [32, T], BF16) for i in range(2)]
            ystb = [self.sb(es, f's5yst{i}', [32, 512]) for i in range(2)]
            Wb = [{n: self.sb(es, f's5w{i}_' + n, [128, 512]) for n in ('t1', 't2', 't3', 't4', 'wir', 'wii', 'wr', 'wi')} for i in range(2)]
            Ppb = [[self.sb(es, f's5P{i}_{j}', [128, 512], BF16) for i in range(4)] for j in range(2)]
            car = self.sb(es, 's5car', [128, 4])
            lat = [s_ for s_ in enumerate(self.spans) if not s_[1][2]]
            ctxs = [s_ for s_ in enumerate(self.spans) if s_[1][2]]
            orders = [ctxs + lat, ctxs + lat[::-1]]
            it = 0
            for dr in range(2):
                Y = self.YF if dr == 0 else self.YB
                for jb in range(8):
                    cols = slice(dr * 32 + 4 * jb, dr * 32 + 4 * jb + 4)
                    tk = ['tab']
                    S.op('pool', lambda: nc.gpsimd.memset(cosT[:, :, 0:1], 1.0), r=[], w=tk)
                    S.op('pool', lambda: nc.gpsimd.memset(sinT[:, :, 0:1], 0.0), r=[], w=tk)
                    for l in range(9):
                        n = 1 << l
                        pc = PL[:, l, 0, cols].unsqueeze(2).to_broadcast([128, 4, n])
                        ps_ = PL[:, l, 1, cols].unsqueeze(2).to_broadcast([128, 4, n])
                        c0, s0 = cosT[:, :, 0:n], sinT[:, :, 0:n]
                        self.tt(ga[:, :, 0:n], c0, pc, ALU.mult, r=tk + ['PL'], w=['ga'], e='pool')
                        self.tt(gb_[:, :, 0:n], s0, ps_, ALU.mult, r=tk + ['PL'], w=['gb'], e='pool')
                        self.tt(cosT[:, :, n:2 * n], ga[:, :, 0:n], gb_[:, :, 0:n], ALU.subtract, r=['ga', 'gb'], w=tk, e='pool')
                        self.tt(ga[:, :, 0:n], s0, pc, ALU.mult, r=tk + ['PL'], w=['ga'], e='pool')
                        self.tt(gb_[:, :, 0:n], c0, ps_, ALU.mult, r=tk + ['PL'], w=['gb'], e='pool')
                        self.tt(sinT[:, :, n:2 * n], ga[:, :, 0:n], gb_[:, :, 0:n], ALU.add, r=['ga', 'gb'], w=tk, e='pool')
                    self.cp(Rt[:], v['mag'][:, cols].unsqueeze(2).to_broadcast([128, 4, 512]), r=K_, w=tk, e='pool')
                    for jj in range(4):
                        j = 4 * jb + jj
                        dj = dr * 32 + j
                        uj, ukey = ujb[it % 2], ('uj', it % 2)
                        S.dma(uj[:], self.UT[32 * j:32 * j + 32, :], w=[ukey])
                        S.op('dve', lambda: nc.vector.memset(car[:], 0.0), r=[], w=['car'])
                        cT, sT, rT = cosT[:, jj, :], sinT[:, jj, :], Rt[:, jj, :]
                        for n2, (si, (t0, w, isc)) in enumerate(orders[dr]):
                            b2 = n2 % 2
                            W_, Pp, yst = Wb[b2], Ppb[b2], ystb[b2]
                            kk = lambda n: (n, b2)
                            pbr, pbi, py = ps[2 * b2], ps[2 * b2 + 1], ps[4 + b2]
                            usl = uj[:, t0:t0 + w]
                            rhs = usl if dr == 0 else usl[:, ::-1]
                            self.mm(pbr[:, :w], BT[:, dj, 0, :], rhs, True, True, r=['BT', ukey], w=[('ps', 2 * b2)])
                            self.mm(pbi[:, :w], BT[:, dj, 1, :], rhs, True, True, r=['BT', ukey], w=[('ps', 2 * b2 + 1)])
                            self.tt(W_['t1'][:, :w], pbr[:, :w], cT[:, :w], ALU.mult, r=[('ps', 2 * b2)] + tk, w=[kk('t1')])
                            self.tt(W_['t2'][:, :w], pbi[:, :w], sT[:, :w], ALU.mult, r=[('ps', 2 * b2 + 1)] + tk, w=[kk('t2')])
                            self.tt(W_['wir'][:, :w], W_['t1'][:, :w], W_['t2'][:, :w], ALU.add, r=[kk('t1'), kk('t2')], w=[kk('wir')])
                            self.tt(W_['t3'][:, :w], pbi[:, :w], cT[:, :w], ALU.mult, r=[('ps', 2 * b2 + 1)] + tk, w=[kk('t3')])
                            self.tt(W_['t4'][:, :w], pbr[:, :w], sT[:, :w], ALU.mult, r=[('ps', 2 * b2)] + tk, w=[kk('t4')])
                            self.tt(W_['wii'][:, :w], W_['t3'][:, :w], W_['t4'][:, :w], ALU.subtract, r=[kk('t3'), kk('t4')], w=[kk('wii')], e='pool')
                            S.op('dve', lambda: nc.vector.tensor_tensor_scan(out=W_['wr'][:, :w], data0=rT[:, :w], data1=W_['wir'][:, :w],
                                                                           initial=car[:, 0:1], op0=ALU.mult, op1=ALU.add),
                                 r=[kk('wir'), 'car'] + tk, w=[kk('wr')])
                            S.op('dve', lambda: nc.vector.tensor_tensor_scan(out=W_['wi'][:, :w], data0=rT[:, :w], data1=W_['wii'][:, :w],
                                                                           initial=car[:, 1:2], op0=ALU.mult, op1=ALU.add),
                                 r=[kk('wii'), 'car'] + tk, w=[kk('wi')])
                            lv = 0 if w == 256 else 1
                            Gc, Gs, nG = PL[:, 8 + lv, 0, dj:dj + 1], PL[:, 8 + lv, 1, dj:dj + 1], nGs[:, lv, dj:dj + 1]
                            wrl, wil = W_['wr'][:, w - 1:w], W_['wi'][:, w - 1:w]
                            self.ts(car[:, 2:3], wrl, Gc, ALU.mult, r=[kk('wr'), 'PL', 'car'], w=['car'])
                            self.stt(car[:, 0:1], wil, nG, car[:, 2:3], ALU.mult, ALU.add, r=[kk('wi'), 'nGs', 'car'], w=['car'])
                            self.ts(car[:, 3:4], wil, Gc, ALU.mult, r=[kk('wi'), 'PL', 'car'], w=['car'])
                            self.stt(car[:, 1:2], wrl, Gs, car[:, 3:4], ALU.mult, ALU.add, r=[kk('wr'), 'PL', 'car'], w=['car'])
                            self.tt(Pp[0][:, :w], cT[:, :w], W_['wr'][:, :w], ALU.mult, r=tk + [kk('wr')], w=[('P', 0, b2)], e='pool')
                            self.tt(Pp[1][:, :w], sT[:, :w], W_['wi'][:, :w], ALU.mult, r=tk + [kk('wi')], w=[('P', 1, b2)], e='pool')
                            self.tt(Pp[2][:, :w], sT[:, :w], W_['wr'][:, :w], ALU.mult, r=tk + [kk('wr')], w=[('P', 2, b2)], e='pool')
                            self.tt(Pp[3][:, :w], cT[:, :w], W_['wi'][:, :w], ALU.mult, r=tk + [kk('wi')], w=[('P', 3, b2)], e='pool')
                            yk = ('ps', 4 + b2)
                            self.mm(py[0:32, :w], Cw[:, 0, dj, :], Pp[0][:, :w], True, False, r=['Cw', ('P', 0, b2)], w=[yk])
                            self.mm(py[0:32, :w], Cw[:, 1, dj, :], Pp[1][:, :w], False, False, r=['Cw', ('P', 1, b2)], w=[yk])
                            self.mm(py[0:32, :w], Cw[:, 2, dj, :], Pp[2][:, :w], False, False, r=['Cw', ('P', 2, b2)], w=[yk])
                            self.mm(py[0:32, :w], Cw[:, 2, dj, :], Pp[3][:, :w], False, dr == 1, r=['Cw', ('P', 3, b2)], w=[yk])
                            if dr == 0:
                                self.mm(py[0:32, :w], dsk[:, j, :], usl, False, True, r=['dsk', ukey], w=[yk])
                            src = py[0:32, :w] if dr == 0 else py[0:32, :w][:, ::-1]
                            self.cp(yst[:, :w], src, r=[yk], w=[('yst', b2)], e='act')
                            S.dma(Y[32 * j:32 * j + 32, t0:t0 + w], yst[:, :w], r=[('yst', b2)], w=[('Y', dr, j, si)])
                        it += 1
        S.barrier(keep=KEEP)

    def ssd_d1(self, li, ki, ab, abkey):
        nc, S, I = self.nc, self.S, self.I
        ps = self.ps
        with ExitStack() as es:
            scr = self.norm_alloc(es)
            scr['xn'] = self.sb(es, 'd1_xn', [128, KT, 512])
            win = self.sb(es, 'd1_win', [128, KT, 5184], BF16)
            dtb = self.sb(es, 'd1_dtb', [128, 64])
            hsb = [self.sb(es, f'd1_hs{i}', [128, KT, 512]) for i in range(2)]
            ub = self.sb(es, 'd1_ub', [128, KT, 512], BF16)
            zst = [self.sb(es, f'd1_zst{i}', [128, 2 * D], BF16) for i in range(2)]
            xst = [self.sb(es, f'd1_xst{i}', [128, 8, 512], BF16) for i in range(2)]
            dst = [self.sb(es, f'd1_dst{i}', [128, 64]) for i in range(2)]
            for k in range(KT):
                S.dma(win[:, k, :], self.winb[ki][k * 128:(k + 1) * 128, :], r=[('winb', ki)], w=['win'])
            S.dma(dtb[:], I['ssd_dtb'], w=['dtb'])
            nz = nx = nd = 0
            for si, (t0, w, isc) in enumerate(self.spans):
                hs, hkey = hsb[si % 2], ('hs', si % 2)
                r_ = 1 if isc else 0
                S.dma(hs[:, :, :w], self.hT_span(t0, w), r=[('hT', si)], w=[hkey])
                self.norm_span(hs, hkey, w, ab[:, r_, 0, :], ab[:, r_, 1, :], abkey, None, None, ub, 'ub', scr)
                for tt_ in range(w // 128):
                    zt, zk = zst[nz % 2], ('zst', nz % 2)
                    for cg in range(4):
                        b = cg % 2
                        for k in range(KT):
                            self.mm(ps[b][:, :], ub[:, k, tt_ * 128:(tt_ + 1) * 128], win[:, k, cg * 512:(cg + 1) * 512],
                                    k == 0, k == KT - 1, r=['win', 'ub'], w=[('ps', b)])
                        self.act(zt[:, cg * 512:(cg + 1) * 512], ps[b][:, :], AF.Silu, r=[('ps', b)], w=[zk])
                    S.dma(self.ZS[t0 + tt_ * 128:t0 + (tt_ + 1) * 128, :], zt[:], r=[zk], w=[('ZS', nz)])
                    nz += 1
                    dt_, dk = dst[nd % 2], ('dst', nd % 2)
                    for k in range(KT):
                        self.mm(ps[6][:, 0:64], ub[:, k, tt_ * 128:(tt_ + 1) * 128], win[:, k, 5120:5184],
                                k == 0, k == KT - 1, r=['win', 'ub'], w=[('ps', 6)])
                    self.tt(dt_[:], ps[6][:, 0:64], dtb[:], ALU.add, r=[('ps', 6), 'dtb'], w=[dk])
                    self.act(dt_[:], dt_[:], AF.Exp, r=[dk], w=[dk])
                    self.act(dt_[:], dt_[:], AF.Ln, r=[dk], w=[dk], bias=1.0, scale=1.0)
                    S.dma(self.DTs[t0 + tt_ * 128:t0 + (tt_ + 1) * 128, :], dt_[:], r=[dk], w=[('DTs', nd)])
                    nd += 1
                for c8 in range(3):
                    xt, xk = xst[nx % 2], ('xst', nx % 2)
                    for c in range(8):
                        ct = c8 * 8 + c
                        b = 2 + ct % 2
                        for k in range(KT):
                            self.mm(ps[b][:, :w], win[:, k, 2048 + ct * 128:2048 + (ct + 1) * 128], ub[:, k, :w],
                                    k == 0, k == KT - 1, r=['win', 'ub'], w=[('ps', b)])
                        self.cp(xt[:, c, :w], ps[b][:, :w], r=[('ps', b)], w=[xk], e='dve' if c % 2 == 0 else 'act')
                    S.dma(self.XBC.rearrange("(k p) t -> p k t", p=128)[:, c8 * 8:(c8 + 1) * 8, t0:t0 + w], xt[:, :, :w],
                          r=[xk], w=[('XBC', nx)])
                    nx += 1
        S.barrier(keep=KEEP)

    def ssd_d2(self):
        nc, S, I = self.nc, self.S, self.I
        ps = self.ps
        T = self.T
        with ExitStack() as es:
            cw = self.sb(es, 'd2_cw', [128, 24, 5])
            cb = self.sb(es, 'd2_cb', [128, 24])
            xin = [self.sb(es, f'd2_xin{i}', [128, 24, 516], BF16) for i in range(2)]
            xc = [self.sb(es, f'd2_xc{i}', [128, 24, 512], BF16) for i in range(2)]
            cv = [self.sb(es, f'd2_cv{i}', [128, 512]) for i in range(2)]
            xts = [self.sb(es, f'd2_xts{i}', [128, 2 * D], BF16) for i in range(2)]
            bts = [self.sb(es, f'd2_bts{i}', [128, 512], BF16) for i in range(2)]
            S.dma(cw[:].rearrange("p a b -> p (a b)"), I['ssd_cw'], w=['cw'])
            S.dma(cb[:], I['ssd_cb'], w=['cb'])
            nt = 0
            for si, (t0, w, isc) in enumerate(self.spans):
                s0, s1 = (0, TC) if isc else (TC, T)
                lo, hi = max(t0 - 2, s0), min(t0 + w + 2, s1)
                xi, xik = xin[si % 2], ('xin', si % 2)
                if lo > t0 - 2:
                    S.op('dve', lambda: nc.vector.memset(xi[:, :, 0:2], 0.0), w=[xik])
                if hi < t0 + w + 2:
                    S.op('dve', lambda: nc.vector.memset(xi[:, :, w + 2:w + 4], 0.0), w=[xik])
                for c8 in range(3):
                    S.dma(xi[:, c8 * 8:(c8 + 1) * 8, lo - (t0 - 2):hi - (t0 - 2)],
                          self.XBC.rearrange("(k p) t -> p k t", p=128)[:, c8 * 8:(c8 + 1) * 8, lo:hi], w=[xik])
                xo, xok = xc[si % 2], ('xc', si % 2)
                for ct in range(24):
                    c_, ck = cv[ct % 2], ('cv', ct % 2)
                    self.ts(c_[:, :w], xi[:, ct, 0:w], cw[:, ct, 0:1], ALU.mult, r=[xik, 'cw', 'cb'], w=[ck], s2=cb[:, ct:ct + 1], op1=ALU.add)
                    for k in range(1, 5):
                        self.stt(c_[:, :w], xi[:, ct, k:k + w], cw[:, ct, k:k + 1], c_[:, :w], ALU.mult, ALU.add, r=[xik, 'cw', ck], w=[ck])
                    self.act(xo[:, ct, :w], c_[:, :w], AF.Silu, r=[ck], w=[xok])
                S.dma(self.BCf.rearrange("(k p) t -> p k t", p=128)[:, :, t0:t0 + w], xo[:, 16:24, :w], r=[xok], w=[('BCf', si)])
                for tt_ in range(w // 128):
                    tok = t0 + tt_ * 128
                    xt, xtk_ = xts[nt % 2], ('xts', nt % 2)
                    for half in range(2):
                        bank = half
                        pbv = ps[bank][:].bitcast(BF16)
                        for q in range(8):
                            ct = half * 8 + q
                            S.op('pe', lambda: nc.tensor.transpose(out=pbv[:, q * 128:(q + 1) * 128],
                                                                   in_=xo[:, ct, tt_ * 128:(tt_ + 1) * 128], identity=self.identb[:]),
                                 r=[xok, 'identb'], w=[('ps', bank)])
                        self.cp(xt[:, half * 1024:(half + 1) * 1024], pbv[:, 0:1024], r=[('ps', bank)], w=[xtk_],
                                e='dve' if half == 0 else 'act')
                    S.dma(self.XTK[tok:tok + 128, :], xt[:], r=[xtk_], w=[('XTK', nt)])
                    bt, btk_ = bts[nt % 2], ('bts', nt % 2)
                    pbv = ps[2][:].bitcast(BF16)
                    for q in range(4):
                        S.op('pe', lambda: nc.tensor.transpose(out=pbv[:, q * 128:(q + 1) * 128],
                                                               in_=xo[:, 16 + q, tt_ * 128:(tt_ + 1) * 128], identity=self.identb[:]),
                             r=[xok, 'identb'], w=[('ps', 2)])
                    self.cp(bt[:], pbv[:, 0:512], r=[('ps', 2)], w=[btk_])
                    S.dma(self.BTK[tok:tok + 128, :], bt[:], r=[btk_], w=[('BTK', nt)])
                    nt += 1
        S.barrier(keep=KEEP)

    def ssd_d3(self):
        nc, S, I = self.nc, self.S, self.I
        ps = self.ps
        T = self.T
        NB = T // 128
        with ExitStack() as es:
            msk = self.sb(es, 'd3_msk', [128, 4, 128])
            aneg = self.sb(es, 'd3_aneg', [128, 64])
            Sst = self.sb(es, 'd3_S', [128, 4, 512])
            Sb = self.sb(es, 'd3_Sb', [128, 4, 512], BF16)
            xtk = [self.sb(es, f'd3_xtk{i}', [128, 32, 64], BF16) for i in range(2)]
            btk = [self.sb(es, f'd3_btk{i}', [128, 512], BF16) for i in range(2)]
            bcf = [self.sb(es, f'd3_bcf{i}', [128, 8, 128], BF16) for i in range(2)]
            dtt_ = [self.sb(es, f'd3_dt{i}', [128, 32]) for i in range(2)]
            sm = self.sb(es, 'd3_sm', [128, 8, 32])
            cs = self.sb(es, 'd3_cs', [128, 64])
            xdt = self.sb(es, 'd3_xdt', [128, 32, 64], BF16)
            txdt = self.sb(es, 'd3_txdt', [128, 32, 64], BF16)
            dec = [self.sb(es, f'd3_dec{i}', [128, 128]) for i in range(4)]
            MT = [self.sb(es, f'd3_MT{i}', [128, 128], BF16) for i in range(4)]
            ytmp = [self.sb(es, f'd3_ytmp{i}', [128, 512]) for i in range(2)]
            ych = [self.sb(es, f'd3_ych{i}', [128, 2 * D]) for i in range(2)]
            S.dma(msk[:].rearrange("p a b -> p (a b)"), I['ssd_msk'], w=['msk'])
            S.dma(aneg[:], I['ssd_alog'], w=['aneg'])
            self.act(aneg[:], aneg[:], AF.Exp, r=['aneg'], w=['aneg'])
            self.ts(aneg[:], aneg[:], -1.0, ALU.mult, r=['aneg'], w=['aneg'])
            ctx_ch = list(range(TC // 128))
            lat_ch = list(range(TC // 128, NB))
            orders = [ctx_ch + lat_ch, ctx_ch[::-1] + lat_ch[::-1]]
            it = 0
            for dr in range(2):
                S.op('dve', lambda: nc.vector.memset(Sst[:], 0.0), w=['S'])
                S.op('dve', lambda: nc.vector.memset(Sb[:], 0.0), w=['Sb'])
                tri, mneg = msk[:, dr, :], msk[:, 2 + dr, :]
                for c in orders[dr]:
                    tok = c * 128
                    p2 = it % 2
                    x_, xk = xtk[p2], ('xtk', p2)
                    b_, bk = btk[p2], ('btk', p2)
                    f_, fk = bcf[p2], ('bcf', p2)
                    d_, dk = dtt_[p2], ('dt', p2)
                    S.dma(x_[:].rearrange("p h q -> p (h q)"), self.XTK[tok:tok + 128, :], w=[xk])
                    S.dma(b_[:], self.BTK[tok:tok + 128, :], w=[bk])
                    S.dma(f_[:], self.BCf.rearrange("(k p) t -> p k t", p=128)[:, :, tok:tok + 128], w=[fk])
                    with nc.allow_non_contiguous_dma(reason="dt half rows"):
                        S.dma(d_[:], self.DTs[tok:tok + 128, dr * 32:(dr + 1) * 32], w=[dk])
                    smk = ['sm']
                    self.tt(sm[:, 0, :], d_[:], aneg[:, dr * 32:(dr + 1) * 32], ALU.mult, r=[dk, 'aneg'], w=smk)
                    self.mm(ps[0][:, 0:32], tri, sm[:, 0, :], True, True, r=['msk'] + smk, w=[('ps', 0)])
                    self.mm(ps[0][:, 32:64], self.ones32[:], sm[:, 0, :], True, True, r=['ones32'] + smk, w=[('ps', 0)])
                    self.cp(cs[:], ps[0][:, 0:64], r=[('ps', 0)], w=['cs'])
                    self.ts(sm[:, 1, :], cs[:, 0:32], -1.0, ALU.mult, r=['cs'], w=smk)
                    self.act(sm[:, 2, :], cs[:, 0:32], AF.Exp, r=['cs'], w=smk)
                    self.act(sm[:, 3, :], cs[:, 32:64], AF.Exp, r=['cs'], w=smk)
                    self.tt(sm[:, 4, :], cs[:, 32:64], cs[:, 0:32], ALU.subtract, r=['cs'], w=smk)
                    self.act(sm[:, 5, :], sm[:, 4, :], AF.Exp, r=smk, w=smk)
                    self.tt(sm[:, 6, :], d_[:], sm[:, 5, :], ALU.mult, r=[dk] + smk, w=smk)
                    self.tt(xdt[:], x_[:], d_[:].unsqueeze(2).to_broadcast([128, 32, 64]), ALU.mult, r=[xk, dk], w=['xdt'])
                    self.tt(txdt[:], x_[:], sm[:, 6, :].unsqueeze(2).to_broadcast([128, 32, 64]), ALU.mult, r=[xk] + smk, w=['txdt'], e='pool')
                    for g in range(4):
                        self.mm(ps[1][:, g * 128:(g + 1) * 128], f_[:, g, :], f_[:, 4 + g, :], True, True, r=[fk], w=[('ps', 1)])
                    y_, yk = ych[p2], ('ych', p2)

                    def hA(h):
                        bkn = 2 + (h // 4) % 2
                        col = (h % 4) * 128
                        self.mm(ps[bkn][:, col:col + 128], cs[:, h:h + 1].to_broadcast([128, 128]), self.ident[:], True, False,
                                r=['cs', 'ident'], w=[('ps', bkn)])
                        self.mm(ps[bkn][:, col:col + 128], self.ident[:], mneg, False, True, r=['ident', 'msk'], w=[('ps', bkn)])

                    def hB(h):
                        bkn = 2 + (h // 4) % 2
                        col = (h % 4) * 128
                        self.act(dec[h % 4][:], ps[bkn][:, col:col + 128], AF.Exp, r=[('ps', bkn)] + smk, w=[('dec', h % 4)],
                                 bias=sm[:, 1, h:h + 1], scale=1.0)

                    def hCD(h):
                        g, hh = h // 8, h % 8
                        pa = ps[4 + g % 2]
                        self.tt(MT[h % 4][:], dec[h % 4][:], ps[1][:, g * 128:(g + 1) * 128], ALU.mult,
                                r=[('dec', h % 4), ('ps', 1)], w=[('MT', h % 4)])
                        self.mm(pa[:, hh * 64:(hh + 1) * 64], MT[h % 4][:], xdt[:, h, :], True, True,
                                r=[('MT', h % 4), 'xdt'], w=[('ps', 4 + g % 2)])
                    PF = 3
                    for h in range(PF):
                        hA(h)
                        hB(h)
                    for h in range(32):
                        if h + PF < 32:
                            hA(h + PF)
                            hB(h + PF)
                        hCD(h)
                        if h % 8 == 7:
                            g = h // 8
                            pa, pbk = ps[4 + g % 2], ps[6 + g % 2]
                            self.mm(pbk[:, :], f_[:, 4 + g, :], Sb[:, g, :], True, True, r=[fk, 'Sb'], w=[('ps', 6 + g % 2)])
                            self.tt(ytmp[g % 2][:].rearrange("p (h q) -> p h q", h=8), pbk[:, :].rearrange("p (h q) -> p h q", h=8),
                                    sm[:, 2, g * 8:(g + 1) * 8].unsqueeze(2).to_broadcast([128, 8, 64]), ALU.mult,
                                    r=[('ps', 6 + g % 2)] + smk, w=[('ytmp', g % 2)])
                            self.tt(y_[:, g * 512:(g + 1) * 512], pa[:, :], ytmp[g % 2][:], ALU.add,
                                    r=[('ps', 4 + g % 2), ('ytmp', g % 2)], w=[yk])
                    S.dma(self.YD[dr, tok:tok + 128, :], y_[:], r=[yk], w=[('YD', dr, c)])
                    for g in range(4):
                        pst = ps[2 + g % 2]
                        self.mm(pst[:, :], b_[:, g * 128:(g + 1) * 128], txdt[:, g * 8:(g + 1) * 8, :].rearrange("p h q -> p (h q)"),
                                True, True, r=[bk, 'txdt'], w=[('ps', 2 + g % 2)])
                        sv = Sst[:, g, :].rearrange("p (h q) -> p h q", h=8)
                        self.tt(sv, sv, sm[:, 3, g * 8:(g + 1) * 8].unsqueeze(2).to_broadcast([128, 8, 64]), ALU.mult, r=['S'] + smk, w=['S'])
                        self.tt(Sst[:, g, :], Sst[:, g, :], pst[:, :], ALU.add, r=['S', ('ps', 2 + g % 2)], w=['S'])
                        self.cp(Sb[:, g, :], Sst[:, g, :], r=['S'], w=['Sb'], e='pool')
                    it += 1
        S.barrier(keep=KEEP)

    def ssd_d4(self, li, ki, ab, abkey, need_ctx):
        nc, S, I = self.nc, self.S, self.I
        ps = self.ps
        with ExitStack() as es:
            wout = self.sb(es, 'd4_wout', [128, 16, D], BF16)
            ngr = self.sb(es, 'd4_ng', [128, 2 * D])
            dh = self.sb(es, 'd4_dh', [128, 32])
            eps_ = self.sb(es, 'd4_eps', [128, 1])
            hsb = [self.sb(es, f'd4_hs{i}', [128, KT, 512]) for i in range(2)]
            yf = [self.sb(es, f'd4_yf{i}', [128, 2 * D]) for i in range(2)]
            yb = [self.sb(es, f'd4_yb{i}', [128, 2 * D]) for i in range(2)]
            xk_ = [self.sb(es, f'd4_x{i}', [128, 2 * D], BF16) for i in range(2)]
            zs = [self.sb(es, f'd4_z{i}', [128, 2 * D], BF16) for i in range(2)]
            tmp = self.sb(es, 'd4_tmp', [128, 2 * D])
            ynb = self.sb(es, 'd4_ynb', [128, 2 * D], BF16)
            ynT = self.sb(es, 'd4_ynT', [128, 16, 512], BF16)
            st = self.sb(es, 'd4_st', [128, 4])
            S.dma(wout[:], self.woutb[ki].rearrange("(k p) n -> p k n", p=128), r=[('woutb', ki)], w=['wout'])
            S.dma(ngr[:], I['ssd_ng'], w=['ngr'])
            S.dma(dh[:], I['ssd_dh'], w=['dh'])
            S.op('dve', lambda: nc.vector.memset(eps_[:], EPS), w=['eps'])
            spans = [s_ for s_ in enumerate(self.spans) if need_ctx or not s_[1][2]]
            nt = 0
            for n_, (si, (t0, w, isc)) in enumerate(spans):
                hs, hkey = hsb[n_ % 2], ('hs', n_ % 2)
                r_ = 1 if isc else 0
                S.dma(hs[:, :, :w], self.hT_span(t0, w), r=[('hT', si)], w=[hkey])
                for tt_ in range(w // 128):
                    tok = t0 + tt_ * 128
                    p2 = nt % 2
                    S.dma(yf[p2][:], self.YD[0, tok:tok + 128, :], w=[('yf', p2)])
                    S.dma(yb[p2][:], self.YD[1, tok:tok + 128, :], w=[('yb', p2)])
                    S.dma(xk_[p2][:], self.XTK[tok:tok + 128, :], w=[('x', p2)])
                    S.dma(zs[p2][:], self.ZS[tok:tok + 128, :], w=[('z', p2)])
                    y = yf[p2]
                    yk = ('yf', p2)
                    self.tt(y[:], y[:], yb[p2][:], ALU.add, r=[yk, ('yb', p2)], w=[yk], e='pool')
                    self.tt(tmp[:].rearrange("p (h q) -> p h q", h=32), xk_[p2][:].rearrange("p (h q) -> p h q", h=32),
                            dh[:].unsqueeze(2).to_broadcast([128, 32, 64]), ALU.mult, r=[('x', p2), 'dh'], w=['tmp'], e='pool')
                    self.tt(y[:], y[:], tmp[:], ALU.add, r=[yk, 'tmp'], w=[yk])
                    self.tt(y[:], y[:], zs[p2][:], ALU.mult, r=[yk, ('z', p2)], w=[yk])
                    self.act(tmp[:], y[:], AF.Square, r=[yk], w=['tmp', 'st'], accum_out=st[:, 0:1])
                    self.act(st[:, 1:2], st[:, 0:1], AF.Sqrt, r=['st', 'eps'], w=['st'], scale=1.0 / (2 * D), bias=eps_[:, 0:1])
                    S.op('dve', lambda: nc.vector.reciprocal(out=st[:, 2:3], in_=st[:, 1:2]), r=['st'], w=['st'])
                    self.stt(ynb[:], y[:], st[:, 2:3], ngr[:], ALU.mult, ALU.mult, r=[yk, 'st', 'ngr'], w=['ynb'])
                    for half in range(2):
                        bank = half
                        pbv = ps[bank][:].bitcast(BF16)
                        for q in range(8):
                            k = half * 8 + q
                            S.op('pe', lambda: nc.tensor.transpose(out=pbv[:, q * 128:(q + 1) * 128],
                                                                   in_=ynb[:, k * 128:(k + 1) * 128], identity=self.identb[:]),
                                 r=['ynb', 'identb'], w=[('ps', bank)])
                        self.cp(ynT[:, half * 8:(half + 1) * 8, tt_ * 128:(tt_ + 1) * 128],
                                pbv[:, 0:1024].rearrange("p (k t) -> p k t", k=8), r=[('ps', bank)], w=['ynT'],
                                e='dve' if half == 0 else 'act')
                    nt += 1
                for ct in range(KT):
                    pb = ps[2 + ct % 2]
                    for k in range(16):
                        self.mm(pb[:, :w], wout[:, k, ct * 128:(ct + 1) * 128], ynT[:, k, :w], k == 0, k == 15,
                                r=['wout', 'ynT'], w=[('ps', 2 + ct % 2)])
                    self.stt(hs[:, ct, :w], pb[:, :w], ab[:, r_, 2, ct:ct + 1], hs[:, ct, :w], ALU.mult, ALU.add,
                             r=[('ps', 2 + ct % 2), abkey, hkey], w=[hkey])
                S.dma(self.hT_span(t0, w), hs[:, :, :w], r=[hkey], w=[('hT', si)])
        S.barrier(keep=KEEP)

    def layer(self, li, kind, ki, need_ctx):
        S = self.S
        ps = self.ps
        with ExitStack() as es:
            ab = self.layer_scalars(es, li)
            abkey = ('ab', li)
            if kind == 'a':
                self.attn_a1(li, ki, ab, abkey)
                self.attn_a2(need_ctx)
            if kind == 's':
                self.s5_s1(li, ab, abkey)
                self.s5_s2(ki)
            if kind == 'd':
                self.ssd_d1(li, ki, ab, abkey)
                self.ssd_d2()
                self.ssd_d3()
                self.ssd_d4(li, ki, ab, abkey, need_ctx)
            with ExitStack() as es2:
                scr = self.norm_alloc(es2)
                M = self.moe_alloc(es2, li)
                scr['xn'] = M['acc']
                hsb = [self.sb(es2, f'hs{i}', [128, KT, 512]) for i in range(2)]
                u32 = self.sb(es2, 'u32', [128, KT, 512])
                ub = self.sb(es2, 'ub', [128, KT, 512], BF16)
                if kind == 'a':
                    wo = self.sb(es2, 'a3_wo', [128, KT, D], BF16)
                    otb = [self.sb(es2, f'a3_ot{i}', [128, KT, 512], BF16) for i in range(2)]
                    S.dma(wo[:], self.wob[ki].rearrange("(k p) n -> p k n", p=128), r=[('wob', ki)], w=['wo'])
                if kind == 's':
                    wgl = self.sb(es2, 's3_wglu', [128, KT, 2 * D], BF16)
                    bgl = self.sb(es2, 's3_bglu', [128, 16])
                    sgl = [self.sb(es2, f's3_sig{i}', [128, 512]) for i in range(2)]
                    ymx = [self.sb(es2, f's3_ymx{i}', [128, 512]) for i in range(2)]
                    S.dma(wgl[:], self.wglub[ki].rearrange("(k p) n -> p k n", p=128), r=[('wglub', ki)], w=['wgl'])
                    S.dma(bgl[:], self.I['s5_bglu'], w=['bgl'])
                spans = [s for s in enumerate(self.spans) if need_ctx or not s[1][2]]
                for n_, (si, (t0, w, isc)) in enumerate(spans):
                    hs, hkey = hsb[n_ % 2], ('hs', n_ % 2)
                    r_ = 1 if isc else 0
                    S.dma(hs[:, :, :w], self.hT_span(t0, w), r=[('hT', si)], w=[hkey])
                    if kind == 's':
                        acc = M['acc']
                        akeys = [('acc', k) for k in range(KT)]
                        S.dma(u32[:, :, :w], self.YF.rearrange("(k p) t -> p k t", p=128)[:, :, t0:t0 + w], w=['u32'])
                        S.dma(acc[:, :, :w], self.YB.rearrange("(k p) t -> p k t", p=128)[:, :, t0:t0 + w], w=akeys)
                        self.tt(u32[:, :, :w], u32[:, :, :w], acc[:, :, :w], ALU.add, r=['u32'] + akeys, w=['u32'])
                        self.tt(acc[:, :, :w], u32[:, :, :w], u32[:, :, :w], ALU.mult, r=['u32'], w=akeys, e='pool')
                        self.ts(acc[:, :, :w], acc[:, :, :w], 0.044715, ALU.mult, r=akeys, w=akeys, s2=1.0, op1=ALU.add)
                        self.tt(acc[:, :, :w], acc[:, :, :w], u32[:, :, :w], ALU.mult, r=['u32'] + akeys, w=akeys, e='pool')
                        self.act(acc[:, :, :w], acc[:, :, :w], AF.Sigmoid, r=akeys, w=akeys, scale=2.0 * float(np.sqrt(2.0 / np.pi)))
                        self.tt(ub[:, :, :w], acc[:, :, :w], u32[:, :, :w], ALU.mult, r=['u32'] + akeys, w=['ub'])
                        for ct in range(KT):
                            b = ct % 2
                            pa, pb2 = ps[b], ps[2 + b]
                            for k in range(KT):
                                self.mm(pa[:, :w], wgl[:, k, ct * 128:(ct + 1) * 128], ub[:, k, :w], k == 0, k == KT - 1,
                                        r=['wgl', 'ub'], w=[('ps', b)])
                            for k in range(KT):
                                self.mm(pb2[:, :w], wgl[:, k, D + ct * 128:D + (ct + 1) * 128], ub[:, k, :w], k == 0, k == KT - 1,
                                        r=['wgl', 'ub'], w=[('ps', 2 + b)])
                            self.act(sgl[b][:, :w], pb2[:, :w], AF.Sigmoid, r=[('ps', 2 + b), 'bgl'], w=[('sgl', b)],
                                     bias=bgl[:, 8 + ct:9 + ct], scale=1.0)
                            self.stt(ymx[b][:, :w], pa[:, :w], bgl[:, ct:ct + 1], sgl[b][:, :w], ALU.add, ALU.mult,
                                     r=[('ps', b), 'bgl', ('sgl', b)], w=[('ymx', b)])
                            self.stt(hs[:, ct, :w], ymx[b][:, :w], ab[:, r_, 2, ct:ct + 1], hs[:, ct, :w], ALU.mult, ALU.add,
                                     r=[('ymx', b), abkey, hkey], w=[hkey])
                    if kind == 'a':
                        ot, okey = otb[n_ % 2], ('ot', n_ % 2)
                        S.dma(ot[:, :, :w], self.OT.rearrange("(k p) t -> p k t", p=128)[:, :, t0:t0 + w], w=[okey])
                        for ct in range(KT):
                            pb = ps[ct % 2]
                            for k in range(KT):
                                self.mm(pb[:, :w], wo[:, k, ct * 128:(ct + 1) * 128], ot[:, k, :w], k == 0, k == KT - 1,
                                        r=['wo', okey], w=[('ps', ct % 2)])
                            self.stt(hs[:, ct, :w], pb[:, :w], ab[:, r_, 2, ct:ct + 1], hs[:, ct, :w], ALU.mult, ALU.add,
                                     r=[('ps', ct % 2), abkey, hkey], w=[hkey])
                    self.norm_span(hs, hkey, w, ab[:, r_, 3, :], ab[:, r_, 4, :], abkey, u32, 'u32', ub, 'ub', scr)
                    self.moe_span(li, hs, hkey, w, u32, ub, ab[:, r_, 5, :], abkey, M)
                    S.dma(self.hT_span(t0, w), hs[:, :, :w], r=[hkey], w=[('hT', si)])
            S.barrier(keep=KEEP)


_ROPE = {}


def rope_consts(TL):
    if TL in _ROPE:
        return _ROPE[TL]
    f = np.float32
    t = np.arange(TL)
    pos = np.stack([t // 64, t % 64], axis=-1).astype(f)
    inv = (f(10000.0) ** (-np.arange(32, dtype=f) / f(32))).astype(f)
    ang = np.broadcast_to(pos[:, :, None, None] * inv, (TL, 2, 2, 32)).reshape(TL, 128).astype(f)
    cos = np.concatenate([np.ones((TC, 128), f), np.cos(ang).astype(f)], axis=0).T
    sin = np.concatenate([np.zeros((TC, 128), f), np.sin(ang).astype(f)], axis=0).T
    pm = np.zeros((128, 128), f)
    for a in range(2):
        for j in range(32):
            pm[a * 64 + 32 + j, a * 64 + j] = -1.0
            pm[a * 64 + j, a * 64 + 32 + j] = 1.0
    _ROPE[TL] = (np.ascontiguousarray(cos), np.ascontiguousarray(sin), pm)
    return _ROPE[TL]


def ssd_masks():
    f = np.float32
    k = np.arange(128)[:, None]
    i = np.arange(128)[None, :]
    trif = (k <= i).astype(f)
    trib = (k >= i).astype(f)
    mf = np.where(k <= i, 0.0, -30000.0).astype(f)
    mb = np.where(k >= i, 0.0, -30000.0).astype(f)
    return np.ascontiguousarray(np.stack([trif, trib, mf, mb], axis=1).reshape(128, 512))


def host_inputs(inp, b, TL, layers):
    f = np.float32
    d = {}
    d['x'] = np.ascontiguousarray(inp['x'][b, :TL])
    d['ctx'] = np.ascontiguousarray(inp['ctx'][b])
    d['cc'] = np.ascontiguousarray(np.stack([inp['c'][b], inp['c_ctx']]))
    d['ident'] = np.eye(128, dtype=f)
    d['mod_w'] = inp['mod_w']
    d['mod_b'] = inp['mod_b']
    ng = np.stack([inp['norm1_g'], inp['norm2_g']])
    d['ng'] = np.ascontiguousarray(ng.reshape(2, 4, KT, 128).transpose(3, 0, 1, 2).reshape(128, -1))
    d['attn_wqkv'] = inp['attn_w_qkv']
    d['attn_wo'] = inp['attn_w_o']
    d['attn_g'] = np.ascontiguousarray(np.stack([inp['attn_q_gain'], inp['attn_k_gain']], axis=1).reshape(4, 128).T)
    f = np.float32
    ki = 0
    def gl_p(a):
        sh = a.shape
        a = a.reshape((2, 32, 2, 64) + sh[3:])
        return np.moveaxis(a, (2, 3), (0, 1)).reshape((128, 2, 32) + sh[3:])
    ldt_b = np.broadcast_to(inp['s5_log_dt'][ki][:, :, None], (2, 64, 64))
    par = np.stack([gl_p(inp['s5_a_re'][ki]), gl_p(inp['s5_a_im'][ki]), gl_p(np.ascontiguousarray(ldt_b))], axis=1)
    d['s5_par'] = np.ascontiguousarray(par.reshape(128, 3 * 64)).astype(f)
    def blockdiag(a):
        o = np.zeros((128, 2, 32, 2, 16), f)
        o[:64, :, :, 0, :] = a[:64]
        o[64:, :, :, 1, :] = a[64:]
        return o.reshape(128, 2, 32, 32)
    Bre, Bim = blockdiag(gl_p(inp['s5_b_re'][ki])), blockdiag(gl_p(inp['s5_b_im'][ki]))
    d['s5_B'] = np.ascontiguousarray(np.stack([Bre, Bim], axis=1).reshape(128, -1))
    cre = np.swapaxes(inp['s5_c_re'][ki], 2, 3)
    cim = np.swapaxes(inp['s5_c_im'][ki], 2, 3)
    Cre, Cim = blockdiag(gl_p(np.ascontiguousarray(cre))), blockdiag(gl_p(np.ascontiguousarray(cim)))
    d['s5_C'] = np.ascontiguousarray(np.stack([Cre, Cim], axis=1).reshape(128, -1))
    d['s5_d'] = np.ascontiguousarray(inp['s5_d'][ki].reshape(32, 32).T)
    d['s5_wglu'] = inp['s5_w_glu']
    d['s5_bglu'] = np.ascontiguousarray(inp['s5_b_glu'][ki].reshape(16, 128).T)
    d['ssd_win'] = inp['ssd_w_in']
    d['ssd_wout'] = inp['ssd_w_out']
    d['ssd_cw'] = np.ascontiguousarray(inp['ssd_conv_w'][ki].reshape(5, 24, 128).transpose(2, 1, 0).reshape(128, 120))
    d['ssd_cb'] = np.ascontiguousarray(inp['ssd_conv_b'][ki].reshape(24, 128).T)
    d['ssd_dtb'] = np.ascontiguousarray(np.broadcast_to(inp['ssd_dt_bias'][ki].reshape(1, 64), (128, 64)))
    d['ssd_alog'] = np.ascontiguousarray(np.broadcast_to(inp['ssd_a_log'][ki].reshape(1, 64), (128, 64)))
    d['ssd_dh'] = np.ascontiguousarray(np.broadcast_to(inp['ssd_d'][ki].reshape(1, 32), (128, 32)))
    d['ssd_ng'] = np.ascontiguousarray(np.broadcast_to(inp['ssd_norm_g'][ki].reshape(1, 2048), (128, 2048)))
    d['ssd_msk'] = ssd_masks()
    cos, sin, pm = rope_consts(TL)
    d['rope_cos'], d['rope_sin'], d['rope_pm'] = cos, sin, pm
    d['moe_wr'] = np.ascontiguousarray(np.concatenate([inp['moe_w_group'], inp['moe_w_router']], axis=-1))
    d['moe_br'] = np.ascontiguousarray(np.concatenate([inp['moe_b_group'], inp['moe_b_router']], axis=-1))
    d['moe_wg'] = inp['moe_w_gate']
    d['moe_wu'] = inp['moe_w_up']
    d['moe_wd'] = inp['moe_w_down']
    return d


FULL_LAYERS = [(0, 'a', 0, True), (1, 's', 0, True), (2, 'd', 0, True), (3, 'a', 1, False)]


def kernel(**inputs):
    inp = {k: np.asarray(v) for k, v in inputs.items()}
    B, TL = inp['x'].shape[0], inp['x'].shape[1]
    prog = Prog(TL, FULL_LAYERS)
    in_maps = []
    for b in range(B):
        d = host_inputs(inp, b, TL, FULL_LAYERS)
        in_maps.append({k: d[k] for k in prog.in_names})
    res = run_bass_kernel_spmd(prog.nc, in_maps, core_ids=list(range(B)))
    return np.stack([r['out'] for r in res.results], axis=0)
```

```python
import numpy as np
from contextlib import ExitStack
import concourse.bass as bass
import concourse.mybir as mybir
from concourse.bass_utils import run_bass_kernel_spmd

F32, BF16 = mybir.dt.float32, mybir.dt.bfloat16
AF = mybir.ActivationFunctionType
ALU = mybir.AluOpType
AX = mybir.AxisListType

D = 1024
KT = 8
TC = 256
EPS = 1e-6
NE = 32
KEEP = ('wgb', 'wub', 'wdb', 'wqkvb', 'wob', 'wglub', 'winb', 'woutb')
HID = 256


class Sched:
    BLK = 8000
    NDMA = 12

    def __init__(self, nc, es):
        self.nc, self.es = nc, es
        self.eng = {'pe': nc.tensor, 'act': nc.scalar, 'dve': nc.vector,
                    'pool': nc.gpsimd, 'sp': nc.sync}
        self.cnt = {e: 0 for e in self.eng}
        self.sems = {e: [] for e in self.eng}
        self.seen = {e: {} for e in self.eng}
        self.lastw, self.readers = {}, {}
        self.dq = {}
        self.nsem = 0

    def _newsem(self, name):
        self.nsem += 1
        return self.es.enter_context(self.nc.semaphore(name))

    def _deps(self, r, w):
        toks = []
        for k in r:
            t = self.lastw.get(k)
            if t:
                toks.append(t)
        for k in w:
            t = self.lastw.get(k)
            if t:
                toks.append(t)
            toks.extend(self.readers.get(k, {}).values())
        return toks

    def _wait(self, e, toks, skip_pe=False):
        need = {}
        for (te, tb, sem, val) in toks:
            if skip_pe and te == 'pe':
                continue
            cur = need.get(te)
            if cur is None or (tb, val) > (cur[0], cur[1]):
                need[te] = (tb, val, sem)
        for te, (tb, val, sem) in need.items():
            s = self.seen[e].get(te)
            if s is not None and s >= (tb, val):
                continue
            self.eng[e].wait_ge(sem, val)
            self.seen[e][te] = (tb, val)

    def _reg(self, tok, r, w):
        for k in r:
            self.readers.setdefault(k, {})[tok[0]] = tok
        for k in w:
            self.lastw[k] = tok
            self.readers[k] = {}

    def op(self, e, fn, r=(), w=()):
        self._wait(e, self._deps(r, w), skip_pe=(e == 'pe'))
        ins = fn()
        k = self.cnt[e]
        b = k // self.BLK
        while len(self.sems[e]) <= b:
            self.sems[e].append(self._newsem(f"s_{e}_{len(self.sems[e])}"))
        sem, val = self.sems[e][b], k % self.BLK + 1
        ins.then_inc(sem, 1)
        self.cnt[e] += 1
        self._reg((e, b, sem, val), r, w)

    def dma(self, out, in_, r=(), w=(), q='sp', grp='m', **kw):
        key = (q, grp)
        if key not in self.dq:
            self.dq[key] = {'rr': 0, 'sems': [[self._newsem(f"d_{q}_{grp}_{i}"), 0]
                                             for i in range(self.NDMA)]}
        st = self.dq[key]
        i = st['rr']
        st['rr'] = (i + 1) % self.NDMA
        sem, n = st['sems'][i]
        te = ('dma', q, grp, i)
        toks = self._deps(r, w)
        if n > 0:
            toks.append((te, 0, sem, 16 * n))
        self._wait(q, toks)
        ins = self.eng[q].dma_start(out=out, in_=in_, **kw)
        ins.then_inc(sem, 16)
        st['sems'][i][1] = n + 1
        self._reg((te, 0, sem, 16 * (n + 1)), r, w)

    def barrier(self, keep=()):
        toks = []
        for e in ('pe', 'act', 'dve', 'pool'):
            k = self.cnt[e]
            if k > 0:
                b = (k - 1) // self.BLK
                toks.append((e, b, self.sems[e][b], (k - 1) % self.BLK + 1))
        for (q, grp), st in self.dq.items():
            if grp == 'async':
                continue
            for i, (sem, n) in enumerate(st['sems']):
                if n > 0:
                    toks.append((('dma', q, grp, i), 0, sem, 16 * n))
        for e in self.eng:
            self._wait(e, toks)
        lw = {k: v for k, v in self.lastw.items() if k[0] in keep}
        self.lastw, self.readers = lw, {}

    def finish(self):
        toks = []
        for (q, grp), st in self.dq.items():
            for i, (sem, n) in enumerate(st['sems']):
                if n > 0:
                    toks.append((('dma', q, grp, i), 0, sem, 16 * n))
        self._wait('sp', toks)


class Prog:
    def __init__(self, TL, layers, n_layers_w=4, dbg=None):
        self.TL, self.T = TL, TC + TL
        self.layers = layers
        self.dbg = dbg
        self.spans = [(0, TC, True)] + [(TC + i * 512, 512, False) for i in range(TL // 512)]
        self.nc = bass.Bass("TRN2", target_bir_lowering=False)
        self.build()

    def dram_in(self, name, shape, dt=F32):
        self.in_names.append(name)
        return self.nc.dram_tensor(name, list(shape), dt, kind="ExternalInput").ap()

    def dram_scr(self, name, shape, dt=F32):
        return self.nc.dram_tensor(name, list(shape), dt, kind="Internal").ap()

    def sb(self, es, name, shape, dt=F32):
        self._nsb = getattr(self, '_nsb', 0) + 1
        return es.enter_context(self.nc.sbuf_tensor(f"sb{self._nsb}_{name}", list(shape), dt))

    def mm(self, out, lhsT, rhs, start, stop, r, w):
        nc = self.nc
        self.S.op('pe', lambda: nc.tensor.matmul(out, lhsT=lhsT, rhs=rhs, start=start, stop=stop), r=r, w=w)

    def act(self, out, in_, func, r, w, **kw):
        nc = self.nc
        self.S.op('act', lambda: nc.scalar.activation(out=out, in_=in_, func=func, **kw), r=r, w=w)

    def tt(self, out, in0, in1, op, r, w, e='dve'):
        eng = self.S.eng[e]
        self.S.op(e, lambda: eng.tensor_tensor(out=out, in0=in0, in1=in1, op=op), r=r, w=w)

    def ts(self, out, in0, s1, op0, r, w, s2=None, op1=None, e='dve', **kw):
        eng = self.S.eng[e]
        if op1 is None:
            self.S.op(e, lambda: eng.tensor_scalar(out=out, in0=in0, scalar1=s1, scalar2=None, op0=op0, **kw), r=r, w=w)
        else:
            self.S.op(e, lambda: eng.tensor_scalar(out=out, in0=in0, scalar1=s1, scalar2=s2, op0=op0, op1=op1, **kw), r=r, w=w)

    def stt(self, out, in0, scalar, in1, op0, op1, r, w):
        nc = self.nc
        self.S.op('dve', lambda: nc.vector.scalar_tensor_tensor(out=out, in0=in0, scalar=scalar, in1=in1, op0=op0, op1=op1), r=r, w=w)

    def cp(self, out, in_, r, w, e='dve'):
        eng = self.S.eng[e]
        if e == 'act':
            self.S.op(e, lambda: eng.copy(out=out, in_=in_), r=r, w=w)
        else:
            self.S.op(e, lambda: eng.tensor_copy(out=out, in_=in_), r=r, w=w)

    def build(self):
        nc = self.nc
        self.in_names = []
        TL, T = self.TL, self.T
        I = self.I = {}
        I['x'] = self.dram_in('x', [TL, D])
        I['ctx'] = self.dram_in('ctx', [TC, D])
        I['cc'] = self.dram_in('cc', [2, D])
        I['ident'] = self.dram_in('ident', [128, 128])
        I['mod_w'] = self.dram_in('mod_w', [4, D, 6 * D])
        I['mod_b'] = self.dram_in('mod_b', [4, 6 * D])
        I['ng'] = self.dram_in('ng', [128, 2 * 4 * KT])
        I['moe_wr'] = self.dram_in('moe_wr', [4, D, 36])
        I['moe_br'] = self.dram_in('moe_br', [4, 36])
        I['moe_wg'] = self.dram_in('moe_wg', [4, NE, D, HID])
        I['moe_wu'] = self.dram_in('moe_wu', [4, NE, D, HID])
        I['moe_wd'] = self.dram_in('moe_wd', [4, NE, HID, D])
        I['attn_wqkv'] = self.dram_in('attn_wqkv', [2, D, 1536])
        I['attn_wo'] = self.dram_in('attn_wo', [2, D, D])
        I['attn_g'] = self.dram_in('attn_g', [128, 4])
        I['rope_cos'] = self.dram_in('rope_cos', [128, T])
        I['rope_sin'] = self.dram_in('rope_sin', [128, T])
        I['rope_pm'] = self.dram_in('rope_pm', [128, 128])
        self.wqkvb = self.dram_scr('wqkvb', [2, D, 1536], BF16)
        self.wob = self.dram_scr('wob', [2, D, D], BF16)
        self.QT = self.dram_scr('QT', [D, T], BF16)
        self.KT_ = self.dram_scr('KTs', [256, T], BF16)
        self.Vs = self.dram_scr('Vs', [T, 256], BF16)
        self.OT = self.dram_scr('OT', [D, T], BF16)
        I['s5_par'] = self.dram_in('s5_par', [128, 3 * 64])
        I['s5_B'] = self.dram_in('s5_B', [128, 2 * 64 * 32])
        I['s5_C'] = self.dram_in('s5_C', [128, 2 * 64 * 32])
        I['s5_d'] = self.dram_in('s5_d', [32, 32])
        I['s5_wglu'] = self.dram_in('s5_wglu', [1, D, 2 * D])
        I['s5_bglu'] = self.dram_in('s5_bglu', [128, 16])
        self.wglub = self.dram_scr('wglub', [1, D, 2 * D], BF16)
        self.UT = self.dram_scr('UT', [D, T], BF16)
        self.YF = self.dram_scr('YF', [D, T])
        self.YB = self.dram_scr('YB', [D, T])
        I['ssd_win'] = self.dram_in('ssd_win', [1, D, 5184])
        I['ssd_wout'] = self.dram_in('ssd_wout', [1, 2 * D, D])
        I['ssd_cw'] = self.dram_in('ssd_cw', [128, 24 * 5])
        I['ssd_cb'] = self.dram_in('ssd_cb', [128, 24])
        I['ssd_dtb'] = self.dram_in('ssd_dtb', [128, 64])
        I['ssd_alog'] = self.dram_in('ssd_alog', [128, 64])
        I['ssd_dh'] = self.dram_in('ssd_dh', [128, 32])
        I['ssd_ng'] = self.dram_in('ssd_ng', [128, 2 * D])
        I['ssd_msk'] = self.dram_in('ssd_msk', [128, 4 * 128])
        self.winb = self.dram_scr('winb', [1, D, 5184], BF16)
        self.woutb = self.dram_scr('woutb', [1, 2 * D, D], BF16)
        self.ZS = self.dram_scr('ZS', [T, 2 * D], BF16)
        self.XBC = self.dram_scr('XBC', [3072, T], BF16)
        self.DTs = self.dram_scr('DTs', [T, 64])
        self.XTK = self.dram_scr('XTK', [T, 2 * D], BF16)
        self.BTK = self.dram_scr('BTK', [T, 512], BF16)
        self.BCf = self.dram_scr('BCf', [1024, T], BF16)
        self.YD = self.dram_scr('YD', [2, T, 2 * D])
        self.out = nc.dram_tensor('out', [TL, D], F32, kind="ExternalOutput").ap()
        self.hT = self.dram_scr('hT', [D, T])
        self.wgb = self.dram_scr('wgb', [4, NE, D, HID], BF16)
        self.wub = self.dram_scr('wub', [4, NE, D, HID], BF16)
        self.wdb = self.dram_scr('wdb', [4, NE, HID, D], BF16)
        if self.dbg:
            self.dbg_out = nc.dram_tensor('dbg', [D, T], F32, kind="ExternalOutput").ap()

        with ExitStack() as es:
            self.S = S = Sched(nc, es)
            self.ident = self.sb(es, 'ident', [128, 128])
            self.identb = self.sb(es, 'identb', [128, 128], BF16)
            self.ones32 = self.sb(es, 'ones32', [128, 128])
            self.onesb = self.sb(es, 'onesb', [128, 128], BF16)
            self.mv = self.sb(es, 'mv', [128, 4, 96])
            self.ng = self.sb(es, 'ng', [128, 2, 4, KT])
            self.ps = [es.enter_context(nc.psum_tensor(f'ps{i}', [128, 512], F32)) for i in range(8)]
            S.dma(self.ident[:], I['ident'], w=['ident'])
            S.dma(self.ng[:].rearrange("p n l k -> p (n l k)"), I['ng'], w=['ng'])
            self.cp(self.identb[:], self.ident[:], r=['ident'], w=['identb'])
            S.op('dve', lambda: nc.vector.memset(self.ones32[:], 1.0), w=['ones32'])
            S.op('dve', lambda: nc.vector.memset(self.onesb[:], 1.0), w=['onesb'])
            S.barrier()
            self.async_casts(self.layers[0])
            self.phase_mod()
            self.phase_tin()
            for n_, L in enumerate(self.layers):
                if n_ + 1 < len(self.layers):
                    self.async_casts(self.layers[n_ + 1])
                self.layer(*L)
            self.phase_tout()
            S.finish()

    def cast_dram(self, dst, src, key):
        R_, C_ = src.shape
        a = R_ // 128
        sv = src.rearrange("(p a) n -> p a n", p=128)
        dv = dst.rearrange("(p a) n -> p a n", p=128)
        step = max(1, 2048 // C_)
        for a0 in range(0, a, step):
            a1 = min(a, a0 + step)
            self.S.dma(dv[:, a0:a1, :], sv[:, a0:a1, :], w=[key], q='pool', grp='async')

    def async_casts(self, L):
        I = self.I
        li, kind, ki, nctx = L
        if kind == 's':
            self.cast_dram(self.wglub[ki], I['s5_wglu'][ki], ('wglub', ki))
        if kind == 'd':
            self.cast_dram(self.winb[ki], I['ssd_win'][ki], ('winb', ki))
            self.cast_dram(self.woutb[ki], I['ssd_wout'][ki], ('woutb', ki))
        if kind == 'a':
            self.cast_dram(self.wqkvb[ki], I['attn_wqkv'][ki], ('wqkvb', ki))
            self.cast_dram(self.wob[ki], I['attn_wo'][ki], ('wob', ki))
        for e in range(NE):
            self.cast_dram(self.wgb[li, e], I['moe_wg'][li, e], ('wgb', li, e))
            self.cast_dram(self.wub[li, e], I['moe_wu'][li, e], ('wub', li, e))
            self.cast_dram(self.wdb[li, e], I['moe_wd'][li, e], ('wdb', li, e))

    def phase_mod(self):
        nc, S, I = self.nc, self.S, self.I
        with ExitStack() as es:
            ccT = self.sb(es, 'ccT', [128, KT, 2])
            scT = self.sb(es, 'scT', [128, KT, 2])
            wch = [self.sb(es, f'modw{i}', [128, KT, 512]) for i in range(2)]
            brow = self.sb(es, 'modb', [1, 6 * D])
            with nc.allow_non_contiguous_dma(reason="tiny transposed load of c"):
                for r_ in range(2):
                    S.dma(ccT[:, :, r_], I['cc'][r_].rearrange("(k p) -> p k", p=128), w=['ccT'])
            self.act(scT[:], ccT[:], AF.Silu, r=['ccT'], w=['scT'])
            ci = 0
            for (li, kind, ki, nctx) in self.layers:
                S.dma(brow[:], I['mod_b'][li:li + 1, :], w=['modb'])
                pb = self.ps[li % 2]
                for j in range(12):
                    wt = wch[ci % 2]
                    S.dma(wt[:], I['mod_w'][li][:, j * 512:(j + 1) * 512].rearrange("(k p) n -> p k n", p=128),
                          w=[('modw', ci % 2)])
                    for b4 in range(4):
                        blk = j * 4 + b4
                        o = pb[:, blk * 2:blk * 2 + 2]
                        for k in range(KT):
                            self.mm(o, wt[:, k, b4 * 128:(b4 + 1) * 128], scT[:, k, :], k == 0, False,
                                    r=[('modw', ci % 2), 'scT'], w=[('psm', li % 2)])
                        self.mm(o, brow[0:1, blk * 128:(blk + 1) * 128], self.ones32[0:1, 0:2], False, True,
                                r=['modb', 'ones32'], w=[('psm', li % 2)])
                    ci += 1
                self.cp(self.mv[:, li, :], pb[:, 0:96], r=[('psm', li % 2)], w=[('mv', li)])
        S.barrier(keep=KEEP)

    def hT_span(self, t0, w):
        return self.hT.rearrange("(k p) t -> p k t", p=128)[:, :, t0:t0 + w]

    def phase_tin(self):
        nc, S, I = self.nc, self.S, self.I
        with ExitStack() as es:
            xt = [self.sb(es, f'tin_x{i}', [128, D]) for i in range(2)]
            stg = [self.sb(es, f'tin_s{i}', [128, KT, 512]) for i in range(2)]
            ti = 0
            for si, (t0, w, isc) in enumerate(self.spans):
                st = stg[si % 2]
                for tt_ in range(w // 128):
                    tok = t0 + tt_ * 128
                    src = I['ctx'][tok:tok + 128, :] if isc else I['x'][tok - TC:tok - TC + 128, :]
                    xs = xt[ti % 2]
                    S.dma(xs[:], src, w=[('tinx', ti % 2)])
                    for half in range(2):
                        pb = self.ps[(ti * 2 + half) % 4]
                        for q in range(4):
                            k = half * 4 + q
                            S.op('pe', lambda: nc.tensor.transpose(out=pb[:, q * 128:(q + 1) * 128],
                                                                   in_=xs[:, k * 128:(k + 1) * 128], identity=self.ident[:]),
                                 r=[('tinx', ti % 2)], w=[('pst', (ti * 2 + half) % 4)])
                        self.cp(st[:, half * 4:half * 4 + 4, tt_ * 128:(tt_ + 1) * 128],
                                pb[:].rearrange("p (q t) -> p q t", q=4),
                                r=[('pst', (ti * 2 + half) % 4)], w=[('tins', si % 2)],
                                e='dve' if half == 0 else 'act')
                    ti += 1
                S.dma(self.hT_span(t0, w), st[:, :, :w], r=[('tins', si % 2)], w=[('hT', si)])
        S.barrier(keep=KEEP)

    def phase_tout(self):
        nc, S = self.nc, self.S
        if self.dbg:
            with ExitStack() as es:
                t_ = [self.sb(es, f'dbg{i}', [128, KT, 512]) for i in range(2)]
                for si, (t0, w, isc) in enumerate(self.spans):
                    S.dma(t_[si % 2][:, :, :w], self.hT_span(t0, w), w=[('dbgt', si % 2)])
                    S.dma(self.dbg_out.rearrange("(k p) t -> p k t", p=128)[:, :, t0:t0 + w], t_[si % 2][:, :, :w],
                          r=[('dbgt', si % 2)], w=[('dbgo', si)])
            S.barrier()
        with ExitStack() as es:
            hs = [self.sb(es, f'to_h{i}', [128, KT, 512]) for i in range(2)]
            ot = [self.sb(es, f'to_o{i}', [128, D]) for i in range(2)]
            ti = 0
            for si, (t0, w, isc) in enumerate(self.spans):
                if isc:
                    continue
                h = hs[si % 2]
                S.dma(h[:, :, :w], self.hT_span(t0, w), w=[('toh', si % 2)])
                for tt_ in range(w // 128):
                    o = ot[ti % 2]
                    for half in range(2):
                        pb = self.ps[(ti * 2 + half) % 4]
                        for q in range(4):
                            k = half * 4 + q
                            S.op('pe', lambda: nc.tensor.transpose(out=pb[:, q * 128:(q + 1) * 128],
                                                                   in_=h[:, k, tt_ * 128:(tt_ + 1) * 128], identity=self.ident[:]),
                                 r=[('toh', si % 2)], w=[('pst', (ti * 2 + half) % 4)])
                        self.cp(o[:, half * 512:(half + 1) * 512], pb[:],
                                r=[('pst', (ti * 2 + half) % 4)], w=[('too', ti % 2)],
                                e='dve' if half == 0 else 'act')
                    tok = t0 - TC + tt_ * 128
                    S.dma(self.out[tok:tok + 128, :], o[:], r=[('too', ti % 2)], w=[('out', ti)])
                    ti += 1

    def layer_scalars(self, es, li):
        S = self.S
        ab = self.sb(es, f'ab{li}', [128, 2, 6, KT])
        mvv = self.mv[:, li, :].rearrange("p (j k r) -> p r j k", j=6, k=KT, r=2)
        key = ('ab', li)
        for r_ in range(2):
            self.stt(ab[:, r_, 0, :], mvv[:, r_, 1, :], 1.0, self.ng[:, 0, li, :], ALU.add, ALU.mult, r=[('mv', li), 'ng'], w=[key])
            self.cp(ab[:, r_, 1, :], mvv[:, r_, 0, :], r=[('mv', li)], w=[key])
            self.cp(ab[:, r_, 2, :], mvv[:, r_, 2, :], r=[('mv', li)], w=[key])
            self.stt(ab[:, r_, 3, :], mvv[:, r_, 4, :], 1.0, self.ng[:, 1, li, :], ALU.add, ALU.mult, r=[('mv', li), 'ng'], w=[key])
            self.cp(ab[:, r_, 4, :], mvv[:, r_, 3, :], r=[('mv', li)], w=[key])
            self.cp(ab[:, r_, 5, :], mvv[:, r_, 5, :], r=[('mv', li)], w=[key])
        return ab

    def norm_span(self, hs, hkey, w, A, B, abkey, u32, u32key, ub, ubkey, scr):
        nc, S = self.nc, self.S
        sq, rs = scr['sq'], scr['rs']
        pss = self.ps[7]
        self.act(sq[:, :, :w], hs[:, :, :w], AF.Square, r=[hkey], w=['sq'])
        for k in range(KT):
            self.mm(pss[:, :w], self.onesb[:], sq[:, k, :w], k == 0, k == KT - 1, r=['sq', 'onesb'], w=[('ps', 7)])
        self.act(rs[:, :w], pss[:, :w], AF.Sqrt, r=[('ps', 7), 'epsb'], w=['rs'], scale=1.0 / D, bias=self.epsb[:, 0:1])
        S.op('dve', lambda: nc.vector.reciprocal(out=rs[:, :w], in_=rs[:, :w]), r=['rs'], w=['rs'])
        xn = scr['xn']
        self.tt(xn[:, :, :w], hs[:, :, :w], rs[:, :w].unsqueeze(1).to_broadcast([128, KT, w]), ALU.mult, r=[hkey, 'rs'],
                w=[('acc', k) for k in range(KT)])
        for k in range(KT):
            dst = u32[:, k, :w] if u32 is not None else ub[:, k, :w]
            dkey = u32key if u32 is not None else ubkey
            self.act(dst, xn[:, k, :w], AF.Identity, r=[('acc', k), abkey], w=[dkey], scale=A[:, k:k + 1], bias=B[:, k:k + 1])
        if u32 is not None:
            self.cp(ub[:, :, :w], u32[:, :, :w], r=[u32key], w=[ubkey], e='pool')

    def moe_span(self, li, hs, hkey, w, u32, ub, G5, abkey, M):
        nc, S = self.nc, self.S
        ps = self.ps
        ntt = w // 128
        cw, lgs, sm = M['cw'], M['lgs'], M['sm']
        for tt_ in range(ntt):
            pl = ps[6]
            for k in range(KT):
                self.mm(pl[:, 0:36], u32[:, k, tt_ * 128:(tt_ + 1) * 128], M['wr'][:, k, :], k == 0, False,
                        r=['u32', 'wr'], w=[('ps', 6)])
            self.mm(pl[:, 0:36], self.ones32[0:1, :], M['br'][0:1, :], False, True, r=['ones32', 'br'], w=[('ps', 6)])
            L = lgs
            self.cp(L[:, 0:36], pl[:, 0:36], r=[('ps', 6)], w=['lgs'])
            rk, wk = ['lgs', 'sm'], ['sm']
            S.op('dve', lambda: nc.vector.tensor_reduce(out=sm[:, 0:1], in_=L[:, 0:4], op=ALU.max, axis=AX.X), r=rk, w=wk)
            self.ts(sm[:, 1:2], sm[:, 0:1], -1.0, ALU.mult, r=rk, w=wk)
            self.ts(L[:, 36:40], L[:, 0:4], sm[:, 0:1], ALU.is_equal, r=rk, w=['lgs'])
            self.act(L[:, 40:44], L[:, 0:4], AF.Exp, r=rk, w=['lgs', 'sm'], bias=sm[:, 1:2], scale=1.0, accum_out=sm[:, 2:3])
            S.op('dve', lambda: nc.vector.reciprocal(out=sm[:, 3:4], in_=sm[:, 2:3]), r=rk, w=wk)
            self.ts(L[:, 44:52], L[:, 4:12], L[:, 36:37], ALU.mult, r=rk, w=['lgs'])
            for g in range(1, 4):
                self.stt(L[:, 44:52], L[:, 4 + 8 * g:12 + 8 * g], L[:, 36 + g:37 + g], L[:, 44:52], ALU.mult, ALU.add, r=rk, w=['lgs'])
            S.op('dve', lambda: nc.vector.tensor_reduce(out=sm[:, 4:5], in_=L[:, 44:52], op=ALU.max, axis=AX.X), r=rk, w=wk)
            self.ts(L[:, 52:60], L[:, 44:52], sm[:, 4:5], ALU.is_equal, r=rk, w=['lgs'])
            self.stt(L[:, 60:68], L[:, 52:60], -1e30, L[:, 44:52], ALU.mult, ALU.add, r=rk, w=['lgs'])
            S.op('dve', lambda: nc.vector.tensor_reduce(out=sm[:, 5:6], in_=L[:, 60:68], op=ALU.max, axis=AX.X), r=rk, w=wk)
            self.ts(L[:, 68:76], L[:, 60:68], sm[:, 5:6], ALU.is_equal, r=rk, w=['lgs'])
            self.tt(sm[:, 6:7], sm[:, 5:6], sm[:, 4:5], ALU.subtract, r=rk, w=wk)
            self.act(sm[:, 7:8], sm[:, 6:7], AF.Exp, r=rk, w=wk)
            self.ts(sm[:, 8:9], sm[:, 7:8], 1.0, ALU.add, r=rk, w=wk)
            S.op('dve', lambda: nc.vector.reciprocal(out=sm[:, 8:9], in_=sm[:, 8:9]), r=rk, w=wk)
            self.tt(sm[:, 9:10], sm[:, 7:8], sm[:, 8:9], ALU.mult, r=rk, w=wk)
            self.tt(sm[:, 10:11], sm[:, 8:9], sm[:, 3:4], ALU.mult, r=rk, w=wk)
            self.tt(sm[:, 11:12], sm[:, 9:10], sm[:, 3:4], ALU.mult, r=rk, w=wk)
            self.ts(L[:, 76:84], L[:, 52:60], sm[:, 10:11], ALU.mult, r=rk, w=['lgs'])
            self.stt(L[:, 76:84], L[:, 68:76], sm[:, 11:12], L[:, 76:84], ALU.mult, ALU.add, r=rk, w=['lgs'])
            for g in range(4):
                self.ts(cw[:, tt_, g * 8:(g + 1) * 8], L[:, 76:84], L[:, 36 + g:37 + g], ALU.mult, r=rk, w=['cw'])
        wgs, wus, wds = M['wg'], M['wu'], M['wd']
        acc = M['acc']

        def load_w(e):
            sl = e % 3
            S.dma(wgs[sl][:], self.wgb[li, e].rearrange("(k p) n -> p k n", p=128), r=[('wgb', li, e)], w=[('wg', sl)])
            S.dma(wus[sl][:], self.wub[li, e].rearrange("(k p) n -> p k n", p=128), r=[('wub', li, e)], w=[('wu', sl)])
            S.dma(wds[sl][:], self.wdb[li, e].rearrange("(k p) n -> p k n", p=128), r=[('wdb', li, e)], w=[('wd', sl)])
        def down(e):
            sl = e % 3
            for ct in range(KT):
                pd = ps[6 + ct % 2]
                for j in range(2):
                    self.mm(pd[:, :w], wds[sl][:, j, ct * 128:(ct + 1) * 128], M['hid'][sl][:, j, :w], j == 0, j == 1,
                            r=[('wd', sl), ('hid', sl, j)], w=[('ps', 6 + ct % 2)])
                if e == 0:
                    self.cp(acc[:, ct, :w], pd[:, :w], r=[('ps', 6 + ct % 2)], w=[('acc', ct)])
                else:
                    self.tt(acc[:, ct, :w], acc[:, ct, :w], pd[:, :w], ALU.add, r=[('ps', 6 + ct % 2), ('acc', ct)], w=[('acc', ct)])
        load_w(0)
        for e in range(NE):
            sl = e % 3
            if e + 1 < NE:
                load_w(e + 1)
            pc = ps[4 + e % 2]
            for tt_ in range(ntt):
                self.mm(pc[:, tt_ * 128:(tt_ + 1) * 128], cw[:, tt_, e:e + 1].to_broadcast([128, 128]), self.ident[:], True, True,
                        r=['cw', 'ident'], w=[('ps', 4 + e % 2)])
            for j in range(2):
                b0 = 2 * ((e * 2 + j) % 2)
                for (wt, wkey, bank) in ((wgs[sl], ('wg', sl), b0), (wus[sl], ('wu', sl), b0 + 1)):
                    for k in range(KT):
                        self.mm(ps[bank][:, :w], wt[:, k, j * 128:(j + 1) * 128], ub[:, k, :w], k == 0, k == KT - 1,
                                r=[wkey, 'ub'], w=[('ps', bank)])
                sg, t2 = M['sg'][j], M['t2'][j]
                hid = M['hid'][sl]
                self.act(sg[:, :w], ps[b0][:, :w], AF.Silu, r=[('ps', b0)], w=[('sg', j)])
                self.tt(t2[:, :w], sg[:, :w], ps[b0 + 1][:, :w], ALU.mult, r=[('sg', j), ('ps', b0 + 1)], w=[('t2', j)])
                self.tt(hid[:, j, :w], t2[:, :w], pc[:, :w], ALU.mult, r=[('t2', j), ('ps', 4 + e % 2)], w=[('hid', sl, j)])
            if e > 0:
                down(e - 1)
        down(NE - 1)
        for ct in range(KT):
            self.stt(hs[:, ct, :w], acc[:, ct, :w], G5[:, ct:ct + 1], hs[:, ct, :w], ALU.mult, ALU.add,
                     r=[('acc', ct), hkey, abkey], w=[hkey])

    def moe_alloc(self, es, li):
        S, I = self.S, self.I
        M = {}
        M['wr'] = self.sb(es, 'moe_wr', [128, KT, 36])
        M['br'] = self.sb(es, 'moe_br', [1, 36])
        M['cw'] = self.sb(es, 'moe_cw', [128, 4, NE])
        M['lgs'] = self.sb(es, 'moe_lgs', [128, 96])
        M['sm'] = self.sb(es, 'moe_sm', [128, 16])
        M['wg'] = [self.sb(es, f'moe_wg{i}', [128, KT, HID], BF16) for i in range(3)]
        M['wu'] = [self.sb(es, f'moe_wu{i}', [128, KT, HID], BF16) for i in range(3)]
        M['wd'] = [self.sb(es, f'moe_wd{i}', [128, 2, D], BF16) for i in range(3)]
        M['sg'] = [self.sb(es, f'moe_sg{i}', [128, 512]) for i in range(2)]
        M['t2'] = [self.sb(es, f'moe_t2{i}', [128, 512]) for i in range(2)]
        M['hid'] = [self.sb(es, f'moe_hid{i}', [128, 2, 512], BF16) for i in range(3)]
        M['acc'] = self.sb(es, 'moe_acc', [128, KT, 512])
        S.dma(M['wr'][:], I['moe_wr'][li].rearrange("(k p) n -> p k n", p=128), w=['wr'])
        S.dma(M['br'][:], I['moe_br'][li:li + 1, :], w=['br'])
        return M

    def norm_alloc(self, es):
        scr = {'sq': self.sb(es, 'n_sq', [128, KT, 512], BF16), 'rs': self.sb(es, 'n_rs', [128, 512])}
        self.epsb = self.sb(es, 'epsb', [128, 1])
        nc = self.nc
        self.S.op('dve', lambda: nc.vector.memset(self.epsb[:], EPS), w=['epsb'])
        return scr

    def attn_a1(self, li, ki, ab, abkey):
        nc, S, I = self.nc, self.S, self.I
        ps = self.ps
        with ExitStack() as es:
            scr = self.norm_alloc(es)
            scr['xn'] = self.sb(es, 'a1_xn', [128, KT, 512])
            wq = self.sb(es, 'a1_wqkv', [128, KT, 1536], BF16)
            pmb = self.sb(es, 'a1_pmb', [128, 128], BF16)
            pm32 = self.sb(es, 'a1_pm32', [128, 128])
            gq = self.sb(es, 'a1_g', [128, 4])
            hsb = [self.sb(es, f'a1_hs{i}', [128, KT, 512]) for i in range(2)]
            ub = self.sb(es, 'a1_ub', [128, KT, 512], BF16)
            cs = [self.sb(es, f'a1_cos{i}', [128, 512]) for i in range(2)]
            sn = [self.sb(es, f'a1_sin{i}', [128, 512]) for i in range(2)]
            sq = [self.sb(es, f'a1_sq{i}', [128, 512], BF16) for i in range(2)]
            rsq = [self.sb(es, f'a1_rs{i}', [128, 512]) for i in range(2)]
            yb = [self.sb(es, f'a1_yb{i}', [128, 512], BF16) for i in range(2)]
            t1 = [self.sb(es, f'a1_t1{i}', [128, 512]) for i in range(2)]
            t2 = [self.sb(es, f'a1_t2{i}', [128, 512]) for i in range(2)]
            qst = [self.sb(es, f'a1_qst{i}', [128, 10, 512], BF16) for i in range(2)]
            vst = [self.sb(es, f'a1_vst{i}', [128, 4, 256], BF16) for i in range(2)]
            S.dma(wq[:], self.wqkvb[ki].rearrange("(k p) n -> p k n", p=128), r=[('wqkvb', ki)], w=['wq'])
            S.dma(pm32[:], I['rope_pm'], w=['pm32'])
            S.dma(gq[:], I['attn_g'], w=['gq'])
            self.cp(pmb[:], pm32[:], r=['pm32'], w=['pmb'])
            self.ts(gq[:, 2 * ki:2 * ki + 1], gq[:, 2 * ki:2 * ki + 1], 128.0 ** -0.5, ALU.mult, r=['gq'], w=['gq'])
            for si, (t0, w, isc) in enumerate(self.spans):
                hs, hkey = hsb[si % 2], ('hs', si % 2)
                r_ = 1 if isc else 0
                S.dma(hs[:, :, :w], self.hT_span(t0, w), r=[('hT', si)], w=[hkey])
                S.dma(cs[si % 2][:, :w], I['rope_cos'][:, t0:t0 + w], w=[('cos', si % 2)])
                S.dma(sn[si % 2][:, :w], I['rope_sin'][:, t0:t0 + w], w=[('sin', si % 2)])
                self.norm_span(hs, hkey, w, ab[:, r_, 0, :], ab[:, r_, 1, :], abkey, None, None, ub, 'ub', scr)
                qs_ = qst[si % 2]
                for c in range(10):
                    b = c % 2
                    pq, pss_, pr = ps[b], ps[2 + b], ps[4 + b]
                    for k in range(KT):
                        self.mm(pq[:, :w], wq[:, k, c * 128:(c + 1) * 128], ub[:, k, :w], k == 0, k == KT - 1,
                                r=['wq', 'ub'], w=[('ps', b)])
                    self.act(sq[b][:, :w], pq[:, :w], AF.Square, r=[('ps', b)], w=[('sq2', b)])
                    self.mm(pss_[:, :w], self.onesb[:], sq[b][:, :w], True, True, r=[('sq2', b), 'onesb'], w=[('ps', 2 + b)])
                    self.act(rsq[b][:, :w], pss_[:, :w], AF.Sqrt, r=[('ps', 2 + b), 'epsb'], w=[('rsq', b)],
                             scale=1.0 / 128, bias=self.epsb[:, 0:1])
                    S.op('dve', lambda: nc.vector.reciprocal(out=rsq[b][:, :w], in_=rsq[b][:, :w]), r=[('rsq', b)], w=[('rsq', b)])
                    gcol = 2 * ki + (0 if c < 8 else 1)
                    self.stt(yb[b][:, :w], pq[:, :w], gq[:, gcol:gcol + 1], rsq[b][:, :w], ALU.mult, ALU.mult,
                             r=[('ps', b), 'gq', ('rsq', b)], w=[('yb', b)])
                    self.mm(pr[:, :w], pmb[:], yb[b][:, :w], True, True, r=['pmb', ('yb', b)], w=[('ps', 4 + b)])
                    self.tt(t1[b][:, :w], yb[b][:, :w], cs[si % 2][:, :w], ALU.mult, r=[('yb', b), ('cos', si % 2)], w=[('t1', b)])
                    self.tt(t2[b][:, :w], pr[:, :w], sn[si % 2][:, :w], ALU.mult, r=[('ps', 4 + b), ('sin', si % 2)], w=[('t2', b)])
                    self.tt(qs_[:, c, :w], t1[b][:, :w], t2[b][:, :w], ALU.add, r=[('t1', b), ('t2', b)], w=[('qst', si % 2)])
                S.dma(self.QT.rearrange("(h p) t -> p h t", p=128)[:, :, t0:t0 + w], qs_[:, 0:8, :w], r=[('qst', si % 2)], w=[('QT', si)])
                S.dma(self.KT_.rearrange("(h p) t -> p h t", p=128)[:, :, t0:t0 + w], qs_[:, 8:10, :w], r=[('qst', si % 2)], w=[('KT', si)])
                vs_ = vst[si % 2]
                for tt_ in range(w // 128):
                    for k in range(KT):
                        self.mm(ps[6][:, 0:256], ub[:, k, tt_ * 128:(tt_ + 1) * 128], wq[:, k, 1280:1536], k == 0, k == KT - 1,
                                r=['wq', 'ub'], w=[('ps', 6)])
                    self.cp(vs_[:, tt_, :], ps[6][:, 0:256], r=[('ps', 6)], w=[('vst', si % 2)], e='act')
                S.dma(self.Vs[t0:t0 + w, :].rearrange("(a p) n -> p a n", p=128), vs_[:, :w // 128, :], r=[('vst', si % 2)], w=[('Vs', si)])
        S.barrier(keep=KEEP)

    def attn_a2(self, need_ctx):
        nc, S = self.nc, self.S
        ps = self.ps
        T = self.T
        NB = T // 128
        with ExitStack() as es:
            kT = self.sb(es, 'a2_kT', [128, 2, T], BF16)
            vt = self.sb(es, 'a2_v', [128, NB, 256], BF16)
            qsb = [self.sb(es, f'a2_q{i}', [128, 512], BF16) for i in range(2)]
            pt = [self.sb(es, f'a2_p{i}', [128, 512], BF16) for i in range(4)]
            pt2 = [self.sb(es, f'a2_pp{i}', [128, 512], BF16) for i in range(2)]
            rl = [self.sb(es, f'a2_rl{i}', [128, 512]) for i in range(2)]
            ost = [self.sb(es, f'a2_o{i}', [128, 512], BF16) for i in range(2)]
            S.dma(kT[:], self.KT_.rearrange("(h p) t -> p h t", p=128), w=['kT'])
            for a0 in range(0, NB, 8):
                a1 = min(NB, a0 + 8)
                S.dma(vt[:, a0:a1, :], self.Vs[a0 * 128:a1 * 128, :].rearrange("(a p) n -> p a n", p=128), w=['vt'])
            it = 0
            ie = 0
            for g in range(2):
                for hq in range(4):
                    h = g * 4 + hq
                    for si, (t0, w, isc) in enumerate(self.spans):
                        if isc and not need_ctx:
                            continue
                        nkb = 2 if isc else NB
                        q_, qk = qsb[it % 2], ('q', it % 2)
                        S.dma(q_[:, :w], self.QT[h * 128:(h + 1) * 128, t0:t0 + w], w=[qk])
                        po, pl = ps[4 + it % 2], ps[6 + it % 2]
                        base = ie

                        def stA(kb):
                            sb_ = (base + kb) % 4
                            self.mm(ps[sb_][:, :w], kT[:, g, kb * 128:(kb + 1) * 128], q_[:, :w], True, True,
                                    r=['kT', qk], w=[('ps', sb_)])

                        def stB(kb):
                            sb_ = (base + kb) % 4
                            self.act(pt[sb_][:, :w], ps[sb_][:, :w], AF.Exp, r=[('ps', sb_)], w=[('pt', sb_)])

                        def stC(kb):
                            sb_ = (base + kb) % 4
                            self.mm(po[:, :w], vt[:, kb, g * 128:(g + 1) * 128], pt[sb_][:, :w], kb == 0, kb == nkb - 1,
                                    r=['vt', ('pt', sb_)], w=[('ps', 4 + it % 2)])
                            if kb % 2 == 1:
                                sp_ = (base + kb - 1) % 4
                                p2 = pt2[(kb // 2) % 2]
                                self.tt(p2[:, :w], pt[sp_][:, :w], pt[sb_][:, :w], ALU.add,
                                        r=[('pt', sp_), ('pt', sb_)], w=[('pt2', (kb // 2) % 2)])
                                self.mm(pl[:, :w], self.onesb[:], p2[:, :w], kb == 1, kb == nkb - 1,
                                        r=['onesb', ('pt2', (kb // 2) % 2)], w=[('ps', 6 + it % 2)])
                        PF = 2
                        for kb in range(min(PF, nkb)):
                            stA(kb)
                            stB(kb)
                        for kb in range(nkb):
                            if kb + PF < nkb:
                                stA(kb + PF)
                                stB(kb + PF)
                            stC(kb)
                        ie += nkb
                        S.op('dve', lambda: nc.vector.reciprocal(out=rl[it % 2][:, :w], in_=pl[:, :w]),
                             r=[('ps', 6 + it % 2)], w=[('rl', it % 2)])
                        self.tt(ost[it % 2][:, :w], po[:, :w], rl[it % 2][:, :w], ALU.mult,
                                r=[('ps', 4 + it % 2), ('rl', it % 2)], w=[('ost', it % 2)])
                        S.dma(self.OT[h * 128:(h + 1) * 128, t0:t0 + w], ost[it % 2][:, :w], r=[('ost', it % 2)], w=[('OT', h, si)])
                        it += 1
        S.barrier(keep=KEEP)

    def s5_s1(self, li, ab, abkey):
        S = self.S
        with ExitStack() as es:
            scr = self.norm_alloc(es)
            scr['xn'] = self.sb(es, 's1_xn', [128, KT, 512])
            hsb = [self.sb(es, f's1_hs{i}', [128, KT, 512]) for i in range(2)]
            ubb = [self.sb(es, f's1_ub{i}', [128, KT, 512], BF16) for i in range(2)]
            for si, (t0, w, isc) in enumerate(self.spans):
                hs, hkey = hsb[si % 2], ('hs', si % 2)
                r_ = 1 if isc else 0
                S.dma(hs[:, :, :w], self.hT_span(t0, w), r=[('hT', si)], w=[hkey])
                self.norm_span(hs, hkey, w, ab[:, r_, 0, :], ab[:, r_, 1, :], abkey, None, None, ubb[si % 2], ('ub', si % 2), scr)
                S.dma(self.UT.rearrange("(k p) t -> p k t", p=128)[:, :, t0:t0 + w], ubb[si % 2][:, :, :w],
                      r=[('ub', si % 2)], w=[('UT', si)])
        S.barrier(keep=KEEP)

    def s5_s2(self, ki):
        nc, S, I = self.nc, self.S, self.I
        ps = self.ps
        T = self.T
        PI = float(np.pi)
        with ExitStack() as es:
            es0 = ExitStack()
            par = self.sb(es, 's5par', [128, 3, 64])
            d32 = self.sb(es, 's5d', [32, 32])
            dsk = self.sb(es, 's5dsk', [32, 32, 32], BF16)
            BT = self.sb(es, 's5BT', [32, 64, 2, 128], BF16)
            Cw = self.sb(es, 's5Cw', [128, 3, 64, 32], BF16)
            v = {n: self.sb(es, 's5_' + n, [128, 64]) for n in
                 ('dt', 'mag', 'th', 't', 's', 'y', 'm', 'cos', 'sin', 'abr', 'abi', 'den', 'nr', 'cre', 'cim', 'u1', 'u2')}
            vi = self.sb(es, 's5_ti', [128, 64], mybir.dt.int32)
            PL = self.sb(es, 's5PL', [128, 10, 2, 64])
            nGs = self.sb(es, 's5nGs', [128, 2, 64])
            Bt = self.sb(es0, 's5B', [128, 2, 64, 32])
            Ct = self.sb(es0, 's5C', [128, 2, 64, 32])
            bb = self.sb(es0, 's5bb', [128, 2, 64, 32])
            tmpb = self.sb(es0, 's5tmpb', [128, 64, 32])
            with nc.allow_non_contiguous_dma(reason="small parameter tables"):
                S.dma(par[:].rearrange("p a b -> p (a b)"), I['s5_par'], w=['par'])
                S.dma(Bt[:].rearrange("p a b c -> p (a b c)"), I['s5_B'], w=['Bt'])
                S.dma(Ct[:].rearrange("p a b c -> p (a b c)"), I['s5_C'], w=['Ct'])
                S.dma(d32[:], I['s5_d'], w=['d32'])
            K_ = ['dk']

            def TS(o, i, s1, op0, s2=None, op1=None):
                self.ts(o, i, s1, op0, r=K_ + ['par'], w=K_, s2=s2, op1=op1)

            def TT(o, a, b, op):
                self.tt(o, a, b, op, r=K_ + ['par'], w=K_)
            are, aim, ldt = par[:, 0, :], par[:, 1, :], par[:, 2, :]
            self.act(v['dt'][:], ldt, AF.Exp, r=['par'], w=K_)
            TT(v['t'][:], are, v['dt'][:], ALU.mult)
            self.act(v['mag'][:], v['t'][:], AF.Exp, r=K_, w=K_)
            TT(v['th'][:], aim, v['dt'][:], ALU.mult)

            def sin_of(dst, shift):
                TS(v['t'][:], v['th'][:], 1.0 / (2 * PI), ALU.mult, s2=shift / (2 * PI), op1=ALU.add)
                self.cp(vi[:], v['t'][:], r=K_, w=K_)
                self.cp(v['t'][:], vi[:], r=K_, w=K_)
                TS(v['s'][:], v['th'][:], shift, ALU.add)
                self.stt(v['y'][:], v['t'][:], -2 * PI, v['s'][:], ALU.mult, ALU.add, r=K_, w=K_)
                TS(v['m'][:], v['y'][:], PI, ALU.is_gt)
                self.stt(v['y'][:], v['m'][:], -2 * PI, v['y'][:], ALU.mult, ALU.add, r=K_, w=K_)
                TS(v['m'][:], v['y'][:], -PI, ALU.is_lt)
                self.stt(v['y'][:], v['m'][:], 2 * PI, v['y'][:], ALU.mult, ALU.add, r=K_, w=K_)
                self.act(dst, v['y'][:], AF.Sin, r=K_, w=K_)
            sin_of(v['sin'][:], 0.0)
            sin_of(v['cos'][:], PI / 2)
            TT(v['abr'][:], v['mag'][:], v['cos'][:], ALU.mult)
            TT(v['abi'][:], v['mag'][:], v['sin'][:], ALU.mult)
            TT(v['den'][:], are, are, ALU.mult)
            TT(v['u1'][:], aim, aim, ALU.mult)
            TT(v['den'][:], v['den'][:], v['u1'][:], ALU.add)
            S.op('dve', lambda: nc.vector.reciprocal(out=v['den'][:], in_=v['den'][:]), r=K_, w=K_)
            TS(v['nr'][:], v['abr'][:], -1.0, ALU.add)
            TT(v['u1'][:], v['nr'][:], are, ALU.mult)
            TT(v['u2'][:], v['abi'][:], aim, ALU.mult)
            TT(v['u1'][:], v['u1'][:], v['u2'][:], ALU.add)
            TT(v['cre'][:], v['u1'][:], v['den'][:], ALU.mult)
            TT(v['u1'][:], v['abi'][:], are, ALU.mult)
            TT(v['u2'][:], v['nr'][:], aim, ALU.mult)
            TT(v['u1'][:], v['u1'][:], v['u2'][:], ALU.subtract)
            TT(v['cim'][:], v['u1'][:], v['den'][:], ALU.mult)
            creb = v['cre'][:].unsqueeze(2).to_broadcast([128, 64, 32])
            cimb = v['cim'][:].unsqueeze(2).to_broadcast([128, 64, 32])
            self.tt(bb[:, 0], Bt[:, 0], creb, ALU.mult, r=K_ + ['Bt'], w=['bb'])
            self.tt(tmpb[:], Bt[:, 1], cimb, ALU.mult, r=K_ + ['Bt'], w=['tmpb'])
            self.tt(bb[:, 0], bb[:, 0], tmpb[:], ALU.subtract, r=['bb', 'tmpb'], w=['bb'])
            self.tt(bb[:, 1], Bt[:, 1], creb, ALU.mult, r=K_ + ['Bt'], w=['bb'])
            self.tt(tmpb[:], Bt[:, 0], cimb, ALU.mult, r=K_ + ['Bt', 'bb'], w=['tmpb'])
            self.tt(bb[:, 1], bb[:, 1], tmpb[:], ALU.add, r=['bb', 'tmpb'], w=['bb'])
            n_ = 0
            for dj in range(64):
                for ri in range(2):
                    bank = 6 + (n_ // 4) % 2
                    S.op('pe', lambda: nc.tensor.transpose(out=ps[bank][0:32, (n_ % 4) * 128:(n_ % 4 + 1) * 128],
                                                           in_=bb[:, ri, dj, :], identity=self.ident[:]),
                         r=['bb', 'ident'], w=[('ps', bank)])
                    if n_ % 4 == 3:
                        dj0 = dj - 1
                        self.cp(BT[:, dj0:dj0 + 2, :, :].rearrange("p a b c -> p (a b c)"), ps[bank][0:32, :],
                                r=[('ps', bank)], w=['BT'], e='act')
                    n_ += 1
            self.cp(Cw[:, 0], Ct[:, 0], r=['Ct'], w=['Cw'])
            self.ts(Cw[:, 1], Ct[:, 0], -1.0, ALU.mult, r=['Ct'], w=['Cw'])
            self.ts(Cw[:, 2], Ct[:, 1], -1.0, ALU.mult, r=['Ct'], w=['Cw'])
            self.tt(dsk[:], self.ident[0:32, 0:32].unsqueeze(1).to_broadcast([32, 32, 32]),
                    d32[:].unsqueeze(2).to_broadcast([32, 32, 32]), ALU.mult, r=['ident', 'd32'], w=['dsk'])
            self.cp(PL[:, 0, 0, :], v['cos'][:], r=K_, w=['PL'])
            self.cp(PL[:, 0, 1, :], v['sin'][:], r=K_, w=['PL'])
            for l in range(9):
                c_, s_ = PL[:, l, 0, :], PL[:, l, 1, :]
                self.tt(v['u1'][:], c_, c_, ALU.mult, r=['PL'] + K_, w=K_)
                self.tt(v['u2'][:], s_, s_, ALU.mult, r=['PL'] + K_, w=K_)
                self.tt(PL[:, l + 1, 0, :], v['u1'][:], v['u2'][:], ALU.subtract, r=K_ + ['PL'], w=['PL'])
                self.tt(v['u1'][:], c_, s_, ALU.mult, r=['PL'] + K_, w=K_)
                self.ts(PL[:, l + 1, 1, :], v['u1'][:], 2.0, ALU.mult, r=K_ + ['PL'], w=['PL'])
            self.ts(nGs[:, 0, :], PL[:, 8, 1, :], -1.0, ALU.mult, r=['PL'], w=['nGs'])
            self.ts(nGs[:, 1, :], PL[:, 9, 1, :], -1.0, ALU.mult, r=['PL'], w=['nGs'])

            S.barrier(keep=KEEP)
            es0.close()
            cosT = self.sb(es, 's5cosT', [128, 4, 512])
            sinT = self.sb(es, 's5sinT', [128, 4, 512])
            Rt = self.sb(es, 's5Rt', [128, 4, 512])
            ga = self.sb(es, 's5ga', [128, 4, 256])
            gb_ = self.sb(es, 's5gb', [128, 4, 256])
            ujb = [self.sb(es, f's5uj{i}', [32, T], BF16) for i in range(2)]
            ystb = [self.sb(es, f's5yst{i}', [32, 512]) for i in range(2)]
            Wb = [{n: self.sb(es, f's5w{i}_' + n, [128, 512]) for n in ('t1', 't2', 't3', 't4', 'wir', 'wii', 'wr', 'wi')} for i in range(2)]
            Ppb = [[self.sb(es, f's5P{i}_{j}', [128, 512], BF16) for i in range(4)] for j in range(2)]
            car = self.sb(es, 's5car', [128, 4])
            lat = [s_ for s_ in enumerate(self.spans) if not s_[1][2]]
            ctxs = [s_ for s_ in enumerate(self.spans) if s_[1][2]]
            orders = [ctxs + lat, ctxs + lat[::-1]]
            it = 0
            for dr in range(2):
                Y = self.YF if dr == 0 else self.YB
                for jb in range(8):
                    cols = slice(dr * 32 + 4 * jb, dr * 32 + 4 * jb + 4)
                    tk = ['tab']
                    S.op('pool', lambda: nc.gpsimd.memset(cosT[:, :, 0:1], 1.0), r=[], w=tk)
                    S.op('pool', lambda: nc.gpsimd.memset(sinT[:, :, 0:1], 0.0), r=[], w=tk)
                    for l in range(9):
                        n = 1 << l
                        pc = PL[:, l, 0, cols].unsqueeze(2).to_broadcast([128, 4, n])
                        ps_ = PL[:, l, 1, cols].unsqueeze(2).to_broadcast([128, 4, n])
                        c0, s0 = cosT[:, :, 0:n], sinT[:, :, 0:n]
                        self.tt(ga[:, :, 0:n], c0, pc, ALU.mult, r=tk + ['PL'], w=['ga'], e='pool')
                        self.tt(gb_[:, :, 0:n], s0, ps_, ALU.mult, r=tk + ['PL'], w=['gb'], e='pool')
                        self.tt(cosT[:, :, n:2 * n], ga[:, :, 0:n], gb_[:, :, 0:n], ALU.subtract, r=['ga', 'gb'], w=tk, e='pool')
                        self.tt(ga[:, :, 0:n], s0, pc, ALU.mult, r=tk + ['PL'], w=['ga'], e='pool')
                        self.tt(gb_[:, :, 0:n], c0, ps_, ALU.mult, r=tk + ['PL'], w=['gb'], e='pool')
                        self.tt(sinT[:, :, n:2 * n], ga[:, :, 0:n], gb_[:, :, 0:n], ALU.add, r=['ga', 'gb'], w=tk, e='pool')
                    self.cp(Rt[:], v['mag'][:, cols].unsqueeze(2).to_broadcast([128, 4, 512]), r=K_, w=tk, e='pool')
                    for jj in range(4):
                        j = 4 * jb + jj
                        dj = dr * 32 + j
                        uj, ukey = ujb[it % 2], ('uj', it % 2)
                        S.dma(uj[:], self.UT[32 * j:32 * j + 32, :], w=[ukey])
                        S.op('dve', lambda: nc.vector.memset(car[:], 0.0), r=[], w=['car'])
                        cT, sT, rT = cosT[:, jj, :], sinT[:, jj, :], Rt[:, jj, :]
                        for n2, (si, (t0, w, isc)) in enumerate(orders[dr]):
                            b2 = n2 % 2
                            W_, Pp, yst = Wb[b2], Ppb[b2], ystb[b2]
                            kk = lambda n: (n, b2)
                            pbr, pbi, py = ps[2 * b2], ps[2 * b2 + 1], ps[4 + b2]
                            usl = uj[:, t0:t0 + w]
                            rhs = usl if dr == 0 else usl[:, ::-1]
                            self.mm(pbr[:, :w], BT[:, dj, 0, :], rhs, True, True, r=['BT', ukey], w=[('ps', 2 * b2)])
                            self.mm(pbi[:, :w], BT[:, dj, 1, :], rhs, True, True, r=['BT', ukey], w=[('ps', 2 * b2 + 1)])
                            self.tt(W_['t1'][:, :w], pbr[:, :w], cT[:, :w], ALU.mult, r=[('ps', 2 * b2)] + tk, w=[kk('t1')])
                            self.tt(W_['t2'][:, :w], pbi[:, :w], sT[:, :w], ALU.mult, r=[('ps', 2 * b2 + 1)] + tk, w=[kk('t2')])
                            self.tt(W_['t3'][:, :w], pbi[:, :w], cT[:, :w], ALU.mult, r=[('ps', 2 * b2 + 1)] + tk, w=[kk('t3')])
                            self.tt(W_['t4'][:, :w], pbr[:, :w], sT[:, :w], ALU.mult, r=[('ps', 2 * b2)] + tk, w=[kk('t4')])
                            self.tt(W_['wir'][:, :w], W_['t1'][:, :w], W_['t2'][:, :w], ALU.add, r=[kk('t1'), kk('t2')], w=[kk('wir')])
                            self.tt(W_['wii'][:, :w], W_['t3'][:, :w], W_['t4'][:, :w], ALU.subtract, r=[kk('t3'), kk('t4')], w=[kk('wii')])
                            S.op('dve', lambda: nc.vector.tensor_tensor_scan(out=W_['wr'][:, :w], data0=rT[:, :w], data1=W_['wir'][:, :w],
                                                                           initial=car[:, 0:1], op0=ALU.mult, op1=ALU.add),
                                 r=[kk('wir'), 'car'] + tk, w=[kk('wr')])
                            S.op('dve', lambda: nc.vector.tensor_tensor_scan(out=W_['wi'][:, :w], data0=rT[:, :w], data1=W_['wii'][:, :w],
                                                                           initial=car[:, 1:2], op0=ALU.mult, op1=ALU.add),
                                 r=[kk('wii'), 'car'] + tk, w=[kk('wi')])
                            lv = 0 if w == 256 else 1
                            Gc, Gs, nG = PL[:, 8 + lv, 0, dj:dj + 1], PL[:, 8 + lv, 1, dj:dj + 1], nGs[:, lv, dj:dj + 1]
                            wrl, wil = W_['wr'][:, w - 1:w], W_['wi'][:, w - 1:w]
                            self.ts(car[:, 2:3], wrl, Gc, ALU.mult, r=[kk('wr'), 'PL', 'car'], w=['car'])
                            self.stt(car[:, 0:1], wil, nG, car[:, 2:3], ALU.mult, ALU.add, r=[kk('wi'), 'nGs', 'car'], w=['car'])
                            self.ts(car[:, 3:4], wil, Gc, ALU.mult, r=[kk('wi'), 'PL', 'car'], w=['car'])
                            self.stt(car[:, 1:2], wrl, Gs, car[:, 3:4], ALU.mult, ALU.add, r=[kk('wr'), 'PL', 'car'], w=['car'])
                            self.tt(Pp[0][:, :w], cT[:, :w], W_['wr'][:, :w], ALU.mult, r=tk + [kk('wr')], w=[('P', 0, b2)], e='pool')
                            self.tt(Pp[1][:, :w], sT[:, :w], W_['wi'][:, :w], ALU.mult, r=tk + [kk('wi')], w=[('P', 1, b2)], e='pool')
                            self.tt(Pp[2][:, :w], sT[:, :w], W_['wr'][:, :w], ALU.mult, r=tk + [kk('wr')], w=[('P', 2, b2)], e='pool')
                            self.tt(Pp[3][:, :w], cT[:, :w], W_['wi'][:, :w], ALU.mult, r=tk + [kk('wi')], w=[('P', 3, b2)], e='pool')
                            yk = ('ps', 4 + b2)
                            self.mm(py[0:32, :w], Cw[:, 0, dj, :], Pp[0][:, :w], True, False, r=['Cw', ('P', 0, b2)], w=[yk])
                            self.mm(py[0:32, :w], Cw[:, 1, dj, :], Pp[1][:, :w], False, False, r=['Cw', ('P', 1, b2)], w=[yk])
                            self.mm(py[0:32, :w], Cw[:, 2, dj, :], Pp[2][:, :w], False, False, r=['Cw', ('P', 2, b2)], w=[yk])
                            self.mm(py[0:32, :w], Cw[:, 2, dj, :], Pp[3][:, :w], False, dr == 1, r=['Cw', ('P', 3, b2)], w=[yk])
                            if dr == 0:
                                self.mm(py[0:32, :w], dsk[:, j, :], usl, False, True, r=['dsk', ukey], w=[yk])
                            src = py[0:32, :w] if dr == 0 else py[0:32, :w][:, ::-1]
                            self.cp(yst[:, :w], src, r=[yk], w=[('yst', b2)], e='act')
                            S.dma(Y[32 * j:32 * j + 32, t0:t0 + w], yst[:, :w], r=[('yst', b2)], w=[('Y', dr, j, si)])
                        it += 1
        S.barrier(keep=KEEP)

    def ssd_d1(self, li, ki, ab, abkey):
        nc, S, I = self.nc, self.S, self.I
        ps = self.ps
        with ExitStack() as es:
            scr = self.norm_alloc(es)
            scr['xn'] = self.sb(es, 'd1_xn', [128, KT, 512])
            win = self.sb(es, 'd1_win', [128, KT, 5184], BF16)
            dtb = self.sb(es, 'd1_dtb', [128, 64])
            hsb = [self.sb(es, f'd1_hs{i}', [128, KT, 512]) for i in range(2)]
            ub = self.sb(es, 'd1_ub', [128, KT, 512], BF16)
            zst = [self.sb(es, f'd1_zst{i}', [128, 2 * D], BF16) for i in range(2)]
            xst = [self.sb(es, f'd1_xst{i}', [128, 8, 512], BF16) for i in range(2)]
            dst = [self.sb(es, f'd1_dst{i}', [128, 64]) for i in range(2)]
            for k in range(KT):
                S.dma(win[:, k, :], self.winb[ki][k * 128:(k + 1) * 128, :], r=[('winb', ki)], w=['win'])
            S.dma(dtb[:], I['ssd_dtb'], w=['dtb'])
            nz = nx = nd = 0
            for si, (t0, w, isc) in enumerate(self.spans):
                hs, hkey = hsb[si % 2], ('hs', si % 2)
                r_ = 1 if isc else 0
                S.dma(hs[:, :, :w], self.hT_span(t0, w), r=[('hT', si)], w=[hkey])
                self.norm_span(hs, hkey, w, ab[:, r_, 0, :], ab[:, r_, 1, :], abkey, None, None, ub, 'ub', scr)
                for tt_ in range(w // 128):
                    zt, zk = zst[nz % 2], ('zst', nz % 2)
                    for cg in range(4):
                        b = cg % 2
                        for k in range(KT):
                            self.mm(ps[b][:, :], ub[:, k, tt_ * 128:(tt_ + 1) * 128], win[:, k, cg * 512:(cg + 1) * 512],
                                    k == 0, k == KT - 1, r=['win', 'ub'], w=[('ps', b)])
                        self.act(zt[:, cg * 512:(cg + 1) * 512], ps[b][:, :], AF.Silu, r=[('ps', b)], w=[zk])
                    S.dma(self.ZS[t0 + tt_ * 128:t0 + (tt_ + 1) * 128, :], zt[:], r=[zk], w=[('ZS', nz)])
                    nz += 1
                    dt_, dk = dst[nd % 2], ('dst', nd % 2)
                    for k in range(KT):
                        self.mm(ps[6][:, 0:64], ub[:, k, tt_ * 128:(tt_ + 1) * 128], win[:, k, 5120:5184],
                                k == 0, k == KT - 1, r=['win', 'ub'], w=[('ps', 6)])
                    self.tt(dt_[:], ps[6][:, 0:64], dtb[:], ALU.add, r=[('ps', 6), 'dtb'], w=[dk])
                    self.act(dt_[:], dt_[:], AF.Exp, r=[dk], w=[dk])
                    self.act(dt_[:], dt_[:], AF.Ln, r=[dk], w=[dk], bias=1.0, scale=1.0)
                    S.dma(self.DTs[t0 + tt_ * 128:t0 + (tt_ + 1) * 128, :], dt_[:], r=[dk], w=[('DTs', nd)])
                    nd += 1
                for c8 in range(3):
                    xt, xk = xst[nx % 2], ('xst', nx % 2)
                    for c in range(8):
                        ct = c8 * 8 + c
                        b = 2 + ct % 2
                        for k in range(KT):
                            self.mm(ps[b][:, :w], win[:, k, 2048 + ct * 128:2048 + (ct + 1) * 128], ub[:, k, :w],
                                    k == 0, k == KT - 1, r=['win', 'ub'], w=[('ps', b)])
                        self.cp(xt[:, c, :w], ps[b][:, :w], r=[('ps', b)], w=[xk], e='dve' if c % 2 == 0 else 'act')
                    S.dma(self.XBC.rearrange("(k p) t -> p k t", p=128)[:, c8 * 8:(c8 + 1) * 8, t0:t0 + w], xt[:, :, :w],
                          r=[xk], w=[('XBC', nx)])
                    nx += 1
        S.barrier(keep=KEEP)

    def ssd_d2(self):
        nc, S, I = self.nc, self.S, self.I
        ps = self.ps
        T = self.T
        with ExitStack() as es:
            cw = self.sb(es, 'd2_cw', [128, 24, 5])
            cb = self.sb(es, 'd2_cb', [128, 24])
            xin = [self.sb(es, f'd2_xin{i}', [128, 24, 516], BF16) for i in range(2)]
            xc = [self.sb(es, f'd2_xc{i}', [128, 24, 512], BF16) for i in range(2)]
            cv = [self.sb(es, f'd2_cv{i}', [128, 512]) for i in range(2)]
            xts = [self.sb(es, f'd2_xts{i}', [128, 2 * D], BF16) for i in range(2)]
            bts = [self.sb(es, f'd2_bts{i}', [128, 512], BF16) for i in range(2)]
            S.dma(cw[:].rearrange("p a b -> p (a b)"), I['ssd_cw'], w=['cw'])
            S.dma(cb[:], I['ssd_cb'], w=['cb'])
            nt = 0
            for si, (t0, w, isc) in enumerate(self.spans):
                s0, s1 = (0, TC) if isc else (TC, T)
                lo, hi = max(t0 - 2, s0), min(t0 + w + 2, s1)
                xi, xik = xin[si % 2], ('xin', si % 2)
                if lo > t0 - 2:
                    S.op('dve', lambda: nc.vector.memset(xi[:, :, 0:2], 0.0), w=[xik])
                if hi < t0 + w + 2:
                    S.op('dve', lambda: nc.vector.memset(xi[:, :, w + 2:w + 4], 0.0), w=[xik])
                for c8 in range(3):
                    S.dma(xi[:, c8 * 8:(c8 + 1) * 8, lo - (t0 - 2):hi - (t0 - 2)],
                          self.XBC.rearrange("(k p) t -> p k t", p=128)[:, c8 * 8:(c8 + 1) * 8, lo:hi], w=[xik])
                xo, xok = xc[si % 2], ('xc', si % 2)
                for ct in range(24):
                    c_, ck = cv[ct % 2], ('cv', ct % 2)
                    self.ts(c_[:, :w], xi[:, ct, 0:w], cw[:, ct, 0:1], ALU.mult, r=[xik, 'cw', 'cb'], w=[ck], s2=cb[:, ct:ct + 1], op1=ALU.add)
                    for k in range(1, 5):
                        self.stt(c_[:, :w], xi[:, ct, k:k + w], cw[:, ct, k:k + 1], c_[:, :w], ALU.mult, ALU.add, r=[xik, 'cw', ck], w=[ck])
                    self.act(xo[:, ct, :w], c_[:, :w], AF.Silu, r=[ck], w=[xok])
                S.dma(self.BCf.rearrange("(k p) t -> p k t", p=128)[:, :, t0:t0 + w], xo[:, 16:24, :w], r=[xok], w=[('BCf', si)])
                for tt_ in range(w // 128):
                    tok = t0 + tt_ * 128
                    xt, xtk_ = xts[nt % 2], ('xts', nt % 2)
                    for half in range(2):
                        bank = half
                        pbv = ps[bank][:].bitcast(BF16)
                        for q in range(8):
                            ct = half * 8 + q
                            S.op('pe', lambda: nc.tensor.transpose(out=pbv[:, q * 128:(q + 1) * 128],
                                                                   in_=xo[:, ct, tt_ * 128:(tt_ + 1) * 128], identity=self.identb[:]),
                                 r=[xok, 'identb'], w=[('ps', bank)])
                        self.cp(xt[:, half * 1024:(half + 1) * 1024], pbv[:, 0:1024], r=[('ps', bank)], w=[xtk_],
                                e='dve' if half == 0 else 'act')
                    S.dma(self.XTK[tok:tok + 128, :], xt[:], r=[xtk_], w=[('XTK', nt)])
                    bt, btk_ = bts[nt % 2], ('bts', nt % 2)
                    pbv = ps[2][:].bitcast(BF16)
                    for q in range(4):
                        S.op('pe', lambda: nc.tensor.transpose(out=pbv[:, q * 128:(q + 1) * 128],
                                                               in_=xo[:, 16 + q, tt_ * 128:(tt_ + 1) * 128], identity=self.identb[:]),
                             r=[xok, 'identb'], w=[('ps', 2)])
                    self.cp(bt[:], pbv[:, 0:512], r=[('ps', 2)], w=[btk_])
                    S.dma(self.BTK[tok:tok + 128, :], bt[:], r=[btk_], w=[('BTK', nt)])
                    nt += 1
        S.barrier(keep=KEEP)

    def ssd_d3(self):
        nc, S, I = self.nc, self.S, self.I
        ps = self.ps
        T = self.T
        NB = T // 128
        with ExitStack() as es:
            msk = self.sb(es, 'd3_msk', [128, 4, 128])
            aneg = self.sb(es, 'd3_aneg', [128, 64])
            Sst = self.sb(es, 'd3_S', [128, 4, 512])
            Sb = self.sb(es, 'd3_Sb', [128, 4, 512], BF16)
            xtk = [self.sb(es, f'd3_xtk{i}', [128, 32, 64], BF16) for i in range(2)]
            btk = [self.sb(es, f'd3_btk{i}', [128, 512], BF16) for i in range(2)]
            bcf = [self.sb(es, f'd3_bcf{i}', [128, 8, 128], BF16) for i in range(2)]
            dtt_ = [self.sb(es, f'd3_dt{i}', [128, 32]) for i in range(2)]
            sm = self.sb(es, 'd3_sm', [128, 8, 32])
            cs = self.sb(es, 'd3_cs', [128, 64])
            xdt = self.sb(es, 'd3_xdt', [128, 32, 64], BF16)
            txdt = self.sb(es, 'd3_txdt', [128, 32, 64], BF16)
            dec = [self.sb(es, f'd3_dec{i}', [128, 128]) for i in range(4)]
            MT = [self.sb(es, f'd3_MT{i}', [128, 128], BF16) for i in range(4)]
            ytmp = [self.sb(es, f'd3_ytmp{i}', [128, 512]) for i in range(2)]
            ych = [self.sb(es, f'd3_ych{i}', [128, 2 * D]) for i in range(2)]
            S.dma(msk[:].rearrange("p a b -> p (a b)"), I['ssd_msk'], w=['msk'])
            S.dma(aneg[:], I['ssd_alog'], w=['aneg'])
            self.act(aneg[:], aneg[:], AF.Exp, r=['aneg'], w=['aneg'])
            self.ts(aneg[:], aneg[:], -1.0, ALU.mult, r=['aneg'], w=['aneg'])
            ctx_ch = list(range(TC // 128))
            lat_ch = list(range(TC // 128, NB))
            orders = [ctx_ch + lat_ch, ctx_ch[::-1] + lat_ch[::-1]]
            it = 0
            for dr in range(2):
                S.op('dve', lambda: nc.vector.memset(Sst[:], 0.0), w=['S'])
                S.op('dve', lambda: nc.vector.memset(Sb[:], 0.0), w=['Sb'])
                tri, mneg = msk[:, dr, :], msk[:, 2 + dr, :]
                for c in orders[dr]:
                    tok = c * 128
                    p2 = it % 2
                    x_, xk = xtk[p2], ('xtk', p2)
                    b_, bk = btk[p2], ('btk', p2)
                    f_, fk = bcf[p2], ('bcf', p2)
                    d_, dk = dtt_[p2], ('dt', p2)
                    S.dma(x_[:].rearrange("p h q -> p (h q)"), self.XTK[tok:tok + 128, :], w=[xk])
                    S.dma(b_[:], self.BTK[tok:tok + 128, :], w=[bk])
                    S.dma(f_[:], self.BCf.rearrange("(k p) t -> p k t", p=128)[:, :, tok:tok + 128], w=[fk])
                    with nc.allow_non_contiguous_dma(reason="dt half rows"):
                        S.dma(d_[:], self.DTs[tok:tok + 128, dr * 32:(dr + 1) * 32], w=[dk])
                    smk = ['sm']
                    self.tt(sm[:, 0, :], d_[:], aneg[:, dr * 32:(dr + 1) * 32], ALU.mult, r=[dk, 'aneg'], w=smk)
                    self.mm(ps[0][:, 0:32], tri, sm[:, 0, :], True, True, r=['msk'] + smk, w=[('ps', 0)])
                    self.mm(ps[0][:, 32:64], self.ones32[:], sm[:, 0, :], True, True, r=['ones32'] + smk, w=[('ps', 0)])
                    self.cp(cs[:], ps[0][:, 0:64], r=[('ps', 0)], w=['cs'])
                    self.ts(sm[:, 1, :], cs[:, 0:32], -1.0, ALU.mult, r=['cs'], w=smk)
                    self.act(sm[:, 2, :], cs[:, 0:32], AF.Exp, r=['cs'], w=smk)
                    self.act(sm[:, 3, :], cs[:, 32:64], AF.Exp, r=['cs'], w=smk)
                    self.tt(sm[:, 4, :], cs[:, 32:64], cs[:, 0:32], ALU.subtract, r=['cs'], w=smk)
                    self.act(sm[:, 5, :], sm[:, 4, :], AF.Exp, r=smk, w=smk)
                    self.tt(sm[:, 6, :], d_[:], sm[:, 5, :], ALU.mult, r=[dk] + smk, w=smk)
                    self.tt(xdt[:], x_[:], d_[:].unsqueeze(2).to_broadcast([128, 32, 64]), ALU.mult, r=[xk, dk], w=['xdt'])
                    self.tt(txdt[:], x_[:], sm[:, 6, :].unsqueeze(2).to_broadcast([128, 32, 64]), ALU.mult, r=[xk] + smk, w=['txdt'], e='pool')
                    for g in range(4):
                        self.mm(ps[1][:, g * 128:(g + 1) * 128], f_[:, g, :], f_[:, 4 + g, :], True, True, r=[fk], w=[('ps', 1)])
                    y_, yk = ych[p2], ('ych', p2)

                    def hA(h):
                        bkn = 2 + (h // 4) % 2
                        col = (h % 4) * 128
                        self.mm(ps[bkn][:, col:col + 128], cs[:, h:h + 1].to_broadcast([128, 128]), self.ident[:], True, False,
                                r=['cs', 'ident'], w=[('ps', bkn)])
                        self.mm(ps[bkn][:, col:col + 128], self.ident[:], mneg, False, True, r=['ident', 'msk'], w=[('ps', bkn)])

                    def hB(h):
                        bkn = 2 + (h // 4) % 2
                        col = (h % 4) * 128
                        self.act(dec[h % 4][:], ps[bkn][:, col:col + 128], AF.Exp, r=[('ps', bkn)] + smk, w=[('dec', h % 4)],
                                 bias=sm[:, 1, h:h + 1], scale=1.0)

                    def hCD(h):
                        g, hh = h // 8, h % 8
                        pa = ps[4 + g % 2]
                        self.tt(MT[h % 4][:], dec[h % 4][:], ps[1][:, g * 128:(g + 1) * 128], ALU.mult,
                                r=[('dec', h % 4), ('ps', 1)], w=[('MT', h % 4)])
                        self.mm(pa[:, hh * 64:(hh + 1) * 64], MT[h % 4][:], xdt[:, h, :], True, True,
                                r=[('MT', h % 4), 'xdt'], w=[('ps', 4 + g % 2)])
                    PF = 3
                    for h in range(PF):
                        hA(h)
                        hB(h)
                    for h in range(32):
                        if h + PF < 32:
                            hA(h + PF)
                            hB(h + PF)
                        hCD(h)
                        if h % 8 == 7:
                            g = h // 8
                            pa, pbk = ps[4 + g % 2], ps[6 + g % 2]
                            self.mm(pbk[:, :], f_[:, 4 + g, :], Sb[:, g, :], True, True, r=[fk, 'Sb'], w=[('ps', 6 + g % 2)])
                            self.tt(ytmp[g % 2][:].rearrange("p (h q) -> p h q", h=8), pbk[:, :].rearrange("p (h q) -> p h q", h=8),
                                    sm[:, 2, g * 8:(g + 1) * 8].unsqueeze(2).to_broadcast([128, 8, 64]), ALU.mult,
                                    r=[('ps', 6 + g % 2)] + smk, w=[('ytmp', g % 2)])
                            self.tt(y_[:, g * 512:(g + 1) * 512], pa[:, :], ytmp[g % 2][:], ALU.add,
                                    r=[('ps', 4 + g % 2), ('ytmp', g % 2)], w=[yk])
                    S.dma(self.YD[dr, tok:tok + 128, :], y_[:], r=[yk], w=[('YD', dr, c)])
                    for g in range(4):
                        pst = ps[2 + g % 2]
                        self.mm(pst[:, :], b_[:, g * 128:(g + 1) * 128], txdt[:, g * 8:(g + 1) * 8, :].rearrange("p h q -> p (h q)"),
                                True, True, r=[bk, 'txdt'], w=[('ps', 2 + g % 2)])
                        sv = Sst[:, g, :].rearrange("p (h q) -> p h q", h=8)
                        self.tt(sv, sv, sm[:, 3, g * 8:(g + 1) * 8].unsqueeze(2).to_broadcast([128, 8, 64]), ALU.mult, r=['S'] + smk, w=['S'])
                        self.tt(Sst[:, g, :], Sst[:, g, :], pst[:, :], ALU.add, r=['S', ('ps', 2 + g % 2)], w=['S'])
                        self.cp(Sb[:, g, :], Sst[:, g, :], r=['S'], w=['Sb'], e='pool')
                    it += 1
        S.barrier(keep=KEEP)

    def ssd_d4(self, li, ki, ab, abkey, need_ctx):
        nc, S, I = self.nc, self.S, self.I
        ps = self.ps
        with ExitStack() as es:
            wout = self.sb(es, 'd4_wout', [128, 16, D], BF16)
            ngr = self.sb(es, 'd4_ng', [128, 2 * D])
            dh = self.sb(es, 'd4_dh', [128, 32])
            eps_ = self.sb(es, 'd4_eps', [128, 1])
            hsb = [self.sb(es, f'd4_hs{i}', [128, KT, 512]) for i in range(2)]
            yf = [self.sb(es, f'd4_yf{i}', [128, 2 * D]) for i in range(2)]
            yb = [self.sb(es, f'd4_yb{i}', [128, 2 * D]) for i in range(2)]
            xk_ = [self.sb(es, f'd4_x{i}', [128, 2 * D], BF16) for i in range(2)]
            zs = [self.sb(es, f'd4_z{i}', [128, 2 * D], BF16) for i in range(2)]
            tmp = self.sb(es, 'd4_tmp', [128, 2 * D])
            ynb = self.sb(es, 'd4_ynb', [128, 2 * D], BF16)
            ynT = self.sb(es, 'd4_ynT', [128, 16, 512], BF16)
            st = self.sb(es, 'd4_st', [128, 4])
            S.dma(wout[:], self.woutb[ki].rearrange("(k p) n -> p k n", p=128), r=[('woutb', ki)], w=['wout'])
            S.dma(ngr[:], I['ssd_ng'], w=['ngr'])
            S.dma(dh[:], I['ssd_dh'], w=['dh'])
            S.op('dve', lambda: nc.vector.memset(eps_[:], EPS), w=['eps'])
            spans = [s_ for s_ in enumerate(self.spans) if need_ctx or not s_[1][2]]
            nt = 0
            for n_, (si, (t0, w, isc)) in enumerate(spans):
                hs, hkey = hsb[n_ % 2], ('hs', n_ % 2)
                r_ = 1 if isc else 0
                S.dma(hs[:, :, :w], self.hT_span(t0, w), r=[('hT', si)], w=[hkey])
                for tt_ in range(w // 128):
                    tok = t0 + tt_ * 128
                    p2 = nt % 2
                    S.dma(yf[p2][:], self.YD[0, tok:tok + 128, :], w=[('yf', p2)])
                    S.dma(yb[p2][:], self.YD[1, tok:tok + 128, :], w=[('yb', p2)])
                    S.dma(xk_[p2][:], self.XTK[tok:tok + 128, :], w=[('x', p2)])
                    S.dma(zs[p2][:], self.ZS[tok:tok + 128, :], w=[('z', p2)])
                    y = yf[p2]
                    yk = ('yf', p2)
                    self.tt(y[:], y[:], yb[p2][:], ALU.add, r=[yk, ('yb', p2)], w=[yk], e='pool')
                    self.tt(tmp[:].rearrange("p (h q) -> p h q", h=32), xk_[p2][:].rearrange("p (h q) -> p h q", h=32),
                            dh[:].unsqueeze(2).to_broadcast([128, 32, 64]), ALU.mult, r=[('x', p2), 'dh'], w=['tmp'], e='pool')
                    self.tt(y[:], y[:], tmp[:], ALU.add, r=[yk, 'tmp'], w=[yk])
                    self.tt(y[:], y[:], zs[p2][:], ALU.mult, r=[yk, ('z', p2)], w=[yk])
                    self.act(tmp[:], y[:], AF.Square, r=[yk], w=['tmp', 'st'], accum_out=st[:, 0:1])
                    self.act(st[:, 1:2], st[:, 0:1], AF.Sqrt, r=['st', 'eps'], w=['st'], scale=1.0 / (2 * D), bias=eps_[:, 0:1])
                    S.op('dve', lambda: nc.vector.reciprocal(out=st[:, 2:3], in_=st[:, 1:2]), r=['st'], w=['st'])
                    self.stt(ynb[:], y[:], st[:, 2:3], ngr[:], ALU.mult, ALU.mult, r=[yk, 'st', 'ngr'], w=['ynb'])
                    for half in range(2):
                        bank = half
                        pbv = ps[bank][:].bitcast(BF16)
                        for q in range(8):
                            k = half * 8 + q
                            S.op('pe', lambda: nc.tensor.transpose(out=pbv[:, q * 128:(q + 1) * 128],
                                                                   in_=ynb[:, k * 128:(k + 1) * 128], identity=self.identb[:]),
                                 r=['ynb', 'identb'], w=[('ps', bank)])
                        self.cp(ynT[:, half * 8:(half + 1) * 8, tt_ * 128:(tt_ + 1) * 128],
                                pbv[:, 0:1024].rearrange("p (k t) -> p k t", k=8), r=[('ps', bank)], w=['ynT'],
                                e='dve' if half == 0 else 'act')
                    nt += 1
                for ct in range(KT):
                    pb = ps[2 + ct % 2]
                    for k in range(16):
                        self.mm(pb[:, :w], wout[:, k, ct * 128:(ct + 1) * 128], ynT[:, k, :w], k == 0, k == 15,
                                r=['wout', 'ynT'], w=[('ps', 2 + ct % 2)])
                    self.stt(hs[:, ct, :w], pb[:, :w], ab[:, r_, 2, ct:ct + 1], hs[:, ct, :w], ALU.mult, ALU.add,
                             r=[('ps', 2 + ct % 2), abkey, hkey], w=[hkey])
                S.dma(self.hT_span(t0, w), hs[:, :, :w], r=[hkey], w=[('hT', si)])
        S.barrier(keep=KEEP)

    def layer(self, li, kind, ki, need_ctx):
        S = self.S
        ps = self.ps
        with ExitStack() as es:
            ab = self.layer_scalars(es, li)
            abkey = ('ab', li)
            if kind == 'a':
                self.attn_a1(li, ki, ab, abkey)
                self.attn_a2(need_ctx)
            if kind == 's':
                self.s5_s1(li, ab, abkey)
                self.s5_s2(ki)
            if kind == 'd':
                self.ssd_d1(li, ki, ab, abkey)
                self.ssd_d2()
                self.ssd_d3()
                self.ssd_d4(li, ki, ab, abkey, need_ctx)
            with ExitStack() as es2:
                scr = self.norm_alloc(es2)
                M = self.moe_alloc(es2, li)
                scr['xn'] = M['acc']
                hsb = [self.sb(es2, f'hs{i}', [128, KT, 512]) for i in range(2)]
                u32 = self.sb(es2, 'u32', [128, KT, 512])
                ub = self.sb(es2, 'ub', [128, KT, 512], BF16)
                if kind == 'a':
                    wo = self.sb(es2, 'a3_wo', [128, KT, D], BF16)
                    otb = [self.sb(es2, f'a3_ot{i}', [128, KT, 512], BF16) for i in range(2)]
                    S.dma(wo[:], self.wob[ki].rearrange("(k p) n -> p k n", p=128), r=[('wob', ki)], w=['wo'])
                if kind == 's':
                    wgl = self.sb(es2, 's3_wglu', [128, KT, 2 * D], BF16)
                    bgl = self.sb(es2, 's3_bglu', [128, 16])
                    sgl = [self.sb(es2, f's3_sig{i}', [128, 512]) for i in range(2)]
                    ymx = [self.sb(es2, f's3_ymx{i}', [128, 512]) for i in range(2)]
                    S.dma(wgl[:], self.wglub[ki].rearrange("(k p) n -> p k n", p=128), r=[('wglub', ki)], w=['wgl'])
                    S.dma(bgl[:], self.I['s5_bglu'], w=['bgl'])
                spans = [s for s in enumerate(self.spans) if need_ctx or not s[1][2]]
                for n_, (si, (t0, w, isc)) in enumerate(spans):
                    hs, hkey = hsb[n_ % 2], ('hs', n_ % 2)
                    r_ = 1 if isc else 0
                    S.dma(hs[:, :, :w], self.hT_span(t0, w), r=[('hT', si)], w=[hkey])
                    if kind == 's':
                        acc = M['acc']
                        akeys = [('acc', k) for k in range(KT)]
                        S.dma(u32[:, :, :w], self.YF.rearrange("(k p) t -> p k t", p=128)[:, :, t0:t0 + w], w=['u32'])
                        S.dma(acc[:, :, :w], self.YB.rearrange("(k p) t -> p k t", p=128)[:, :, t0:t0 + w], w=akeys)
                        self.tt(u32[:, :, :w], u32[:, :, :w], acc[:, :, :w], ALU.add, r=['u32'] + akeys, w=['u32'])
                        self.tt(acc[:, :, :w], u32[:, :, :w], u32[:, :, :w], ALU.mult, r=['u32'], w=akeys, e='pool')
                        self.ts(acc[:, :, :w], acc[:, :, :w], 0.044715, ALU.mult, r=akeys, w=akeys, s2=1.0, op1=ALU.add)
                        self.tt(acc[:, :, :w], acc[:, :, :w], u32[:, :, :w], ALU.mult, r=['u32'] + akeys, w=akeys, e='pool')
                        self.act(acc[:, :, :w], acc[:, :, :w], AF.Sigmoid, r=akeys, w=akeys, scale=2.0 * float(np.sqrt(2.0 / np.pi)))
                        self.tt(ub[:, :, :w], acc[:, :, :w], u32[:, :, :w], ALU.mult, r=['u32'] + akeys, w=['ub'])
                        for ct in range(KT):
                            b = ct % 2
                            pa, pb2 = ps[b], ps[2 + b]
                            for k in range(KT):
                                self.mm(pa[:, :w], wgl[:, k, ct * 128:(ct + 1) * 128], ub[:, k, :w], k == 0, k == KT - 1,
                                        r=['wgl', 'ub'], w=[('ps', b)])
                            for k in range(KT):
                                self.mm(pb2[:, :w], wgl[:, k, D + ct * 128:D + (ct + 1) * 128], ub[:, k, :w], k == 0, k == KT - 1,
                                        r=['wgl', 'ub'], w=[('ps', 2 + b)])
                            self.act(sgl[b][:, :w], pb2[:, :w], AF.Sigmoid, r=[('ps', 2 + b), 'bgl'], w=[('sgl', b)],
                                     bias=bgl[:, 8 + ct:9 + ct], scale=1.0)
                            self.stt(ymx[b][:, :w], pa[:, :w], bgl[:, ct:ct + 1], sgl[b][:, :w], ALU.add, ALU.mult,
                                     r=[('ps', b), 'bgl', ('sgl', b)], w=[('ymx', b)])
                            self.stt(hs[:, ct, :w], ymx[b][:, :w], ab[:, r_, 2, ct:ct + 1], hs[:, ct, :w], ALU.mult, ALU.add,
                                     r=[('ymx', b), abkey, hkey], w=[hkey])
                    if kind == 'a':
                        ot, okey = otb[n_ % 2], ('ot', n_ % 2)
                        S.dma(ot[:, :, :w], self.OT.rearrange("(k p) t -> p k t", p=128)[:, :, t0:t0 + w], w=[okey])
                        for ct in range(KT):
                            pb = ps[ct % 2]
                            for k in range(KT):
                                self.mm(pb[:, :w], wo[:, k, ct * 128:(ct + 1) * 128], ot[:, k, :w], k == 0, k == KT - 1,
                                        r=['wo', okey], w=[('ps', ct % 2)])
                            self.stt(hs[:, ct, :w], pb[:, :w], ab[:, r_, 2, ct:ct + 1], hs[:, ct, :w], ALU.mult, ALU.add,
                                     r=[('ps', ct % 2), abkey, hkey], w=[hkey])
                    self.norm_span(hs, hkey, w, ab[:, r_, 3, :], ab[:, r_, 4, :], abkey, u32, 'u32', ub, 'ub', scr)
                    self.moe_span(li, hs, hkey, w, u32, ub, ab[:, r_, 5, :], abkey, M)
                    S.dma(self.hT_span(t0, w), hs[:, :, :w], r=[hkey], w=[('hT', si)])
            S.barrier(keep=KEEP)


_ROPE = {}


def rope_consts(TL):
    if TL in _ROPE:
        return _ROPE[TL]
    f = np.float32
    t = np.arange(TL)
    pos = np.stack([t // 64, t % 64], axis=-1).astype(f)
    inv = (f(10000.0) ** (-np.arange(32, dtype=f) / f(32))).astype(f)
    ang = np.broadcast_to(pos[:, :, None, None] * inv, (TL, 2, 2, 32)).reshape(TL, 128).astype(f)
    cos = np.concatenate([np.ones((TC, 128), f), np.cos(ang).astype(f)], axis=0).T
    sin = np.concatenate([np.zeros((TC, 128), f), np.sin(ang).astype(f)], axis=0).T
    pm = np.zeros((128, 128), f)
    for a in range(2):
        for j in range(32):
            pm[a * 64 + 32 + j, a * 64 + j] = -1.0
            pm[a * 64 + j, a * 64 + 32 + j] = 1.0
    _ROPE[TL] = (np.ascontiguousarray(cos), np.ascontiguousarray(sin), pm)
    return _ROPE[TL]


def ssd_masks():
    f = np.float32
    k = np.arange(128)[:, None]
    i = np.arange(128)[None, :]
    trif = (k <= i).astype(f)
    trib = (k >= i).astype(f)
    mf = np.where(k <= i, 0.0, -30000.0).astype(f)
    mb = np.where(k >= i, 0.0, -30000.0).astype(f)
    return np.ascontiguousarray(np.stack([trif, trib, mf, mb], axis=1).reshape(128, 512))


def host_inputs(inp, b, TL, layers):
    f = np.float32
    d = {}
    d['x'] = np.ascontiguousarray(inp['x'][b, :TL])
    d['ctx'] = np.ascontiguousarray(inp['ctx'][b])
    d['cc'] = np.ascontiguousarray(np.stack([inp['c'][b], inp['c_ctx']]))
    d['ident'] = np.eye(128, dtype=f)
    d['mod_w'] = inp['mod_w']
    d['mod_b'] = inp['mod_b']
    ng = np.stack([inp['norm1_g'], inp['norm2_g']])
    d['ng'] = np.ascontiguousarray(ng.reshape(2, 4, KT, 128).transpose(3, 0, 1, 2).reshape(128, -1))
    d['attn_wqkv'] = inp['attn_w_qkv']
    d['attn_wo'] = inp['attn_w_o']
    d['attn_g'] = np.ascontiguousarray(np.stack([inp['attn_q_gain'], inp['attn_k_gain']], axis=1).reshape(4, 128).T)
    f = np.float32
    ki = 0
    def gl_p(a):
        sh = a.shape
        a = a.reshape((2, 32, 2, 64) + sh[3:])
        return np.moveaxis(a, (2, 3), (0, 1)).reshape((128, 2, 32) + sh[3:])
    ldt_b = np.broadcast_to(inp['s5_log_dt'][ki][:, :, None], (2, 64, 64))
    par = np.stack([gl_p(inp['s5_a_re'][ki]), gl_p(inp['s5_a_im'][ki]), gl_p(np.ascontiguousarray(ldt_b))], axis=1)
    d['s5_par'] = np.ascontiguousarray(par.reshape(128, 3 * 64)).astype(f)
    def blockdiag(a):
        o = np.zeros((128, 2, 32, 2, 16), f)
        o[:64, :, :, 0, :] = a[:64]
        o[64:, :, :, 1, :] = a[64:]
        return o.reshape(128, 2, 32, 32)
    Bre, Bim = blockdiag(gl_p(inp['s5_b_re'][ki])), blockdiag(gl_p(inp['s5_b_im'][ki]))
    d['s5_B'] = np.ascontiguousarray(np.stack([Bre, Bim], axis=1).reshape(128, -1))
    cre = np.swapaxes(inp['s5_c_re'][ki], 2, 3)
    cim = np.swapaxes(inp['s5_c_im'][ki], 2, 3)
    Cre, Cim = blockdiag(gl_p(np.ascontiguousarray(cre))), blockdiag(gl_p(np.ascontiguousarray(cim)))
    d['s5_C'] = np.ascontiguousarray(np.stack([Cre, Cim], axis=1).reshape(128, -1))
    d['s5_d'] = np.ascontiguousarray(inp['s5_d'][ki].reshape(32, 32).T)
    d['s5_wglu'] = inp['s5_w_glu']
    d['s5_bglu'] = np.ascontiguousarray(inp['s5_b_glu'][ki].reshape(16, 128).T)
    d['ssd_win'] = inp['ssd_w_in']
    d['ssd_wout'] = inp['ssd_w_out']
    d['ssd_cw'] = np.ascontiguousarray(inp['ssd_conv_w'][ki].reshape(5, 24, 128).transpose(2, 1, 0).reshape(128, 120))
    d['ssd_cb'] = np.ascontiguousarray(inp['ssd_conv_b'][ki].reshape(24, 128).T)
    d['ssd_dtb'] = np.ascontiguousarray(np.broadcast_to(inp['ssd_dt_bias'][ki].reshape(1, 64), (128, 64)))
    d['ssd_alog'] = np.ascontiguousarray(np.broadcast_to(inp['ssd_a_log'][ki].reshape(1, 64), (128, 64)))
    d['ssd_dh'] = np.ascontiguousarray(np.broadcast_to(inp['ssd_d'][ki].reshape(1, 32), (128, 32)))
    d['ssd_ng'] = np.ascontiguousarray(np.broadcast_to(inp['ssd_norm_g'][ki].reshape(1, 2048), (128, 2048)))
    d['ssd_msk'] = ssd_masks()
    cos, sin, pm = rope_consts(TL)
    d['rope_cos'], d['rope_sin'], d['rope_pm'] = cos, sin, pm
    d['moe_wr'] = np.ascontiguousarray(np.concatenate([inp['moe_w_group'], inp['moe_w_router']], axis=-1))
    d['moe_br'] = np.ascontiguousarray(np.concatenate([inp['moe_b_group'], inp['moe_b_router']], axis=-1))
    d['moe_wg'] = inp['moe_w_gate']
    d['moe_wu'] = inp['moe_w_up']
    d['moe_wd'] = inp['moe_w_down']
    return d


FULL_LAYERS = [(0, 'a', 0, True), (1, 's', 0, True), (2, 'd', 0, True), (3, 'a', 1, False)]


def kernel(**inputs):
    inp = {k: np.asarray(v) for k, v in inputs.items()}
    B, TL = inp['x'].shape[0], inp['x'].shape[1]
    prog = Prog(TL, FULL_LAYERS)
    in_maps = []
    for b in range(B):
        d = host_inputs(inp, b, TL, FULL_LAYERS)
        in_maps.append({k: d[k] for k in prog.in_names})
    res = run_bass_kernel_spmd(prog.nc, in_maps, core_ids=list(range(B)))
    return np.stack([r['out'] for r in res.results], axis=0)
```

```python
import numpy as np
from contextlib import ExitStack
import concourse.bass as bass
import concourse.mybir as mybir
from concourse.bass_utils import run_bass_kernel_spmd

F32, BF16 = mybir.dt.float32, mybir.dt.bfloat16
AF = mybir.ActivationFunctionType
ALU = mybir.AluOpType
AX = mybir.AxisListType

D = 1024
KT = 8
TC = 256
EPS = 1e-6
NE = 32
KEEP = ('wgb', 'wub', 'wdb', 'wqkvb', 'wob', 'wglub', 'winb', 'woutb')
HID = 256


class Sched:
    BLK = 8000
    NDMA = 12

    def __init__(self, nc, es):
        self.nc, self.es = nc, es
        self.eng = {'pe': nc.tensor, 'act': nc.scalar, 'dve': nc.vector,
                    'pool': nc.gpsimd, 'sp': nc.sync}
        self.cnt = {e: 0 for e in self.eng}
        self.sems = {e: [] for e in self.eng}
        self.seen = {e: {} for e in self.eng}
        self.lastw, self.readers = {}, {}
        self.dq = {}
        self.nsem = 0

    def _newsem(self, name):
        self.nsem += 1
        return self.es.enter_context(self.nc.semaphore(name))

    def _deps(self, r, w):
        toks = []
        for k in r:
            t = self.lastw.get(k)
            if t:
                toks.append(t)
        for k in w:
            t = self.lastw.get(k)
            if t:
                toks.append(t)
            toks.extend(self.readers.get(k, {}).values())
        return toks

    def _wait(self, e, toks, skip_pe=False):
        need = {}
        for (te, tb, sem, val) in toks:
            if skip_pe and te == 'pe':
                continue
            cur = need.get(te)
            if cur is None or (tb, val) > (cur[0], cur[1]):
                need[te] = (tb, val, sem)
        for te, (tb, val, sem) in need.items():
            s = self.seen[e].get(te)
            if s is not None and s >= (tb, val):
                continue
            self.eng[e].wait_ge(sem, val)
            self.seen[e][te] = (tb, val)

    def _reg(self, tok, r, w):
        for k in r:
            self.readers.setdefault(k, {})[tok[0]] = tok
        for k in w:
            self.lastw[k] = tok
            self.readers[k] = {}

    def op(self, e, fn, r=(), w=()):
        self._wait(e, self._deps(r, w), skip_pe=(e == 'pe'))
        ins = fn()
        k = self.cnt[e]
        b = k // self.BLK
        while len(self.sems[e]) <= b:
            self.sems[e].append(self._newsem(f"s_{e}_{len(self.sems[e])}"))
        sem, val = self.sems[e][b], k % self.BLK + 1
        ins.then_inc(sem, 1)
        self.cnt[e] += 1
        self._reg((e, b, sem, val), r, w)

    def dma(self, out, in_, r=(), w=(), q='sp', grp='m', **kw):
        key = (q, grp)
        if key not in self.dq:
            self.dq[key] = {'rr': 0, 'sems': [[self._newsem(f"d_{q}_{grp}_{i}"), 0]
                                             for i in range(self.NDMA)]}
        st = self.dq[key]
        i = st['rr']
        st['rr'] = (i + 1) % self.NDMA
        sem, n = st['sems'][i]
        te = ('dma', q, grp, i)
        toks = self._deps(r, w)
        if n > 0:
            toks.append((te, 0, sem, 16 * n))
        self._wait(q, toks)
        ins = self.eng[q].dma_start(out=out, in_=in_, **kw)
        ins.then_inc(sem, 16)
        st['sems'][i][1] = n + 1
        self._reg((te, 0, sem, 16 * (n + 1)), r, w)

    def barrier(self, keep=()):
        toks = []
        for e in ('pe', 'act', 'dve', 'pool'):
            k = self.cnt[e]
            if k > 0:
                b = (k - 1) // self.BLK
                toks.append((e, b, self.sems[e][b], (k - 1) % self.BLK + 1))
        for (q, grp), st in self.dq.items():
            if grp == 'async':
                continue
            for i, (sem, n) in enumerate(st['sems']):
                if n > 0:
                    toks.append((('dma', q, grp, i), 0, sem, 16 * n))
        for e in self.eng:
            self._wait(e, toks)
        lw = {k: v for k, v in self.lastw.items() if k[0] in keep}
        self.lastw, self.readers = lw, {}

    def finish(self):
        toks = []
        for (q, grp), st in self.dq.items():
            for i, (sem, n) in enumerate(st['sems']):
                if n > 0:
                    toks.append((('dma', q, grp, i), 0, sem, 16 * n))
        self._wait('sp', toks)


class Prog:
    def __init__(self, TL, layers, n_layers_w=4, dbg=None):
        self.TL, self.T = TL, TC + TL
        self.layers = layers
        self.dbg = dbg
        self.spans = [(0, TC, True)] + [(TC + i * 512, 512, False) for i in range(TL // 512)]
        self.nc = bass.Bass("TRN2", target_bir_lowering=False)
        self.build()

    def dram_in(self, name, shape, dt=F32):
        self.in_names.append(name)
        return self.nc.dram_tensor(name, list(shape), dt, kind="ExternalInput").ap()

    def dram_scr(self, name, shape, dt=F32):
        return self.nc.dram_tensor(name, list(shape), dt, kind="Internal").ap()

    def sb(self, es, name, shape, dt=F32):
        self._nsb = getattr(self, '_nsb', 0) + 1
        return es.enter_context(self.nc.sbuf_tensor(f"sb{self._nsb}_{name}", list(shape), dt))

    def mm(self, out, lhsT, rhs, start, stop, r, w):
        nc = self.nc
        self.S.op('pe', lambda: nc.tensor.matmul(out, lhsT=lhsT, rhs=rhs, start=start, stop=stop), r=r, w=w)

    def act(self, out, in_, func, r, w, **kw):
        nc = self.nc
        self.S.op('act', lambda: nc.scalar.activation(out=out, in_=in_, func=func, **kw), r=r, w=w)

    def tt(self, out, in0, in1, op, r, w, e='dve'):
        eng = self.S.eng[e]
        self.S.op(e, lambda: eng.tensor_tensor(out=out, in0=in0, in1=in1, op=op), r=r, w=w)

    def ts(self, out, in0, s1, op0, r, w, s2=None, op1=None, e='dve', **kw):
        eng = self.S.eng[e]
        if op1 is None:
            self.S.op(e, lambda: eng.tensor_scalar(out=out, in0=in0, scalar1=s1, scalar2=None, op0=op0, **kw), r=r, w=w)
        else:
            self.S.op(e, lambda: eng.tensor_scalar(out=out, in0=in0, scalar1=s1, scalar2=s2, op0=op0, op1=op1, **kw), r=r, w=w)

    def stt(self, out, in0, scalar, in1, op0, op1, r, w):
        nc = self.nc
        self.S.op('dve', lambda: nc.vector.scalar_tensor_tensor(out=out, in0=in0, scalar=scalar, in1=in1, op0=op0, op1=op1), r=r, w=w)

    def cp(self, out, in_, r, w, e='dve'):
        eng = self.S.eng[e]
        if e == 'act':
            self.S.op(e, lambda: eng.copy(out=out, in_=in_), r=r, w=w)
        else:
            self.S.op(e, lambda: eng.tensor_copy(out=out, in_=in_), r=r, w=w)

    def build(self):
        nc = self.nc
        self.in_names = []
        TL, T = self.TL, self.T
        I = self.I = {}
        I['x'] = self.dram_in('x', [TL, D])
        I['ctx'] = self.dram_in('ctx', [TC, D])
        I['cc'] = self.dram_in('cc', [2, D])
        I['ident'] = self.dram_in('ident', [128, 128])
        I['mod_w'] = self.dram_in('mod_w', [4, D, 6 * D])
        I['mod_b'] = self.dram_in('mod_b', [4, 6 * D])
        I['ng'] = self.dram_in('ng', [128, 2 * 4 * KT])
        I['moe_wr'] = self.dram_in('moe_wr', [4, D, 36])
        I['moe_br'] = self.dram_in('moe_br', [4, 36])
        I['moe_wg'] = self.dram_in('moe_wg', [4, NE, D, HID])
        I['moe_wu'] = self.dram_in('moe_wu', [4, NE, D, HID])
        I['moe_wd'] = self.dram_in('moe_wd', [4, NE, HID, D])
        I['attn_wqkv'] = self.dram_in('attn_wqkv', [2, D, 1536])
        I['attn_wo'] = self.dram_in('attn_wo', [2, D, D])
        I['attn_g'] = self.dram_in('attn_g', [128, 4])
        I['rope_cos'] = self.dram_in('rope_cos', [128, T])
        I['rope_sin'] = self.dram_in('rope_sin', [128, T])
        I['rope_pm'] = self.dram_in('rope_pm', [128, 128])
        self.wqkvb = self.dram_scr('wqkvb', [2, D, 1536], BF16)
        self.wob = self.dram_scr('wob', [2, D, D], BF16)
        self.QT = self.dram_scr('QT', [D, T], BF16)
        self.KT_ = self.dram_scr('KTs', [256, T], BF16)
        self.Vs = self.dram_scr('Vs', [T, 256], BF16)
        self.OT = self.dram_scr('OT', [D, T], BF16)
        I['s5_par'] = self.dram_in('s5_par', [128, 3 * 64])
        I['s5_B'] = self.dram_in('s5_B', [128, 2 * 64 * 32])
        I['s5_C'] = self.dram_in('s5_C', [128, 2 * 64 * 32])
        I['s5_d'] = self.dram_in('s5_d', [32, 32])
        I['s5_wglu'] = self.dram_in('s5_wglu', [1, D, 2 * D])
        I['s5_bglu'] = self.dram_in('s5_bglu', [128, 16])
        self.wglub = self.dram_scr('wglub', [1, D, 2 * D], BF16)
        self.UT = self.dram_scr('UT', [D, T], BF16)
        self.YF = self.dram_scr('YF', [D, T])
        self.YB = self.dram_scr('YB', [D, T])
        I['ssd_win'] = self.dram_in('ssd_win', [1, D, 5184])
        I['ssd_wout'] = self.dram_in('ssd_wout', [1, 2 * D, D])
        I['ssd_cw'] = self.dram_in('ssd_cw', [128, 24 * 5])
        I['ssd_cb'] = self.dram_in('ssd_cb', [128, 24])
        I['ssd_dtb'] = self.dram_in('ssd_dtb', [128, 64])
        I['ssd_alog'] = self.dram_in('ssd_alog', [128, 64])
        I['ssd_dh'] = self.dram_in('ssd_dh', [128, 32])
        I['ssd_ng'] = self.dram_in('ssd_ng', [128, 2 * D])
        I['ssd_msk'] = self.dram_in('ssd_msk', [128, 4 * 128])
        self.winb = self.dram_scr('winb', [1, D, 5184], BF16)
        self.woutb = self.dram_scr('woutb', [1, 2 * D, D], BF16)
        self.ZS = self.dram_scr('ZS', [T, 2 * D], BF16)
        self.XBC = self.dram_scr('XBC', [3072, T], BF16)
        self.DTs = self.dram_scr('DTs', [T, 64])
        self.XTK = self.dram_scr('XTK', [T, 2 * D], BF16)
        self.BTK = self.dram_scr('BTK', [T, 512], BF16)
        self.BCf = self.dram_scr('BCf', [1024, T], BF16)
        self.YD = self.dram_scr('YD', [2, T, 2 * D])
        self.out = nc.dram_tensor('out', [TL, D], F32, kind="ExternalOutput").ap()
        self.hT = self.dram_scr('hT', [D, T])
        self.wgb = self.dram_scr('wgb', [4, NE, D, HID], BF16)
        self.wub = self.dram_scr('wub', [4, NE, D, HID], BF16)
        self.wdb = self.dram_scr('wdb', [4, NE, HID, D], BF16)
        if self.dbg:
            self.dbg_out = nc.dram_tensor('dbg', [D, T], F32, kind="ExternalOutput").ap()

        with ExitStack() as es:
            self.S = S = Sched(nc, es)
            self.ident = self.sb(es, 'ident', [128, 128])
            self.identb = self.sb(es, 'identb', [128, 128], BF16)
            self.ones32 = self.sb(es, 'ones32', [128, 128])
            self.onesb = self.sb(es, 'onesb', [128, 128], BF16)
            self.mv = self.sb(es, 'mv', [128, 4, 96])
            self.ng = self.sb(es, 'ng', [128, 2, 4, KT])
            self.ps = [es.enter_context(nc.psum_tensor(f'ps{i}', [128, 512], F32)) for i in range(8)]
            S.dma(self.ident[:], I['ident'], w=['ident'])
            S.dma(self.ng[:].rearrange("p n l k -> p (n l k)"), I['ng'], w=['ng'])
            self.cp(self.identb[:], self.ident[:], r=['ident'], w=['identb'])
            S.op('dve', lambda: nc.vector.memset(self.ones32[:], 1.0), w=['ones32'])
            S.op('dve', lambda: nc.vector.memset(self.onesb[:], 1.0), w=['onesb'])
            S.barrier()
            self.async_casts(self.layers[0])
            self.phase_mod()
            self.phase_tin()
            for n_, L in enumerate(self.layers):
                if n_ + 1 < len(self.layers):
                    self.async_casts(self.layers[n_ + 1])
                self.layer(*L)
            self.phase_tout()
            S.finish()

    def cast_dram(self, dst, src, key):
        R_, C_ = src.shape
        a = R_ // 128
        sv = src.rearrange("(p a) n -> p a n", p=128)
        dv = dst.rearrange("(p a) n -> p a n", p=128)
        step = max(1, 2048 // C_)
        for a0 in range(0, a, step):
            a1 = min(a, a0 + step)
            self.S.dma(dv[:, a0:a1, :], sv[:, a0:a1, :], w=[key], q='pool', grp='async')

    def async_casts(self, L):
        I = self.I
        li, kind, ki, nctx = L
        if kind == 's':
            self.cast_dram(self.wglub[ki], I['s5_wglu'][ki], ('wglub', ki))
        if kind == 'd':
            self.cast_dram(self.winb[ki], I['ssd_win'][ki], ('winb', ki))
            self.cast_dram(self.woutb[ki], I['ssd_wout'][ki], ('woutb', ki))
        if kind == 'a':
            self.cast_dram(self.wqkvb[ki], I['attn_wqkv'][ki], ('wqkvb', ki))
            self.cast_dram(self.wob[ki], I['attn_wo'][ki], ('wob', ki))
        for e in range(NE):
            self.cast_dram(self.wgb[li, e], I['moe_wg'][li, e], ('wgb', li, e))
            self.cast_dram(self.wub[li, e], I['moe_wu'][li, e], ('wub', li, e))
            self.cast_dram(self.wdb[li, e], I['moe_wd'][li, e], ('wdb', li, e))

    def phase_mod(self):
        nc, S, I = self.nc, self.S, self.I
        with ExitStack() as es:
            ccT = self.sb(es, 'ccT', [128, KT, 2])
            scT = self.sb(es, 'scT', [128, KT, 2])
            wch = [self.sb(es, f'modw{i}', [128, KT, 512]) for i in range(2)]
            brow = self.sb(es, 'modb', [1, 6 * D])
            with nc.allow_non_contiguous_dma(reason="tiny transposed load of c"):
                for r_ in range(2):
                    S.dma(ccT[:, :, r_], I['cc'][r_].rearrange("(k p) -> p k", p=128), w=['ccT'])
            self.act(scT[:], ccT[:], AF.Silu, r=['ccT'], w=['scT'])
            ci = 0
            for (li, kind, ki, nctx) in self.layers:
                S.dma(brow[:], I['mod_b'][li:li + 1, :], w=['modb'])
                pb = self.ps[li % 2]
                for j in range(12):
                    wt = wch[ci % 2]
                    S.dma(wt[:], I['mod_w'][li][:, j * 512:(j + 1) * 512].rearrange("(k p) n -> p k n", p=128),
                          w=[('modw', ci % 2)])
                    for b4 in range(4):
                        blk = j * 4 + b4
                        o = pb[:, blk * 2:blk * 2 + 2]
                        for k in range(KT):
                            self.mm(o, wt[:, k, b4 * 128:(b4 + 1) * 128], scT[:, k, :], k == 0, False,
                                    r=[('modw', ci % 2), 'scT'], w=[('psm', li % 2)])
                        self.mm(o, brow[0:1, blk * 128:(blk + 1) * 128], self.ones32[0:1, 0:2], False, True,
                                r=['modb', 'ones32'], w=[('psm', li % 2)])
                    ci += 1
                self.cp(self.mv[:, li, :], pb[:, 0:96], r=[('psm', li % 2)], w=[('mv', li)])
        S.barrier(keep=KEEP)

    def hT_span(self, t0, w):
        return self.hT.rearrange("(k p) t -> p k t", p=128)[:, :, t0:t0 + w]

    def phase_tin(self):
        nc, S, I = self.nc, self.S, self.I
        with ExitStack() as es:
            xt = [self.sb(es, f'tin_x{i}', [128, D]) for i in range(2)]
            stg = [self.sb(es, f'tin_s{i}', [128, KT, 512]) for i in range(2)]
            ti = 0
            for si, (t0, w, isc) in enumerate(self.spans):
                st = stg[si % 2]
                for tt_ in range(w // 128):
                    tok = t0 + tt_ * 128
                    src = I['ctx'][tok:tok + 128, :] if isc else I['x'][tok - TC:tok - TC + 128, :]
                    xs = xt[ti % 2]
                    S.dma(xs[:], src, w=[('tinx', ti % 2)])
                    for half in range(2):
                        pb = self.ps[(ti * 2 + half) % 4]
                        for q in range(4):
                            k = half * 4 + q
                            S.op('pe', lambda: nc.tensor.transpose(out=pb[:, q * 128:(q + 1) * 128],
                                                                   in_=xs[:, k * 128:(k + 1) * 128], identity=self.ident[:]),
                                 r=[('tinx', ti % 2)], w=[('pst', (ti * 2 + half) % 4)])
                        self.cp(st[:, half * 4:half * 4 + 4, tt_ * 128:(tt_ + 1) * 128],
                                pb[:].rearrange("p (q t) -> p q t", q=4),
                                r=[('pst', (ti * 2 + half) % 4)], w=[('tins', si % 2)],
                                e='dve' if half == 0 else 'act')
                    ti += 1
                S.dma(self.hT_span(t0, w), st[:, :, :w], r=[('tins', si % 2)], w=[('hT', si)])
        S.barrier(keep=KEEP)

    def phase_tout(self):
        nc, S = self.nc, self.S
        if self.dbg:
            with ExitStack() as es:
                t_ = [self.sb(es, f'dbg{i}', [128, KT, 512]) for i in range(2)]
                for si, (t0, w, isc) in enumerate(self.spans):
                    S.dma(t_[si % 2][:, :, :w], self.hT_span(t0, w), w=[('dbgt', si % 2)])
                    S.dma(self.dbg_out.rearrange("(k p) t -> p k t", p=128)[:, :, t0:t0 + w], t_[si % 2][:, :, :w],
                          r=[('dbgt', si % 2)], w=[('dbgo', si)])
            S.barrier()
        with ExitStack() as es:
            hs = [self.sb(es, f'to_h{i}', [128, KT, 512]) for i in range(2)]
            ot = [self.sb(es, f'to_o{i}', [128, D]) for i in range(2)]
            ti = 0
            for si, (t0, w, isc) in enumerate(self.spans):
                if isc:
                    continue
                h = hs[si % 2]
                S.dma(h[:, :, :w], self.hT_span(t0, w), w=[('toh', si % 2)])
                for tt_ in range(w // 128):
                    o = ot[ti % 2]
                    for half in range(2):
                        pb = self.ps[(ti * 2 + half) % 4]
                        for q in range(4):
                            k = half * 4 + q
                            S.op('pe', lambda: nc.tensor.transpose(out=pb[:, q * 128:(q + 1) * 128],
                                                                   in_=h[:, k, tt_ * 128:(tt_ + 1) * 128], identity=self.ident[:]),
                                 r=[('toh', si % 2)], w=[('pst', (ti * 2 + half) % 4)])
                        self.cp(o[:, half * 512:(half + 1) * 512], pb[:],
                                r=[('pst', (ti * 2 + half) % 4)], w=[('too', ti % 2)],
                                e='dve' if half == 0 else 'act')
                    tok = t0 - TC + tt_ * 128
                    S.dma(self.out[tok:tok + 128, :], o[:], r=[('too', ti % 2)], w=[('out', ti)])
                    ti += 1

    def layer_scalars(self, es, li):
        S = self.S
        ab = self.sb(es, f'ab{li}', [128, 2, 6, KT])
        mvv = self.mv[:, li, :].rearrange("p (j k r) -> p r j k", j=6, k=KT, r=2)
        key = ('ab', li)
        for r_ in range(2):
            self.stt(ab[:, r_, 0, :], mvv[:, r_, 1, :], 1.0, self.ng[:, 0, li, :], ALU.add, ALU.mult, r=[('mv', li), 'ng'], w=[key])
            self.cp(ab[:, r_, 1, :], mvv[:, r_, 0, :], r=[('mv', li)], w=[key])
            self.cp(ab[:, r_, 2, :], mvv[:, r_, 2, :], r=[('mv', li)], w=[key])
            self.stt(ab[:, r_, 3, :], mvv[:, r_, 4, :], 1.0, self.ng[:, 1, li, :], ALU.add, ALU.mult, r=[('mv', li), 'ng'], w=[key])
            self.cp(ab[:, r_, 4, :], mvv[:, r_, 3, :], r=[('mv', li)], w=[key])
            self.cp(ab[:, r_, 5, :], mvv[:, r_, 5, :], r=[('mv', li)], w=[key])
        return ab

    def norm_span(self, hs, hkey, w, A, B, abkey, u32, u32key, ub, ubkey, scr):
        nc, S = self.nc, self.S
        sq, rs = scr['sq'], scr['rs']
        pss = self.ps[7]
        self.act(sq[:, :, :w], hs[:, :, :w], AF.Square, r=[hkey], w=['sq'])
        for k in range(KT):
            self.mm(pss[:, :w], self.onesb[:], sq[:, k, :w], k == 0, k == KT - 1, r=['sq', 'onesb'], w=[('ps', 7)])
        self.act(rs[:, :w], pss[:, :w], AF.Sqrt, r=[('ps', 7), 'epsb'], w=['rs'], scale=1.0 / D, bias=self.epsb[:, 0:1])
        S.op('dve', lambda: nc.vector.reciprocal(out=rs[:, :w], in_=rs[:, :w]), r=['rs'], w=['rs'])
        xn = scr['xn']
        self.tt(xn[:, :, :w], hs[:, :, :w], rs[:, :w].unsqueeze(1).to_broadcast([128, KT, w]), ALU.mult, r=[hkey, 'rs'],
                w=[('acc', k) for k in range(KT)])
        for k in range(KT):
            dst = u32[:, k, :w] if u32 is not None else ub[:, k, :w]
            dkey = u32key if u32 is not None else ubkey
            self.act(dst, xn[:, k, :w], AF.Identity, r=[('acc', k), abkey], w=[dkey], scale=A[:, k:k + 1], bias=B[:, k:k + 1])
        if u32 is not None:
            self.cp(ub[:, :, :w], u32[:, :, :w], r=[u32key], w=[ubkey], e='pool')

    def moe_span(self, li, hs, hkey, w, u32, ub, G5, abkey, M):
        nc, S = self.nc, self.S
        ps = self.ps
        ntt = w // 128
        cw, lgs, sm = M['cw'], M['lgs'], M['sm']
        for tt_ in range(ntt):
            pl = ps[6]
            for k in range(KT):
                self.mm(pl[:, 0:36], u32[:, k, tt_ * 128:(tt_ + 1) * 128], M['wr'][:, k, :], k == 0, False,
                        r=['u32', 'wr'], w=[('ps', 6)])
            self.mm(pl[:, 0:36], self.ones32[0:1, :], M['br'][0:1, :], False, True, r=['ones32', 'br'], w=[('ps', 6)])
            L = lgs
            self.cp(L[:, 0:36], pl[:, 0:36], r=[('ps', 6)], w=['lgs'])
            rk, wk = ['lgs', 'sm'], ['sm']
            S.op('dve', lambda: nc.vector.tensor_reduce(out=sm[:, 0:1], in_=L[:, 0:4], op=ALU.max, axis=AX.X), r=rk, w=wk)
            self.ts(sm[:, 1:2], sm[:, 0:1], -1.0, ALU.mult, r=rk, w=wk)
            self.ts(L[:, 36:40], L[:, 0:4], sm[:, 0:1], ALU.is_equal, r=rk, w=['lgs'])
            self.act(L[:, 40:44], L[:, 0:4], AF.Exp, r=rk, w=['lgs', 'sm'], bias=sm[:, 1:2], scale=1.0, accum_out=sm[:, 2:3])
            S.op('dve', lambda: nc.vector.reciprocal(out=sm[:, 3:4], in_=sm[:, 2:3]), r=rk, w=wk)
            self.ts(L[:, 44:52], L[:, 4:12], L[:, 36:37], ALU.mult, r=rk, w=['lgs'])
            for g in range(1, 4):
                self.stt(L[:, 44:52], L[:, 4 + 8 * g:12 + 8 * g], L[:, 36 + g:37 + g], L[:, 44:52], ALU.mult, ALU.add, r=rk, w=['lgs'])
            S.op('dve', lambda: nc.vector.tensor_reduce(out=sm[:, 4:5], in_=L[:, 44:52], op=ALU.max, axis=AX.X), r=rk, w=wk)
            self.ts(L[:, 52:60], L[:, 44:52], sm[:, 4:5], ALU.is_equal, r=rk, w=['lgs'])
            self.stt(L[:, 60:68], L[:, 52:60], -1e30, L[:, 44:52], ALU.mult, ALU.add, r=rk, w=['lgs'])
            S.op('dve', lambda: nc.vector.tensor_reduce(out=sm[:, 5:6], in_=L[:, 60:68], op=ALU.max, axis=AX.X), r=rk, w=wk)
            self.ts(L[:, 68:76], L[:, 60:68], sm[:, 5:6], ALU.is_equal, r=rk, w=['lgs'])
            self.tt(sm[:, 6:7], sm[:, 5:6], sm[:, 4:5], ALU.subtract, r=rk, w=wk)
            self.act(sm[:, 7:8], sm[:, 6:7], AF.Exp, r=rk, w=wk)
            self.ts(sm[:, 8:9], sm[:, 7:8], 1.0, ALU.add, r=rk, w=wk)
            S.op('dve', lambda: nc.vector.reciprocal(out=sm[:, 8:9], in_=sm[:, 8:9]), r=rk, w=wk)
            self.tt(sm[:, 9:10], sm[:, 7:8], sm[:, 8:9], ALU.mult, r=rk, w=wk)
            self.tt(sm[:, 10:11], sm[:, 8:9], sm[:, 3:4], ALU.mult, r=rk, w=wk)
            self.tt(sm[:, 11:12], sm[:, 9:10], sm[:, 3:4], ALU.mult, r=rk, w=wk)
            self.ts(L[:, 76:84], L[:, 52:60], sm[:, 10:11], ALU.mult, r=rk, w=['lgs'])
            self.stt(L[:, 76:84], L[:, 68:76], sm[:, 11:12], L[:, 76:84], ALU.mult, ALU.add, r=rk, w=['lgs'])
            for g in range(4):
                self.ts(cw[:, tt_, g * 8:(g + 1) * 8], L[:, 76:84], L[:, 36 + g:37 + g], ALU.mult, r=rk, w=['cw'])
        wgs, wus, wds = M['wg'], M['wu'], M['wd']
        acc = M['acc']

        def load_w(e):
            sl = e % 3
            S.dma(wgs[sl][:], self.wgb[li, e].rearrange("(k p) n -> p k n", p=128), r=[('wgb', li, e)], w=[('wg', sl)])
            S.dma(wus[sl][:], self.wub[li, e].rearrange("(k p) n -> p k n", p=128), r=[('wub', li, e)], w=[('wu', sl)])
            S.dma(wds[sl][:], self.wdb[li, e].rearrange("(k p) n -> p k n", p=128), r=[('wdb', li, e)], w=[('wd', sl)])
        def down(e):
            sl = e % 3
            for ct in range(KT):
                pd = ps[6 + ct % 2]
                for j in range(2):
                    self.mm(pd[:, :w], wds[sl][:, j, ct * 128:(ct + 1) * 128], M['hid'][sl][:, j, :w], j == 0, j == 1,
                            r=[('wd', sl), ('hid', sl, j)], w=[('ps', 6 + ct % 2)])
                if e == 0:
                    self.cp(acc[:, ct, :w], pd[:, :w], r=[('ps', 6 + ct % 2)], w=[('acc', ct)])
                else:
                    self.tt(acc[:, ct, :w], acc[:, ct, :w], pd[:, :w], ALU.add, r=[('ps', 6 + ct % 2), ('acc', ct)], w=[('acc', ct)])
        load_w(0)
        for e in range(NE):
            sl = e % 3
            if e + 1 < NE:
                load_w(e + 1)
            pc = ps[4 + e % 2]
            for tt_ in range(ntt):
                self.mm(pc[:, tt_ * 128:(tt_ + 1) * 128], cw[:, tt_, e:e + 1].to_broadcast([128, 128]), self.ident[:], True, True,
                        r=['cw', 'ident'], w=[('ps', 4 + e % 2)])
            for j in range(2):
                b0 = 2 * ((e * 2 + j) % 2)
                for (wt, wkey, bank) in ((wgs[sl], ('wg', sl), b0), (wus[sl], ('wu', sl), b0 + 1)):
                    for k in range(KT):
                        self.mm(ps[bank][:, :w], wt[:, k, j * 128:(j + 1) * 128], ub[:, k, :w], k == 0, k == KT - 1,
                                r=[wkey, 'ub'], w=[('ps', bank)])
                sg, t2 = M['sg'][j], M['t2'][j]
                hid = M['hid'][sl]
                self.act(sg[:, :w], ps[b0][:, :w], AF.Silu, r=[('ps', b0)], w=[('sg', j)])
                self.tt(t2[:, :w], sg[:, :w], ps[b0 + 1][:, :w], ALU.mult, r=[('sg', j), ('ps', b0 + 1)], w=[('t2', j)])
                self.tt(hid[:, j, :w], t2[:, :w], pc[:, :w], ALU.mult, r=[('t2', j), ('ps', 4 + e % 2)], w=[('hid', sl, j)])
            if e > 0:
                down(e - 1)
        down(NE - 1)
        for ct in range(KT):
            self.stt(hs[:, ct, :w], acc[:, ct, :w], G5[:, ct:ct + 1], hs[:, ct, :w], ALU.mult, ALU.add,
                     r=[('acc', ct), hkey, abkey], w=[hkey])

    def moe_alloc(self, es, li):
        S, I = self.S, self.I
        M = {}
        M['wr'] = self.sb(es, 'moe_wr', [128, KT, 36])
        M['br'] = self.sb(es, 'moe_br', [1, 36])
        M['cw'] = self.sb(es, 'moe_cw', [128, 4, NE])
        M['lgs'] = self.sb(es, 'moe_lgs', [128, 96])
        M['sm'] = self.sb(es, 'moe_sm', [128, 16])
        M['wg'] = [self.sb(es, f'moe_wg{i}', [128, KT, HID], BF16) for i in range(3)]
        M['wu'] = [self.sb(es, f'moe_wu{i}', [128, KT, HID], BF16) for i in range(3)]
        M['wd'] = [self.sb(es, f'moe_wd{i}', [128, 2, D], BF16) for i in range(3)]
        M['sg'] = [self.sb(es, f'moe_sg{i}', [128, 512]) for i in range(2)]
        M['t2'] = [self.sb(es, f'moe_t2{i}', [128, 512]) for i in range(2)]
        M['hid'] = [self.sb(es, f'moe_hid{i}', [128, 2, 512], BF16) for i in range(3)]
        M['acc'] = self.sb(es, 'moe_acc', [128, KT, 512])
        S.dma(M['wr'][:], I['moe_wr'][li].rearrange("(k p) n -> p k n", p=128), w=['wr'])
        S.dma(M['br'][:], I['moe_br'][li:li + 1, :], w=['br'])
        return M

    def norm_alloc(self, es):
        scr = {'sq': self.sb(es, 'n_sq', [128, KT, 512], BF16), 'rs': self.sb(es, 'n_rs', [128, 512])}
        self.epsb = self.sb(es, 'epsb', [128, 1])
        nc = self.nc
        self.S.op('dve', lambda: nc.vector.memset(self.epsb[:], EPS), w=['epsb'])
        return scr

    def attn_a1(self, li, ki, ab, abkey):
        nc, S, I = self.nc, self.S, self.I
        ps = self.ps
        with ExitStack() as es:
            scr = self.norm_alloc(es)
            scr['xn'] = self.sb(es, 'a1_xn', [128, KT, 512])
            wq = self.sb(es, 'a1_wqkv', [128, KT, 1536], BF16)
            pmb = self.sb(es, 'a1_pmb', [128, 128], BF16)
            pm32 = self.sb(es, 'a1_pm32', [128, 128])
            gq = self.sb(es, 'a1_g', [128, 4])
            hsb = [self.sb(es, f'a1_hs{i}', [128, KT, 512]) for i in range(2)]
            ub = self.sb(es, 'a1_ub', [128, KT, 512], BF16)
            cs = [self.sb(es, f'a1_cos{i}', [128, 512]) for i in range(2)]
            sn = [self.sb(es, f'a1_sin{i}', [128, 512]) for i in range(2)]
            sq = [self.sb(es, f'a1_sq{i}', [128, 512], BF16) for i in range(2)]
            rsq = [self.sb(es, f'a1_rs{i}', [128, 512]) for i in range(2)]
            yb = [self.sb(es, f'a1_yb{i}', [128, 512], BF16) for i in range(2)]
            t1 = [self.sb(es, f'a1_t1{i}', [128, 512]) for i in range(2)]
            t2 = [self.sb(es, f'a1_t2{i}', [128, 512]) for i in range(2)]
            qst = [self.sb(es, f'a1_qst{i}', [128, 10, 512], BF16) for i in range(2)]
            vst = [self.sb(es, f'a1_vst{i}', [128, 4, 256], BF16) for i in range(2)]
            S.dma(wq[:], self.wqkvb[ki].rearrange("(k p) n -> p k n", p=128), r=[('wqkvb', ki)], w=['wq'])
            S.dma(pm32[:], I['rope_pm'], w=['pm32'])
            S.dma(gq[:], I['attn_g'], w=['gq'])
            self.cp(pmb[:], pm32[:], r=['pm32'], w=['pmb'])
            self.ts(gq[:, 2 * ki:2 * ki + 1], gq[:, 2 * ki:2 * ki + 1], 128.0 ** -0.5, ALU.mult, r=['gq'], w=['gq'])
            for si, (t0, w, isc) in enumerate(self.spans):
                hs, hkey = hsb[si % 2], ('hs', si % 2)
                r_ = 1 if isc else 0
                S.dma(hs[:, :, :w], self.hT_span(t0, w), r=[('hT', si)], w=[hkey])
                S.dma(cs[si % 2][:, :w], I['rope_cos'][:, t0:t0 + w], w=[('cos', si % 2)])
                S.dma(sn[si % 2][:, :w], I['rope_sin'][:, t0:t0 + w], w=[('sin', si % 2)])
                self.norm_span(hs, hkey, w, ab[:, r_, 0, :], ab[:, r_, 1, :], abkey, None, None, ub, 'ub', scr)
                qs_ = qst[si % 2]
                for c in range(10):
                    b = c % 2
                    pq, pss_, pr = ps[b], ps[2 + b], ps[4 + b]
                    for k in range(KT):
                        self.mm(pq[:, :w], wq[:, k, c * 128:(c + 1) * 128], ub[:, k, :w], k == 0, k == KT - 1,
                                r=['wq', 'ub'], w=[('ps', b)])
                    self.act(sq[b][:, :w], pq[:, :w], AF.Square, r=[('ps', b)], w=[('sq2', b)])
                    self.mm(pss_[:, :w], self.onesb[:], sq[b][:, :w], True, True, r=[('sq2', b), 'onesb'], w=[('ps', 2 + b)])
                    self.act(rsq[b][:, :w], pss_[:, :w], AF.Sqrt, r=[('ps', 2 + b), 'epsb'], w=[('rsq', b)],
                             scale=1.0 / 128, bias=self.epsb[:, 0:1])
                    S.op('dve', lambda: nc.vector.reciprocal(out=rsq[b][:, :w], in_=rsq[b][:, :w]), r=[('rsq', b)], w=[('rsq', b)])
                    gcol = 2 * ki + (0 if c < 8 else 1)
                    self.stt(yb[b][:, :w], pq[:, :w], gq[:, gcol:gcol + 1], rsq[b][:, :w], ALU.mult, ALU.mult,
                             r=[('ps', b), 'gq', ('rsq', b)], w=[('yb', b)])
                    self.mm(pr[:, :w], pmb[:], yb[b][:, :w], True, True, r=['pmb', ('yb', b)], w=[('ps', 4 + b)])
                    self.tt(t1[b][:, :w], yb[b][:, :w], cs[si % 2][:, :w], ALU.mult, r=[('yb', b), ('cos', si % 2)], w=[('t1', b)])
                    self.tt(t2[b][:, :w], pr[:, :w], sn[si % 2][:, :w], ALU.mult, r=[('ps', 4 + b), ('sin', si % 2)], w=[('t2', b)])
                    self.tt(qs_[:, c, :w], t1[b][:, :w], t2[b][:, :w], ALU.add, r=[('t1', b), ('t2', b)], w=[('qst', si % 2)])
                S.dma(self.QT.rearrange("(h p) t -> p h t", p=128)[:, :, t0:t0 + w], qs_[:, 0:8, :w], r=[('qst', si % 2)], w=[('QT', si)])
                S.dma(self.KT_.rearrange("(h p) t -> p h t", p=128)[:, :, t0:t0 + w], qs_[:, 8:10, :w], r=[('qst', si % 2)], w=[('KT', si)])
                vs_ = vst[si % 2]
                for tt_ in range(w // 128):
                    for k in range(KT):
                        self.mm(ps[6][:, 0:256], ub[:, k, tt_ * 128:(tt_ + 1) * 128], wq[:, k, 1280:1536], k == 0, k == KT - 1,
                                r=['wq', 'ub'], w=[('ps', 6)])
                    self.cp(vs_[:, tt_, :], ps[6][:, 0:256], r=[('ps', 6)], w=[('vst', si % 2)], e='act')
                S.dma(self.Vs[t0:t0 + w, :].rearrange("(a p) n -> p a n", p=128), vs_[:, :w // 128, :], r=[('vst', si % 2)], w=[('Vs', si)])
        S.barrier(keep=KEEP)

    def attn_a2(self, need_ctx):
        nc, S = self.nc, self.S
        ps = self.ps
        T = self.T
        NB = T // 128
        with ExitStack() as es:
            kT = self.sb(es, 'a2_kT', [128, 2, T], BF16)
            vt = self.sb(es, 'a2_v', [128, NB, 256], BF16)
            qsb = [self.sb(es, f'a2_q{i}', [128, 512], BF16) for i in range(2)]
            pt = [self.sb(es, f'a2_p{i}', [128, 512], BF16) for i in range(4)]
            pt2 = [self.sb(es, f'a2_pp{i}', [128, 512], BF16) for i in range(2)]
            rl = [self.sb(es, f'a2_rl{i}', [128, 512]) for i in range(2)]
            ost = [self.sb(es, f'a2_o{i}', [128, 512], BF16) for i in range(2)]
            S.dma(kT[:], self.KT_.rearrange("(h p) t -> p h t", p=128), w=['kT'])
            for a0 in range(0, NB, 8):
                a1 = min(NB, a0 + 8)
                S.dma(vt[:, a0:a1, :], self.Vs[a0 * 128:a1 * 128, :].rearrange("(a p) n -> p a n", p=128), w=['vt'])
            it = 0
            ie = 0
            for g in range(2):
                for hq in range(4):
                    h = g * 4 + hq
                    for si, (t0, w, isc) in enumerate(self.spans):
                        if isc and not need_ctx:
                            continue
                        nkb = 2 if isc else NB
                        q_, qk = qsb[it % 2], ('q', it % 2)
                        S.dma(q_[:, :w], self.QT[h * 128:(h + 1) * 128, t0:t0 + w], w=[qk])
                        po, pl = ps[4 + it % 2], ps[6 + it % 2]
                        base = ie

                        def stA(kb):
                            sb_ = (base + kb) % 4
                            self.mm(ps[sb_][:, :w], kT[:, g, kb * 128:(kb + 1) * 128], q_[:, :w], True, True,
                                    r=['kT', qk], w=[('ps', sb_)])

                        def stB(kb):
                            sb_ = (base + kb) % 4
                            self.act(pt[sb_][:, :w], ps[sb_][:, :w], AF.Exp, r=[('ps', sb_)], w=[('pt', sb_)])

                        def stC(kb):
                            sb_ = (base + kb) % 4
                            self.mm(po[:, :w], vt[:, kb, g * 128:(g + 1) * 128], pt[sb_][:, :w], kb == 0, kb == nkb - 1,
                                    r=['vt', ('pt', sb_)], w=[('ps', 4 + it % 2)])
                            if kb % 2 == 1:
                                sp_ = (base + kb - 1) % 4
                                p2 = pt2[(kb // 2) % 2]
                                self.tt(p2[:, :w], pt[sp_][:, :w], pt[sb_][:, :w], ALU.add,
                                        r=[('pt', sp_), ('pt', sb_)], w=[('pt2', (kb // 2) % 2)])
                                self.mm(pl[:, :w], self.onesb[:], p2[:, :w], kb == 1, kb == nkb - 1,
                                        r=['onesb', ('pt2', (kb // 2) % 2)], w=[('ps', 6 + it % 2)])
                        PF = 2
                        for kb in range(min(PF, nkb)):
                            stA(kb)
                            stB(kb)
                        for kb in range(nkb):
                            if kb + PF < nkb:
                                stA(kb + PF)
                                stB(kb + PF)
                            stC(kb)
                        ie += nkb
                        S.op('dve', lambda: nc.vector.reciprocal(out=rl[it % 2][:, :w], in_=pl[:, :w]),
                             r=[('ps', 6 + it % 2)], w=[('rl', it % 2)])
                        self.tt(ost[it % 2][:, :w], po[:, :w], rl[it % 2][:, :w], ALU.mult,
                                r=[('ps', 4 + it % 2), ('rl', it % 2)], w=[('ost', it % 2)])
                        S.dma(self.OT[h * 128:(h + 1) * 128, t0:t0 + w], ost[it % 2][:, :w], r=[('ost', it % 2)], w=[('OT', h, si)])
                        it += 1
        S.barrier(keep=KEEP)

    def s5_s1(self, li, ab, abkey):
        S = self.S
        with ExitStack() as es:
            scr = self.norm_alloc(es)
            scr['xn'] = self.sb(es, 's1_xn', [128, KT, 512])
            hsb = [self.sb(es, f's1_hs{i}', [128, KT, 512]) for i in range(2)]
            ubb = [self.sb(es, f's1_ub{i}', [128, KT, 512], BF16) for i in range(2)]
            for si, (t0, w, isc) in enumerate(self.spans):
                hs, hkey = hsb[si % 2], ('hs', si % 2)
                r_ = 1 if isc else 0
                S.dma(hs[:, :, :w], self.hT_span(t0, w), r=[('hT', si)], w=[hkey])
                self.norm_span(hs, hkey, w, ab[:, r_, 0, :], ab[:, r_, 1, :], abkey, None, None, ubb[si % 2], ('ub', si % 2), scr)
                S.dma(self.UT.rearrange("(k p) t -> p k t", p=128)[:, :, t0:t0 + w], ubb[si % 2][:, :, :w],
                      r=[('ub', si % 2)], w=[('UT', si)])
        S.barrier(keep=KEEP)

    def s5_s2(self, ki):
        nc, S, I = self.nc, self.S, self.I
        ps = self.ps
        T = self.T
        PI = float(np.pi)
        with ExitStack() as es:
            es0 = ExitStack()
            par = self.sb(es, 's5par', [128, 3, 64])
            d32 = self.sb(es, 's5d', [32, 32])
            dsk = self.sb(es, 's5dsk', [32, 32, 32], BF16)
            BT = self.sb(es, 's5BT', [32, 64, 2, 128], BF16)
            Cw = self.sb(es, 's5Cw', [128, 3, 64, 32], BF16)
            v = {n: self.sb(es, 's5_' + n, [128, 64]) for n in
                 ('dt', 'mag', 'th', 't', 's', 'y', 'm', 'cos', 'sin', 'abr', 'abi', 'den', 'nr', 'cre', 'cim', 'u1', 'u2')}
            vi = self.sb(es, 's5_ti', [128, 64], mybir.dt.int32)
            PL = self.sb(es, 's5PL', [128, 10, 2, 64])
            nGs = self.sb(es, 's5nGs', [128, 2, 64])
            Bt = self.sb(es0, 's5B', [128, 2, 64, 32])
            Ct = self.sb(es0, 's5C', [128, 2, 64, 32])
            bb = self.sb(es0, 's5bb', [128, 2, 64, 32])
            tmpb = self.sb(es0, 's5tmpb', [128, 64, 32])
            with nc.allow_non_contiguous_dma(reason="small parameter tables"):
                S.dma(par[:].rearrange("p a b -> p (a b)"), I['s5_par'], w=['par'])
                S.dma(Bt[:].rearrange("p a b c -> p (a b c)"), I['s5_B'], w=['Bt'])
                S.dma(Ct[:].rearrange("p a b c -> p (a b c)"), I['s5_C'], w=['Ct'])
                S.dma(d32[:], I['s5_d'], w=['d32'])
            K_ = ['dk']

            def TS(o, i, s1, op0, s2=None, op1=None):
                self.ts(o, i, s1, op0, r=K_ + ['par'], w=K_, s2=s2, op1=op1)

            def TT(o, a, b, op):
                self.tt(o, a, b, op, r=K_ + ['par'], w=K_)
            are, aim, ldt = par[:, 0, :], par[:, 1, :], par[:, 2, :]
            self.act(v['dt'][:], ldt, AF.Exp, r=['par'], w=K_)
            TT(v['t'][:], are, v['dt'][:], ALU.mult)
            self.act(v['mag'][:], v['t'][:], AF.Exp, r=K_, w=K_)
            TT(v['th'][:], aim, v['dt'][:], ALU.mult)

            def sin_of(dst, shift):
                TS(v['t'][:], v['th'][:], 1.0 / (2 * PI), ALU.mult, s2=shift / (2 * PI), op1=ALU.add)
                self.cp(vi[:], v['t'][:], r=K_, w=K_)
                self.cp(v['t'][:], vi[:], r=K_, w=K_)
                TS(v['s'][:], v['th'][:], shift, ALU.add)
                self.stt(v['y'][:], v['t'][:], -2 * PI, v['s'][:], ALU.mult, ALU.add, r=K_, w=K_)
                TS(v['m'][:], v['y'][:], PI, ALU.is_gt)
                self.stt(v['y'][:], v['m'][:], -2 * PI, v['y'][:], ALU.mult, ALU.add, r=K_, w=K_)
                TS(v['m'][:], v['y'][:], -PI, ALU.is_lt)
                self.stt(v['y'][:], v['m'][:], 2 * PI, v['y'][:], ALU.mult, ALU.add, r=K_, w=K_)
                self.act(dst, v['y'][:], AF.Sin, r=K_, w=K_)
            sin_of(v['sin'][:], 0.0)
            sin_of(v['cos'][:], PI / 2)
            TT(v['abr'][:], v['mag'][:], v['cos'][:], ALU.mult)
            TT(v['abi'][:], v['mag'][:], v['sin'][:], ALU.mult)
            TT(v['den'][:], are, are, ALU.mult)
            TT(v['u1'][:], aim, aim, ALU.mult)
            TT(v['den'][:], v['den'][:], v['u1'][:], ALU.add)
            S.op('dve', lambda: nc.vector.reciprocal(out=v['den'][:], in_=v['den'][:]), r=K_, w=K_)
            TS(v['nr'][:], v['abr'][:], -1.0, ALU.add)
            TT(v['u1'][:], v['nr'][:], are, ALU.mult)
            TT(v['u2'][:], v['abi'][:], aim, ALU.mult)
            TT(v['u1'][:], v['u1'][:], v['u2'][:], ALU.add)
            TT(v['cre'][:], v['u1'][:], v['den'][:], ALU.mult)
            TT(v['u1'][:], v['abi'][:], are, ALU.mult)
            TT(v['u2'][:], v['nr'][:], aim, ALU.mult)
            TT(v['u1'][:], v['u1'][:], v['u2'][:], ALU.subtract)
            TT(v['cim'][:], v['u1'][:], v['den'][:], ALU.mult)
            creb = v['cre'][:].unsqueeze(2).to_broadcast([128, 64, 32])
            cimb = v['cim'][:].unsqueeze(2).to_broadcast([128, 64, 32])
            self.tt(bb[:, 0], Bt[:, 0], creb, ALU.mult, r=K_ + ['Bt'], w=['bb'])
            self.tt(tmpb[:], Bt[:, 1], cimb, ALU.mult, r=K_ + ['Bt'], w=['tmpb'])
            self.tt(bb[:, 0], bb[:, 0], tmpb[:], ALU.subtract, r=['bb', 'tmpb'], w=['bb'])
            self.tt(bb[:, 1], Bt[:, 1], creb, ALU.mult, r=K_ + ['Bt'], w=['bb'])
            self.tt(tmpb[:], Bt[:, 0], cimb, ALU.mult, r=K_ + ['Bt', 'bb'], w=['tmpb'])
            self.tt(bb[:, 1], bb[:, 1], tmpb[:], ALU.add, r=['bb', 'tmpb'], w=['bb'])
            n_ = 0
            for dj in range(64):
                for ri in range(2):
                    bank = 6 + (n_ // 4) % 2
                    S.op('pe', lambda: nc.tensor.transpose(out=ps[bank][0:32, (n_ % 4) * 128:(n_ % 4 + 1) * 128],
                                                           in_=bb[:, ri, dj, :], identity=self.ident[:]),
                         r=['bb', 'ident'], w=[('ps', bank)])
                    if n_ % 4 == 3:
                        dj0 = dj - 1
                        self.cp(BT[:, dj0:dj0 + 2, :, :].rearrange("p a b c -> p (a b c)"), ps[bank][0:32, :],
                                r=[('ps', bank)], w=['BT'], e='act')
                    n_ += 1
            self.cp(Cw[:, 0], Ct[:, 0], r=['Ct'], w=['Cw'])
            self.ts(Cw[:, 1], Ct[:, 0], -1.0, ALU.mult, r=['Ct'], w=['Cw'])
            self.ts(Cw[:, 2], Ct[:, 1], -1.0, ALU.mult, r=['Ct'], w=['Cw'])
            self.tt(dsk[:], self.ident[0:32, 0:32].unsqueeze(1).to_broadcast([32, 32, 32]),
                    d32[:].unsqueeze(2).to_broadcast([32, 32, 32]), ALU.mult, r=['ident', 'd32'], w=['dsk'])
            self.cp(PL[:, 0, 0, :], v['cos'][:], r=K_, w=['PL'])
            self.cp(PL[:, 0, 1, :], v['sin'][:], r=K_, w=['PL'])
            for l in range(9):
                c_, s_ = PL[:, l, 0, :], PL[:, l, 1, :]
                self.tt(v['u1'][:], c_, c_, ALU.mult, r=['PL'] + K_, w=K_)
                self.tt(v['u2'][:], s_, s_, ALU.mult, r=['PL'] + K_, w=K_)
                self.tt(PL[:, l + 1, 0, :], v['u1'][:], v['u2'][:], ALU.subtract, r=K_ + ['PL'], w=['PL'])
                self.tt(v['u1'][:], c_, s_, ALU.mult, r=['PL'] + K_, w=K_)
                self.ts(PL[:, l + 1, 1, :], v['u1'][:], 2.0, ALU.mult, r=K_ + ['PL'], w=['PL'])
            self.ts(nGs[:, 0, :], PL[:, 8, 1, :], -1.0, ALU.mult, r=['PL'], w=['nGs'])
            self.ts(nGs[:, 1, :], PL[:, 9, 1, :], -1.0, ALU.mult, r=['PL'], w=['nGs'])

            S.barrier(keep=KEEP)
            es0.close()
            cosT = self.sb(es, 's5cosT', [128, 4, 512])
            sinT = self.sb(es, 's5sinT', [128, 4, 512])
            Rt = self.sb(es, 's5Rt', [128, 4, 512])
            ga = self.sb(es, 's5ga', [128, 4, 256])
            gb_ = self.sb(es, 's5gb', [128, 4, 256])
            ujb = [self.sb(es, f's5uj{i}', [32, T], BF16) for i in range(2)]
            ystb = [self.sb(es, f's5yst{i}', [32, 512]) for i in range(2)]
            Wb = [{n: self.sb(es, f's5w{i}_' + n, [128, 512]) for n in ('t1', 't2', 't3', 't4', 'wir', 'wii', 'wr', 'wi')} for i in range(2)]
            Ppb = [[self.sb(es, f's5P{i}_{j}', [128, 512], BF16) for i in range(4)] for j in range(2)]
            car = self.sb(es, 's5car', [128, 4])
            lat = [s_ for s_ in enumerate(self.spans) if not s_[1][2]]
            ctxs = [s_ for s_ in enumerate(self.spans) if s_[1][2]]
            orders = [ctxs + lat, ctxs + lat[::-1]]
            it = 0
            for dr in range(2):
                Y = self.YF if dr == 0 else self.YB
                for jb in range(8):
                    cols = slice(dr * 32 + 4 * jb, dr * 32 + 4 * jb + 4)
                    tk = ['tab']
                    S.op('pool', lambda: nc.gpsimd.memset(cosT[:, :, 0:1], 1.0), r=[], w=tk)
                    S.op('pool', lambda: nc.gpsimd.memset(sinT[:, :, 0:1], 0.0), r=[], w=tk)
                    for l in range(9):
                        n = 1 << l
                        pc = PL[:, l, 0, cols].unsqueeze(2).to_broadcast([128, 4, n])
                        ps_ = PL[:, l, 1, cols].unsqueeze(2).to_broadcast([128, 4, n])
                        c0, s0 = cosT[:, :, 0:n], sinT[:, :, 0:n]
                        self.tt(ga[:, :, 0:n], c0, pc, ALU.mult, r=tk + ['PL'], w=['ga'], e='pool')
                        self.tt(gb_[:, :, 0:n], s0, ps_, ALU.mult, r=tk + ['PL'], w=['gb'], e='pool')
                        self.tt(cosT[:, :, n:2 * n], ga[:, :, 0:n], gb_[:, :, 0:n], ALU.subtract, r=['ga', 'gb'], w=tk, e='pool')
                        self.tt(ga[:, :, 0:n], s0, pc, ALU.mult, r=tk + ['PL'], w=['ga'], e='pool')
                        self.tt(gb_[:, :, 0:n], c0, ps_, ALU.mult, r=tk + ['PL'], w=['gb'], e='pool')
                        self.tt(sinT[:, :, n:2 * n], ga[:, :, 0:n], gb_[:, :, 0:n], ALU.add, r=['ga', 'gb'], w=tk, e='pool')
                    self.cp(Rt[:], v['mag'][:, cols].unsqueeze(2).to_broadcast([128, 4, 512]), r=K_, w=tk, e='pool')
                    for jj in range(4):
                        j = 4 * jb + jj
                        dj = dr * 32 + j
                        uj, ukey = ujb[it % 2], ('uj', it % 2)
                        S.dma(uj[:], self.UT[32 * j:32 * j + 32, :], w=[ukey])
                        S.op('dve', lambda: nc.vector.memset(car[:], 0.0), r=[], w=['car'])
                        cT, sT, rT = cosT[:, jj, :], sinT[:, jj, :], Rt[:, jj, :]
                        def emitB(n2):
                            si, (t0, w, isc) = orders[dr][n2]
                            b2 = n2 % 2
                            usl = uj[:, t0:t0 + w]
                            rhs = usl if dr == 0 else usl[:, ::-1]
                            self.mm(ps[2 * b2][:, :w], BT[:, dj, 0, :], rhs, True, True, r=['BT', ukey], w=[('ps', 2 * b2)])
                            self.mm(ps[2 * b2 + 1][:, :w], BT[:, dj, 1, :], rhs, True, True, r=['BT', ukey], w=[('ps', 2 * b2 + 1)])
                        emitB(0)
                        for n2, (si, (t0, w, isc)) in enumerate(orders[dr]):
                            b2 = n2 % 2
                            W_, Pp, yst = Wb[b2], Ppb[b2], ystb[b2]
                            kk = lambda n: (n, b2)
                            pbr, pbi, py = ps[2 * b2], ps[2 * b2 + 1], ps[4 + b2]
                            usl = uj[:, t0:t0 + w]
                            if n2 + 1 < len(orders[dr]):
                                emitB(n2 + 1)
                            self.tt(W_['t1'][:, :w], pbr[:, :w], cT[:, :w], ALU.mult, r=[('ps', 2 * b2)] + tk, w=[kk('t1')])
                            self.tt(W_['t2'][:, :w], pbi[:, :w], sT[:, :w], ALU.mult, r=[('ps', 2 * b2 + 1)] + tk, w=[kk('t2')])
                            self.tt(W_['t3'][:, :w], pbi[:, :w], cT[:, :w], ALU.mult, r=[('ps', 2 * b2 + 1)] + tk, w=[kk('t3')])
                            self.tt(W_['t4'][:, :w], pbr[:, :w], sT[:, :w], ALU.mult, r=[('ps', 2 * b2)] + tk, w=[kk('t4')])
                            self.tt(W_['wir'][:, :w], W_['t1'][:, :w], W_['t2'][:, :w], ALU.add, r=[kk('t1'), kk('t2')], w=[kk('wir')])
                            self.tt(W_['wii'][:, :w], W_['t3'][:, :w], W_['t4'][:, :w], ALU.subtract, r=[kk('t3'), kk('t4')], w=[kk('wii')])
                            S.op('dve', lambda: nc.vector.tensor_tensor_scan(out=W_['wr'][:, :w], data0=rT[:, :w], data1=W_['wir'][:, :w],
                                                                           initial=car[:, 0:1], op0=ALU.mult, op1=ALU.add),
                                 r=[kk('wir'), 'car'] + tk, w=[kk('wr')])
                            S.op('dve', lambda: nc.vector.tensor_tensor_scan(out=W_['wi'][:, :w], data0=rT[:, :w], data1=W_['wii'][:, :w],
                                                                           initial=car[:, 1:2], op0=ALU.mult, op1=ALU.add),
                                 r=[kk('wii'), 'car'] + tk, w=[kk('wi')])
                            lv = 0 if w == 256 else 1
                            Gc, Gs, nG = PL[:, 8 + lv, 0, dj:dj + 1], PL[:, 8 + lv, 1, dj:dj + 1], nGs[:, lv, dj:dj + 1]
                            wrl, wil = W_['wr'][:, w - 1:w], W_['wi'][:, w - 1:w]
                            self.ts(car[:, 2:3], wrl, Gc, ALU.mult, r=[kk('wr'), 'PL', 'car'], w=['car'])
                            self.stt(car[:, 0:1], wil, nG, car[:, 2:3], ALU.mult, ALU.add, r=[kk('wi'), 'nGs', 'car'], w=['car'])
                            self.ts(car[:, 3:4], wil, Gc, ALU.mult, r=[kk('wi'), 'PL', 'car'], w=['car'])
                            self.stt(car[:, 1:2], wrl, Gs, car[:, 3:4], ALU.mult, ALU.add, r=[kk('wr'), 'PL', 'car'], w=['car'])
                            self.tt(Pp[0][:, :w], cT[:, :w], W_['wr'][:, :w], ALU.mult, r=tk + [kk('wr')], w=[('P', 0, b2)], e='pool')
                            self.tt(Pp[1][:, :w], sT[:, :w], W_['wi'][:, :w], ALU.mult, r=tk + [kk('wi')], w=[('P', 1, b2)], e='pool')
                            self.tt(Pp[2][:, :w], sT[:, :w], W_['wr'][:, :w], ALU.mult, r=tk + [kk('wr')], w=[('P', 2, b2)], e='pool')
                            self.tt(Pp[3][:, :w], cT[:, :w], W_['wi'][:, :w], ALU.mult, r=tk + [kk('wi')], w=[('P', 3, b2)], e='pool')
                            yk = ('ps', 4 + b2)
                            self.mm(py[0:32, :w], Cw[:, 0, dj, :], Pp[0][:, :w], True, False, r=['Cw', ('P', 0, b2)], w=[yk])
                            self.mm(py[0:32, :w], Cw[:, 1, dj, :], Pp[1][:, :w], False, False, r=['Cw', ('P', 1, b2)], w=[yk])
                            self.mm(py[0:32, :w], Cw[:, 2, dj, :], Pp[2][:, :w], False, False, r=['Cw', ('P', 2, b2)], w=[yk])
                            self.mm(py[0:32, :w], Cw[:, 2, dj, :], Pp[3][:, :w], False, dr == 1, r=['Cw', ('P', 3, b2)], w=[yk])
                            if dr == 0:
                                self.mm(py[0:32, :w], dsk[:, j, :], usl, False, True, r=['dsk', ukey], w=[yk])
                            src = py[0:32, :w] if dr == 0 else py[0:32, :w][:, ::-1]
                            self.cp(yst[:, :w], src, r=[yk], w=[('yst', b2)], e='act')
                            S.dma(Y[32 * j:32 * j + 32, t0:t0 + w], yst[:, :w], r=[('yst', b2)], w=[('Y', dr, j, si)])
                        it += 1
        S.barrier(keep=KEEP)

    def ssd_d1(self, li, ki, ab, abkey):
        nc, S, I = self.nc, self.S, self.I
        ps = self.ps
        with ExitStack() as es:
            scr = self.norm_alloc(es)
            scr['xn'] = self.sb(es, 'd1_xn', [128, KT, 512])
            win = self.sb(es, 'd1_win', [128, KT, 5184], BF16)
            dtb = self.sb(es, 'd1_dtb', [128, 64])
            hsb = [self.sb(es, f'd1_hs{i}', [128, KT, 512]) for i in range(2)]
            ub = self.sb(es, 'd1_ub', [128, KT, 512], BF16)
            zst = [self.sb(es, f'd1_zst{i}', [128, 2 * D], BF16) for i in range(2)]
            xst = [self.sb(es, f'd1_xst{i}', [128, 8, 512], BF16) for i in range(2)]
            dst = [self.sb(es, f'd1_dst{i}', [128, 64]) for i in range(2)]
            for k in range(KT):
                S.dma(win[:, k, :], self.winb[ki][k * 128:(k + 1) * 128, :], r=[('winb', ki)], w=['win'])
            S.dma(dtb[:], I['ssd_dtb'], w=['dtb'])
            nz = nx = nd = 0
            for si, (t0, w, isc) in enumerate(self.spans):
                hs, hkey = hsb[si % 2], ('hs', si % 2)
                r_ = 1 if isc else 0
                S.dma(hs[:, :, :w], self.hT_span(t0, w), r=[('hT', si)], w=[hkey])
                self.norm_span(hs, hkey, w, ab[:, r_, 0, :], ab[:, r_, 1, :], abkey, None, None, ub, 'ub', scr)
                for tt_ in range(w // 128):
                    zt, zk = zst[nz % 2], ('zst', nz % 2)
                    for cg in range(4):
                        b = cg % 2
                        for k in range(KT):
                            self.mm(ps[b][:, :], ub[:, k, tt_ * 128:(tt_ + 1) * 128], win[:, k, cg * 512:(cg + 1) * 512],
                                    k == 0, k == KT - 1, r=['win', 'ub'], w=[('ps', b)])
                        self.act(zt[:, cg * 512:(cg + 1) * 512], ps[b][:, :], AF.Silu, r=[('ps', b)], w=[zk])
                    S.dma(self.ZS[t0 + tt_ * 128:t0 + (tt_ + 1) * 128, :], zt[:], r=[zk], w=[('ZS', nz)])
                    nz += 1
                    dt_, dk = dst[nd % 2], ('dst', nd % 2)
                    for k in range(KT):
                        self.mm(ps[6][:, 0:64], ub[:, k, tt_ * 128:(tt_ + 1) * 128], win[:, k, 5120:5184],
                                k == 0, k == KT - 1, r=['win', 'ub'], w=[('ps', 6)])
                    self.tt(dt_[:], ps[6][:, 0:64], dtb[:], ALU.add, r=[('ps', 6), 'dtb'], w=[dk])
                    self.act(dt_[:], dt_[:], AF.Exp, r=[dk], w=[dk])
                    self.act(dt_[:], dt_[:], AF.Ln, r=[dk], w=[dk], bias=1.0, scale=1.0)
                    S.dma(self.DTs[t0 + tt_ * 128:t0 + (tt_ + 1) * 128, :], dt_[:], r=[dk], w=[('DTs', nd)])
                    nd += 1
                for c8 in range(3):
                    xt, xk = xst[nx % 2], ('xst', nx % 2)
                    for c in range(8):
                        ct = c8 * 8 + c
                        b = 2 + ct % 2
                        for k in range(KT):
                            self.mm(ps[b][:, :w], win[:, k, 2048 + ct * 128:2048 + (ct + 1) * 128], ub[:, k, :w],
                                    k == 0, k == KT - 1, r=['win', 'ub'], w=[('ps', b)])
                        self.cp(xt[:, c, :w], ps[b][:, :w], r=[('ps', b)], w=[xk], e='dve' if c % 2 == 0 else 'act')
                    S.dma(self.XBC.rearrange("(k p) t -> p k t", p=128)[:, c8 * 8:(c8 + 1) * 8, t0:t0 + w], xt[:, :, :w],
                          r=[xk], w=[('XBC', nx)])
                    nx += 1
        S.barrier(keep=KEEP)

    def ssd_d2(self):
        nc, S, I = self.nc, self.S, self.I
        ps = self.ps
        T = self.T
        with ExitStack() as es:
            cw = self.sb(es, 'd2_cw', [128, 24, 5])
            cb = self.sb(es, 'd2_cb', [128, 24])
            xin = [self.sb(es, f'd2_xin{i}', [128, 24, 516], BF16) for i in range(2)]
            xc = [self.sb(es, f'd2_xc{i}', [128, 24, 512], BF16) for i in range(2)]
            cv = [self.sb(es, f'd2_cv{i}', [128, 512]) for i in range(2)]
            xts = [self.sb(es, f'd2_xts{i}', [128, 2 * D], BF16) for i in range(2)]
            bts = [self.sb(es, f'd2_bts{i}', [128, 512], BF16) for i in range(2)]
            S.dma(cw[:].rearrange("p a b -> p (a b)"), I['ssd_cw'], w=['cw'])
            S.dma(cb[:], I['ssd_cb'], w=['cb'])
            nt = 0
            for si, (t0, w, isc) in enumerate(self.spans):
                s0, s1 = (0, TC) if isc else (TC, T)
                lo, hi = max(t0 - 2, s0), min(t0 + w + 2, s1)
                xi, xik = xin[si % 2], ('xin', si % 2)
                if lo > t0 - 2:
                    S.op('dve', lambda: nc.vector.memset(xi[:, :, 0:2], 0.0), w=[xik])
                if hi < t0 + w + 2:
                    S.op('dve', lambda: nc.vector.memset(xi[:, :, w + 2:w + 4], 0.0), w=[xik])
                for c8 in range(3):
                    S.dma(xi[:, c8 * 8:(c8 + 1) * 8, lo - (t0 - 2):hi - (t0 - 2)],
                          self.XBC.rearrange("(k p) t -> p k t", p=128)[:, c8 * 8:(c8 + 1) * 8, lo:hi], w=[xik])
                xo, xok = xc[si % 2], ('xc', si % 2)
                for ct in range(24):
                    c_, ck = cv[ct % 2], ('cv', ct % 2)
                    self.ts(c_[:, :w], xi[:, ct, 0:w], cw[:, ct, 0:1], ALU.mult, r=[xik, 'cw', 'cb'], w=[ck], s2=cb[:, ct:ct + 1], op1=ALU.add)
                    for k in range(1, 5):
                        self.stt(c_[:, :w], xi[:, ct, k:k + w], cw[:, ct, k:k + 1], c_[:, :w], ALU.mult, ALU.add, r=[xik, 'cw', ck], w=[ck])
                    self.act(xo[:, ct, :w], c_[:, :w], AF.Silu, r=[ck], w=[xok])
                S.dma(self.BCf.rearrange("(k p) t -> p k t", p=128)[:, :, t0:t0 + w], xo[:, 16:24, :w], r=[xok], w=[('BCf', si)])
                for tt_ in range(w // 128):
                    tok = t0 + tt_ * 128
                    xt, xtk_ = xts[nt % 2], ('xts', nt % 2)
                    for half in range(2):
                        bank = half
                        pbv = ps[bank][:].bitcast(BF16)
                        for q in range(8):
                            ct = half * 8 + q
                            S.op('pe', lambda: nc.tensor.transpose(out=pbv[:, q * 128:(q + 1) * 128],
                                                                   in_=xo[:, ct, tt_ * 128:(tt_ + 1) * 128], identity=self.identb[:]),
                                 r=[xok, 'identb'], w=[('ps', bank)])
                        self.cp(xt[:, half * 1024:(half + 1) * 1024], pbv[:, 0:1024], r=[('ps', bank)], w=[xtk_],
                                e='dve' if half == 0 else 'act')
                    S.dma(self.XTK[tok:tok + 128, :], xt[:], r=[xtk_], w=[('XTK', nt)])
                    bt, btk_ = bts[nt % 2], ('bts', nt % 2)
                    pbv = ps[2][:].bitcast(BF16)
                    for q in range(4):
                        S.op('pe', lambda: nc.tensor.transpose(out=pbv[:, q * 128:(q + 1) * 128],
                                                               in_=xo[:, 16 + q, tt_ * 128:(tt_ + 1) * 128], identity=self.identb[:]),
                             r=[xok, 'identb'], w=[('ps', 2)])
                    self.cp(bt[:], pbv[:, 0:512], r=[('ps', 2)], w=[btk_])
                    S.dma(self.BTK[tok:tok + 128, :], bt[:], r=[btk_], w=[('BTK', nt)])
                    nt += 1
        S.barrier(keep=KEEP)

    def ssd_d3(self):
        nc, S, I = self.nc, self.S, self.I
        ps = self.ps
        T = self.T
        NB = T // 128
        with ExitStack() as es:
            msk = self.sb(es, 'd3_msk', [128, 4, 128])
            aneg = self.sb(es, 'd3_aneg', [128, 64])
            Sst = self.sb(es, 'd3_S', [128, 4, 512])
            Sb = self.sb(es, 'd3_Sb', [128, 4, 512], BF16)
            xtk = [self.sb(es, f'd3_xtk{i}', [128, 32, 64], BF16) for i in range(2)]
            btk = [self.sb(es, f'd3_btk{i}', [128, 512], BF16) for i in range(2)]
            bcf = [self.sb(es, f'd3_bcf{i}', [128, 8, 128], BF16) for i in range(2)]
            dtt_ = [self.sb(es, f'd3_dt{i}', [128, 32]) for i in range(2)]
            sm = self.sb(es, 'd3_sm', [128, 8, 32])
            cs = self.sb(es, 'd3_cs', [128, 64])
            chl = self.sb(es, 'd3_chl', [128, 2, 32], BF16)
            ch32 = self.sb(es, 'd3_ch32', [128, 2, 32])
            mskb = self.sb(es, 'd3_mskb', [128, 2, 128], BF16)
            xdt = self.sb(es, 'd3_xdt', [128, 32, 64], BF16)
            txdt = self.sb(es, 'd3_txdt', [128, 32, 64], BF16)
            dec = [self.sb(es, f'd3_dec{i}', [128, 128]) for i in range(4)]
            MT = [self.sb(es, f'd3_MT{i}', [128, 128], BF16) for i in range(4)]
            ytmp = [self.sb(es, f'd3_ytmp{i}', [128, 512]) for i in range(2)]
            ych = [self.sb(es, f'd3_ych{i}', [128, 2 * D]) for i in range(2)]
            S.dma(msk[:].rearrange("p a b -> p (a b)"), I['ssd_msk'], w=['msk'])
            S.dma(aneg[:], I['ssd_alog'], w=['aneg'])
            self.act(aneg[:], aneg[:], AF.Exp, r=['aneg'], w=['aneg'])
            self.ts(aneg[:], aneg[:], -1.0, ALU.mult, r=['aneg'], w=['aneg'])
            self.cp(mskb[:], msk[:, 2:4, :], r=['msk'], w=['mskb'])
            ctx_ch = list(range(TC // 128))
            lat_ch = list(range(TC // 128, NB))
            orders = [ctx_ch + lat_ch, ctx_ch[::-1] + lat_ch[::-1]]
            it = 0
            for dr in range(2):
                S.op('dve', lambda: nc.vector.memset(Sst[:], 0.0), w=['S'])
                S.op('dve', lambda: nc.vector.memset(Sb[:], 0.0), w=['Sb'])
                tri, mneg = msk[:, dr, :], msk[:, 2 + dr, :]
                for c in orders[dr]:
                    tok = c * 128
                    p2 = it % 2
                    x_, xk = xtk[p2], ('xtk', p2)
                    b_, bk = btk[p2], ('btk', p2)
                    f_, fk = bcf[p2], ('bcf', p2)
                    d_, dk = dtt_[p2], ('dt', p2)
                    S.dma(x_[:].rearrange("p h q -> p (h q)"), self.XTK[tok:tok + 128, :], w=[xk])
                    S.dma(b_[:], self.BTK[tok:tok + 128, :], w=[bk])
                    S.dma(f_[:], self.BCf.rearrange("(k p) t -> p k t", p=128)[:, :, tok:tok + 128], w=[fk])
                    with nc.allow_non_contiguous_dma(reason="dt half rows"):
                        S.dma(d_[:], self.DTs[tok:tok + 128, dr * 32:(dr + 1) * 32], w=[dk])
                    smk = ['sm']
                    self.tt(sm[:, 0, :], d_[:], aneg[:, dr * 32:(dr + 1) * 32], ALU.mult, r=[dk, 'aneg'], w=smk)
                    self.mm(ps[0][:, 0:32], tri, sm[:, 0, :], True, True, r=['msk'] + smk, w=[('ps', 0)])
                    self.mm(ps[0][:, 32:64], self.ones32[:], sm[:, 0, :], True, True, r=['ones32'] + smk, w=[('ps', 0)])
                    self.cp(cs[:], ps[0][:, 0:64], r=[('ps', 0)], w=['cs'])
                    self.ts(sm[:, 1, :], cs[:, 0:32], -1.0, ALU.mult, r=['cs'], w=smk)
                    self.cp(chl[:, 0, :], cs[:, 0:32], r=['cs'], w=['chl'])
                    self.cp(ch32[:, 0, :], chl[:, 0, :], r=['chl'], w=['ch32'])
                    self.tt(ch32[:, 1, :], cs[:, 0:32], ch32[:, 0, :], ALU.subtract, r=['cs', 'ch32'], w=['ch32'])
                    self.cp(chl[:, 1, :], ch32[:, 1, :], r=['ch32'], w=['chl'])
                    self.act(sm[:, 2, :], cs[:, 0:32], AF.Exp, r=['cs'], w=smk)
                    self.act(sm[:, 3, :], cs[:, 32:64], AF.Exp, r=['cs'], w=smk)
                    self.tt(sm[:, 4, :], cs[:, 32:64], cs[:, 0:32], ALU.subtract, r=['cs'], w=smk)
                    self.act(sm[:, 5, :], sm[:, 4, :], AF.Exp, r=smk, w=smk)
                    self.tt(sm[:, 6, :], d_[:], sm[:, 5, :], ALU.mult, r=[dk] + smk, w=smk)
                    self.tt(xdt[:], x_[:], d_[:].unsqueeze(2).to_broadcast([128, 32, 64]), ALU.mult, r=[xk, dk], w=['xdt'])
                    self.tt(txdt[:], x_[:], sm[:, 6, :].unsqueeze(2).to_broadcast([128, 32, 64]), ALU.mult, r=[xk] + smk, w=['txdt'], e='pool')
                    for g in range(4):
                        self.mm(ps[1][:, g * 128:(g + 1) * 128], f_[:, g, :], f_[:, 4 + g, :], True, True, r=[fk], w=[('ps', 1)])
                    y_, yk = ych[p2], ('ych', p2)

                    def hA(h):
                        bkn = 2 + (h // 4) % 2
                        col = (h % 4) * 128
                        self.mm(ps[bkn][:, col:col + 128], chl[:, 0, h:h + 1].to_broadcast([128, 128]), self.identb[:], True, False,
                                r=['chl', 'identb'], w=[('ps', bkn)])
                        self.mm(ps[bkn][:, col:col + 128], chl[:, 1, h:h + 1].to_broadcast([128, 128]), self.identb[:], False, False,
                                r=['chl', 'identb'], w=[('ps', bkn)])
                        self.mm(ps[bkn][:, col:col + 128], self.identb[:], mskb[:, dr, :], False, True, r=['identb', 'mskb'], w=[('ps', bkn)])

                    def hB(h):
                        bkn = 2 + (h // 4) % 2
                        col = (h % 4) * 128
                        self.act(dec[h % 4][:], ps[bkn][:, col:col + 128], AF.Exp, r=[('ps', bkn)] + smk, w=[('dec', h % 4)],
                                 bias=sm[:, 1, h:h + 1], scale=1.0)

                    def hCD(h):
                        g, hh = h // 8, h % 8
                        pa = ps[4 + g % 2]
                        self.tt(MT[h % 4][:], dec[h % 4][:], ps[1][:, g * 128:(g + 1) * 128], ALU.mult,
                                r=[('dec', h % 4), ('ps', 1)], w=[('MT', h % 4)])
                        self.mm(pa[:, hh * 64:(hh + 1) * 64], MT[h % 4][:], xdt[:, h, :], True, True,
                                r=[('MT', h % 4), 'xdt'], w=[('ps', 4 + g % 2)])
                    PF = 3
                    for h in range(PF):
                        hA(h)
                        hB(h)
                    for h in range(32):
                        if h + PF < 32:
                            hA(h + PF)
                            hB(h + PF)
                        hCD(h)
                        if h % 8 == 7:
                            g = h // 8
                            pa, pbk = ps[4 + g % 2], ps[6 + g % 2]
                            self.mm(pbk[:, :], f_[:, 4 + g, :], Sb[:, g, :], True, True, r=[fk, 'Sb'], w=[('ps', 6 + g % 2)])
                            self.tt(ytmp[g % 2][:].rearrange("p (h q) -> p h q", h=8), pbk[:, :].rearrange("p (h q) -> p h q", h=8),
                                    sm[:, 2, g * 8:(g + 1) * 8].unsqueeze(2).to_broadcast([128, 8, 64]), ALU.mult,
                                    r=[('ps', 6 + g % 2)] + smk, w=[('ytmp', g % 2)])
                            self.tt(y_[:, g * 512:(g + 1) * 512], pa[:, :], ytmp[g % 2][:], ALU.add,
                                    r=[('ps', 4 + g % 2), ('ytmp', g % 2)], w=[yk])
                    S.dma(self.YD[dr, tok:tok + 128, :], y_[:], r=[yk], w=[('YD', dr, c)])
                    for g in range(4):
                        pst = ps[2 + g % 2]
                        self.mm(pst[:, :], b_[:, g * 128:(g + 1) * 128], txdt[:, g * 8:(g + 1) * 8, :].rearrange("p h q -> p (h q)"),
                                True, True, r=[bk, 'txdt'], w=[('ps', 2 + g % 2)])
                        sv = Sst[:, g, :].rearrange("p (h q) -> p h q", h=8)
                        self.tt(sv, sv, sm[:, 3, g * 8:(g + 1) * 8].unsqueeze(2).to_broadcast([128, 8, 64]), ALU.mult, r=['S'] + smk, w=['S'])
                        self.tt(Sst[:, g, :], Sst[:, g, :], pst[:, :], ALU.add, r=['S', ('ps', 2 + g % 2)], w=['S'])
                        self.cp(Sb[:, g, :], Sst[:, g, :], r=['S'], w=['Sb'], e='pool')
                    it += 1
        S.barrier(keep=KEEP)

    def ssd_d4(self, li, ki, ab, abkey, need_ctx):
        nc, S, I = self.nc, self.S, self.I
        ps = self.ps
        with ExitStack() as es:
            wout = self.sb(es, 'd4_wout', [128, 16, D], BF16)
            ngr = self.sb(es, 'd4_ng', [128, 2 * D])
            dh = self.sb(es, 'd4_dh', [128, 32])
            eps_ = self.sb(es, 'd4_eps', [128, 1])
            hsb = [self.sb(es, f'd4_hs{i}', [128, KT, 512]) for i in range(2)]
            yf = [self.sb(es, f'd4_yf{i}', [128, 2 * D]) for i in range(2)]
            yb = [self.sb(es, f'd4_yb{i}', [128, 2 * D]) for i in range(2)]
            xk_ = [self.sb(es, f'd4_x{i}', [128, 2 * D], BF16) for i in range(2)]
            zs = [self.sb(es, f'd4_z{i}', [128, 2 * D], BF16) for i in range(2)]
            tmp = self.sb(es, 'd4_tmp', [128, 2 * D])
            ynb = self.sb(es, 'd4_ynb', [128, 2 * D], BF16)
            ynT = self.sb(es, 'd4_ynT', [128, 16, 512], BF16)
            st = self.sb(es, 'd4_st', [128, 4])
            S.dma(wout[:], self.woutb[ki].rearrange("(k p) n -> p k n", p=128), r=[('woutb', ki)], w=['wout'])
            S.dma(ngr[:], I['ssd_ng'], w=['ngr'])
            S.dma(dh[:], I['ssd_dh'], w=['dh'])
            S.op('dve', lambda: nc.vector.memset(eps_[:], EPS), w=['eps'])
            spans = [s_ for s_ in enumerate(self.spans) if need_ctx or not s_[1][2]]
            nt = 0
            for n_, (si, (t0, w, isc)) in enumerate(spans):
                hs, hkey = hsb[n_ % 2], ('hs', n_ % 2)
                r_ = 1 if isc else 0
                S.dma(hs[:, :, :w], self.hT_span(t0, w), r=[('hT', si)], w=[hkey])
                for tt_ in range(w // 128):
                    tok = t0 + tt_ * 128
                    p2 = nt % 2
                    S.dma(yf[p2][:], self.YD[0, tok:tok + 128, :], w=[('yf', p2)])
                    S.dma(yb[p2][:], self.YD[1, tok:tok + 128, :], w=[('yb', p2)])
                    S.dma(xk_[p2][:], self.XTK[tok:tok + 128, :], w=[('x', p2)])
                    S.dma(zs[p2][:], self.ZS[tok:tok + 128, :], w=[('z', p2)])
                    y = yf[p2]
                    yk = ('yf', p2)
                    self.tt(y[:], y[:], yb[p2][:], ALU.add, r=[yk, ('yb', p2)], w=[yk], e='pool')
                    self.tt(tmp[:].rearrange("p (h q) -> p h q", h=32), xk_[p2][:].rearrange("p (h q) -> p h q", h=32),
                            dh[:].unsqueeze(2).to_broadcast([128, 32, 64]), ALU.mult, r=[('x', p2), 'dh'], w=['tmp'], e='pool')
                    self.tt(y[:], y[:], tmp[:], ALU.add, r=[yk, 'tmp'], w=[yk])
                    self.tt(y[:], y[:], zs[p2][:], ALU.mult, r=[yk, ('z', p2)], w=[yk])
                    self.act(tmp[:], y[:], AF.Square, r=[yk], w=['tmp', 'st'], accum_out=st[:, 0:1])
                    self.act(st[:, 1:2], st[:, 0:1], AF.Sqrt, r=['st', 'eps'], w=['st'], scale=1.0 / (2 * D), bias=eps_[:, 0:1])
                    S.op('dve', lambda: nc.vector.reciprocal(out=st[:, 2:3], in_=st[:, 1:2]), r=['st'], w=['st'])
                    self.stt(ynb[:], y[:], st[:, 2:3], ngr[:], ALU.mult, ALU.mult, r=[yk, 'st', 'ngr'], w=['ynb'])
                    for half in range(2):
                        bank = half
                        pbv = ps[bank][:].bitcast(BF16)
                        for q in range(8):
                            k = half * 8 + q
                            S.op('pe', lambda: nc.tensor.transpose(out=pbv[:, q * 128:(q + 1) * 128],
                                                                   in_=ynb[:, k * 128:(k + 1) * 128], identity=self.identb[:]),
                                 r=['ynb', 'identb'], w=[('ps', bank)])
                        self.cp(ynT[:, half * 8:(half + 1) * 8, tt_ * 128:(tt_ + 1) * 128],
                                pbv[:, 0:1024].rearrange("p (k t) -> p k t", k=8), r=[('ps', bank)], w=['ynT'],
                                e='dve' if half == 0 else 'act')
                    nt += 1
                for ct in range(KT):
                    pb = ps[2 + ct % 2]
                    for k in range(16):
                        self.mm(pb[:, :w], wout[:, k, ct * 128:(ct + 1) * 128], ynT[:, k, :w], k == 0, k == 15,
                                r=['wout', 'ynT'], w=[('ps', 2 + ct % 2)])
                    self.stt(hs[:, ct, :w], pb[:, :w], ab[:, r_, 2, ct:ct + 1], hs[:, ct, :w], ALU.mult, ALU.add,
                             r=[('ps', 2 + ct % 2), abkey, hkey], w=[hkey])
                S.dma(self.hT_span(t0, w), hs[:, :, :w], r=[hkey], w=[('hT', si)])
        S.barrier(keep=KEEP)

    def layer(self, li, kind, ki, need_ctx):
        S = self.S
        ps = self.ps
        with ExitStack() as es:
            ab = self.layer_scalars(es, li)
            abkey = ('ab', li)
            if kind == 'a':
                self.attn_a1(li, ki, ab, abkey)
                self.attn_a2(need_ctx)
            if kind == 's':
                self.s5_s1(li, ab, abkey)
                self.s5_s2(ki)
            if kind == 'd':
                self.ssd_d1(li, ki, ab, abkey)
                self.ssd_d2()
                self.ssd_d3()
                self.ssd_d4(li, ki, ab, abkey, need_ctx)
            with ExitStack() as es2:
                scr = self.norm_alloc(es2)
                M = self.moe_alloc(es2, li)
                scr['xn'] = M['acc']
                hsb = [self.sb(es2, f'hs{i}', [128, KT, 512]) for i in range(2)]
                u32 = self.sb(es2, 'u32', [128, KT, 512])
                ub = self.sb(es2, 'ub', [128, KT, 512], BF16)
                if kind == 'a':
                    wo = self.sb(es2, 'a3_wo', [128, KT, D], BF16)
                    otb = [self.sb(es2, f'a3_ot{i}', [128, KT, 512], BF16) for i in range(2)]
                    S.dma(wo[:], self.wob[ki].rearrange("(k p) n -> p k n", p=128), r=[('wob', ki)], w=['wo'])
                if kind == 's':
                    wgl = self.sb(es2, 's3_wglu', [128, KT, 2 * D], BF16)
                    bgl = self.sb(es2, 's3_bglu', [128, 16])
                    sgl = [self.sb(es2, f's3_sig{i}', [128, 512]) for i in range(2)]
                    ymx = [self.sb(es2, f's3_ymx{i}', [128, 512]) for i in range(2)]
                    S.dma(wgl[:], self.wglub[ki].rearrange("(k p) n -> p k n", p=128), r=[('wglub', ki)], w=['wgl'])
                    S.dma(bgl[:], self.I['s5_bglu'], w=['bgl'])
                spans = [s for s in enumerate(self.spans) if need_ctx or not s[1][2]]
                for n_, (si, (t0, w, isc)) in enumerate(spans):
                    hs, hkey = hsb[n_ % 2], ('hs', n_ % 2)
                    r_ = 1 if isc else 0
                    S.dma(hs[:, :, :w], self.hT_span(t0, w), r=[('hT', si)], w=[hkey])
                    if kind == 's':
                        acc = M['acc']
                        akeys = [('acc', k) for k in range(KT)]
                        S.dma(u32[:, :, :w], self.YF.rearrange("(k p) t -> p k t", p=128)[:, :, t0:t0 + w], w=['u32'])
                        S.dma(acc[:, :, :w], self.YB.rearrange("(k p) t -> p k t", p=128)[:, :, t0:t0 + w], w=akeys)
                        self.tt(u32[:, :, :w], u32[:, :, :w], acc[:, :, :w], ALU.add, r=['u32'] + akeys, w=['u32'])
                        self.tt(acc[:, :, :w], u32[:, :, :w], u32[:, :, :w], ALU.mult, r=['u32'], w=akeys, e='pool')
                        self.ts(acc[:, :, :w], acc[:, :, :w], 0.044715, ALU.mult, r=akeys, w=akeys, s2=1.0, op1=ALU.add)
                        self.tt(acc[:, :, :w], acc[:, :, :w], u32[:, :, :w], ALU.mult, r=['u32'] + akeys, w=akeys, e='pool')
                        self.act(acc[:, :, :w], acc[:, :, :w], AF.Sigmoid, r=akeys, w=akeys, scale=2.0 * float(np.sqrt(2.0 / np.pi)))
                        self.tt(ub[:, :, :w], acc[:, :, :w], u32[:, :, :w], ALU.mult, r=['u32'] + akeys, w=['ub'])
                        for ct in range(KT):
                            b = ct % 2
                            pa, pb2 = ps[b], ps[2 + b]
                            for k in range(KT):
                                self.mm(pa[:, :w], wgl[:, k, ct * 128:(ct + 1) * 128], ub[:, k, :w], k == 0, k == KT - 1,
                                        r=['wgl', 'ub'], w=[('ps', b)])
                            for k in range(KT):
                                self.mm(pb2[:, :w], wgl[:, k, D + ct * 128:D + (ct + 1) * 128], ub[:, k, :w], k == 0, k == KT - 1,
                                        r=['wgl', 'ub'], w=[('ps', 2 + b)])
                            self.act(sgl[b][:, :w], pb2[:, :w], AF.Sigmoid, r=[('ps', 2 + b), 'bgl'], w=[('sgl', b)],
                                     bias=bgl[:, 8 + ct:9 + ct], scale=1.0)
                            self.stt(ymx[b][:, :w], pa[:, :w], bgl[:, ct:ct + 1], sgl[b][:, :w], ALU.add, ALU.mult,
                                     r=[('ps', b), 'bgl', ('sgl', b)], w=[('ymx', b)])
                            self.stt(hs[:, ct, :w], ymx[b][:, :w], ab[:, r_, 2, ct:ct + 1], hs[:, ct, :w], ALU.mult, ALU.add,
                                     r=[('ymx', b), abkey, hkey], w=[hkey])
                    if kind == 'a':
                        ot, okey = otb[n_ % 2], ('ot', n_ % 2)
                        S.dma(ot[:, :, :w], self.OT.rearrange("(k p) t -> p k t", p=128)[:, :, t0:t0 + w], w=[okey])
                        for ct in range(KT):
                            pb = ps[ct % 2]
                            for k in range(KT):
                                self.mm(pb[:, :w], wo[:, k, ct * 128:(ct + 1) * 128], ot[:, k, :w], k == 0, k == KT - 1,
                                        r=['wo', okey], w=[('ps', ct % 2)])
                            self.stt(hs[:, ct, :w], pb[:, :w], ab[:, r_, 2, ct:ct + 1], hs[:, ct, :w], ALU.mult, ALU.add,
                                     r=[('ps', ct % 2), abkey, hkey], w=[hkey])
                    self.norm_span(hs, hkey, w, ab[:, r_, 3, :], ab[:, r_, 4, :], abkey, u32, 'u32', ub, 'ub', scr)
                    self.moe_span(li, hs, hkey, w, u32, ub, ab[:, r_, 5, :], abkey, M)
                    S.dma(self.hT_span(t0, w), hs[:, :, :w], r=[hkey], w=[('hT', si)])
            S.barrier(keep=KEEP)


_ROPE = {}


def rope_consts(TL):
    if TL in _ROPE:
        return _ROPE[TL]
    f = np.float32
    t = np.arange(TL)
    pos = np.stack([t // 64, t % 64], axis=-1).astype(f)
    inv = (f(10000.0) ** (-np.arange(32, dtype=f) / f(32))).astype(f)
    ang = np.broadcast_to(pos[:, :, None, None] * inv, (TL, 2, 2, 32)).reshape(TL, 128).astype(f)
    cos = np.concatenate([np.ones((TC, 128), f), np.cos(ang).astype(f)], axis=0).T
    sin = np.concatenate([np.zeros((TC, 128), f), np.sin(ang).astype(f)], axis=0).T
    pm = np.zeros((128, 128), f)
    for a in range(2):
        for j in range(32):
            pm[a * 64 + 32 + j, a * 64 + j] = -1.0
            pm[a * 64 + j, a * 64 + 32 + j] = 1.0
    _ROPE[TL] = (np.ascontiguousarray(cos), np.ascontiguousarray(sin), pm)
    return _ROPE[TL]


def ssd_masks():
    f = np.float32
    k = np.arange(128)[:, None]
    i = np.arange(128)[None, :]
    trif = (k <= i).astype(f)
    trib = (k >= i).astype(f)
    mf = np.where(k <= i, 0.0, -30000.0).astype(f)
    mb = np.where(k >= i, 0.0, -30000.0).astype(f)
    return np.ascontiguousarray(np.stack([trif, trib, mf, mb], axis=1).reshape(128, 512))


def host_inputs(inp, b, TL, layers):
    f = np.float32
    d = {}
    d['x'] = np.ascontiguousarray(inp['x'][b, :TL])
    d['ctx'] = np.ascontiguousarray(inp['ctx'][b])
    d['cc'] = np.ascontiguousarray(np.stack([inp['c'][b], inp['c_ctx']]))
    d['ident'] = np.eye(128, dtype=f)
    d['mod_w'] = inp['mod_w']
    d['mod_b'] = inp['mod_b']
    ng = np.stack([inp['norm1_g'], inp['norm2_g']])
    d['ng'] = np.ascontiguousarray(ng.reshape(2, 4, KT, 128).transpose(3, 0, 1, 2).reshape(128, -1))
    d['attn_wqkv'] = inp['attn_w_qkv']
    d['attn_wo'] = inp['attn_w_o']
    d['attn_g'] = np.ascontiguousarray(np.stack([inp['attn_q_gain'], inp['attn_k_gain']], axis=1).reshape(4, 128).T)
    f = np.float32
    ki = 0
    def gl_p(a):
        sh = a.shape
        a = a.reshape((2, 32, 2, 64) + sh[3:])
        return np.moveaxis(a, (2, 3), (0, 1)).reshape((128, 2, 32) + sh[3:])
    ldt_b = np.broadcast_to(inp['s5_log_dt'][ki][:, :, None], (2, 64, 64))
    par = np.stack([gl_p(inp['s5_a_re'][ki]), gl_p(inp['s5_a_im'][ki]), gl_p(np.ascontiguousarray(ldt_b))], axis=1)
    d['s5_par'] = np.ascontiguousarray(par.reshape(128, 3 * 64)).astype(f)
    def blockdiag(a):
        o = np.zeros((128, 2, 32, 2, 16), f)
        o[:64, :, :, 0, :] = a[:64]
        o[64:, :, :, 1, :] = a[64:]
        return o.reshape(128, 2, 32, 32)
    Bre, Bim = blockdiag(gl_p(inp['s5_b_re'][ki])), blockdiag(gl_p(inp['s5_b_im'][ki]))
    d['s5_B'] = np.ascontiguousarray(np.stack([Bre, Bim], axis=1).reshape(128, -1))
    cre = np.swapaxes(inp['s5_c_re'][ki], 2, 3)
    cim = np.swapaxes(inp['s5_c_im'][ki], 2, 3)
    Cre, Cim = blockdiag(gl_p(np.ascontiguousarray(cre))), blockdiag(gl_p(np.ascontiguousarray(cim)))
    d['s5_C'] = np.ascontiguousarray(np.stack([Cre, Cim], axis=1).reshape(128, -1))
    d['s5_d'] = np.ascontiguousarray(inp['s5_d'][ki].reshape(32, 32).T)
    d['s5_wglu'] = inp['s5_w_glu']
    d['s5_bglu'] = np.ascontiguousarray(inp['s5_b_glu'][ki].reshape(16, 128).T)
    d['ssd_win'] = inp['ssd_w_in']
    d['ssd_wout'] = inp['ssd_w_out']
    d['ssd_cw'] = np.ascontiguousarray(inp['ssd_conv_w'][ki].reshape(5, 24, 128).transpose(2, 1, 0).reshape(128, 120))
    d['ssd_cb'] = np.ascontiguousarray(inp['ssd_conv_b'][ki].reshape(24, 128).T)
    d['ssd_dtb'] = np.ascontiguousarray(np.broadcast_to(inp['ssd_dt_bias'][ki].reshape(1, 64), (128, 64)))
    d['ssd_alog'] = np.ascontiguousarray(np.broadcast_to(inp['ssd_a_log'][ki].reshape(1, 64), (128, 64)))
    d['ssd_dh'] = np.ascontiguousarray(np.broadcast_to(inp['ssd_d'][ki].reshape(1, 32), (128, 32)))
    d['ssd_ng'] = np.ascontiguousarray(np.broadcast_to(inp['ssd_norm_g'][ki].reshape(1, 2048), (128, 2048)))
    d['ssd_msk'] = ssd_masks()
    cos, sin, pm = rope_consts(TL)
    d['rope_cos'], d['rope_sin'], d['rope_pm'] = cos, sin, pm
    d['moe_wr'] = np.ascontiguousarray(np.concatenate([inp['moe_w_group'], inp['moe_w_router']], axis=-1))
    d['moe_br'] = np.ascontiguousarray(np.concatenate([inp['moe_b_group'], inp['moe_b_router']], axis=-1))
    d['moe_wg'] = inp['moe_w_gate']
    d['moe_wu'] = inp['moe_w_up']
    d['moe_wd'] = inp['moe_w_down']
    return d


FULL_LAYERS = [(0, 'a', 0, True), (1, 's', 0, True), (2, 'd', 0, True), (3, 'a', 1, False)]


def kernel(**inputs):
    inp = {k: np.asarray(v) for k, v in inputs.items()}
    B, TL = inp['x'].shape[0], inp['x'].shape[1]
    prog = Prog(TL, FULL_LAYERS)
    in_maps = []
    for b in range(B):
        d = host_inputs(inp, b, TL, FULL_LAYERS)
        in_maps.append({k: d[k] for k in prog.in_names})
    res = run_bass_kernel_spmd(prog.nc, in_maps, core_ids=list(range(B)))
    return np.stack([r['out'] for r in res.results], axis=0)
```

```python
import numpy as np
from contextlib import ExitStack
import concourse.bass as bass
import concourse.mybir as mybir
from concourse.bass_utils import run_bass_kernel_spmd

F32, BF16 = mybir.dt.float32, mybir.dt.bfloat16
AF = mybir.ActivationFunctionType
ALU = mybir.AluOpType
AX = mybir.AxisListType

D = 1024
KT = 8
TC = 256
EPS = 1e-6
NE = 32
KEEP = ('wgb', 'wub', 'wdb', 'wqkvb', 'wob', 'wglub', 'winb', 'woutb')
HID = 256


class Sched:
    BLK = 8000
    NDMA = 12

    def __init__(self, nc, es):
        self.nc, self.es = nc, es
        self.eng = {'pe': nc.tensor, 'act': nc.scalar, 'dve': nc.vector,
                    'pool': nc.gpsimd, 'sp': nc.sync}
        self.cnt = {e: 0 for e in self.eng}
        self.sems = {e: [] for e in self.eng}
        self.seen = {e: {} for e in self.eng}
        self.lastw, self.readers = {}, {}
        self.dq = {}
        self.nsem = 0

    def _newsem(self, name):
        self.nsem += 1
        return self.es.enter_context(self.nc.semaphore(name))

    def _deps(self, r, w):
        toks = []
        for k in r:
            t = self.lastw.get(k)
            if t:
                toks.append(t)
        for k in w:
            t = self.lastw.get(k)
            if t:
                toks.append(t)
            toks.extend(self.readers.get(k, {}).values())
        return toks

    def _wait(self, e, toks, skip_pe=False):
        need = {}
        for (te, tb, sem, val) in toks:
            if skip_pe and te == 'pe':
                continue
            cur = need.get(te)
            if cur is None or (tb, val) > (cur[0], cur[1]):
                need[te] = (tb, val, sem)
        for te, (tb, val, sem) in need.items():
            s = self.seen[e].get(te)
            if s is not None and s >= (tb, val):
                continue
            self.eng[e].wait_ge(sem, val)
            self.seen[e][te] = (tb, val)

    def _reg(self, tok, r, w):
        for k in r:
            self.readers.setdefault(k, {})[tok[0]] = tok
        for k in w:
            self.lastw[k] = tok
            self.readers[k] = {}

    def op(self, e, fn, r=(), w=()):
        self._wait(e, self._deps(r, w), skip_pe=(e == 'pe'))
        ins = fn()
        k = self.cnt[e]
        b = k // self.BLK
        while len(self.sems[e]) <= b:
            self.sems[e].append(self._newsem(f"s_{e}_{len(self.sems[e])}"))
        sem, val = self.sems[e][b], k % self.BLK + 1
        ins.then_inc(sem, 1)
        self.cnt[e] += 1
        self._reg((e, b, sem, val), r, w)

    def dma(self, out, in_, r=(), w=(), q='sp', grp='m', **kw):
        key = (q, grp)
        if key not in self.dq:
            self.dq[key] = {'rr': 0, 'sems': [[self._newsem(f"d_{q}_{grp}_{i}"), 0]
                                             for i in range(self.NDMA)]}
        st = self.dq[key]
        i = st['rr']
        st['rr'] = (i + 1) % self.NDMA
        sem, n = st['sems'][i]
        te = ('dma', q, grp, i)
        toks = self._deps(r, w)
        if n > 0:
            toks.append((te, 0, sem, 16 * n))
        self._wait(q, toks)
        ins = self.eng[q].dma_start(out=out, in_=in_, **kw)
        ins.then_inc(sem, 16)
        st['sems'][i][1] = n + 1
        self._reg((te, 0, sem, 16 * (n + 1)), r, w)

    def barrier(self, keep=()):
        toks = []
        for e in ('pe', 'act', 'dve', 'pool'):
            k = self.cnt[e]
            if k > 0:
                b = (k - 1) // self.BLK
                toks.append((e, b, self.sems[e][b], (k - 1) % self.BLK + 1))
        for (q, grp), st in self.dq.items():
            if grp == 'async':
                continue
            for i, (sem, n) in enumerate(st['sems']):
                if n > 0:
                    toks.append((('dma', q, grp, i), 0, sem, 16 * n))
        for e in self.eng:
            self._wait(e, toks)
        lw = {k: v for k, v in self.lastw.items() if k[0] in keep}
        self.lastw, self.readers = lw, {}

    def finish(self):
        toks = []
        for (q, grp), st in self.dq.items():
            for i, (sem, n) in enumerate(st['sems']):
                if n > 0:
                    toks.append((('dma', q, grp, i), 0, sem, 16 * n))
        self._wait('sp', toks)


class Prog:
    def __init__(self, TL, layers, n_layers_w=4, dbg=None):
        self.TL, self.T = TL, TC + TL
        self.layers = layers
        self.dbg = dbg
        self.spans = [(0, TC, True)] + [(TC + i * 512, 512, False) for i in range(TL // 512)]
        self.nc = bass.Bass("TRN2", target_bir_lowering=False)
        self.build()

    def dram_in(self, name, shape, dt=F32):
        self.in_names.append(name)
        return self.nc.dram_tensor(name, list(shape), dt, kind="ExternalInput").ap()

    def dram_scr(self, name, shape, dt=F32):
        return self.nc.dram_tensor(name, list(shape), dt, kind="Internal").ap()

    def sb(self, es, name, shape, dt=F32):
        self._nsb = getattr(self, '_nsb', 0) + 1
        return es.enter_context(self.nc.sbuf_tensor(f"sb{self._nsb}_{name}", list(shape), dt))

    def mm(self, out, lhsT, rhs, start, stop, r, w):
        nc = self.nc
        self.S.op('pe', lambda: nc.tensor.matmul(out, lhsT=lhsT, rhs=rhs, start=start, stop=stop), r=r, w=w)

    def act(self, out, in_, func, r, w, **kw):
        nc = self.nc
        self.S.op('act', lambda: nc.scalar.activation(out=out, in_=in_, func=func, **kw), r=r, w=w)

    def tt(self, out, in0, in1, op, r, w, e='dve'):
        eng = self.S.eng[e]
        self.S.op(e, lambda: eng.tensor_tensor(out=out, in0=in0, in1=in1, op=op), r=r, w=w)

    def ts(self, out, in0, s1, op0, r, w, s2=None, op1=None, e='dve', **kw):
        eng = self.S.eng[e]
        if op1 is None:
            self.S.op(e, lambda: eng.tensor_scalar(out=out, in0=in0, scalar1=s1, scalar2=None, op0=op0, **kw), r=r, w=w)
        else:
            self.S.op(e, lambda: eng.tensor_scalar(out=out, in0=in0, scalar1=s1, scalar2=s2, op0=op0, op1=op1, **kw), r=r, w=w)

    def stt(self, out, in0, scalar, in1, op0, op1, r, w):
        nc = self.nc
        self.S.op('dve', lambda: nc.vector.scalar_tensor_tensor(out=out, in0=in0, scalar=scalar, in1=in1, op0=op0, op1=op1), r=r, w=w)

    def cp(self, out, in_, r, w, e='dve'):
        eng = self.S.eng[e]
        if e == 'act':
            self.S.op(e, lambda: eng.copy(out=out, in_=in_), r=r, w=w)
        else:
            self.S.op(e, lambda: eng.tensor_copy(out=out, in_=in_), r=r, w=w)

    def build(self):
        nc = self.nc
        self.in_names = []
        TL, T = self.TL, self.T
        I = self.I = {}
        I['x'] = self.dram_in('x', [TL, D])
        I['ctx'] = self.dram_in('ctx', [TC, D])
        I['cc'] = self.dram_in('cc', [2, D])
        I['ident'] = self.dram_in('ident', [128, 128])
        I['mod_w'] = self.dram_in('mod_w', [4, D, 6 * D])
        I['mod_b'] = self.dram_in('mod_b', [4, 6 * D])
        I['ng'] = self.dram_in('ng', [128, 2 * 4 * KT])
        I['moe_wr'] = self.dram_in('moe_wr', [4, D, 36])
        I['moe_br'] = self.dram_in('moe_br', [4, 36])
        I['moe_wg'] = self.dram_in('moe_wg', [4, NE, D, HID])
        I['moe_wu'] = self.dram_in('moe_wu', [4, NE, D, HID])
        I['moe_wd'] = self.dram_in('moe_wd', [4, NE, HID, D])
        I['attn_wqkv'] = self.dram_in('attn_wqkv', [2, D, 1536])
        I['attn_wo'] = self.dram_in('attn_wo', [2, D, D])
        I['attn_g'] = self.dram_in('attn_g', [128, 4])
        I['rope_cos'] = self.dram_in('rope_cos', [128, T])
        I['rope_sin'] = self.dram_in('rope_sin', [128, T])
        I['rope_pm'] = self.dram_in('rope_pm', [128, 128])
        self.wqkvb = self.dram_scr('wqkvb', [2, D, 1536], BF16)
        self.wob = self.dram_scr('wob', [2, D, D], BF16)
        self.QT = self.dram_scr('QT', [D, T], BF16)
        self.KT_ = self.dram_scr('KTs', [256, T], BF16)
        self.Vs = self.dram_scr('Vs', [T, 256], BF16)
        self.OT = self.dram_scr('OT', [D, T], BF16)
        I['s5_par'] = self.dram_in('s5_par', [128, 3 * 64])
        I['s5_B'] = self.dram_in('s5_B', [128, 2 * 64 * 32])
        I['s5_C'] = self.dram_in('s5_C', [128, 2 * 64 * 32])
        I['s5_d'] = self.dram_in('s5_d', [32, 32])
        I['s5_wglu'] = self.dram_in('s5_wglu', [1, D, 2 * D])
        I['s5_bglu'] = self.dram_in('s5_bglu', [128, 16])
        self.wglub = self.dram_scr('wglub', [1, D, 2 * D], BF16)
        self.UT = self.dram_scr('UT', [D, T], BF16)
        self.YF = self.dram_scr('YF', [D, T])
        self.YB = self.dram_scr('YB', [D, T])
        I['ssd_win'] = self.dram_in('ssd_win', [1, D, 5184])
        I['ssd_wout'] = self.dram_in('ssd_wout', [1, 2 * D, D])
        I['ssd_cw'] = self.dram_in('ssd_cw', [128, 24 * 5])
        I['ssd_cb'] = self.dram_in('ssd_cb', [128, 24])
        I['ssd_dtb'] = self.dram_in('ssd_dtb', [128, 64])
        I['ssd_alog'] = self.dram_in('ssd_alog', [128, 64])
        I['ssd_dh'] = self.dram_in('ssd_dh', [128, 32])
        I['ssd_ng'] = self.dram_in('ssd_ng', [128, 2 * D])
        I['ssd_msk'] = self.dram_in('ssd_msk', [128, 4 * 128])
        self.winb = self.dram_scr('winb', [1, D, 5184], BF16)
        self.woutb = self.dram_scr('woutb', [1, 2 * D, D], BF16)
        self.ZS = self.dram_scr('ZS', [T, 2 * D], BF16)
        self.XBC = self.dram_scr('XBC', [3072, T], BF16)
        self.DTs = self.dram_scr('DTs', [T, 64])
        self.XTK = self.dram_scr('XTK', [T, 2 * D], BF16)
        self.BTK = self.dram_scr('BTK', [T, 512], BF16)
        self.BCf = self.dram_scr('BCf', [1024, T], BF16)
        self.YD = self.dram_scr('YD', [2, T, 2 * D])
        self.out = nc.dram_tensor('out', [TL, D], F32, kind="ExternalOutput").ap()
        self.hT = self.dram_scr('hT', [D, T])
        self.wgb = self.dram_scr('wgb', [4, NE, 128, KT * HID], BF16)
        self.wub = self.dram_scr('wub', [4, NE, 128, KT * HID], BF16)
        self.wdb = self.dram_scr('wdb', [4, NE, 128, 2 * D], BF16)
        if self.dbg:
            self.dbg_out = nc.dram_tensor('dbg', [D, T], F32, kind="ExternalOutput").ap()

        with ExitStack() as es:
            self.S = S = Sched(nc, es)
            self.ident = self.sb(es, 'ident', [128, 128])
            self.identb = self.sb(es, 'identb', [128, 128], BF16)
            self.ones32 = self.sb(es, 'ones32', [128, 128])
            self.onesb = self.sb(es, 'onesb', [128, 128], BF16)
            self.mv = self.sb(es, 'mv', [128, 4, 96])
            self.ng = self.sb(es, 'ng', [128, 2, 4, KT])
            self.ps = [es.enter_context(nc.psum_tensor(f'ps{i}', [128, 512], F32)) for i in range(8)]
            S.dma(self.ident[:], I['ident'], w=['ident'])
            S.dma(self.ng[:].rearrange("p n l k -> p (n l k)"), I['ng'], w=['ng'])
            self.cp(self.identb[:], self.ident[:], r=['ident'], w=['identb'])
            S.op('dve', lambda: nc.vector.memset(self.ones32[:], 1.0), w=['ones32'])
            S.op('dve', lambda: nc.vector.memset(self.onesb[:], 1.0), w=['onesb'])
            S.barrier()
            self.async_casts(self.layers[0])
            self.phase_mod()
            self.phase_tin()
            for n_, L in enumerate(self.layers):
                if n_ + 1 < len(self.layers):
                    self.async_casts(self.layers[n_ + 1])
                self.layer(*L)
            self.phase_tout()
            S.finish()

    def cast_dram(self, dst, src, key):
        R_, C_ = src.shape
        a = R_ // 128
        sv = src.rearrange("(p a) n -> p a n", p=128)
        dv = dst.rearrange("(p a) n -> p a n", p=128)
        step = max(1, 2048 // C_)
        for a0 in range(0, a, step):
            a1 = min(a, a0 + step)
            self.S.dma(dv[:, a0:a1, :], sv[:, a0:a1, :], w=[key], q='pool', grp='async')

    def async_casts(self, L):
        I = self.I
        li, kind, ki, nctx = L
        if kind == 's':
            self.cast_dram(self.wglub[ki], I['s5_wglu'][ki], ('wglub', ki))
        if kind == 'd':
            self.cast_dram(self.winb[ki], I['ssd_win'][ki], ('winb', ki))
            self.cast_dram(self.woutb[ki], I['ssd_wout'][ki], ('woutb', ki))
        if kind == 'a':
            self.cast_dram(self.wqkvb[ki], I['attn_wqkv'][ki], ('wqkvb', ki))
            self.cast_dram(self.wob[ki], I['attn_wo'][ki], ('wob', ki))
        for e in range(NE):
            for (dst, src, key, k_) in ((self.wgb, I['moe_wg'], 'wgb', KT), (self.wub, I['moe_wu'], 'wub', KT), (self.wdb, I['moe_wd'], 'wdb', 2)):
                self.S.dma(dst[li, e].rearrange("p (k n) -> p k n", k=k_), src[li, e].rearrange("(k p) n -> p k n", p=128),
                           w=[(key, li, e)], q='pool', grp='async')

    def phase_mod(self):
        nc, S, I = self.nc, self.S, self.I
        with ExitStack() as es:
            ccT = self.sb(es, 'ccT', [128, KT, 2])
            scT = self.sb(es, 'scT', [128, KT, 2])
            wch = [self.sb(es, f'modw{i}', [128, KT, 512]) for i in range(2)]
            brow = self.sb(es, 'modb', [1, 6 * D])
            with nc.allow_non_contiguous_dma(reason="tiny transposed load of c"):
                for r_ in range(2):
                    S.dma(ccT[:, :, r_], I['cc'][r_].rearrange("(k p) -> p k", p=128), w=['ccT'])
            self.act(scT[:], ccT[:], AF.Silu, r=['ccT'], w=['scT'])
            ci = 0
            for (li, kind, ki, nctx) in self.layers:
                S.dma(brow[:], I['mod_b'][li:li + 1, :], w=['modb'])
                pb = self.ps[li % 2]
                for j in range(12):
                    wt = wch[ci % 2]
                    S.dma(wt[:], I['mod_w'][li][:, j * 512:(j + 1) * 512].rearrange("(k p) n -> p k n", p=128),
                          w=[('modw', ci % 2)])
                    for b4 in range(4):
                        blk = j * 4 + b4
                        o = pb[:, blk * 2:blk * 2 + 2]
                        for k in range(KT):
                            self.mm(o, wt[:, k, b4 * 128:(b4 + 1) * 128], scT[:, k, :], k == 0, False,
                                    r=[('modw', ci % 2), 'scT'], w=[('psm', li % 2)])
                        self.mm(o, brow[0:1, blk * 128:(blk + 1) * 128], self.ones32[0:1, 0:2], False, True,
                                r=['modb', 'ones32'], w=[('psm', li % 2)])
                    ci += 1
                self.cp(self.mv[:, li, :], pb[:, 0:96], r=[('psm', li % 2)], w=[('mv', li)])
        S.barrier(keep=KEEP)

    def hT_span(self, t0, w):
        return self.hT.rearrange("(k p) t -> p k t", p=128)[:, :, t0:t0 + w]

    def phase_tin(self):
        nc, S, I = self.nc, self.S, self.I
        with ExitStack() as es:
            xt = [self.sb(es, f'tin_x{i}', [128, D]) for i in range(2)]
            stg = [self.sb(es, f'tin_s{i}', [128, KT, 512]) for i in range(2)]
            ti = 0
            for si, (t0, w, isc) in enumerate(self.spans):
                st = stg[si % 2]
                for tt_ in range(w // 128):
                    tok = t0 + tt_ * 128
                    src = I['ctx'][tok:tok + 128, :] if isc else I['x'][tok - TC:tok - TC + 128, :]
                    xs = xt[ti % 2]
                    S.dma(xs[:], src, w=[('tinx', ti % 2)])
                    for half in range(2):
                        pb = self.ps[(ti * 2 + half) % 4]
                        for q in range(4):
                            k = half * 4 + q
                            S.op('pe', lambda: nc.tensor.transpose(out=pb[:, q * 128:(q + 1) * 128],
                                                                   in_=xs[:, k * 128:(k + 1) * 128], identity=self.ident[:]),
                                 r=[('tinx', ti % 2)], w=[('pst', (ti * 2 + half) % 4)])
                        self.cp(st[:, half * 4:half * 4 + 4, tt_ * 128:(tt_ + 1) * 128],
                                pb[:].rearrange("p (q t) -> p q t", q=4),
                                r=[('pst', (ti * 2 + half) % 4)], w=[('tins', si % 2)],
                                e='dve' if half == 0 else 'act')
                    ti += 1
                S.dma(self.hT_span(t0, w), st[:, :, :w], r=[('tins', si % 2)], w=[('hT', si)])
        S.barrier(keep=KEEP)

    def phase_tout(self):
        nc, S = self.nc, self.S
        if self.dbg:
            with ExitStack() as es:
                t_ = [self.sb(es, f'dbg{i}', [128, KT, 512]) for i in range(2)]
                for si, (t0, w, isc) in enumerate(self.spans):
                    S.dma(t_[si % 2][:, :, :w], self.hT_span(t0, w), w=[('dbgt', si % 2)])
                    S.dma(self.dbg_out.rearrange("(k p) t -> p k t", p=128)[:, :, t0:t0 + w], t_[si % 2][:, :, :w],
                          r=[('dbgt', si % 2)], w=[('dbgo', si)])
            S.barrier()
        with ExitStack() as es:
            hs = [self.sb(es, f'to_h{i}', [128, KT, 512]) for i in range(2)]
            ot = [self.sb(es, f'to_o{i}', [128, D]) for i in range(2)]
            ti = 0
            for si, (t0, w, isc) in enumerate(self.spans):
                if isc:
                    continue
                h = hs[si % 2]
                S.dma(h[:, :, :w], self.hT_span(t0, w), w=[('toh', si % 2)])
                for tt_ in range(w // 128):
                    o = ot[ti % 2]
                    for half in range(2):
                        pb = self.ps[(ti * 2 + half) % 4]
                        for q in range(4):
                            k = half * 4 + q
                            S.op('pe', lambda: nc.tensor.transpose(out=pb[:, q * 128:(q + 1) * 128],
                                                                   in_=h[:, k, tt_ * 128:(tt_ + 1) * 128], identity=self.ident[:]),
                                 r=[('toh', si % 2)], w=[('pst', (ti * 2 + half) % 4)])
                        self.cp(o[:, half * 512:(half + 1) * 512], pb[:],
                                r=[('pst', (ti * 2 + half) % 4)], w=[('too', ti % 2)],
                                e='dve' if half == 0 else 'act')
                    tok = t0 - TC + tt_ * 128
                    S.dma(self.out[tok:tok + 128, :], o[:], r=[('too', ti % 2)], w=[('out', ti)])
                    ti += 1

    def layer_scalars(self, es, li):
        S = self.S
        ab = self.sb(es, f'ab{li}', [128, 2, 6, KT])
        mvv = self.mv[:, li, :].rearrange("p (j k r) -> p r j k", j=6, k=KT, r=2)
        key = ('ab', li)
        for r_ in range(2):
            self.stt(ab[:, r_, 0, :], mvv[:, r_, 1, :], 1.0, self.ng[:, 0, li, :], ALU.add, ALU.mult, r=[('mv', li), 'ng'], w=[key])
            self.cp(ab[:, r_, 1, :], mvv[:, r_, 0, :], r=[('mv', li)], w=[key])
            self.cp(ab[:, r_, 2, :], mvv[:, r_, 2, :], r=[('mv', li)], w=[key])
            self.stt(ab[:, r_, 3, :], mvv[:, r_, 4, :], 1.0, self.ng[:, 1, li, :], ALU.add, ALU.mult, r=[('mv', li), 'ng'], w=[key])
            self.cp(ab[:, r_, 4, :], mvv[:, r_, 3, :], r=[('mv', li)], w=[key])
            self.cp(ab[:, r_, 5, :], mvv[:, r_, 5, :], r=[('mv', li)], w=[key])
        return ab

    def norm_span(self, hs, hkey, w, A, B, abkey, u32, u32key, ub, ubkey, scr):
        nc, S = self.nc, self.S
        sq, rs = scr['sq'], scr['rs']
        pss = self.ps[7]
        self.act(sq[:, :, :w], hs[:, :, :w], AF.Square, r=[hkey], w=['sq'])
        for k in range(KT):
            self.mm(pss[:, :w], self.onesb[:], sq[:, k, :w], k == 0, k == KT - 1, r=['sq', 'onesb'], w=[('ps', 7)])
        self.act(rs[:, :w], pss[:, :w], AF.Sqrt, r=[('ps', 7), 'epsb'], w=['rs'], scale=1.0 / D, bias=self.epsb[:, 0:1])
        S.op('dve', lambda: nc.vector.reciprocal(out=rs[:, :w], in_=rs[:, :w]), r=['rs'], w=['rs'])
        xn = scr['xn']
        self.tt(xn[:, :, :w], hs[:, :, :w], rs[:, :w].unsqueeze(1).to_broadcast([128, KT, w]), ALU.mult, r=[hkey, 'rs'],
                w=[('acc', k) for k in range(KT)])
        for k in range(KT):
            dst = u32[:, k, :w] if u32 is not None else ub[:, k, :w]
            dkey = u32key if u32 is not None else ubkey
            self.act(dst, xn[:, k, :w], AF.Identity, r=[('acc', k), abkey], w=[dkey], scale=A[:, k:k + 1], bias=B[:, k:k + 1])
        if u32 is not None:
            self.cp(ub[:, :, :w], u32[:, :, :w], r=[u32key], w=[ubkey], e='pool')

    def moe_span(self, li, hs, hkey, w, u32, ub, G5, abkey, M):
        nc, S = self.nc, self.S
        ps = self.ps
        ntt = w // 128
        cw, lgs, sm = M['cw'], M['lgs'], M['sm']
        for tt_ in range(ntt):
            pl = ps[6]
            for k in range(KT):
                self.mm(pl[:, 0:36], u32[:, k, tt_ * 128:(tt_ + 1) * 128], M['wr'][:, k, :], k == 0, False,
                        r=['u32', 'wr'], w=[('ps', 6)])
            self.mm(pl[:, 0:36], self.ones32[0:1, :], M['br'][0:1, :], False, True, r=['ones32', 'br'], w=[('ps', 6)])
            L = lgs
            self.cp(L[:, 0:36], pl[:, 0:36], r=[('ps', 6)], w=['lgs'])
            rk, wk = ['lgs', 'sm'], ['sm']
            S.op('dve', lambda: nc.vector.tensor_reduce(out=sm[:, 0:1], in_=L[:, 0:4], op=ALU.max, axis=AX.X), r=rk, w=wk)
            self.ts(sm[:, 1:2], sm[:, 0:1], -1.0, ALU.mult, r=rk, w=wk)
            self.ts(L[:, 36:40], L[:, 0:4], sm[:, 0:1], ALU.is_equal, r=rk, w=['lgs'])
            self.act(L[:, 40:44], L[:, 0:4], AF.Exp, r=rk, w=['lgs', 'sm'], bias=sm[:, 1:2], scale=1.0, accum_out=sm[:, 2:3])
            S.op('dve', lambda: nc.vector.reciprocal(out=sm[:, 3:4], in_=sm[:, 2:3]), r=rk, w=wk)
            self.ts(L[:, 44:52], L[:, 4:12], L[:, 36:37], ALU.mult, r=rk, w=['lgs'])
            for g in range(1, 4):
                self.stt(L[:, 44:52], L[:, 4 + 8 * g:12 + 8 * g], L[:, 36 + g:37 + g], L[:, 44:52], ALU.mult, ALU.add, r=rk, w=['lgs'])
            S.op('dve', lambda: nc.vector.tensor_reduce(out=sm[:, 4:5], in_=L[:, 44:52], op=ALU.max, axis=AX.X), r=rk, w=wk)
            self.ts(L[:, 52:60], L[:, 44:52], sm[:, 4:5], ALU.is_equal, r=rk, w=['lgs'])
            self.stt(L[:, 60:68], L[:, 52:60], -1e30, L[:, 44:52], ALU.mult, ALU.add, r=rk, w=['lgs'])
            S.op('dve', lambda: nc.vector.tensor_reduce(out=sm[:, 5:6], in_=L[:, 60:68], op=ALU.max, axis=AX.X), r=rk, w=wk)
            self.ts(L[:, 68:76], L[:, 60:68], sm[:, 5:6], ALU.is_equal, r=rk, w=['lgs'])
            self.tt(sm[:, 6:7], sm[:, 5:6], sm[:, 4:5], ALU.subtract, r=rk, w=wk)
            self.act(sm[:, 7:8], sm[:, 6:7], AF.Exp, r=rk, w=wk)
            self.ts(sm[:, 8:9], sm[:, 7:8], 1.0, ALU.add, r=rk, w=wk)
            S.op('dve', lambda: nc.vector.reciprocal(out=sm[:, 8:9], in_=sm[:, 8:9]), r=rk, w=wk)
            self.tt(sm[:, 9:10], sm[:, 7:8], sm[:, 8:9], ALU.mult, r=rk, w=wk)
            self.tt(sm[:, 10:11], sm[:, 8:9], sm[:, 3:4], ALU.mult, r=rk, w=wk)
            self.tt(sm[:, 11:12], sm[:, 9:10], sm[:, 3:4], ALU.mult, r=rk, w=wk)
            self.ts(L[:, 76:84], L[:, 52:60], sm[:, 10:11], ALU.mult, r=rk, w=['lgs'])
            self.stt(L[:, 76:84], L[:, 68:76], sm[:, 11:12], L[:, 76:84], ALU.mult, ALU.add, r=rk, w=['lgs'])
            for g in range(4):
                self.ts(cw[:, tt_, g * 8:(g + 1) * 8], L[:, 76:84], L[:, 36 + g:37 + g], ALU.mult, r=rk, w=['cw'])
        cwT, cw32 = M['cwT'], M['cw32']
        for tt_ in range(ntt):
            S.op('pe', lambda: nc.tensor.transpose(out=ps[5][0:32, tt_ * 128:(tt_ + 1) * 128], in_=cw[:, tt_, :], identity=self.ident[:]),
                 r=['cw', 'ident'], w=[('ps', 5)])
        self.cp(cw32[:, 0, :w], ps[5][0:32, :w], r=[('ps', 5)], w=['cw32'])
        self.cp(cwT[:, 0, :w], cw32[:, 0, :w], r=['cw32'], w=['cwT'])
        self.cp(cw32[:, 1, :w], cwT[:, 0, :w], r=['cwT'], w=['cw32'])
        self.tt(cw32[:, 1, :w], cw32[:, 0, :w], cw32[:, 1, :w], ALU.subtract, r=['cw32'], w=['cw32'])
        self.cp(cwT[:, 1, :w], cw32[:, 1, :w], r=['cw32'], w=['cwT'])
        wgs, wus, wds = M['wg'], M['wu'], M['wd']
        acc = M['acc']

        def load_w(e):
            sl = e % 3
            S.dma(wgs[sl][:].rearrange("p k n -> p (k n)"), self.wgb[li, e], r=[('wgb', li, e)], w=[('wg', sl)])
            S.dma(wus[sl][:].rearrange("p k n -> p (k n)"), self.wub[li, e], r=[('wub', li, e)], w=[('wu', sl)])
            S.dma(wds[sl][:].rearrange("p k n -> p (k n)"), self.wdb[li, e], r=[('wdb', li, e)], w=[('wd', sl)])
        def down(e):
            sl = e % 3
            for ct in range(KT):
                pd = ps[6 + ct % 2]
                for j in range(2):
                    self.mm(pd[:, :w], wds[sl][:, j, ct * 128:(ct + 1) * 128], M['hid'][sl][:, j, :w], j == 0, j == 1,
                            r=[('wd', sl), ('hid', sl, j)], w=[('ps', 6 + ct % 2)])
                if e == 0:
                    self.cp(acc[:, ct, :w], pd[:, :w], r=[('ps', 6 + ct % 2)], w=[('acc', ct)], e='act')
                else:
                    dn = M['dtmp'][ct % 4]
                    self.cp(dn[:, :w], pd[:, :w], r=[('ps', 6 + ct % 2)], w=[('dtmp', ct % 4)], e='act')
                    self.tt(acc[:, ct, :w], acc[:, ct, :w], dn[:, :w], ALU.add, r=[('dtmp', ct % 4), ('acc', ct)], w=[('acc', ct)], e='pool')
        load_w(0)
        for e in range(NE):
            sl = e % 3
            if e + 1 < NE:
                load_w(e + 1)
            pc = ps[4 + e % 2]
            sel = self.identb[0:32, e:e + 1].to_broadcast([32, 128])
            self.mm(pc[:, :w], sel, cwT[:, 0, :w], True, False, r=['cwT', 'identb'], w=[('ps', 4 + e % 2)])
            self.mm(pc[:, :w], sel, cwT[:, 1, :w], False, True, r=['cwT', 'identb'], w=[('ps', 4 + e % 2)])
            for j in range(2):
                b0 = 2 * ((e * 2 + j) % 2)
                for k in range(KT):
                    for (wt, wkey, bank) in ((wgs[sl], ('wg', sl), b0), (wus[sl], ('wu', sl), b0 + 1)):
                        self.mm(ps[bank][:, :w], wt[:, k, j * 128:(j + 1) * 128], ub[:, k, :w], k == 0, k == KT - 1,
                                r=[wkey, 'ub'], w=[('ps', bank)])
                sg, t2 = M['sg'][j], M['t2'][j]
                hid = M['hid'][sl]
                self.act(sg[:, :w], ps[b0][:, :w], AF.Silu, r=[('ps', b0)], w=[('sg', j)])
                self.tt(t2[:, :w], sg[:, :w], ps[b0 + 1][:, :w], ALU.mult, r=[('sg', j), ('ps', b0 + 1)], w=[('t2', j)])
                self.tt(hid[:, j, :w], t2[:, :w], pc[:, :w], ALU.mult, r=[('t2', j), ('ps', 4 + e % 2)], w=[('hid', sl, j)])
            if e > 0:
                down(e - 1)
        down(NE - 1)
        for ct in range(KT):
            self.stt(hs[:, ct, :w], acc[:, ct, :w], G5[:, ct:ct + 1], hs[:, ct, :w], ALU.mult, ALU.add,
                     r=[('acc', ct), hkey, abkey], w=[hkey])

    def moe_alloc(self, es, li):
        S, I = self.S, self.I
        M = {}
        M['wr'] = self.sb(es, 'moe_wr', [128, KT, 36])
        M['br'] = self.sb(es, 'moe_br', [1, 36])
        M['cw'] = self.sb(es, 'moe_cw', [128, 4, NE])
        M['lgs'] = self.sb(es, 'moe_lgs', [128, 96])
        M['cwT'] = self.sb(es, 'moe_cwT', [32, 2, 512], BF16)
        M['cw32'] = self.sb(es, 'moe_cw32', [32, 2, 512])
        M['sm'] = self.sb(es, 'moe_sm', [128, 16])
        M['wg'] = [self.sb(es, f'moe_wg{i}', [128, KT, HID], BF16) for i in range(3)]
        M['wu'] = [self.sb(es, f'moe_wu{i}', [128, KT, HID], BF16) for i in range(3)]
        M['wd'] = [self.sb(es, f'moe_wd{i}', [128, 2, D], BF16) for i in range(3)]
        M['sg'] = [self.sb(es, f'moe_sg{i}', [128, 512]) for i in range(2)]
        M['t2'] = [self.sb(es, f'moe_t2{i}', [128, 512]) for i in range(2)]
        M['hid'] = [self.sb(es, f'moe_hid{i}', [128, 2, 512], BF16) for i in range(3)]
        M['acc'] = self.sb(es, 'moe_acc', [128, KT, 512])
        M['dtmp'] = [self.sb(es, f'moe_dtmp{i}', [128, 512]) for i in range(4)]
        S.dma(M['wr'][:], I['moe_wr'][li].rearrange("(k p) n -> p k n", p=128), w=['wr'])
        S.dma(M['br'][:], I['moe_br'][li:li + 1, :], w=['br'])
        return M

    def norm_alloc(self, es):
        scr = {'sq': self.sb(es, 'n_sq', [128, KT, 512], BF16), 'rs': self.sb(es, 'n_rs', [128, 512])}
        self.epsb = self.sb(es, 'epsb', [128, 1])
        nc = self.nc
        self.S.op('dve', lambda: nc.vector.memset(self.epsb[:], EPS), w=['epsb'])
        return scr

    def attn_a1(self, li, ki, ab, abkey):
        nc, S, I = self.nc, self.S, self.I
        ps = self.ps
        with ExitStack() as es:
            scr = self.norm_alloc(es)
            scr['xn'] = self.sb(es, 'a1_xn', [128, KT, 512])
            wq = self.sb(es, 'a1_wqkv', [128, KT, 1536], BF16)
            pmb = self.sb(es, 'a1_pmb', [128, 128], BF16)
            pm32 = self.sb(es, 'a1_pm32', [128, 128])
            gq = self.sb(es, 'a1_g', [128, 4])
            hsb = [self.sb(es, f'a1_hs{i}', [128, KT, 512]) for i in range(2)]
            ub = self.sb(es, 'a1_ub', [128, KT, 512], BF16)
            cs = [self.sb(es, f'a1_cos{i}', [128, 512]) for i in range(2)]
            sn = [self.sb(es, f'a1_sin{i}', [128, 512]) for i in range(2)]
            sq = [self.sb(es, f'a1_sq{i}', [128, 512], BF16) for i in range(2)]
            rsq = [self.sb(es, f'a1_rs{i}', [128, 512]) for i in range(2)]
            yb = [self.sb(es, f'a1_yb{i}', [128, 512], BF16) for i in range(2)]
            t1 = [self.sb(es, f'a1_t1{i}', [128, 512]) for i in range(2)]
            t2 = [self.sb(es, f'a1_t2{i}', [128, 512]) for i in range(2)]
            qst = [self.sb(es, f'a1_qst{i}', [128, 10, 512], BF16) for i in range(2)]
            vst = [self.sb(es, f'a1_vst{i}', [128, 4, 256], BF16) for i in range(2)]
            S.dma(wq[:], self.wqkvb[ki].rearrange("(k p) n -> p k n", p=128), r=[('wqkvb', ki)], w=['wq'])
            S.dma(pm32[:], I['rope_pm'], w=['pm32'])
            S.dma(gq[:], I['attn_g'], w=['gq'])
            self.cp(pmb[:], pm32[:], r=['pm32'], w=['pmb'])
            self.ts(gq[:, 2 * ki:2 * ki + 1], gq[:, 2 * ki:2 * ki + 1], 128.0 ** -0.5, ALU.mult, r=['gq'], w=['gq'])
            for si, (t0, w, isc) in enumerate(self.spans):
                hs, hkey = hsb[si % 2], ('hs', si % 2)
                r_ = 1 if isc else 0
                S.dma(hs[:, :, :w], self.hT_span(t0, w), r=[('hT', si)], w=[hkey])
                S.dma(cs[si % 2][:, :w], I['rope_cos'][:, t0:t0 + w], w=[('cos', si % 2)])
                S.dma(sn[si % 2][:, :w], I['rope_sin'][:, t0:t0 + w], w=[('sin', si % 2)])
                self.norm_span(hs, hkey, w, ab[:, r_, 0, :], ab[:, r_, 1, :], abkey, None, None, ub, 'ub', scr)
                qs_ = qst[si % 2]
                for c in range(10):
                    b = c % 2
                    pq, pss_, pr = ps[b], ps[2 + b], ps[4 + b]
                    for k in range(KT):
                        self.mm(pq[:, :w], wq[:, k, c * 128:(c + 1) * 128], ub[:, k, :w], k == 0, k == KT - 1,
                                r=['wq', 'ub'], w=[('ps', b)])
                    self.act(sq[b][:, :w], pq[:, :w], AF.Square, r=[('ps', b)], w=[('sq2', b)])
                    self.mm(pss_[:, :w], self.onesb[:], sq[b][:, :w], True, True, r=[('sq2', b), 'onesb'], w=[('ps', 2 + b)])
                    self.act(rsq[b][:, :w], pss_[:, :w], AF.Sqrt, r=[('ps', 2 + b), 'epsb'], w=[('rsq', b)],
                             scale=1.0 / 128, bias=self.epsb[:, 0:1])
                    S.op('dve', lambda: nc.vector.reciprocal(out=rsq[b][:, :w], in_=rsq[b][:, :w]), r=[('rsq', b)], w=[('rsq', b)])
                    gcol = 2 * ki + (0 if c < 8 else 1)
                    self.stt(yb[b][:, :w], pq[:, :w], gq[:, gcol:gcol + 1], rsq[b][:, :w], ALU.mult, ALU.mult,
                             r=[('ps', b), 'gq', ('rsq', b)], w=[('yb', b)])
                    self.mm(pr[:, :w], pmb[:], yb[b][:, :w], True, True, r=['pmb', ('yb', b)], w=[('ps', 4 + b)])
                    self.tt(t1[b][:, :w], yb[b][:, :w], cs[si % 2][:, :w], ALU.mult, r=[('yb', b), ('cos', si % 2)], w=[('t1', b)])
                    self.tt(t2[b][:, :w], pr[:, :w], sn[si % 2][:, :w], ALU.mult, r=[('ps', 4 + b), ('sin', si % 2)], w=[('t2', b)])
                    self.tt(qs_[:, c, :w], t1[b][:, :w], t2[b][:, :w], ALU.add, r=[('t1', b), ('t2', b)], w=[('qst', si % 2)])
                S.dma(self.QT.rearrange("(h p) t -> p h t", p=128)[:, :, t0:t0 + w], qs_[:, 0:8, :w], r=[('qst', si % 2)], w=[('QT', si)])
                S.dma(self.KT_.rearrange("(h p) t -> p h t", p=128)[:, :, t0:t0 + w], qs_[:, 8:10, :w], r=[('qst', si % 2)], w=[('KT', si)])
                vs_ = vst[si % 2]
                for tt_ in range(w // 128):
                    for k in range(KT):
                        self.mm(ps[6][:, 0:256], ub[:, k, tt_ * 128:(tt_ + 1) * 128], wq[:, k, 1280:1536], k == 0, k == KT - 1,
                                r=['wq', 'ub'], w=[('ps', 6)])
                    self.cp(vs_[:, tt_, :], ps[6][:, 0:256], r=[('ps', 6)], w=[('vst', si % 2)], e='act')
                S.dma(self.Vs[t0:t0 + w, :].rearrange("(a p) n -> p a n", p=128), vs_[:, :w // 128, :], r=[('vst', si % 2)], w=[('Vs', si)])
        S.barrier(keep=KEEP)

    def attn_a2(self, need_ctx):
        nc, S = self.nc, self.S
        ps = self.ps
        T = self.T
        NB = T // 128
        with ExitStack() as es:
            kT = self.sb(es, 'a2_kT', [128, 2, T], BF16)
            vt = self.sb(es, 'a2_v', [128, NB, 256], BF16)
            qsb = [self.sb(es, f'a2_q{i}', [128, 512], BF16) for i in range(2)]
            pt = [self.sb(es, f'a2_p{i}', [128, 512], BF16) for i in range(4)]
            pt2 = [self.sb(es, f'a2_pp{i}', [128, 512], BF16) for i in range(2)]
            pt4 = [self.sb(es, f'a2_pq{i}', [128, 512], BF16) for i in range(2)]
            rl = [self.sb(es, f'a2_rl{i}', [128, 512]) for i in range(2)]
            ost = [self.sb(es, f'a2_o{i}', [128, 512], BF16) for i in range(2)]
            S.dma(kT[:], self.KT_.rearrange("(h p) t -> p h t", p=128), w=['kT'])
            for a0 in range(0, NB, 8):
                a1 = min(NB, a0 + 8)
                S.dma(vt[:, a0:a1, :], self.Vs[a0 * 128:a1 * 128, :].rearrange("(a p) n -> p a n", p=128), w=['vt'])
            it = 0
            ie = 0
            for g in range(2):
                for hq in range(4):
                    h = g * 4 + hq
                    for si, (t0, w, isc) in enumerate(self.spans):
                        if isc and not need_ctx:
                            continue
                        nkb = 2 if isc else NB
                        q_, qk = qsb[it % 2], ('q', it % 2)
                        S.dma(q_[:, :w], self.QT[h * 128:(h + 1) * 128, t0:t0 + w], w=[qk])
                        po, pl = ps[4 + it % 2], ps[6 + it % 2]
                        base = ie

                        def stA(kb):
                            sb_ = (base + kb) % 4
                            self.mm(ps[sb_][:, :w], kT[:, g, kb * 128:(kb + 1) * 128], q_[:, :w], True, True,
                                    r=['kT', qk], w=[('ps', sb_)])

                        def stB(kb):
                            sb_ = (base + kb) % 4
                            self.act(pt[sb_][:, :w], ps[sb_][:, :w], AF.Exp, r=[('ps', sb_)], w=[('pt', sb_)])

                        def stC(kb):
                            sb_ = (base + kb) % 4
                            self.mm(po[:, :w], vt[:, kb, g * 128:(g + 1) * 128], pt[sb_][:, :w], kb == 0, kb == nkb - 1,
                                    r=['vt', ('pt', sb_)], w=[('ps', 4 + it % 2)])
                            if kb % 2 == 1:
                                sp_ = (base + kb - 1) % 4
                                pi = (kb // 2) % 2
                                p2 = pt2[pi]
                                self.tt(p2[:, :w], pt[sp_][:, :w], pt[sb_][:, :w], ALU.add,
                                        r=[('pt', sp_), ('pt', sb_)], w=[('pt2', pi)])
                                last = (kb == nkb - 1)
                                if pi == 1:
                                    p4 = pt4[(kb // 4) % 2]
                                    self.tt(p4[:, :w], pt2[0][:, :w], pt2[1][:, :w], ALU.add,
                                            r=[('pt2', 0), ('pt2', 1)], w=[('pt4', (kb // 4) % 2)])
                                    self.mm(pl[:, :w], self.onesb[:], p4[:, :w], kb == 3, last,
                                            r=['onesb', ('pt4', (kb // 4) % 2)], w=[('ps', 6 + it % 2)])
                                elif last:
                                    self.mm(pl[:, :w], self.onesb[:], p2[:, :w], kb == 1, True,
                                            r=['onesb', ('pt2', pi)], w=[('ps', 6 + it % 2)])
                        PF = 2
                        for kb in range(min(PF, nkb)):
                            stA(kb)
                            stB(kb)
                        for kb in range(nkb):
                            if kb + PF < nkb:
                                stA(kb + PF)
                                stB(kb + PF)
                            stC(kb)
                        ie += nkb
                        S.op('dve', lambda: nc.vector.reciprocal(out=rl[it % 2][:, :w], in_=pl[:, :w]),
                             r=[('ps', 6 + it % 2)], w=[('rl', it % 2)])
                        self.tt(ost[it % 2][:, :w], po[:, :w], rl[it % 2][:, :w], ALU.mult,
                                r=[('ps', 4 + it % 2), ('rl', it % 2)], w=[('ost', it % 2)])
                        S.dma(self.OT[h * 128:(h + 1) * 128, t0:t0 + w], ost[it % 2][:, :w], r=[('ost', it % 2)], w=[('OT', h, si)])
                        it += 1
        S.barrier(keep=KEEP)

    def s5_s1(self, li, ab, abkey):
        S = self.S
        with ExitStack() as es:
            scr = self.norm_alloc(es)
            scr['xn'] = self.sb(es, 's1_xn', [128, KT, 512])
            hsb = [self.sb(es, f's1_hs{i}', [128, KT, 512]) for i in range(2)]
            ubb = [self.sb(es, f's1_ub{i}', [128, KT, 512], BF16) for i in range(2)]
            for si, (t0, w, isc) in enumerate(self.spans):
                hs, hkey = hsb[si % 2], ('hs', si % 2)
                r_ = 1 if isc else 0
                S.dma(hs[:, :, :w], self.hT_span(t0, w), r=[('hT', si)], w=[hkey])
                self.norm_span(hs, hkey, w, ab[:, r_, 0, :], ab[:, r_, 1, :], abkey, None, None, ubb[si % 2], ('ub', si % 2), scr)
                S.dma(self.UT.rearrange("(k p) t -> p k t", p=128)[:, :, t0:t0 + w], ubb[si % 2][:, :, :w],
                      r=[('ub', si % 2)], w=[('UT', si)])
        S.barrier(keep=KEEP)

    def s5_s2(self, ki):
        nc, S, I = self.nc, self.S, self.I
        ps = self.ps
        T = self.T
        PI = float(np.pi)
        with ExitStack() as es:
            es0 = ExitStack()
            par = self.sb(es, 's5par', [128, 3, 64])
            d32 = self.sb(es, 's5d', [32, 32])
            dsk = self.sb(es, 's5dsk', [32, 32, 32], BF16)
            BT = self.sb(es, 's5BT', [32, 64, 2, 128], BF16)
            Cw = self.sb(es, 's5Cw', [128, 3, 64, 32], BF16)
            v = {n: self.sb(es, 's5_' + n, [128, 64]) for n in
                 ('dt', 'mag', 'th', 't', 's', 'y', 'm', 'cos', 'sin', 'abr', 'abi', 'den', 'nr', 'cre', 'cim', 'u1', 'u2')}
            vi = self.sb(es, 's5_ti', [128, 64], mybir.dt.int32)
            PL = self.sb(es, 's5PL', [128, 10, 2, 64])
            nGs = self.sb(es, 's5nGs', [128, 2, 64])
            Bt = self.sb(es0, 's5B', [128, 2, 64, 32])
            Ct = self.sb(es0, 's5C', [128, 2, 64, 32])
            bb = self.sb(es0, 's5bb', [128, 2, 64, 32])
            tmpb = self.sb(es0, 's5tmpb', [128, 64, 32])
            with nc.allow_non_contiguous_dma(reason="small parameter tables"):
                S.dma(par[:].rearrange("p a b -> p (a b)"), I['s5_par'], w=['par'])
                S.dma(Bt[:].rearrange("p a b c -> p (a b c)"), I['s5_B'], w=['Bt'])
                S.dma(Ct[:].rearrange("p a b c -> p (a b c)"), I['s5_C'], w=['Ct'])
                S.dma(d32[:], I['s5_d'], w=['d32'])
            K_ = ['dk']

            def TS(o, i, s1, op0, s2=None, op1=None):
                self.ts(o, i, s1, op0, r=K_ + ['par'], w=K_, s2=s2, op1=op1)

            def TT(o, a, b, op):
                self.tt(o, a, b, op, r=K_ + ['par'], w=K_)
            are, aim, ldt = par[:, 0, :], par[:, 1, :], par[:, 2, :]
            self.act(v['dt'][:], ldt, AF.Exp, r=['par'], w=K_)
            TT(v['t'][:], are, v['dt'][:], ALU.mult)
            self.act(v['mag'][:], v['t'][:], AF.Exp, r=K_, w=K_)
            TT(v['th'][:], aim, v['dt'][:], ALU.mult)

            def sin_of(dst, shift):
                TS(v['t'][:], v['th'][:], 1.0 / (2 * PI), ALU.mult, s2=shift / (2 * PI), op1=ALU.add)
                self.cp(vi[:], v['t'][:], r=K_, w=K_)
                self.cp(v['t'][:], vi[:], r=K_, w=K_)
                TS(v['s'][:], v['th'][:], shift, ALU.add)
                self.stt(v['y'][:], v['t'][:], -2 * PI, v['s'][:], ALU.mult, ALU.add, r=K_, w=K_)
                TS(v['m'][:], v['y'][:], PI, ALU.is_gt)
                self.stt(v['y'][:], v['m'][:], -2 * PI, v['y'][:], ALU.mult, ALU.add, r=K_, w=K_)
                TS(v['m'][:], v['y'][:], -PI, ALU.is_lt)
                self.stt(v['y'][:], v['m'][:], 2 * PI, v['y'][:], ALU.mult, ALU.add, r=K_, w=K_)
                self.act(dst, v['y'][:], AF.Sin, r=K_, w=K_)
            sin_of(v['sin'][:], 0.0)
            sin_of(v['cos'][:], PI / 2)
            TT(v['abr'][:], v['mag'][:], v['cos'][:], ALU.mult)
            TT(v['abi'][:], v['mag'][:], v['sin'][:], ALU.mult)
            TT(v['den'][:], are, are, ALU.mult)
            TT(v['u1'][:], aim, aim, ALU.mult)
            TT(v['den'][:], v['den'][:], v['u1'][:], ALU.add)
            S.op('dve', lambda: nc.vector.reciprocal(out=v['den'][:], in_=v['den'][:]), r=K_, w=K_)
            TS(v['nr'][:], v['abr'][:], -1.0, ALU.add)
            TT(v['u1'][:], v['nr'][:], are, ALU.mult)
            TT(v['u2'][:], v['abi'][:], aim, ALU.mult)
            TT(v['u1'][:], v['u1'][:], v['u2'][:], ALU.add)
            TT(v['cre'][:], v['u1'][:], v['den'][:], ALU.mult)
            TT(v['u1'][:], v['abi'][:], are, ALU.mult)
            TT(v['u2'][:], v['nr'][:], aim, ALU.mult)
            TT(v['u1'][:], v['u1'][:], v['u2'][:], ALU.subtract)
            TT(v['cim'][:], v['u1'][:], v['den'][:], ALU.mult)
            creb = v['cre'][:].unsqueeze(2).to_broadcast([128, 64, 32])
            cimb = v['cim'][:].unsqueeze(2).to_broadcast([128, 64, 32])
            self.tt(bb[:, 0], Bt[:, 0], creb, ALU.mult, r=K_ + ['Bt'], w=['bb'])
            self.tt(tmpb[:], Bt[:, 1], cimb, ALU.mult, r=K_ + ['Bt'], w=['tmpb'])
            self.tt(bb[:, 0], bb[:, 0], tmpb[:], ALU.subtract, r=['bb', 'tmpb'], w=['bb'])
            self.tt(bb[:, 1], Bt[:, 1], creb, ALU.mult, r=K_ + ['Bt'], w=['bb'])
            self.tt(tmpb[:], Bt[:, 0], cimb, ALU.mult, r=K_ + ['Bt', 'bb'], w=['tmpb'])
            self.tt(bb[:, 1], bb[:, 1], tmpb[:], ALU.add, r=['bb', 'tmpb'], w=['bb'])
            n_ = 0
            for dj in range(64):
                for ri in range(2):
                    bank = 6 + (n_ // 4) % 2
                    S.op('pe', lambda: nc.tensor.transpose(out=ps[bank][0:32, (n_ % 4) * 128:(n_ % 4 + 1) * 128],
                                                           in_=bb[:, ri, dj, :], identity=self.ident[:]),
                         r=['bb', 'ident'], w=[('ps', bank)])
                    if n_ % 4 == 3:
                        dj0 = dj - 1
                        self.cp(BT[:, dj0:dj0 + 2, :, :].rearrange("p a b c -> p (a b c)"), ps[bank][0:32, :],
                                r=[('ps', bank)], w=['BT'], e='act')
                    n_ += 1
            self.cp(Cw[:, 0], Ct[:, 0], r=['Ct'], w=['Cw'])
            self.ts(Cw[:, 1], Ct[:, 0], -1.0, ALU.mult, r=['Ct'], w=['Cw'])
            self.ts(Cw[:, 2], Ct[:, 1], -1.0, ALU.mult, r=['Ct'], w=['Cw'])
            self.tt(dsk[:], self.ident[0:32, 0:32].unsqueeze(1).to_broadcast([32, 32, 32]),
                    d32[:].unsqueeze(2).to_broadcast([32, 32, 32]), ALU.mult, r=['ident', 'd32'], w=['dsk'])
            self.cp(PL[:, 0, 0, :], v['cos'][:], r=K_, w=['PL'])
            self.cp(PL[:, 0, 1, :], v['sin'][:], r=K_, w=['PL'])
            for l in range(9):
                c_, s_ = PL[:, l, 0, :], PL[:, l, 1, :]
                self.tt(v['u1'][:], c_, c_, ALU.mult, r=['PL'] + K_, w=K_)
                self.tt(v['u2'][:], s_, s_, ALU.mult, r=['PL'] + K_, w=K_)
                self.tt(PL[:, l + 1, 0, :], v['u1'][:], v['u2'][:], ALU.subtract, r=K_ + ['PL'], w=['PL'])
                self.tt(v['u1'][:], c_, s_, ALU.mult, r=['PL'] + K_, w=K_)
                self.ts(PL[:, l + 1, 1, :], v['u1'][:], 2.0, ALU.mult, r=K_ + ['PL'], w=['PL'])
            self.ts(nGs[:, 0, :], PL[:, 8, 1, :], -1.0, ALU.mult, r=['PL'], w=['nGs'])
            self.ts(nGs[:, 1, :], PL[:, 9, 1, :], -1.0, ALU.mult, r=['PL'], w=['nGs'])

            S.barrier(keep=KEEP)
            es0.close()
            cosT = self.sb(es, 's5cosT', [128, 4, 512])
            sinT = self.sb(es, 's5sinT', [128, 4, 512])
            Rt = self.sb(es, 's5Rt', [128, 4, 512])
            ga = self.sb(es, 's5ga', [128, 4, 256])
            gb_ = self.sb(es, 's5gb', [128, 4, 256])
            ujb = [self.sb(es, f's5uj{i}', [32, T], BF16) for i in range(2)]
            ystb = [self.sb(es, f's5yst{i}', [32, 512]) for i in range(2)]
            Wb = [{n: self.sb(es, f's5w{i}_' + n, [128, 512]) for n in ('t1', 't2', 't3', 't4', 'wir', 'wii', 'wr', 'wi')} for i in range(2)]
            Ppb = [[self.sb(es, f's5P{i}_{j}', [128, 512], BF16) for i in range(4)] for j in range(2)]
            car = self.sb(es, 's5car', [128, 4])
            lat = [s_ for s_ in enumerate(self.spans) if not s_[1][2]]
            ctxs = [s_ for s_ in enumerate(self.spans) if s_[1][2]]
            orders = [ctxs + lat, ctxs + lat[::-1]]
            it = 0
            for dr in range(2):
                Y = self.YF if dr == 0 else self.YB
                for jb in range(8):
                    cols = slice(dr * 32 + 4 * jb, dr * 32 + 4 * jb + 4)
                    tk = ['tab']
                    S.op('pool', lambda: nc.gpsimd.memset(cosT[:, :, 0:1], 1.0), r=[], w=tk)
                    S.op('pool', lambda: nc.gpsimd.memset(sinT[:, :, 0:1], 0.0), r=[], w=tk)
                    for l in range(9):
                        n = 1 << l
                        pc = PL[:, l, 0, cols].unsqueeze(2).to_broadcast([128, 4, n])
                        ps_ = PL[:, l, 1, cols].unsqueeze(2).to_broadcast([128, 4, n])
                        c0, s0 = cosT[:, :, 0:n], sinT[:, :, 0:n]
                        self.tt(ga[:, :, 0:n], c0, pc, ALU.mult, r=tk + ['PL'], w=['ga'], e='pool')
                        self.tt(gb_[:, :, 0:n], s0, ps_, ALU.mult, r=tk + ['PL'], w=['gb'], e='pool')
                        self.tt(cosT[:, :, n:2 * n], ga[:, :, 0:n], gb_[:, :, 0:n], ALU.subtract, r=['ga', 'gb'], w=tk, e='pool')
                        self.tt(ga[:, :, 0:n], s0, pc, ALU.mult, r=tk + ['PL'], w=['ga'], e='pool')
                        self.tt(gb_[:, :, 0:n], c0, ps_, ALU.mult, r=tk + ['PL'], w=['gb'], e='pool')
                        self.tt(sinT[:, :, n:2 * n], ga[:, :, 0:n], gb_[:, :, 0:n], ALU.add, r=['ga', 'gb'], w=tk, e='pool')
                    self.cp(Rt[:], v['mag'][:, cols].unsqueeze(2).to_broadcast([128, 4, 512]), r=K_, w=tk, e='pool')
                    for jj in range(4):
                        j = 4 * jb + jj
                        dj = dr * 32 + j
                        uj, ukey = ujb[it % 2], ('uj', it % 2)
                        S.dma(uj[:], self.UT[32 * j:32 * j + 32, :], w=[ukey])
                        S.op('dve', lambda: nc.vector.memset(car[:], 0.0), r=[], w=['car'])
                        cT, sT, rT = cosT[:, jj, :], sinT[:, jj, :], Rt[:, jj, :]
                        def emitB(n2):
                            si, (t0, w, isc) = orders[dr][n2]
                            b2 = n2 % 2
                            usl = uj[:, t0:t0 + w]
                            rhs = usl if dr == 0 else usl[:, ::-1]
                            self.mm(ps[2 * b2][:, :w], BT[:, dj, 0, :], rhs, True, True, r=['BT', ukey], w=[('ps', 2 * b2)])
                            self.mm(ps[2 * b2 + 1][:, :w], BT[:, dj, 1, :], rhs, True, True, r=['BT', ukey], w=[('ps', 2 * b2 + 1)])
                        emitB(0)
                        for n2, (si, (t0, w, isc)) in enumerate(orders[dr]):
                            b2 = n2 % 2
                            W_, Pp, yst = Wb[b2], Ppb[b2], ystb[b2]
                            kk = lambda n: (n, b2)
                            pbr, pbi, py = ps[2 * b2], ps[2 * b2 + 1], ps[4 + b2]
                            usl = uj[:, t0:t0 + w]
                            if n2 + 1 < len(orders[dr]):
                                emitB(n2 + 1)
                            self.tt(W_['t1'][:, :w], pbr[:, :w], cT[:, :w], ALU.mult, r=[('ps', 2 * b2)] + tk, w=[kk('t1')])
                            self.tt(W_['t2'][:, :w], pbi[:, :w], sT[:, :w], ALU.mult, r=[('ps', 2 * b2 + 1)] + tk, w=[kk('t2')])
                            self.tt(W_['t3'][:, :w], pbi[:, :w], cT[:, :w], ALU.mult, r=[('ps', 2 * b2 + 1)] + tk, w=[kk('t3')])
                            self.tt(W_['t4'][:, :w], pbr[:, :w], sT[:, :w], ALU.mult, r=[('ps', 2 * b2)] + tk, w=[kk('t4')])
                            self.tt(W_['wir'][:, :w], W_['t1'][:, :w], W_['t2'][:, :w], ALU.add, r=[kk('t1'), kk('t2')], w=[kk('wir')])
                            self.tt(W_['wii'][:, :w], W_['t3'][:, :w], W_['t4'][:, :w], ALU.subtract, r=[kk('t3'), kk('t4')], w=[kk('wii')])
                            S.op('dve', lambda: nc.vector.tensor_tensor_scan(out=W_['wr'][:, :w], data0=rT[:, :w], data1=W_['wir'][:, :w],
                                                                           initial=car[:, 0:1], op0=ALU.mult, op1=ALU.add),
                                 r=[kk('wir'), 'car'] + tk, w=[kk('wr')])
                            S.op('dve', lambda: nc.vector.tensor_tensor_scan(out=W_['wi'][:, :w], data0=rT[:, :w], data1=W_['wii'][:, :w],
                                                                           initial=car[:, 1:2], op0=ALU.mult, op1=ALU.add),
                                 r=[kk('wii'), 'car'] + tk, w=[kk('wi')])
                            lv = 0 if w == 256 else 1
                            Gc, Gs, nG = PL[:, 8 + lv, 0, dj:dj + 1], PL[:, 8 + lv, 1, dj:dj + 1], nGs[:, lv, dj:dj + 1]
                            wrl, wil = W_['wr'][:, w - 1:w], W_['wi'][:, w - 1:w]
                            self.ts(car[:, 2:3], wrl, Gc, ALU.mult, r=[kk('wr'), 'PL', 'car'], w=['car'])
                            self.stt(car[:, 0:1], wil, nG, car[:, 2:3], ALU.mult, ALU.add, r=[kk('wi'), 'nGs', 'car'], w=['car'])
                            self.ts(car[:, 3:4], wil, Gc, ALU.mult, r=[kk('wi'), 'PL', 'car'], w=['car'])
                            self.stt(car[:, 1:2], wrl, Gs, car[:, 3:4], ALU.mult, ALU.add, r=[kk('wr'), 'PL', 'car'], w=['car'])
                            self.tt(Pp[0][:, :w], cT[:, :w], W_['wr'][:, :w], ALU.mult, r=tk + [kk('wr')], w=[('P', 0, b2)], e='pool')
                            self.tt(Pp[1][:, :w], sT[:, :w], W_['wi'][:, :w], ALU.mult, r=tk + [kk('wi')], w=[('P', 1, b2)], e='pool')
                            self.tt(Pp[2][:, :w], sT[:, :w], W_['wr'][:, :w], ALU.mult, r=tk + [kk('wr')], w=[('P', 2, b2)], e='pool')
                            self.tt(Pp[3][:, :w], cT[:, :w], W_['wi'][:, :w], ALU.mult, r=tk + [kk('wi')], w=[('P', 3, b2)], e='pool')
                            yk = ('ps', 4 + b2)
                            self.mm(py[0:32, :w], Cw[:, 0, dj, :], Pp[0][:, :w], True, False, r=['Cw', ('P', 0, b2)], w=[yk])
                            self.mm(py[0:32, :w], Cw[:, 1, dj, :], Pp[1][:, :w], False, False, r=['Cw', ('P', 1, b2)], w=[yk])
                            self.mm(py[0:32, :w], Cw[:, 2, dj, :], Pp[2][:, :w], False, False, r=['Cw', ('P', 2, b2)], w=[yk])
                            self.mm(py[0:32, :w], Cw[:, 2, dj, :], Pp[3][:, :w], False, dr == 1, r=['Cw', ('P', 3, b2)], w=[yk])
                            if dr == 0:
                                self.mm(py[0:32, :w], dsk[:, j, :], usl, False, True, r=['dsk', ukey], w=[yk])
                            src = py[0:32, :w] if dr == 0 else py[0:32, :w][:, ::-1]
                            self.cp(yst[:, :w], src, r=[yk], w=[('yst', b2)], e='act')
                            S.dma(Y[32 * j:32 * j + 32, t0:t0 + w], yst[:, :w], r=[('yst', b2)], w=[('Y', dr, j, si)])
                        it += 1
        S.barrier(keep=KEEP)

    def ssd_d1(self, li, ki, ab, abkey):
        nc, S, I = self.nc, self.S, self.I
        ps = self.ps
        with ExitStack() as es:
            scr = self.norm_alloc(es)
            scr['xn'] = self.sb(es, 'd1_xn', [128, KT, 512])
            win = self.sb(es, 'd1_win', [128, KT, 5184], BF16)
            dtb = self.sb(es, 'd1_dtb', [128, 64])
            hsb = [self.sb(es, f'd1_hs{i}', [128, KT, 512]) for i in range(2)]
            ub = self.sb(es, 'd1_ub', [128, KT, 512], BF16)
            zst = [self.sb(es, f'd1_zst{i}', [128, 2 * D], BF16) for i in range(2)]
            xst = [self.sb(es, f'd1_xst{i}', [128, 8, 512], BF16) for i in range(2)]
            dst = [self.sb(es, f'd1_dst{i}', [128, 64]) for i in range(2)]
            for k in range(KT):
                S.dma(win[:, k, :], self.winb[ki][k * 128:(k + 1) * 128, :], r=[('winb', ki)], w=['win'])
            S.dma(dtb[:], I['ssd_dtb'], w=['dtb'])
            nz = nx = nd = 0
            for si, (t0, w, isc) in enumerate(self.spans):
                hs, hkey = hsb[si % 2], ('hs', si % 2)
                r_ = 1 if isc else 0
                S.dma(hs[:, :, :w], self.hT_span(t0, w), r=[('hT', si)], w=[hkey])
                self.norm_span(hs, hkey, w, ab[:, r_, 0, :], ab[:, r_, 1, :], abkey, None, None, ub, 'ub', scr)
                for tt_ in range(w // 128):
                    zt, zk = zst[nz % 2], ('zst', nz % 2)
                    for cg in range(4):
                        b = cg % 2
                        for k in range(KT):
                            self.mm(ps[b][:, :], ub[:, k, tt_ * 128:(tt_ + 1) * 128], win[:, k, cg * 512:(cg + 1) * 512],
                                    k == 0, k == KT - 1, r=['win', 'ub'], w=[('ps', b)])
                        self.act(zt[:, cg * 512:(cg + 1) * 512], ps[b][:, :], AF.Silu, r=[('ps', b)], w=[zk])
                    S.dma(self.ZS[t0 + tt_ * 128:t0 + (tt_ + 1) * 128, :], zt[:], r=[zk], w=[('ZS', nz)])
                    nz += 1
                    dt_, dk = dst[nd % 2], ('dst', nd % 2)
                    for k in range(KT):
                        self.mm(ps[6][:, 0:64], ub[:, k, tt_ * 128:(tt_ + 1) * 128], win[:, k, 5120:5184],
                                k == 0, k == KT - 1, r=['win', 'ub'], w=[('ps', 6)])
                    self.tt(dt_[:], ps[6][:, 0:64], dtb[:], ALU.add, r=[('ps', 6), 'dtb'], w=[dk])
                    self.act(dt_[:], dt_[:], AF.Exp, r=[dk], w=[dk])
                    self.act(dt_[:], dt_[:], AF.Ln, r=[dk], w=[dk], bias=1.0, scale=1.0)
                    S.dma(self.DTs[t0 + tt_ * 128:t0 + (tt_ + 1) * 128, :], dt_[:], r=[dk], w=[('DTs', nd)])
                    nd += 1
                for c8 in range(3):
                    xt, xk = xst[nx % 2], ('xst', nx % 2)
                    for c in range(8):
                        ct = c8 * 8 + c
                        b = 2 + ct % 2
                        for k in range(KT):
                            self.mm(ps[b][:, :w], win[:, k, 2048 + ct * 128:2048 + (ct + 1) * 128], ub[:, k, :w],
                                    k == 0, k == KT - 1, r=['win', 'ub'], w=[('ps', b)])
                        self.cp(xt[:, c, :w], ps[b][:, :w], r=[('ps', b)], w=[xk], e='dve' if c % 2 == 0 else 'act')
                    S.dma(self.XBC.rearrange("(k p) t -> p k t", p=128)[:, c8 * 8:(c8 + 1) * 8, t0:t0 + w], xt[:, :, :w],
                          r=[xk], w=[('XBC', nx)])
                    nx += 1
        S.barrier(keep=KEEP)

    def ssd_d2(self):
        nc, S, I = self.nc, self.S, self.I
        ps = self.ps
        T = self.T
        with ExitStack() as es:
            cw = self.sb(es, 'd2_cw', [128, 24, 5])
            cb = self.sb(es, 'd2_cb', [128, 24])
            xin = [self.sb(es, f'd2_xin{i}', [128, 24, 516], BF16) for i in range(2)]
            xc = [self.sb(es, f'd2_xc{i}', [128, 24, 512], BF16) for i in range(2)]
            cv = [self.sb(es, f'd2_cv{i}', [128, 512]) for i in range(2)]
            xts = [self.sb(es, f'd2_xts{i}', [128, 2 * D], BF16) for i in range(2)]
            bts = [self.sb(es, f'd2_bts{i}', [128, 512], BF16) for i in range(2)]
            S.dma(cw[:].rearrange("p a b -> p (a b)"), I['ssd_cw'], w=['cw'])
            S.dma(cb[:], I['ssd_cb'], w=['cb'])
            nt = 0
            for si, (t0, w, isc) in enumerate(self.spans):
                s0, s1 = (0, TC) if isc else (TC, T)
                lo, hi = max(t0 - 2, s0), min(t0 + w + 2, s1)
                xi, xik = xin[si % 2], ('xin', si % 2)
                if lo > t0 - 2:
                    S.op('dve', lambda: nc.vector.memset(xi[:, :, 0:2], 0.0), w=[xik])
                if hi < t0 + w + 2:
                    S.op('dve', lambda: nc.vector.memset(xi[:, :, w + 2:w + 4], 0.0), w=[xik])
                for c8 in range(3):
                    S.dma(xi[:, c8 * 8:(c8 + 1) * 8, lo - (t0 - 2):hi - (t0 - 2)],
                          self.XBC.rearrange("(k p) t -> p k t", p=128)[:, c8 * 8:(c8 + 1) * 8, lo:hi], w=[xik])
                xo, xok = xc[si % 2], ('xc', si % 2)
                for ct in range(24):
                    c_, ck = cv[ct % 2], ('cv', ct % 2)
                    self.ts(c_[:, :w], xi[:, ct, 0:w], cw[:, ct, 0:1], ALU.mult, r=[xik, 'cw', 'cb'], w=[ck], s2=cb[:, ct:ct + 1], op1=ALU.add)
                    for k in range(1, 5):
                        self.stt(c_[:, :w], xi[:, ct, k:k + w], cw[:, ct, k:k + 1], c_[:, :w], ALU.mult, ALU.add, r=[xik, 'cw', ck], w=[ck])
                    self.act(xo[:, ct, :w], c_[:, :w], AF.Silu, r=[ck], w=[xok])
                S.dma(self.BCf.rearrange("(k p) t -> p k t", p=128)[:, :, t0:t0 + w], xo[:, 16:24, :w], r=[xok], w=[('BCf', si)])
                for tt_ in range(w // 128):
                    tok = t0 + tt_ * 128
                    xt, xtk_ = xts[nt % 2], ('xts', nt % 2)
                    for half in range(2):
                        bank = half
                        pbv = ps[bank][:].bitcast(BF16)
                        for q in range(8):
                            ct = half * 8 + q
                            S.op('pe', lambda: nc.tensor.transpose(out=pbv[:, q * 128:(q + 1) * 128],
                                                                   in_=xo[:, ct, tt_ * 128:(tt_ + 1) * 128], identity=self.identb[:]),
                                 r=[xok, 'identb'], w=[('ps', bank)])
                        self.cp(xt[:, half * 1024:(half + 1) * 1024], pbv[:, 0:1024], r=[('ps', bank)], w=[xtk_],
                                e='dve' if half == 0 else 'act')
                    S.dma(self.XTK[tok:tok + 128, :], xt[:], r=[xtk_], w=[('XTK', nt)])
                    bt, btk_ = bts[nt % 2], ('bts', nt % 2)
                    pbv = ps[2][:].bitcast(BF16)
                    for q in range(4):
                        S.op('pe', lambda: nc.tensor.transpose(out=pbv[:, q * 128:(q + 1) * 128],
                                                               in_=xo[:, 16 + q, tt_ * 128:(tt_ + 1) * 128], identity=self.identb[:]),
                             r=[xok, 'identb'], w=[('ps', 2)])
                    self.cp(bt[:], pbv[:, 0:512], r=[('ps', 2)], w=[btk_])
                    S.dma(self.BTK[tok:tok + 128, :], bt[:], r=[btk_], w=[('BTK', nt)])
                    nt += 1
        S.barrier(keep=KEEP)

    def ssd_d3(self):
        nc, S, I = self.nc, self.S, self.I
        ps = self.ps
        T = self.T
        NB = T // 128
        with ExitStack() as es:
            msk = self.sb(es, 'd3_msk', [128, 4, 128])
            aneg = self.sb(es, 'd3_aneg', [128, 64])
            Sst = self.sb(es, 'd3_S', [128, 4, 512])
            Sb = self.sb(es, 'd3_Sb', [128, 4, 512], BF16)
            xtk = [self.sb(es, f'd3_xtk{i}', [128, 32, 64], BF16) for i in range(2)]
            btk = [self.sb(es, f'd3_btk{i}', [128, 512], BF16) for i in range(2)]
            bcf = [self.sb(es, f'd3_bcf{i}', [128, 8, 128], BF16) for i in range(2)]
            dtt_ = [self.sb(es, f'd3_dt{i}', [128, 32]) for i in range(2)]
            sm = self.sb(es, 'd3_sm', [128, 8, 32])
            cs = self.sb(es, 'd3_cs', [128, 64])
            chl = self.sb(es, 'd3_chl', [128, 2, 32], BF16)
            ch32 = self.sb(es, 'd3_ch32', [128, 2, 32])
            mskb = self.sb(es, 'd3_mskb', [128, 2, 128], BF16)
            xdt = self.sb(es, 'd3_xdt', [128, 32, 64], BF16)
            txdt = self.sb(es, 'd3_txdt', [128, 32, 64], BF16)
            dec = [self.sb(es, f'd3_dec{i}', [128, 128]) for i in range(4)]
            MT = [self.sb(es, f'd3_MT{i}', [128, 128], BF16) for i in range(4)]
            ytmp = [self.sb(es, f'd3_ytmp{i}', [128, 512]) for i in range(2)]
            ych = [self.sb(es, f'd3_ych{i}', [128, 2 * D]) for i in range(2)]
            S.dma(msk[:].rearrange("p a b -> p (a b)"), I['ssd_msk'], w=['msk'])
            S.dma(aneg[:], I['ssd_alog'], w=['aneg'])
            self.act(aneg[:], aneg[:], AF.Exp, r=['aneg'], w=['aneg'])
            self.ts(aneg[:], aneg[:], -1.0, ALU.mult, r=['aneg'], w=['aneg'])
            self.cp(mskb[:], msk[:, 2:4, :], r=['msk'], w=['mskb'])
            ctx_ch = list(range(TC // 128))
            lat_ch = list(range(TC // 128, NB))
            orders = [ctx_ch + lat_ch, ctx_ch[::-1] + lat_ch[::-1]]
            it = 0
            for dr in range(2):
                S.op('dve', lambda: nc.vector.memset(Sst[:], 0.0), w=['S'])
                S.op('dve', lambda: nc.vector.memset(Sb[:], 0.0), w=['Sb'])
                tri, mneg = msk[:, dr, :], msk[:, 2 + dr, :]
                for c in orders[dr]:
                    tok = c * 128
                    p2 = it % 2
                    x_, xk = xtk[p2], ('xtk', p2)
                    b_, bk = btk[p2], ('btk', p2)
                    f_, fk = bcf[p2], ('bcf', p2)
                    d_, dk = dtt_[p2], ('dt', p2)
                    S.dma(x_[:].rearrange("p h q -> p (h q)"), self.XTK[tok:tok + 128, :], w=[xk])
                    S.dma(b_[:], self.BTK[tok:tok + 128, :], w=[bk])
                    S.dma(f_[:], self.BCf.rearrange("(k p) t -> p k t", p=128)[:, :, tok:tok + 128], w=[fk])
                    with nc.allow_non_contiguous_dma(reason="dt half rows"):
                        S.dma(d_[:], self.DTs[tok:tok + 128, dr * 32:(dr + 1) * 32], w=[dk])
                    smk = ['sm']
                    self.tt(sm[:, 0, :], d_[:], aneg[:, dr * 32:(dr + 1) * 32], ALU.mult, r=[dk, 'aneg'], w=smk)
                    self.mm(ps[0][:, 0:32], tri, sm[:, 0, :], True, True, r=['msk'] + smk, w=[('ps', 0)])
                    self.mm(ps[0][:, 32:64], self.ones32[:], sm[:, 0, :], True, True, r=['ones32'] + smk, w=[('ps', 0)])
                    self.cp(cs[:], ps[0][:, 0:64], r=[('ps', 0)], w=['cs'])
                    self.ts(sm[:, 1, :], cs[:, 0:32], -1.0, ALU.mult, r=['cs'], w=smk)
                    self.cp(chl[:, 0, :], cs[:, 0:32], r=['cs'], w=['chl'])
                    self.cp(ch32[:, 0, :], chl[:, 0, :], r=['chl'], w=['ch32'])
                    self.tt(ch32[:, 1, :], cs[:, 0:32], ch32[:, 0, :], ALU.subtract, r=['cs', 'ch32'], w=['ch32'])
                    self.cp(chl[:, 1, :], ch32[:, 1, :], r=['ch32'], w=['chl'])
                    self.act(sm[:, 2, :], cs[:, 0:32], AF.Exp, r=['cs'], w=smk)
                    self.act(sm[:, 3, :], cs[:, 32:64], AF.Exp, r=['cs'], w=smk)
                    self.tt(sm[:, 4, :], cs[:, 32:64], cs[:, 0:32], ALU.subtract, r=['cs'], w=smk)
                    self.act(sm[:, 5, :], sm[:, 4, :], AF.Exp, r=smk, w=smk)
                    self.tt(sm[:, 6, :], d_[:], sm[:, 5, :], ALU.mult, r=[dk] + smk, w=smk)
                    self.tt(xdt[:], x_[:], d_[:].unsqueeze(2).to_broadcast([128, 32, 64]), ALU.mult, r=[xk, dk], w=['xdt'])
                    self.tt(txdt[:], x_[:], sm[:, 6, :].unsqueeze(2).to_broadcast([128, 32, 64]), ALU.mult, r=[xk] + smk, w=['txdt'], e='pool')
                    for g in range(4):
                        self.mm(ps[1][:, g * 128:(g + 1) * 128], f_[:, g, :], f_[:, 4 + g, :], True, True, r=[fk], w=[('ps', 1)])
                    y_, yk = ych[p2], ('ych', p2)

                    def hA(h):
                        bkn = 2 + (h // 4) % 2
                        col = (h % 4) * 128
                        self.mm(ps[bkn][:, col:col + 128], chl[:, 0, h:h + 1].to_broadcast([128, 128]), self.identb[:], True, False,
                                r=['chl', 'identb'], w=[('ps', bkn)])
                        self.mm(ps[bkn][:, col:col + 128], chl[:, 1, h:h + 1].to_broadcast([128, 128]), self.identb[:], False, False,
                                r=['chl', 'identb'], w=[('ps', bkn)])
                        self.mm(ps[bkn][:, col:col + 128], self.identb[:], mskb[:, dr, :], False, True, r=['identb', 'mskb'], w=[('ps', bkn)])

                    def hB(h):
                        bkn = 2 + (h // 4) % 2
                        col = (h % 4) * 128
                        self.act(dec[h % 4][:], ps[bkn][:, col:col + 128], AF.Exp, r=[('ps', bkn)] + smk, w=[('dec', h % 4)],
                                 bias=sm[:, 1, h:h + 1], scale=1.0)

                    def hCD(h):
                        g, hh = h // 8, h % 8
                        pa = ps[4 + g % 2]
                        self.tt(MT[h % 4][:], dec[h % 4][:], ps[1][:, g * 128:(g + 1) * 128], ALU.mult,
                                r=[('dec', h % 4), ('ps', 1)], w=[('MT', h % 4)])
                        self.mm(pa[:, hh * 64:(hh + 1) * 64], MT[h % 4][:], xdt[:, h, :], True, True,
                                r=[('MT', h % 4), 'xdt'], w=[('ps', 4 + g % 2)])
                    PF = 3
                    for h in range(PF):
                        hA(h)
                        hB(h)
                    for h in range(32):
                        if h + PF < 32:
                            hA(h + PF)
                            hB(h + PF)
                        hCD(h)
                        if h % 8 == 7:
                            g = h // 8
                            pa, pbk = ps[4 + g % 2], ps[6 + g % 2]
                            self.mm(pbk[:, :], f_[:, 4 + g, :], Sb[:, g, :], True, True, r=[fk, 'Sb'], w=[('ps', 6 + g % 2)])
                            self.tt(ytmp[g % 2][:].rearrange("p (h q) -> p h q", h=8), pbk[:, :].rearrange("p (h q) -> p h q", h=8),
                                    sm[:, 2, g * 8:(g + 1) * 8].unsqueeze(2).to_broadcast([128, 8, 64]), ALU.mult,
                                    r=[('ps', 6 + g % 2)] + smk, w=[('ytmp', g % 2)])
                            self.tt(y_[:, g * 512:(g + 1) * 512], pa[:, :], ytmp[g % 2][:], ALU.add,
                                    r=[('ps', 4 + g % 2), ('ytmp', g % 2)], w=[yk])
                    S.dma(self.YD[dr, tok:tok + 128, :], y_[:], r=[yk], w=[('YD', dr, c)])
                    for g in range(4):
                        pst = ps[2 + g % 2]
                        self.mm(pst[:, :], b_[:, g * 128:(g + 1) * 128], txdt[:, g * 8:(g + 1) * 8, :].rearrange("p h q -> p (h q)"),
                                True, True, r=[bk, 'txdt'], w=[('ps', 2 + g % 2)])
                        sv = Sst[:, g, :].rearrange("p (h q) -> p h q", h=8)
                        self.tt(sv, sv, sm[:, 3, g * 8:(g + 1) * 8].unsqueeze(2).to_broadcast([128, 8, 64]), ALU.mult, r=['S'] + smk, w=['S'])
                        self.tt(Sst[:, g, :], Sst[:, g, :], pst[:, :], ALU.add, r=['S', ('ps', 2 + g % 2)], w=['S'])
                        self.cp(Sb[:, g, :], Sst[:, g, :], r=['S'], w=['Sb'], e='pool')
                    it += 1
        S.barrier(keep=KEEP)

    def ssd_d4(self, li, ki, ab, abkey, need_ctx):
        nc, S, I = self.nc, self.S, self.I
        ps = self.ps
        with ExitStack() as es:
            wout = self.sb(es, 'd4_wout', [128, 16, D], BF16)
            ngr = self.sb(es, 'd4_ng', [128, 2 * D])
            dh = self.sb(es, 'd4_dh', [128, 32])
            eps_ = self.sb(es, 'd4_eps', [128, 1])
            hsb = [self.sb(es, f'd4_hs{i}', [128, KT, 512]) for i in range(2)]
            yf = [self.sb(es, f'd4_yf{i}', [128, 2 * D]) for i in range(2)]
            yb = [self.sb(es, f'd4_yb{i}', [128, 2 * D]) for i in range(2)]
            xk_ = [self.sb(es, f'd4_x{i}', [128, 2 * D], BF16) for i in range(2)]
            zs = [self.sb(es, f'd4_z{i}', [128, 2 * D], BF16) for i in range(2)]
            tmp = self.sb(es, 'd4_tmp', [128, 2 * D])
            ynb = self.sb(es, 'd4_ynb', [128, 2 * D], BF16)
            ynT = self.sb(es, 'd4_ynT', [128, 16, 512], BF16)
            st = self.sb(es, 'd4_st', [128, 4])
            S.dma(wout[:], self.woutb[ki].rearrange("(k p) n -> p k n", p=128), r=[('woutb', ki)], w=['wout'])
            S.dma(ngr[:], I['ssd_ng'], w=['ngr'])
            S.dma(dh[:], I['ssd_dh'], w=['dh'])
            S.op('dve', lambda: nc.vector.memset(eps_[:], EPS), w=['eps'])
            spans = [s_ for s_ in enumerate(self.spans) if need_ctx or not s_[1][2]]
            nt = 0
            for n_, (si, (t0, w, isc)) in enumerate(spans):
                hs, hkey = hsb[n_ % 2], ('hs', n_ % 2)
                r_ = 1 if isc else 0
                S.dma(hs[:, :, :w], self.hT_span(t0, w), r=[('hT', si)], w=[hkey])
                for tt_ in range(w // 128):
                    tok = t0 + tt_ * 128
                    p2 = nt % 2
                    S.dma(yf[p2][:], self.YD[0, tok:tok + 128, :], w=[('yf', p2)])
                    S.dma(yb[p2][:], self.YD[1, tok:tok + 128, :], w=[('yb', p2)])
                    S.dma(xk_[p2][:], self.XTK[tok:tok + 128, :], w=[('x', p2)])
                    S.dma(zs[p2][:], self.ZS[tok:tok + 128, :], w=[('z', p2)])
                    y = yf[p2]
                    yk = ('yf', p2)
                    self.tt(y[:], y[:], yb[p2][:], ALU.add, r=[yk, ('yb', p2)], w=[yk], e='pool')
                    self.tt(tmp[:].rearrange("p (h q) -> p h q", h=32), xk_[p2][:].rearrange("p (h q) -> p h q", h=32),
                            dh[:].unsqueeze(2).to_broadcast([128, 32, 64]), ALU.mult, r=[('x', p2), 'dh'], w=['tmp'], e='pool')
                    self.tt(y[:], y[:], tmp[:], ALU.add, r=[yk, 'tmp'], w=[yk])
                    self.tt(y[:], y[:], zs[p2][:], ALU.mult, r=[yk, ('z', p2)], w=[yk])
                    self.act(tmp[:], y[:], AF.Square, r=[yk], w=['tmp', 'st'], accum_out=st[:, 0:1])
                    self.act(st[:, 1:2], st[:, 0:1], AF.Sqrt, r=['st', 'eps'], w=['st'], scale=1.0 / (2 * D), bias=eps_[:, 0:1])
                    S.op('dve', lambda: nc.vector.reciprocal(out=st[:, 2:3], in_=st[:, 1:2]), r=['st'], w=['st'])
                    self.stt(ynb[:], y[:], st[:, 2:3], ngr[:], ALU.mult, ALU.mult, r=[yk, 'st', 'ngr'], w=['ynb'])
                    for half in range(2):
                        bank = half
                        pbv = ps[bank][:].bitcast(BF16)
                        for q in range(8):
                            k = half * 8 + q
                            S.op('pe', lambda: nc.tensor.transpose(out=pbv[:, q * 128:(q + 1) * 128],
                                                                   in_=ynb[:, k * 128:(k + 1) * 128], identity=self.identb[:]),
                                 r=['ynb', 'identb'], w=[('ps', bank)])
                        self.cp(ynT[:, half * 8:(half + 1) * 8, tt_ * 128:(tt_ + 1) * 128],
                                pbv[:, 0:1024].rearrange("p (k t) -> p k t", k=8), r=[('ps', bank)], w=['ynT'],
                                e='dve' if half == 0 else 'act')
                    nt += 1
                for ct in range(KT):
                    pb = ps[2 + ct % 2]
                    for k in range(16):
                        self.mm(pb[:, :w], wout[:, k, ct * 128:(ct + 1) * 128], ynT[:, k, :w], k == 0, k == 15,
                                r=['wout', 'ynT'], w=[('ps', 2 + ct % 2)])
                    self.stt(hs[:, ct, :w], pb[:, :w], ab[:, r_, 2, ct:ct + 1], hs[:, ct, :w], ALU.mult, ALU.add,
                             r=[('ps', 2 + ct % 2), abkey, hkey], w=[hkey])
                S.dma(self.hT_span(t0, w), hs[:, :, :w], r=[hkey], w=[('hT', si)])
        S.barrier(keep=KEEP)

    def layer(self, li, kind, ki, need_ctx):
        S = self.S
        ps = self.ps
        with ExitStack() as es:
            ab = self.layer_scalars(es, li)
            abkey = ('ab', li)
            if kind == 'a':
                self.attn_a1(li, ki, ab, abkey)
                self.attn_a2(need_ctx)
            if kind == 's':
                self.s5_s1(li, ab, abkey)
                self.s5_s2(ki)
            if kind == 'd':
                self.ssd_d1(li, ki, ab, abkey)
                self.ssd_d2()
                self.ssd_d3()
                self.ssd_d4(li, ki, ab, abkey, need_ctx)
            with ExitStack() as es2:
                scr = self.norm_alloc(es2)
                M = self.moe_alloc(es2, li)
                scr['xn'] = M['acc']
                hsb = [self.sb(es2, f'hs{i}', [128, KT, 512]) for i in range(2)]
                u32 = self.sb(es2, 'u32', [128, KT, 512])
                ub = self.sb(es2, 'ub', [128, KT, 512], BF16)
                if kind == 'a':
                    wo = self.sb(es2, 'a3_wo', [128, KT, D], BF16)
                    otb = [self.sb(es2, f'a3_ot{i}', [128, KT, 512], BF16) for i in range(2)]
                    S.dma(wo[:], self.wob[ki].rearrange("(k p) n -> p k n", p=128), r=[('wob', ki)], w=['wo'])
                if kind == 's':
                    wgl = self.sb(es2, 's3_wglu', [128, KT, 2 * D], BF16)
                    bgl = self.sb(es2, 's3_bglu', [128, 16])
                    sgl = [self.sb(es2, f's3_sig{i}', [128, 512]) for i in range(2)]
                    ymx = [self.sb(es2, f's3_ymx{i}', [128, 512]) for i in range(2)]
                    S.dma(wgl[:], self.wglub[ki].rearrange("(k p) n -> p k n", p=128), r=[('wglub', ki)], w=['wgl'])
                    S.dma(bgl[:], self.I['s5_bglu'], w=['bgl'])
                spans = [s for s in enumerate(self.spans) if need_ctx or not s[1][2]]
                for n_, (si, (t0, w, isc)) in enumerate(spans):
                    hs, hkey = hsb[n_ % 2], ('hs', n_ % 2)
                    r_ = 1 if isc else 0
                    S.dma(hs[:, :, :w], self.hT_span(t0, w), r=[('hT', si)], w=[hkey])
                    if kind == 's':
                        acc = M['acc']
                        akeys = [('acc', k) for k in range(KT)]
                        S.dma(u32[:, :, :w], self.YF.rearrange("(k p) t -> p k t", p=128)[:, :, t0:t0 + w], w=['u32'])
                        S.dma(acc[:, :, :w], self.YB.rearrange("(k p) t -> p k t", p=128)[:, :, t0:t0 + w], w=akeys)
                        self.tt(u32[:, :, :w], u32[:, :, :w], acc[:, :, :w], ALU.add, r=['u32'] + akeys, w=['u32'])
                        self.tt(acc[:, :, :w], u32[:, :, :w], u32[:, :, :w], ALU.mult, r=['u32'], w=akeys, e='pool')
                        self.ts(acc[:, :, :w], acc[:, :, :w], 0.044715, ALU.mult, r=akeys, w=akeys, s2=1.0, op1=ALU.add)
                        self.tt(acc[:, :, :w], acc[:, :, :w], u32[:, :, :w], ALU.mult, r=['u32'] + akeys, w=akeys, e='pool')
                        self.act(acc[:, :, :w], acc[:, :, :w], AF.Sigmoid, r=akeys, w=akeys, scale=2.0 * float(np.sqrt(2.0 / np.pi)))
                        self.tt(ub[:, :, :w], acc[:, :, :w], u32[:, :, :w], ALU.mult, r=['u32'] + akeys, w=['ub'])
                        for ct in range(KT):
                            b = ct % 2
                            pa, pb2 = ps[b], ps[2 + b]
                            for k in range(KT):
                                self.mm(pa[:, :w], wgl[:, k, ct * 128:(ct + 1) * 128], ub[:, k, :w], k == 0, k == KT - 1,
                                        r=['wgl', 'ub'], w=[('ps', b)])
                            for k in range(KT):
                                self.mm(pb2[:, :w], wgl[:, k, D + ct * 128:D + (ct + 1) * 128], ub[:, k, :w], k == 0, k == KT - 1,
                                        r=['wgl', 'ub'], w=[('ps', 2 + b)])
                            self.act(sgl[b][:, :w], pb2[:, :w], AF.Sigmoid, r=[('ps', 2 + b), 'bgl'], w=[('sgl', b)],
                                     bias=bgl[:, 8 + ct:9 + ct], scale=1.0)
                            self.stt(ymx[b][:, :w], pa[:, :w], bgl[:, ct:ct + 1], sgl[b][:, :w], ALU.add, ALU.mult,
                                     r=[('ps', b), 'bgl', ('sgl', b)], w=[('ymx', b)])
                            self.stt(hs[:, ct, :w], ymx[b][:, :w], ab[:, r_, 2, ct:ct + 1], hs[:, ct, :w], ALU.mult, ALU.add,
                                     r=[('ymx', b), abkey, hkey], w=[hkey])
                    if kind == 'a':
                        ot, okey = otb[n_ % 2], ('ot', n_ % 2)
                        S.dma(ot[:, :, :w], self.OT.rearrange("(k p) t -> p k t", p=128)[:, :, t0:t0 + w], w=[okey])
                        for ct in range(KT):
                            pb = ps[ct % 2]
                            for k in range(KT):
                                self.mm(pb[:, :w], wo[:, k, ct * 128:(ct + 1) * 128], ot[:, k, :w], k == 0, k == KT - 1,
                                        r=['wo', okey], w=[('ps', ct % 2)])
                            self.stt(hs[:, ct, :w], pb[:, :w], ab[:, r_, 2, ct:ct + 1], hs[:, ct, :w], ALU.mult, ALU.add,
                                     r=[('ps', ct % 2), abkey, hkey], w=[hkey])
                    self.norm_span(hs, hkey, w, ab[:, r_, 3, :], ab[:, r_, 4, :], abkey, u32, 'u32', ub, 'ub', scr)
                    self.moe_span(li, hs, hkey, w, u32, ub, ab[:, r_, 5, :], abkey, M)
                    S.dma(self.hT_span(t0, w), hs[:, :, :w], r=[hkey], w=[('hT', si)])
            S.barrier(keep=KEEP)


_ROPE = {}


def rope_consts(TL):
    if TL in _ROPE:
        return _ROPE[TL]
    f = np.float32
    t = np.arange(TL)
    pos = np.stack([t // 64, t % 64], axis=-1).astype(f)
    inv = (f(10000.0) ** (-np.arange(32, dtype=f) / f(32))).astype(f)
    ang = np.broadcast_to(pos[:, :, None, None] * inv, (TL, 2, 2, 32)).reshape(TL, 128).astype(f)
    cos = np.concatenate([np.ones((TC, 128), f), np.cos(ang).astype(f)], axis=0).T
    sin = np.concatenate([np.zeros((TC, 128), f), np.sin(ang).astype(f)], axis=0).T
    pm = np.zeros((128, 128), f)
    for a in range(2):
        for j in range(32):
            pm[a * 64 + 32 + j, a * 64 + j] = -1.0
            pm[a * 64 + j, a * 64 + 32 + j] = 1.0
    _ROPE[TL] = (np.ascontiguousarray(cos), np.ascontiguousarray(sin), pm)
    return _ROPE[TL]


def ssd_masks():
    f = np.float32
    k = np.arange(128)[:, None]
    i = np.arange(128)[None, :]
    trif = (k <= i).astype(f)
    trib = (k >= i).astype(f)
    mf = np.where(k <= i, 0.0, -30000.0).astype(f)
    mb = np.where(k >= i, 0.0, -30000.0).astype(f)
    return np.ascontiguousarray(np.stack([trif, trib, mf, mb], axis=1).reshape(128, 512))


def host_inputs(inp, b, TL, layers):
    f = np.float32
    d = {}
    d['x'] = np.ascontiguousarray(inp['x'][b, :TL])
    d['ctx'] = np.ascontiguousarray(inp['ctx'][b])
    d['cc'] = np.ascontiguousarray(np.stack([inp['c'][b], inp['c_ctx']]))
    d['ident'] = np.eye(128, dtype=f)
    d['mod_w'] = inp['mod_w']
    d['mod_b'] = inp['mod_b']
    ng = np.stack([inp['norm1_g'], inp['norm2_g']])
    d['ng'] = np.ascontiguousarray(ng.reshape(2, 4, KT, 128).transpose(3, 0, 1, 2).reshape(128, -1))
    d['attn_wqkv'] = inp['attn_w_qkv']
    d['attn_wo'] = inp['attn_w_o']
    d['attn_g'] = np.ascontiguousarray(np.stack([inp['attn_q_gain'], inp['attn_k_gain']], axis=1).reshape(4, 128).T)
    f = np.float32
    ki = 0
    def gl_p(a):
        sh = a.shape
        a = a.reshape((2, 32, 2, 64) + sh[3:])
        return np.moveaxis(a, (2, 3), (0, 1)).reshape((128, 2, 32) + sh[3:])
    ldt_b = np.broadcast_to(inp['s5_log_dt'][ki][:, :, None], (2, 64, 64))
    par = np.stack([gl_p(inp['s5_a_re'][ki]), gl_p(inp['s5_a_im'][ki]), gl_p(np.ascontiguousarray(ldt_b))], axis=1)
    d['s5_par'] = np.ascontiguousarray(par.reshape(128, 3 * 64)).astype(f)
    def blockdiag(a):
        o = np.zeros((128, 2, 32, 2, 16), f)
        o[:64, :, :, 0, :] = a[:64]
        o[64:, :, :, 1, :] = a[64:]
        return o.reshape(128, 2, 32, 32)
    Bre, Bim = blockdiag(gl_p(inp['s5_b_re'][ki])), blockdiag(gl_p(inp['s5_b_im'][ki]))
    d['s5_B'] = np.ascontiguousarray(np.stack([Bre, Bim], axis=1).reshape(128, -1))
    cre = np.swapaxes(inp['s5_c_re'][ki], 2, 3)
    cim = np.swapaxes(inp['s5_c_im'][ki], 2, 3)
    Cre, Cim = blockdiag(gl_p(np.ascontiguousarray(cre))), blockdiag(gl_p(np.ascontiguousarray(cim)))
    d['s5_C'] = np.ascontiguousarray(np.stack([Cre, Cim], axis=1).reshape(128, -1))
    d['s5_d'] = np.ascontiguousarray(inp['s5_d'][ki].reshape(32, 32).T)
    d['s5_wglu'] = inp['s5_w_glu']
    d['s5_bglu'] = np.ascontiguousarray(inp['s5_b_glu'][ki].reshape(16, 128).T)
    d['ssd_win'] = inp['ssd_w_in']
    d['ssd_wout'] = inp['ssd_w_out']
    d['ssd_cw'] = np.ascontiguousarray(inp['ssd_conv_w'][ki].reshape(5, 24, 128).transpose(2, 1, 0).reshape(128, 120))
    d['ssd_cb'] = np.ascontiguousarray(inp['ssd_conv_b'][ki].reshape(24, 128).T)
    d['ssd_dtb'] = np.ascontiguousarray(np.broadcast_to(inp['ssd_dt_bias'][ki].reshape(1, 64), (128, 64)))
    d['ssd_alog'] = np.ascontiguousarray(np.broadcast_to(inp['ssd_a_log'][ki].reshape(1, 64), (128, 64)))
    d['ssd_dh'] = np.ascontiguousarray(np.broadcast_to(inp['ssd_d'][ki].reshape(1, 32), (128, 32)))
    d['ssd_ng'] = np.ascontiguousarray(np.broadcast_to(inp['ssd_norm_g'][ki].reshape(1, 2048), (128, 2048)))
    d['ssd_msk'] = ssd_masks()
    cos, sin, pm = rope_consts(TL)
    d['rope_cos'], d['rope_sin'], d['rope_pm'] = cos, sin, pm
    d['moe_wr'] = np.ascontiguousarray(np.concatenate([inp['moe_w_group'], inp['moe_w_router']], axis=-1))
    d['moe_br'] = np.ascontiguousarray(np.concatenate([inp['moe_b_group'], inp['moe_b_router']], axis=-1))
    d['moe_wg'] = inp['moe_w_gate']
    d['moe_wu'] = inp['moe_w_up']
    d['moe_wd'] = inp['moe_w_down']
    return d


FULL_LAYERS = [(0, 'a', 0, True), (1, 's', 0, True), (2, 'd', 0, True), (3, 'a', 1, False)]


def kernel(**inputs):
    inp = {k: np.asarray(v) for k, v in inputs.items()}
    B, TL = inp['x'].shape[0], inp['x'].shape[1]
    prog = Prog(TL, FULL_LAYERS)
    in_maps = []
    for b in range(B):
        d = host_inputs(inp, b, TL, FULL_LAYERS)
        in_maps.append({k: d[k] for k in prog.in_names})
    res = run_bass_kernel_spmd(prog.nc, in_maps, core_ids=list(range(B)))
    return np.stack([r['out'] for r in res.results], axis=0)
```
